# Optimizing a Trainium2 kernel written in Bass

```python
import functools
import jax, jax.numpy as jnp
from jax import lax
import numpy as np

D_MODEL = 1024
BATCH = 8
SEQ = 4096
DEPTH = 4

GRID_W = 64
CTX_LEN = 256
N_MIXERS = 3
EPS = 1e-6

LRU_WIDTH = 1280
LRU_BLOCKS = 10
LRU_BLOCK = LRU_WIDTH // LRU_BLOCKS
LRU_CONV = 4
LRU_C = 8.0

RET_HEADS = 4
RET_DK = 256
RET_DV = 512
RET_DK_TOT = RET_HEADS * RET_DK
RET_DV_TOT = RET_HEADS * RET_DV
RET_CHUNK = 128
RET_THETA_BASE = 10000.0

SWA_HQ = 16
SWA_HKV = 4
SWA_DH = 64
SWA_WINDOW = 128
SWA_BLOCK = 128
ROPE_BASE = 10000.0

D_FF = 3584
N_EXPERTS = 8
TOP_K = 2

N_LAYERS_A = (DEPTH + 2) // 3
N_LAYERS_B = (DEPTH + 1) // 3
N_LAYERS_C = DEPTH // 3
N_DENSE = (DEPTH + 1) // 2
N_MOE = DEPTH // 2

kernel_name = 'hybrid_rglru_retention_swa_moe_dit'


def _rmsnorm(x, g):
    xf = x.astype(jnp.float32)
    y = xf * lax.rsqrt(jnp.mean(xf * xf, axis=-1, keepdims=True) + EPS)
    return (y * g.astype(jnp.float32)).astype(x.dtype)


def _modulate(x, g, shift, scale):
    return _rmsnorm(x, g) * (1 + scale) + shift


def _rotate(x, ang):
    cos = jnp.cos(ang)[:, None, :].astype(x.dtype)
    sin = jnp.sin(ang)[:, None, :].astype(x.dtype)
    x1, x2 = jnp.split(x, 2, axis=-1)
    return jnp.concatenate([x1 * cos - x2 * sin, x2 * cos + x1 * sin], axis=-1)


def _axial_angles(length, dim):
    rows = length // GRID_W
    row = jnp.repeat(jnp.arange(rows, dtype=jnp.float32), GRID_W)
    col = jnp.tile(jnp.arange(GRID_W, dtype=jnp.float32), rows)
    n_freq = dim // 4
    freq = ROPE_BASE ** (-jnp.arange(n_freq, dtype=jnp.float32) / n_freq)
    return jnp.concatenate([row[:, None] * freq, col[:, None] * freq], axis=-1)


def _ret_angles(length):
    n_freq = RET_DK // 2
    theta = RET_THETA_BASE ** (-jnp.arange(n_freq, dtype=jnp.float32) / n_freq)
    return jnp.arange(length, dtype=jnp.float32)[:, None] * theta


def _centred_dwconv(x, w, b):
    length = x.shape[1]
    left = LRU_CONV // 2
    right = LRU_CONV - 1 - left
    xp = jnp.pad(x, ((0, 0), (left, right), (0, 0)))
    return b + sum(xp[:, k:k + length] * w[k] for k in range(LRU_CONV))


def _rglru_coeffs(xr, w_gate, b_gate, lam):
    bsz, length, _ = xr.shape
    xb = xr.reshape(bsz, length, LRU_BLOCKS, LRU_BLOCK)
    gates = jnp.einsum('blnj,gnjk->gblnk', xb, w_gate).reshape(2, bsz, length, LRU_WIDTH)
    gates = gates + b_gate[:, None, None, :]
    r = jax.nn.sigmoid(gates[0])
    i = jax.nn.sigmoid(gates[1])
    log_a = -LRU_C * r * jax.nn.softplus(-lam)
    a = jnp.exp(log_a)
    bx = jnp.sqrt(-jnp.expm1(2.0 * log_a)) * (i * xr)
    return a, bx


def _linear_scan(a, bx, h0, reverse):
    def combine(e1, e2):
        a1, b1 = e1
        a2, b2 = e2
        return a1 * a2, a2 * b1 + b2
    a_cum, b_cum = lax.associative_scan(combine, (a, bx), axis=1, reverse=reverse)
    if h0 is None:
        return b_cum
    return a_cum * h0[:, None, :] + b_cum


def _mixer_rglru(hl, hc, w_in, conv_w, conv_b, w_gate, b_gate, lam, w_out, ctx_out):
    def branches(h):
        y, xr = jnp.split(h @ w_in, 2, axis=-1)
        xr = _centred_dwconv(xr, conv_w, conv_b).astype(jnp.float32)
        return jax.nn.gelu(y), xr
    yl, ul = branches(hl)
    yc, uc = branches(hc)
    h_lat, h_ctx_sum = None, None
    for d, rev in enumerate((False, True)):
        a_c, b_c = _rglru_coeffs(uc, w_gate[d], b_gate[d], lam[d])
        h_ctx = _linear_scan(a_c, b_c, None, rev)
        h0 = h_ctx[:, 0] if rev else h_ctx[:, -1]
        a_l, b_l = _rglru_coeffs(ul, w_gate[d], b_gate[d], lam[d])
        h_l = _linear_scan(a_l, b_l, h0, rev)
        h_lat = h_l if h_lat is None else h_lat + h_l
        h_ctx_sum = h_ctx if h_ctx_sum is None else h_ctx_sum + h_ctx
    ol = (yl * h_lat.astype(yl.dtype)) @ w_out
    oc = (yc * h_ctx_sum.astype(yc.dtype)) @ w_out if ctx_out else None
    return ol, oc


def _retention_dir(q, k, v, log_g, state0):
    bsz, length, heads, _ = q.shape
    dv = v.shape[-1]
    n_chunks = length // RET_CHUNK
    pos = jnp.arange(RET_CHUNK, dtype=jnp.float32)
    lg = log_g.astype(jnp.float32)[:, None]
    diff = pos[:, None] - pos[None, :]
    intra = jnp.where(diff >= 0, jnp.exp(lg[:, :, None] * jnp.maximum(diff, 0.0)), 0.0)
    q_dec = jnp.exp(lg * (pos + 1.0))
    k_dec = jnp.exp(lg * (RET_CHUNK - 1.0 - pos))
    c_dec = jnp.exp(lg * RET_CHUNK)

    def chunks(t):
        return t.reshape(bsz, n_chunks, RET_CHUNK, heads, -1).transpose(1, 0, 3, 2, 4)

    def step(state, qkv):
        qc, kc, vc = qkv
        s = jnp.einsum('bhid,bhjd->bhij', qc, kc) * intra
        o = jnp.einsum('bhij,bhje->bhie', s, vc)
        o = o + jnp.einsum('bhid,bhde->bhie', qc * q_dec[..., None], state)
        state = c_dec[..., None] * state + jnp.einsum('bhjd,bhje->bhde', kc * k_dec[..., None], vc)
        return state, o

    state, o = lax.scan(step, state0, (chunks(q), chunks(k), chunks(v)))
    return o.transpose(1, 0, 3, 2, 4).reshape(bsz, length, heads, dv), state


def _mixer_retention(hl, hc, w_in, log_decay, gn_gain, w_out, ctx_out):
    def project(h, rotate):
        bsz, length, _ = h.shape
        q, k, v, g = jnp.split(h @ w_in, [RET_DK_TOT, 2 * RET_DK_TOT, 2 * RET_DK_TOT + RET_DV_TOT], axis=-1)
        q = q.reshape(bsz, length, RET_HEADS, RET_DK)
        k = k.reshape(bsz, length, RET_HEADS, RET_DK) * (RET_DK ** -0.5)
        v = v.reshape(bsz, length, RET_HEADS, RET_DV)
        if rotate:
            ang = _ret_angles(length)
            q, k = _rotate(q, ang), _rotate(k, ang)
        return q.astype(jnp.float32), k.astype(jnp.float32), v.astype(jnp.float32), g

    def out(o, g):
        bsz, length = o.shape[:2]
        o = o * lax.rsqrt(jnp.mean(o * o, axis=-1, keepdims=True) + EPS)
        o = o.reshape(bsz, length, RET_DV_TOT).astype(g.dtype) * gn_gain
        return (jax.nn.silu(g) * o) @ w_out

    flip = lambda t: jnp.flip(t, axis=1)
    ql, kl, vl, gl = project(hl, True)
    qc, kc, vc, gc = project(hc, False)
    zero = jnp.zeros((hl.shape[0], RET_HEADS, RET_DK, RET_DV), jnp.float32)
    oc_f, st_f = _retention_dir(qc, kc, vc, log_decay[0], zero)
    oc_b, st_b = _retention_dir(flip(qc), flip(kc), flip(vc), log_decay[1], zero)
    ol_f, _ = _retention_dir(ql, kl, vl, log_decay[0], st_f)
    ol_b, _ = _retention_dir(flip(ql), flip(kl), flip(vl), log_decay[1], st_b)
    ol = out(ol_f + flip(ol_b), gl)
    oc = out(oc_f + flip(oc_b), gc) if ctx_out else None
    return ol, oc


def _mixer_swa(hl, hc, w_in, sink, w_out, ctx_out):
    def project(h):
        bsz, length, _ = h.shape
        q, k, v = jnp.split(h @ w_in, [SWA_HQ * SWA_DH, (SWA_HQ + SWA_HKV) * SWA_DH], axis=-1)
        return (q.reshape(bsz, length, SWA_HQ, SWA_DH),
                k.reshape(bsz, length, SWA_HKV, SWA_DH),
                v.reshape(bsz, length, SWA_HKV, SWA_DH))

    ql, kl, vl = project(hl)
    qc, kc, vc = project(hc)
    bsz, seq = hl.shape[:2]
    n_ctx = hc.shape[1]
    groups = SWA_HQ // SWA_HKV
    scale = SWA_DH ** -0.5
    sink_g = sink.reshape(SWA_HKV, groups).astype(jnp.float32)
    ang = _axial_angles(seq, SWA_DH)
    ql, kl = _rotate(ql, ang), _rotate(kl, ang)

    n_blk = seq // SWA_BLOCK
    qb = ql.reshape(bsz, n_blk, SWA_BLOCK, SWA_HKV, groups, SWA_DH)

    def band(t):
        tp = jnp.pad(t, ((0, 0), (SWA_BLOCK, SWA_BLOCK), (0, 0), (0, 0)))
        tp = tp.reshape(bsz, n_blk + 2, SWA_BLOCK, SWA_HKV, SWA_DH)
        return jnp.concatenate([tp[:, :-2], tp[:, 1:-1], tp[:, 2:]], axis=2)

    kb, vb = band(kl), band(vl)
    s_band = jnp.einsum('bnqhgd,bnkhd->bhgnqk', qb, kb, preferred_element_type=jnp.float32) * scale
    s_ctx = jnp.einsum('bnqhgd,bchd->bhgnqc', qb, kc, preferred_element_type=jnp.float32) * scale
    blk = jnp.arange(n_blk)[:, None, None]
    qpos = blk * SWA_BLOCK + jnp.arange(SWA_BLOCK)[None, :, None]
    kpos = (blk - 1) * SWA_BLOCK + jnp.arange(3 * SWA_BLOCK)[None, None, :]
    valid = (jnp.abs(qpos - kpos) <= SWA_WINDOW) & (kpos >= 0) & (kpos < seq)
    s_band = jnp.where(valid, s_band, -jnp.inf)
    sink_col = jnp.broadcast_to(sink_g[None, :, :, None, None, None],
                                (bsz, SWA_HKV, groups, n_blk, SWA_BLOCK, 1))
    p = jax.nn.softmax(jnp.concatenate([s_band, s_ctx, sink_col], axis=-1), axis=-1).astype(vl.dtype)
    pb = p[..., :3 * SWA_BLOCK]
    pc = p[..., 3 * SWA_BLOCK:3 * SWA_BLOCK + n_ctx]
    ol = jnp.einsum('bhgnqk,bnkhd->bnqhgd', pb, vb) + jnp.einsum('bhgnqc,bchd->bnqhgd', pc, vc)
    ol = ol.reshape(bsz, seq, SWA_HQ * SWA_DH) @ w_out
    oc = None
    if ctx_out:
        qcg = qc.reshape(bsz, n_ctx, SWA_HKV, groups, SWA_DH)
        sc = jnp.einsum('bqhgd,bkhd->bhgqk', qcg, kc, preferred_element_type=jnp.float32) * scale
        sink_c = jnp.broadcast_to(sink_g[None, :, :, None, None], (bsz, SWA_HKV, groups, n_ctx, 1))
        pcc = jax.nn.softmax(jnp.concatenate([sc, sink_c], axis=-1), axis=-1)[..., :n_ctx].astype(vc.dtype)
        oc = jnp.einsum('bhgqk,bkhd->bqhgd', pcc, vc).reshape(bsz, n_ctx, SWA_HQ * SWA_DH) @ w_out
    return ol, oc


def _swiglu(h, w_gu, w_down):
    g, u = jnp.split(h @ w_gu, 2, axis=-1)
    return (jax.nn.silu(g) * u) @ w_down


def _moe_swiglu(h, router, w_gu, w_down):
    logits = (h @ router).astype(jnp.float32)
    top_v, top_i = lax.top_k(logits, TOP_K)
    top_w = jax.nn.softmax(top_v, axis=-1)
    combine = jnp.sum(jax.nn.one_hot(top_i, N_EXPERTS, dtype=jnp.float32) * top_w[..., None], axis=1)
    combine = combine.astype(h.dtype)
    out = jnp.zeros_like(h)
    for e in range(N_EXPERTS):
        out = out + combine[:, e:e + 1] * _swiglu(h, w_gu[e], w_down[e])
    return out


def setup_inputs(seed: int = 0) -> dict:
    key = jax.random.key(seed)
    keys = iter(jax.random.split(key, 40))
    f32 = jnp.float32
    D = D_MODEL

    def nrm(shape, fan_in, scale=1.0):
        return scale * (fan_in ** -0.5) * jax.random.normal(next(keys), shape, f32)

    def noise(shape, scale):
        return scale * jax.random.normal(next(keys), shape, f32)

    x = jax.random.normal(next(keys), (BATCH, SEQ, D), f32)
    c = jax.random.normal(next(keys), (BATCH, D), f32)
    ctx = jax.random.normal(next(keys), (BATCH, CTX_LEN, D), f32)
    c_ctx = jax.random.normal(next(keys), (D,), f32)
    ada_w = nrm((DEPTH, D, 6 * D), D, 0.5)
    ada_b = noise((DEPTH, 6 * D), 0.02)
    norm_g = 1.0 + noise((DEPTH, 2, D), 0.05)
    final_g = 1.0 + noise((D,), 0.05)

    lru_w_in = nrm((N_LAYERS_A, D, 2 * LRU_WIDTH), D)
    lru_conv_w = nrm((N_LAYERS_A, LRU_CONV, LRU_WIDTH), LRU_CONV)
    lru_conv_b = noise((N_LAYERS_A, LRU_WIDTH), 0.02)
    lru_w_gate = nrm((N_LAYERS_A, 2, 2, LRU_BLOCKS, LRU_BLOCK, LRU_BLOCK), LRU_BLOCK)
    lru_b_gate = noise((N_LAYERS_A, 2, 2, LRU_WIDTH), 0.1)
    a_pow = jax.random.uniform(next(keys), (N_LAYERS_A, 2, LRU_WIDTH), f32, 0.9, 0.999)
    p = a_pow ** (1.0 / LRU_C)
    lru_lambda = jnp.log(p) - jnp.log1p(-p)
    lru_w_out = nrm((N_LAYERS_A, LRU_WIDTH, D), LRU_WIDTH)

    ret_w_in = nrm((N_LAYERS_B, D, 2 * RET_DK_TOT + 2 * RET_DV_TOT), D)
    head = jnp.arange(RET_HEADS, dtype=f32)
    jitter = jax.random.uniform(next(keys), (N_LAYERS_B, 2, RET_HEADS), f32, 0.0, 0.5)
    ret_log_decay = jnp.log(1.0 - 2.0 ** (-5.0 - head - jitter))
    ret_gn_gain = 1.0 + noise((N_LAYERS_B, RET_DV_TOT), 0.05)
    ret_w_out = nrm((N_LAYERS_B, RET_DV_TOT, D), RET_DV_TOT)

    swa_w_in = nrm((N_LAYERS_C, D, (SWA_HQ + 2 * SWA_HKV) * SWA_DH), D)
    swa_sink = noise((N_LAYERS_C, SWA_HQ), 0.5)
    swa_w_out = nrm((N_LAYERS_C, SWA_HQ * SWA_DH, D), SWA_HQ * SWA_DH)

    ffn_w_gu = nrm((N_DENSE, D, 2 * D_FF), D)
    ffn_w_down = nrm((N_DENSE, D_FF, D), D_FF)
    moe_router = nrm((N_MOE, D, N_EXPERTS), D)
    moe_w_gu = nrm((N_MOE, N_EXPERTS, D, 2 * D_FF), D)
    moe_w_down = nrm((N_MOE, N_EXPERTS, D_FF, D), D_FF)
    return {
        'x': x, 'c': c, 'ctx': ctx, 'c_ctx': c_ctx,
        'ada_w': ada_w, 'ada_b': ada_b, 'norm_g': norm_g, 'final_g': final_g,
        'lru_w_in': lru_w_in, 'lru_conv_w': lru_conv_w, 'lru_conv_b': lru_conv_b,
        'lru_w_gate': lru_w_gate, 'lru_b_gate': lru_b_gate, 'lru_lambda': lru_lambda,
        'lru_w_out': lru_w_out,
        'ret_w_in': ret_w_in, 'ret_log_decay': ret_log_decay, 'ret_gn_gain': ret_gn_gain,
        'ret_w_out': ret_w_out,
        'swa_w_in': swa_w_in, 'swa_sink': swa_sink, 'swa_w_out': swa_w_out,
        'ffn_w_gu': ffn_w_gu, 'ffn_w_down': ffn_w_down,
        'moe_router': moe_router, 'moe_w_gu': moe_w_gu, 'moe_w_down': moe_w_down,
    }


def reference(x, c, ctx, c_ctx, ada_w, ada_b, norm_g, final_g,
              lru_w_in, lru_conv_w, lru_conv_b, lru_w_gate, lru_b_gate, lru_lambda, lru_w_out,
              ret_w_in, ret_log_decay, ret_gn_gain, ret_w_out,
              swa_w_in, swa_sink, swa_w_out,
              ffn_w_gu, ffn_w_down,
              moe_router, moe_w_gu, moe_w_down):
    bsz, seq, dm = x.shape
    n_ctx = ctx.shape[1]
    cond_l = jax.nn.silu(c)
    cond_c = jax.nn.silu(c_ctx)
    xl, xc = x, ctx
    for i in range(DEPTH):
        last = i == DEPTH - 1
        mod_l = jnp.split((cond_l @ ada_w[i] + ada_b[i])[:, None, :], 6, axis=-1)
        mod_c = jnp.split(cond_c @ ada_w[i] + ada_b[i], 6, axis=-1)
        hl = _modulate(xl, norm_g[i, 0], mod_l[0], mod_l[1])
        hc = _modulate(xc, norm_g[i, 0], mod_c[0], mod_c[1])
        j = i // N_MIXERS
        if i % N_MIXERS == 0:
            ol, oc = _mixer_rglru(hl, hc, lru_w_in[j], lru_conv_w[j], lru_conv_b[j], lru_w_gate[j],
                                  lru_b_gate[j], lru_lambda[j], lru_w_out[j], not last)
        elif i % N_MIXERS == 1:
            ol, oc = _mixer_retention(hl, hc, ret_w_in[j], ret_log_decay[j], ret_gn_gain[j],
                                      ret_w_out[j], not last)
        else:
            ol, oc = _mixer_swa(hl, hc, swa_w_in[j], swa_sink[j], swa_w_out[j], not last)
        xl = xl + mod_l[2] * ol
        hl = _modulate(xl, norm_g[i, 1], mod_l[3], mod_l[4])
        f = i // 2
        if i % 2 == 0:
            ffn = functools.partial(_swiglu, w_gu=ffn_w_gu[f], w_down=ffn_w_down[f])
        else:
            ffn = functools.partial(_moe_swiglu, router=moe_router[f], w_gu=moe_w_gu[f], w_down=moe_w_down[f])
        if last:
            xl = xl + mod_l[5] * ffn(hl.reshape(-1, dm)).reshape(bsz, seq, dm)
        else:
            xc = xc + mod_c[2] * oc
            hc = _modulate(xc, norm_g[i, 1], mod_c[3], mod_c[4])
            y = ffn(jnp.concatenate([hl.reshape(-1, dm), hc.reshape(-1, dm)], axis=0))
            xl = xl + mod_l[5] * y[:bsz * seq].reshape(bsz, seq, dm)
            xc = xc + mod_c[5] * y[bsz * seq:].reshape(bsz, n_ctx, dm)
    return _rmsnorm(xl, final_g)
```

```python
import numpy as np
from contextlib import ExitStack
import concourse.bass as bass
import concourse.mybir as mybir
from concourse.bass_utils import run_bass_kernel_spmd

F32 = mybir.dt.float32
BF16 = mybir.dt.bfloat16
AF = mybir.ActivationFunctionType
ALU = mybir.AluOpType
AX = mybir.AxisListType

NPOOL = 8
SPARSE_MOE = True
SAME_ENGINE_SYNC = True

T = 4352
NCTX = 256
SEQ = 4096
D = 1024
DFF = 3584
EPS = 1e-6
TILES = [(0, 256, 1)] + [(256 + 512 * i, 512, 0) for i in range(8)]


class Prog:
    def __init__(self, nc, stack):
        self.nc = nc
        self.stack = stack
        self.eng = {'pe': nc.tensor, 'act': nc.scalar, 'dve': nc.vector, 'pool': nc.gpsimd, 'sp': nc.sync}
        self.sems = {}
        self.cnt = {e: 0 for e in self.eng}
        self.seen = {e: {} for e in self.eng}
        self.keys = {}
        self.dma_n = {'sp': 0, 'pool': 0}
        self.n_inst = 0

    def sem(self, sk):
        if sk not in self.sems:
            name = 's_' + (sk if isinstance(sk, str) else f'{sk[0]}q{sk[1]}')
            self.sems[sk] = self.stack.enter_context(self.nc.semaphore(name))
        return self.sems[sk]

    def op(self, eng, fn, reads=(), writes=(), dma=False):
        deps = {}
        own = None if dma else eng

        def add(sk, v):
            if deps.get(sk, 0) < v:
                deps[sk] = v
        for k in reads:
            st = self.keys.get(k)
            if st is not None and st[0] is not None:
                add(*st[0])
        for k in writes:
            st = self.keys.get(k)
            if st is not None:
                if st[0] is not None and st[0][0] != own:
                    add(*st[0])
                for sk, v in st[1].items():
                    if sk != own:
                        add(sk, v)
        if dma:
            n = self.dma_n[eng]
            slot = n % NPOOL
            semkey = (eng, slot)
            val = 16 * (n // NPOOL + 1)
            if n >= NPOOL:
                add(semkey, 16 * (n // NPOOL))
            self.dma_n[eng] += 1
            inc = 16
        else:
            self.cnt[eng] += 1
            semkey = eng
            val = self.cnt[eng]
            inc = 1
        h = self.eng[eng]
        for sk, v in deps.items():
            if sk == eng and (eng == 'pe' or not SAME_ENGINE_SYNC):
                continue
            if self.seen[eng].get(sk, 0) >= v:
                continue
            self.seen[eng][sk] = v
            h.wait_ge(self.sem(sk), v)
        inst = fn(h)
        inst.then_inc(self.sem(semkey), inc)
        self.n_inst += 1
        for k in reads:
            st = self.keys.setdefault(k, [None, {}])
            if st[1].get(semkey, 0) < val:
                st[1][semkey] = val
        for k in writes:
            self.keys[k] = [(semkey, val), {}]

    def _all_now(self):
        cur = {}
        for e in ('pe', 'act', 'dve', 'pool'):
            if self.cnt[e] > 0:
                cur[e] = self.cnt[e]
        for q, n in self.dma_n.items():
            for slot in range(min(n, NPOOL)):
                cur[(q, slot)] = 16 * (((n - 1 - slot) // NPOOL) + 1)
        return cur

    def barrier(self, engines=('pe', 'act', 'dve', 'pool', 'sp')):
        cur = self._all_now()
        for e in engines:
            h = self.eng[e]
            for sk, v in cur.items():
                if sk == e:
                    continue
                if self.seen[e].get(sk, 0) >= v:
                    continue
                self.seen[e][sk] = v
                h.wait_ge(self.sem(sk), v)
        if len(engines) == 5:
            self.keys.clear()

    def finish(self):
        self.barrier(engines=('sp',))


class K:
    pass


_SB_N = [0]


def sb(st, nc, name, shape, dt):
    _SB_N[0] += 1
    h = st.enter_context(nc.sbuf_tensor(f"{name}_u{_SB_N[0]}", shape, dt))
    return h[tuple(slice(None) for _ in shape)]


def col(v, n):
    return np.ascontiguousarray(np.asarray(v).reshape(n, 128).T)


def build_program(parts, debug=False):
    nc = bass.Bass("TRN2", target_bir_lowering=False)
    k = K()
    k.nc = nc
    dr = {}

    def din(name, shape, dt=F32):
        dr[name] = nc.dram_tensor(name, list(shape), dt, kind="ExternalInput").ap()
        return dr[name]

    k.xT = din("xT", [D, T])
    k.cc = din("cc", [128, 8, 2])
    k.ada_w = din("ada_w", [4, D, 6 * D])
    k.adab = din("adab", [128, 4, 48])
    k.adab_row = din("adab_row", [4, 2, 6144])
    k.ng = din("ng", [128, 4, 2, 8])
    k.fg = din("fg", [128, 8])
    k.ones_in = din("ones_f", [128, 128])
    k.ident_in = din("ident_f", [128, 128])
    k.esel_in = din("esel", [8, 8, 128])
    k.ffn_w_gu = din("ffn_w_gu", [2, D, 2 * DFF])
    k.ffn_w_down = din("ffn_w_down", [2, DFF, D])
    k.router = din("router", [128, 2, 8, 8])
    k.moe_w_gu = din("moe_w_gu", [2, 8, D, 2 * DFF])
    k.moe_w_down = din("moe_w_down", [2, 8, DFF, D])
    k.lru_w_in = din("lru_w_in", [2, D, 2560])
    k.lru_wg = din("lru_wg", [2, 128, 40, 128])
    k.lru_w_out = din("lru_w_out", [2, 1280, D])
    k.lru_cw_in = din("lru_cw", [128, 2, 4, 10])
    k.lru_cb_in = din("lru_cb", [128, 2, 10])
    k.lru_bg_in = din("lru_bg", [128, 2, 2, 2, 10])
    k.lru_lam_in = din("lru_lam", [128, 2, 2, 10])
    k.ret_w_in = din("ret_w_in", [1, D, 6144])
    k.ret_w_out = din("ret_w_out", [1, 2048, D])
    k.ret_ld_in = din("ret_ld", [128, 8])
    k.ret_gain_in = din("ret_gain", [128, 16])
    k.ret_cosT = din("ret_cosT", [128, SEQ])
    k.ret_sinT = din("ret_sinT", [128, SEQ])
    k.ret_costok = din("ret_costok", [SEQ, 128])
    k.ret_sintok = din("ret_sintok", [SEQ, 128])
    k.ret_tab = din("ret_tab", [128, 771])
    k.swa_w_ext = din("swa_w_ext", [D, 2816])
    k.swa_w_out = din("swa_w_out", [1, D, D])
    k.swa_sink_in = din("swa_sink", [64, 16])
    k.swa_cos = din("swa_cos", [128, SEQ])
    k.swa_sin = din("swa_sin", [128, SEQ])
    k.moe_tab = din("moe_tab", [128, 161])
    k.dr = dr

    if debug:
        k.out = nc.dram_tensor("xdump", [D, T], F32, kind="ExternalOutput").ap()
    else:
        k.out = nc.dram_tensor("outT", [D, SEQ], F32, kind="ExternalOutput").ap()
    k.X = nc.dram_tensor("Xres", [D, T], F32, kind="Internal").ap()
    k.WGU = nc.dram_tensor("WGUs", [18, D, 2 * DFF], BF16, kind="Internal").ap() if not SPARSE_MOE else nc.dram_tensor("WGUs", [10, D, 2 * DFF], BF16, kind="Internal").ap()
    k.WD = nc.dram_tensor("WDs", [18, DFF, D], BF16, kind="Internal").ap() if not SPARSE_MOE else nc.dram_tensor("WDs", [10, DFF, D], BF16, kind="Internal").ap()
    k.QT = nc.dram_tensor("QTs", [1024, T], BF16, kind="Internal").ap()
    k.KT = nc.dram_tensor("KTs", [1024, T], BF16, kind="Internal").ap()
    k.KTOK = nc.dram_tensor("KTOKs", [T, 1024], BF16, kind="Internal").ap()
    k.VTOK = nc.dram_tensor("VTOKs", [T, 2048], BF16, kind="Internal").ap()
    k.GS = nc.dram_tensor("GSs", [2048, T], BF16, kind="Internal").ap()
    k.SBd = nc.dram_tensor("SBds", [4, NCH, 256, 512], BF16, kind="Internal").ap()
    k.ZR = nc.dram_tensor("ZRs", [2048, T], BF16, kind="Internal").ap()
    k.QS = nc.dram_tensor("QSs", [64, 16, T], BF16, kind="Internal").ap()
    k.KS = nc.dram_tensor("KSs", [64, 4, T], BF16, kind="Internal").ap()
    k.VS = nc.dram_tensor("VSs", [T, 256], BF16, kind="Internal").ap()
    k.OS = nc.dram_tensor("OSs", [64, 16, T], BF16, kind="Internal").ap()
    k.HC = nc.dram_tensor("HCs", [NBLK_MAX * 512, 1024], BF16, kind="Internal").ap()
    k.YC = nc.dram_tensor("YCs", [NBLK_MAX * 512, 1024], F32, kind="Internal").ap()
    k.WGUx = nc.dram_tensor("WGUx", [16 * 7 * 128, 8192], BF16, kind="Internal").ap()
    k.WDx = nc.dram_tensor("WDx", [16 * 2 * 128, 28 * 512], BF16, kind="Internal").ap()
    k.XR = nc.dram_tensor("XRs", [1280, T], F32, kind="Internal").ap()
    k.YG = nc.dram_tensor("YGs", [1280, T], BF16, kind="Internal").ap()
    k.ZG = nc.dram_tensor("ZGs", [1280, T], BF16, kind="Internal").ap()

    with ExitStack() as st:
        P = Prog(nc, st)
        k.P = P
        k.PS = [st.enter_context(nc.psum_tensor(f"ps{i}", [128, 512], F32))[:, :] for i in range(8)]
        k.ps_n = 0
        k.cond = sb(st, nc, "cond", [128, 8, 2], F32)
        k.mods = sb(st, nc, "mods", [128, 4, 48, 2], F32)
        k.acoef = sb(st, nc, "acoef", [128, 4, 2, 8, 2], F32)
        k.adab_s = sb(st, nc, "adab_s", [128, 4, 48], F32)
        k.ng_s = sb(st, nc, "ng_s", [128, 4, 2, 8], F32)
        k.fg_s = sb(st, nc, "fg_s", [128, 8], F32)
        k.ones_f = sb(st, nc, "ones_fs", [128, 128], F32)
        k.ident_f = sb(st, nc, "ident_fs", [128, 128], F32)
        k.esel = sb(st, nc, "esel_s", [8, 8, 128], F32)
        k.rt = sb(st, nc, "rt_s", [128, 2, 8, 8], F32)
        k.epsb = sb(st, nc, "epsb", [128, 1], F32)
        P.op('dve', lambda e: e.memset(k.epsb, EPS), writes=['epsb'])
        k.ones_b = sb(st, nc, "ones_b", [128, 128], BF16)
        P.op('dve', lambda e: e.memset(k.ones_b, 1.0), writes=['ones_b'])
        k.oneb = sb(st, nc, "oneb", [128, 1], F32)
        P.op('dve', lambda e: e.memset(k.oneb, 1.0), writes=['oneb'])
        k.lru_cw = sb(st, nc, "lru_cw_s", [128, 2, 4, 10], F32)
        k.lru_cb = sb(st, nc, "lru_cb_s", [128, 2, 10], F32)
        k.lru_bg = sb(st, nc, "lru_bg_s", [128, 2, 2, 2, 10], F32)
        k.lru_lam = sb(st, nc, "lru_lam_s", [128, 2, 2, 10], F32)
        for dst, src in ((k.lru_cw, k.lru_cw_in), (k.lru_cb, k.lru_cb_in), (k.lru_bg, k.lru_bg_in), (k.lru_lam, k.lru_lam_in)):
            P.op('sp', lambda e, dst=dst, src=src: e.dma_start(out=dst, in_=src), writes=['lrup'], dma=True)
        k.ret_ld = sb(st, nc, "ret_ld_s", [128, 8], F32)
        k.ret_gain = sb(st, nc, "ret_gain_s", [128, 16], F32)
        for dst, src in ((k.ret_ld, k.ret_ld_in), (k.ret_gain, k.ret_gain_in)):
            P.op('sp', lambda e, dst=dst, src=src: e.dma_start(out=dst, in_=src), writes=['retp'], dma=True)
        k.swa_sink = sb(st, nc, "swa_sink_s", [64, 16], F32)
        P.op('sp', lambda e: e.dma_start(out=k.swa_sink, in_=k.swa_sink_in), writes=['swap'], dma=True)
        for dst, src, key in ((k.adab_s, k.adab, 'adab'), (k.ng_s, k.ng, 'ng'), (k.fg_s, k.fg, 'fg'),
                              (k.ones_f, k.ones_in, 'ones'), (k.ident_f, k.ident_in, 'ident'),
                              (k.esel, k.esel_in, 'esel'), (k.rt, k.router, 'rt')):
            P.op('sp', lambda e, dst=dst, src=src: e.dma_start(out=dst, in_=src), writes=[key], dma=True)

        if 'init' in parts:
            P.op('sp', lambda e: e.dma_start(out=k.X, in_=k.xT), writes=['X'], dma=True)
        k.precast_done = set()
        k.cond_done = False
        k.mods_split = ('mods' in parts and 'mix0' in parts)
        k.fuse_final = ('final' in parts and 'ffn3' in parts and SPARSE_MOE and not debug)
        k.pc_queue = []
        if 'mods' in parts:
            stage_mods(k, layers=(0,) if k.mods_split else (0, 1, 2, 3))
        for i in range(4):
            if f'ffn{i}' in parts:
                if SPARSE_MOE and i % 2 == 1:
                    for e8 in range(8):
                        k.pc_queue.append(lambda pe=(i // 2) * 8 + e8: precast_moe(k, pe))
                else:
                    for e8 in (range(8) if i % 2 == 1 else range(1)):
                        k.pc_queue.append(lambda p=ffn_pass_index(i, e8): precast(k, p))
            if f'mix{i}' in parts:
                if i % 3 == 0:
                    stage_lru(k, i)
                elif i % 3 == 1:
                    stage_ret(k, i)
                else:
                    stage_swa(k, i)
            if f'ffn{i}' in parts:
                if SPARSE_MOE and i % 2 == 1:
                    stage_moe_sparse(k, i)
                else:
                    drain_precast(k, 1000)
                    stage_ffn(k, i)
        if 'final' in parts and not k.fuse_final:
            stage_final(k)
        P.barrier()
        if debug:
            P.op('sp', lambda e: e.dma_start(out=k.out, in_=k.X), dma=True)
        P.finish()
        k.n_inst = P.n_inst
    return nc, k


def psum(k):
    b = k.ps_n % 8
    k.ps_n += 1
    return b, k.PS[b]


def stage_mods(k, layers=(0, 1, 2, 3), final_barrier=True, outer=None, cbw=512):
    nc, P = k.nc, k.P
    with ExitStack() as own:
        st = outer if outer is not None else own
        wa = [sb(st, nc, f"wa{j}", [128, 8, cbw], F32) for j in range(2)]
        mrow = [sb(st, nc, f"mrow{j}", [2, cbw], F32) for j in range(2)]
        nj = cbw // 128
        if not k.cond_done:
            ccs = sb(st, nc, "ccs", [128, 8, 2], F32)
            P.op('sp', lambda e: e.dma_start(out=ccs, in_=k.cc), writes=['ccs'], dma=True)
            P.op('act', lambda e: e.activation(out=k.cond, in_=ccs, func=AF.Silu), reads=['ccs'], writes=['cond'])
            k.cond_done = True
        n = 0
        for i in layers:
            wsrc = k.ada_w[i].rearrange("(kc p) j -> p kc j", p=128)
            for cb in range(6144 // cbw):
                buf = wa[n % 2]
                key = ('wa', n % 2)
                mr = mrow[n % 2]
                kmr = ('mrow', n % 2)
                P.op('sp', lambda e: e.dma_start(out=buf, in_=wsrc[:, :, cb * cbw:(cb + 1) * cbw]), writes=[key], dma=True)
                b, ps = psum(k)
                for kc in range(8):
                    P.op('pe', lambda e: e.matmul(ps[0:2, 0:cbw], lhsT=k.cond[:, kc, :], rhs=buf[:, kc, :], start=(kc == 0), stop=(kc == 7)),
                         reads=[key, 'cond'], writes=[('ps', b)])
                P.op('dve', lambda e: e.tensor_copy(out=mr, in_=ps[0:2, 0:cbw]), reads=[('ps', b)], writes=[kmr])
                b2, ps2 = psum(k)
                for jj in range(nj):
                    P.op('pe', lambda e: e.transpose(out=ps2[:, jj * 2:jj * 2 + 2], in_=mr[0:2, jj * 128:(jj + 1) * 128], identity=k.ident_f[0:2, 0:2]),
                         reads=[kmr, 'ident'], writes=[('ps', b2)])
                P.op('dve', lambda e: e.tensor_tensor(out=k.mods[:, i, cb * nj:(cb + 1) * nj, :], in0=ps2[:, 0:2 * nj].rearrange("p (j s) -> p j s", s=2),
                                                      in1=k.adab_s[:, i, cb * nj:(cb + 1) * nj].unsqueeze(2).to_broadcast([128, nj, 2]), op=ALU.add),
                     reads=[('ps', b2), 'adab'], writes=['mods'])
                n += 1
            for j in range(2):
                for s in range(2):
                    m = (1 + 3 * j) * 8
                    P.op('dve', lambda e: e.tensor_scalar(out=k.acoef[:, i, j, :, s], in0=k.mods[:, i, m:m + 8, s],
                                                          scalar1=1.0, scalar2=None, op0=ALU.add),
                         reads=['mods'], writes=['acoef'])
                    P.op('dve', lambda e: e.tensor_tensor(out=k.acoef[:, i, j, :, s], in0=k.acoef[:, i, j, :, s],
                                                          in1=k.ng_s[:, i, j, :], op=ALU.mult),
                         reads=['acoef', 'ng'], writes=['acoef'])
    if final_barrier:
        P.barrier()


def norm_tile(k, xt, kx, n, i, j, s, hb, kh, sq, rs, tmps, hf=None, khf=None):
    P = k.P
    sqb = sq.bitcast(BF16)[:, :, 0:512]
    P.op('act', lambda e: e.activation(out=sqb[:, :, :n], in_=xt[:, :, :n], func=AF.Square), reads=[kx], writes=['sq'])
    b, ps = psum(k)
    for kc in range(8):
        P.op('pe', lambda e: e.matmul(ps[:, :n], lhsT=k.ones_b[:, :], rhs=sqb[:, kc, :n], start=(kc == 0), stop=(kc == 7)),
             reads=['sq', 'ones_b'], writes=[('ps', b)])
    P.op('act', lambda e: e.activation(out=rs[:, :n], in_=ps[:, :n], func=AF.Ln, scale=1.0 / D, bias=k.epsb[:, 0:1]),
         reads=[('ps', b)], writes=['rs'])
    P.op('act', lambda e: e.activation(out=rs[:, :n], in_=rs[:, :n], func=AF.Exp, scale=-0.5), reads=['rs'], writes=['rs'])
    for kc in range(8):
        tmp = tmps[kc % 2]
        kt = ('ntmp', kc % 2)
        P.op('dve', lambda e: e.scalar_tensor_tensor(out=tmp[:, :n], in0=xt[:, kc, :n], scalar=k.acoef[:, i, j, kc, s:s + 1],
                                                     in1=rs[:, :n], op0=ALU.mult, op1=ALU.mult),
             reads=[kx, 'rs', 'acoef'], writes=[kt])
        sh = k.mods[:, i, (3 * j) * 8 + kc, s:s + 1]
        if hf is None:
            P.op('act', lambda e: e.activation(out=hb[:, kc, :n], in_=tmp[:, :n], func=AF.Identity, bias=sh, scale=1.0),
                 reads=[kt, 'mods'], writes=[kh])
        else:
            P.op('act', lambda e: e.activation(out=hf[:, kc, :n], in_=tmp[:, :n], func=AF.Identity, bias=sh, scale=1.0),
                 reads=[kt, 'mods'], writes=[khf])
    if hf is not None:
        P.op('dve', lambda e: e.tensor_copy(out=hb[:, :, :n], in_=hf[:, :, :n]), reads=[khf], writes=[kh])


def ffn_pass_index(i, e):
    return {0: 0, 1: 1 + e, 2: 9, 3: 10 + e}[i]


def precast(k, p):
    if p in k.precast_done:
        return
    k.precast_done.add(p)
    P = k.P
    if p == 0:
        gu, dn = k.ffn_w_gu[0], k.ffn_w_down[0]
    elif p == 9:
        gu, dn = k.ffn_w_gu[1], k.ffn_w_down[1]
    elif p < 9:
        gu, dn = k.moe_w_gu[0, p - 1], k.moe_w_down[0, p - 1]
    else:
        gu, dn = k.moe_w_gu[1, p - 10], k.moe_w_down[1, p - 10]
    P.op('pool', lambda e: e.dma_start(out=k.WGU[p].rearrange("r (a b) -> (r a) b", b=1024),
                                       in_=gu.rearrange("r (a b) -> (r a) b", b=1024)),
         writes=[('wgus', p)], dma=True)
    P.op('pool', lambda e: e.dma_start(out=k.WD[p], in_=dn), writes=[('wds', p)], dma=True)


def stage_ffn(k, i):
    nc, P = k.nc, k.P
    moe = (i % 2 == 1)
    last = (i == 3)
    f = i // 2
    experts = list(range(8)) if moe else [0]
    tiles = TILES[1:] if last else TILES
    precast(k, ffn_pass_index(i, 0))
    with ExitStack() as st:
        wgu = [sb(st, nc, f"wgu{j}", [128, 8, 2, 512], BF16) for j in range(2)]
        wd = [sb(st, nc, f"wd{j}", [128, 28, 256], BF16) for j in range(2)]
        nbuf = 1 if moe else 2
        Hs = [sb(st, nc, f"Hb{q}", [128, 8, 512], BF16) for q in range(nbuf)]
        xts = [sb(st, nc, f"xt{q}", [128, 8, 512], F32) for q in range(nbuf)]
        act = sb(st, nc, "actb", [128, 28, 512], BF16)
        sq = sb(st, nc, "sq", [128, 8, 512], F32)
        rs = sb(st, nc, "rs", [128, 512], F32)
        tmps = [sb(st, nc, f"ntmp{j}", [128, 512], F32) for j in range(2)]
        sg = [sb(st, nc, f"sg{j}", [128, 512], BF16) for j in range(2)]
        if moe:
            hf = sb(st, nc, "hf", [128, 8, 512], F32)
            yacc = sb(st, nc, "yacc", [128, 8, 512], F32)
            cmul = [sb(st, nc, f"cmul{j}", [128, 512], F32) for j in range(2)]
            combT = sb(st, nc, "combT", [8, 512], F32)
            rsm = sb(st, nc, "rsm", [128, 64], F32)

        seq_gu = [(ti, e, g) for ti in range(len(tiles)) for e in experts for g in range(7)]
        seq_wd = [(ti, e, q) for ti in range(len(tiles)) for e in experts for q in range(4)]
        st_gu = {'n': 0}
        st_wd = {'n': 0}

        def ensure_gu(upto):
            while st_gu['n'] <= upto and st_gu['n'] < len(seq_gu):
                a = st_gu['n']
                ti, e, g = seq_gu[a]
                p = ffn_pass_index(i, e)
                if ti == 0:
                    precast(k, p)
                buf = wgu[a % 2]
                for hh in range(2):
                    src = k.WGU[p].rearrange("(kc q) c -> q kc c", q=128)[:, :, hh * DFF + g * 512:hh * DFF + (g + 1) * 512]
                    P.op('sp', lambda e_: e_.dma_start(out=buf[:, :, hh, :], in_=src), reads=[('wgus', p)],
                         writes=[('wgu', a % 2, hh)], dma=True)
                st_gu['n'] += 1

        def ensure_wd(upto):
            while st_wd['n'] <= upto and st_wd['n'] < len(seq_wd):
                a = st_wd['n']
                ti, e, q = seq_wd[a]
                p = ffn_pass_index(i, e)
                src = k.WD[p].rearrange("(fc q) d -> q fc d", q=128)[:, :, q * 256:(q + 1) * 256]
                buf = wd[a % 2]
                P.op('sp', lambda e_: e_.dma_start(out=buf, in_=src), reads=[('wds', p)], writes=[('wd', a % 2)], dma=True)
                st_wd['n'] += 1

        a_gu = 0
        a_wd = 0
        nsg = 0
        def prep(ti):
            c0, n, s = tiles[ti]
            xt, H = xts[ti % nbuf], Hs[ti % nbuf]
            kxt, kH = ('xt', ti % nbuf), ('H', ti % nbuf)
            P.op('sp', lambda e: e.dma_start(out=xt[:, :, :n], in_=k.X.rearrange("(kc q) t -> q kc t", q=128)[:, :, c0:c0 + n]),
                 reads=['X', ('Xt', c0)], writes=[kxt], dma=True)
            if moe:
                norm_tile(k, xt, kxt, n, i, 1, s, H, kH, sq, rs, tmps, hf=hf, khf='hf')
                route_tile(k, f, n, hf, combT, rsm)
            else:
                norm_tile(k, xt, kxt, n, i, 1, s, H, kH, sq, rs, tmps)

        ensure_gu(0)
        if not moe:
            prep(0)
        for ti, (c0, n, s) in enumerate(tiles):
            xt, H = xts[ti % nbuf], Hs[ti % nbuf]
            kxt, kH = ('xt', ti % nbuf), ('H', ti % nbuf)
            if moe:
                prep(ti)
            elif ti + 1 < len(tiles):
                prep(ti + 1)
            ensure_gu(a_gu)
            for e in experts:
                if moe:
                    cm = cmul[e % 2]
                    kcm = ('cmul', e % 2)
                    b, ps = psum(k)
                    P.op('pe', lambda e_: e_.matmul(ps[:, :n], lhsT=k.esel[0:8, e, :], rhs=combT[0:8, :n], start=True, stop=True),
                         reads=['esel', 'combT'], writes=[('ps', b)])
                    P.op('act', lambda e_: e_.activation(out=cm[:, :n], in_=ps[:, :n], func=AF.Copy),
                         reads=[('ps', b)], writes=[kcm])
                ensure_wd(a_wd)
                for g in range(7):
                    ensure_gu(a_gu + 1)
                    buf = wgu[a_gu % 2]
                    kb0 = ('wgu', a_gu % 2, 0)
                    kb1 = ('wgu', a_gu % 2, 1)
                    for j in range(4):
                        fch = g * 4 + j
                        bg, psg = psum(k)
                        for kc in range(8):
                            P.op('pe', lambda e_: e_.matmul(psg[:, :n], lhsT=buf[:, kc, 0, j * 128:(j + 1) * 128], rhs=H[:, kc, :n],
                                                            start=(kc == 0), stop=(kc == 7)),
                                 reads=[kb0, kH], writes=[('ps', bg)])
                        bu, psu = psum(k)
                        for kc in range(8):
                            P.op('pe', lambda e_: e_.matmul(psu[:, :n], lhsT=buf[:, kc, 1, j * 128:(j + 1) * 128], rhs=H[:, kc, :n],
                                                            start=(kc == 0), stop=(kc == 7)),
                                 reads=[kb1, kH], writes=[('ps', bu)])
                        sgt = sg[nsg % 2]
                        ksg = ('sg', nsg % 2)
                        nsg += 1
                        P.op('act', lambda e_: e_.activation(out=sgt[:, :n], in_=psg[:, :n], func=AF.Silu),
                             reads=[('ps', bg)], writes=[ksg])
                        P.op('dve', lambda e_: e_.tensor_tensor(out=act[:, fch, :n], in0=sgt[:, :n], in1=psu[:, :n], op=ALU.mult),
                             reads=[ksg, ('ps', bu)], writes=[('act', fch)])
                    a_gu += 1
                ensure_gu(a_gu)
                for q in range(4):
                    ensure_wd(a_wd + 1)
                    buf = wd[a_wd % 2]
                    kb = ('wd', a_wd % 2)
                    for dj in range(2):
                        dc = q * 2 + dj
                        b, ps = psum(k)
                        for fc in range(28):
                            P.op('pe', lambda e_: e_.matmul(ps[:, :n], lhsT=buf[:, fc, dj * 128:(dj + 1) * 128], rhs=act[:, fc, :n],
                                                            start=(fc == 0), stop=(fc == 27)),
                                 reads=[kb, ('act', fc)], writes=[('ps', b)])
                        gate = k.mods[:, i, 5 * 8 + dc, s:s + 1]
                        if not moe:
                            P.op('dve', lambda e_: e_.scalar_tensor_tensor(out=xt[:, dc, :n], in0=ps[:, :n], scalar=gate,
                                                                           in1=xt[:, dc, :n], op0=ALU.mult, op1=ALU.add),
                                 reads=[('ps', b), kxt, 'mods'], writes=[kxt])
                        else:
                            if e == 0:
                                P.op('dve', lambda e_: e_.tensor_tensor(out=yacc[:, dc, :n], in0=ps[:, :n], in1=cm[:, :n], op=ALU.mult),
                                     reads=[('ps', b), kcm], writes=[('yacc', dc)])
                            else:
                                tmp = tmps[dc % 2]
                                kt = ('ntmp', dc % 2)
                                P.op('dve', lambda e_: e_.tensor_tensor(out=tmp[:, :n], in0=ps[:, :n], in1=cm[:, :n], op=ALU.mult),
                                     reads=[('ps', b), kcm], writes=[kt])
                                P.op('dve', lambda e_: e_.tensor_tensor(out=yacc[:, dc, :n], in0=yacc[:, dc, :n], in1=tmp[:, :n], op=ALU.add),
                                     reads=[kt, ('yacc', dc)], writes=[('yacc', dc)])
                            if e == experts[-1]:
                                P.op('dve', lambda e_: e_.scalar_tensor_tensor(out=xt[:, dc, :n], in0=yacc[:, dc, :n], scalar=gate,
                                                                               in1=xt[:, dc, :n], op0=ALU.mult, op1=ALU.add),
                                     reads=[('yacc', dc), kxt, 'mods'], writes=[kxt])
                    a_wd += 1
            P.op('sp', lambda e: e.dma_start(out=k.X.rearrange("(kc q) t -> q kc t", q=128)[:, :, c0:c0 + n], in_=xt[:, :, :n]),
                 reads=[kxt], writes=[('Xt', c0)], dma=True)
    P.barrier()


def route_tile(k, f, n, hf, combT, rsm):
    P = k.P
    for bi in range(n // 128):
        b, ps = psum(k)
        for kc in range(8):
            P.op('pe', lambda e: e.matmul(ps[:, 0:8], lhsT=hf[:, kc, bi * 128:(bi + 1) * 128], rhs=k.rt[:, f, kc, :],
                                          start=(kc == 0), stop=(kc == 7)),
                 reads=['hf', 'rt'], writes=[('ps', b)])
        lg, eq1, lg2, eq2, comb = (rsm[:, 8 * j:8 * j + 8] for j in range(5))
        m1, m2, dd, w1, w2 = (rsm[:, 40 + j:41 + j] for j in range(5))
        R = ['rsm']
        P.op('dve', lambda e: e.tensor_copy(out=lg, in_=ps[:, 0:8]), reads=[('ps', b)], writes=R)
        P.op('dve', lambda e: e.tensor_reduce(out=m1, in_=lg, axis=AX.X, op=ALU.max), reads=R, writes=R)
        P.op('dve', lambda e: e.tensor_scalar(out=eq1, in0=lg, scalar1=m1, scalar2=None, op0=ALU.is_equal), reads=R, writes=R)
        P.op('dve', lambda e: e.scalar_tensor_tensor(out=lg2, in0=eq1, scalar=-1e30, in1=lg, op0=ALU.mult, op1=ALU.add), reads=R, writes=R)
        P.op('dve', lambda e: e.tensor_reduce(out=m2, in_=lg2, axis=AX.X, op=ALU.max), reads=R, writes=R)
        P.op('dve', lambda e: e.tensor_scalar(out=eq2, in0=lg2, scalar1=m2, scalar2=None, op0=ALU.is_equal), reads=R, writes=R)
        P.op('dve', lambda e: e.tensor_tensor(out=dd, in0=m2, in1=m1, op=ALU.subtract), reads=R, writes=R)
        P.op('act', lambda e: e.activation(out=w1, in_=dd, func=AF.Sigmoid, scale=-1.0), reads=R, writes=R)
        P.op('act', lambda e: e.activation(out=w2, in_=dd, func=AF.Sigmoid, scale=1.0), reads=R, writes=R)
        P.op('dve', lambda e: e.tensor_scalar(out=comb, in0=eq1, scalar1=w1, scalar2=None, op0=ALU.mult), reads=R, writes=R)
        P.op('dve', lambda e: e.scalar_tensor_tensor(out=comb, in0=eq2, scalar=w2, in1=comb, op0=ALU.mult, op1=ALU.add), reads=R, writes=R)
        b2, ps2 = psum(k)
        P.op('pe', lambda e: e.transpose(out=ps2[0:8, 0:128], in_=comb, identity=k.ident_f[:, :]),
             reads=R + ['ident'], writes=[('ps', b2)])
        P.op('act', lambda e: e.activation(out=combT[0:8, bi * 128:(bi + 1) * 128], in_=ps2[0:8, 0:128], func=AF.Copy),
             reads=[('ps', b2)], writes=['combT'])


def _consts():
    ones = np.ones((128, 128), np.float32)
    ident = np.eye(128, dtype=np.float32)
    esel = np.zeros((8, 8, 128), np.float32)
    for e in range(8):
        esel[e, e, :] = 1.0
    return ones, ident, esel


def _ret_consts():
    f32 = np.float32
    theta = (f32(10000.0) ** (-np.arange(128, dtype=f32) / f32(128))).astype(f32)
    ang = (np.arange(SEQ, dtype=f32)[:, None] * theta[None, :]).astype(f32)
    cos, sin = np.cos(ang).astype(f32), np.sin(ang).astype(f32)
    p = np.arange(128, dtype=f32)[:, None]
    fr = np.arange(128, dtype=f32)[None, :]
    tab = np.zeros((128, 771), f32)
    tab[:, 0:128] = np.maximum(fr - p, 0)
    tab[:, 128:256] = np.maximum(p - fr, 0)
    tab[:, 256:384] = (fr >= p)
    tab[:, 384:512] = (p >= fr)
    tab[:, 512:640] = fr + 1
    tab[:, 640:768] = 128 - fr
    tab[:, 768] = 127 - p[:, 0]
    tab[:, 769] = p[:, 0]
    tab[:, 770] = 128
    return (np.ascontiguousarray(cos.T), np.ascontiguousarray(sin.T), cos, sin, tab)


def _swa_consts(w_in):
    f32 = np.float32
    qk = w_in[:, :1280].reshape(D, 20, 2, 32)
    sw = qk[:, :, ::-1, :].reshape(D, 1280)
    w_ext = np.ascontiguousarray(np.concatenate([w_in, sw], axis=1))
    rows = SEQ // 64
    row = np.repeat(np.arange(rows, dtype=f32), 64)
    colp = np.tile(np.arange(64, dtype=f32), rows)
    freq = (f32(10000.0) ** (-np.arange(16, dtype=f32) / f32(16))).astype(f32)
    ang = np.concatenate([row[:, None] * freq, colp[:, None] * freq], axis=-1).astype(f32)
    cos, sin = np.cos(ang).astype(f32), np.sin(ang).astype(f32)
    cos_full = np.concatenate([cos, cos], axis=1).T
    sin_signed = np.concatenate([-sin, sin], axis=1).T
    return w_ext, np.ascontiguousarray(np.tile(cos_full, (2, 1))), np.ascontiguousarray(np.tile(sin_signed, (2, 1)))


def make_in_maps(inp, cores):
    ones, ident, esel = _consts()
    adab = np.ascontiguousarray(inp['ada_b'].reshape(4, 48, 128).transpose(2, 0, 1))
    ng = np.ascontiguousarray(inp['norm_g'].reshape(4, 2, 8, 128).transpose(3, 0, 1, 2))
    fg = col(inp['final_g'], 8)
    router = np.ascontiguousarray(inp['moe_router'].reshape(2, 8, 128, 8).transpose(2, 0, 1, 3))
    lru_wg = np.ascontiguousarray(inp['lru_w_gate'].transpose(0, 4, 1, 2, 3, 5).reshape(2, 128, 40, 128))
    lru_cw = np.ascontiguousarray(inp['lru_conv_w'].reshape(2, 4, 10, 128).transpose(3, 0, 1, 2))
    lru_cb = np.ascontiguousarray(inp['lru_conv_b'].reshape(2, 10, 128).transpose(2, 0, 1))
    lru_bg = np.ascontiguousarray(inp['lru_b_gate'].reshape(2, 2, 2, 10, 128).transpose(4, 0, 1, 2, 3))
    lru_lam = np.ascontiguousarray(inp['lru_lambda'].reshape(2, 2, 10, 128).transpose(3, 0, 1, 2))
    ret_ld = np.ascontiguousarray(np.broadcast_to(inp['ret_log_decay'].reshape(1, 8), (128, 8))).astype(np.float32)
    ret_gain = col(inp['ret_gn_gain'][0], 16)
    ret_cosT, ret_sinT, ret_costok, ret_sintok, ret_tab = _ret_consts()
    swa_w_ext, swa_cos, swa_sin = _swa_consts(inp['swa_w_in'][0])
    swa_sink = np.ascontiguousarray(np.broadcast_to(inp['swa_sink'].reshape(1, 16), (64, 16))).astype(np.float32)
    adab_row = np.ascontiguousarray(np.broadcast_to(inp['ada_b'][:, None, :], (4, 2, 6144))).astype(np.float32)
    moe_tab = np.zeros((128, 161), np.float32)
    pp_ = np.arange(128, dtype=np.float32)
    moe_tab[:, 0:128] = (pp_[:, None] < pp_[None, :])
    moe_tab[:, 128:152] = np.arange(24, dtype=np.float32)[None, :]
    moe_tab[:, 152:159] = np.arange(7, dtype=np.float32)[None, :] * 128 + pp_[:, None]
    moe_tab[:, 159:161] = np.arange(2, dtype=np.float32)[None, :] * 128 + pp_[:, None]
    maps = []
    for b in cores:
        xT = np.ascontiguousarray(np.concatenate([inp['ctx'][b], inp['x'][b]], axis=0).T)
        cc = np.ascontiguousarray(np.stack([inp['c'][b], inp['c_ctx']], -1).reshape(8, 128, 2).transpose(1, 0, 2))
        maps.append(dict(adab_row=adab_row, moe_tab=moe_tab, swa_w_ext=swa_w_ext, swa_w_out=inp['swa_w_out'], swa_sink=swa_sink, swa_cos=swa_cos, swa_sin=swa_sin,
                         ret_w_in=inp['ret_w_in'], ret_w_out=inp['ret_w_out'], ret_ld=ret_ld, ret_gain=ret_gain, ret_cosT=ret_cosT,
                         ret_sinT=ret_sinT, ret_costok=ret_costok, ret_sintok=ret_sintok, ret_tab=ret_tab,
                         lru_w_in=inp['lru_w_in'], lru_wg=lru_wg, lru_w_out=inp['lru_w_out'], lru_cw=lru_cw, lru_cb=lru_cb,
                         lru_bg=lru_bg, lru_lam=lru_lam, xT=xT, cc=cc, ada_w=inp['ada_w'], adab=adab, ng=ng, fg=fg, ones_f=ones, ident_f=ident,
                         esel=esel, ffn_w_gu=inp['ffn_w_gu'], ffn_w_down=inp['ffn_w_down'], router=router,
                         moe_w_gu=inp['moe_w_gu'], moe_w_down=inp['moe_w_down']))
    return maps


def xrow(k):
    return k.X.rearrange("(kc q) t -> q kc t", q=128)


def load_x_tile(k, xt, c0, n, key='xt'):
    k.P.op('sp', lambda e: e.dma_start(out=xt[:, :, :n], in_=xrow(k)[:, :, c0:c0 + n]),
           reads=['X', ('Xt', c0)], writes=[key], dma=True)


def store_x_tile(k, xt, c0, n):
    k.P.op('sp', lambda e: e.dma_start(out=xrow(k)[:, :, c0:c0 + n], in_=xt[:, :, :n]),
           reads=['xt'], writes=[('Xt', c0)], dma=True)


def out_proj_residual(k, i, tiles, Zd, nz, kz, wo, zt_shape_k, lhs_fn):
    nc, P = k.nc, k.P
    with ExitStack() as st:
        xt = sb(st, nc, "xt_o", [128, 8, 512], F32)
        zt = [sb(st, nc, f"zt_o{j}", [zt_shape_k, nz, 512], BF16) for j in range(2)]
        for ti, (c0, n, s) in enumerate(tiles):
            z = zt[ti % 2]
            kzt = ('zt', ti % 2)
            P.op('sp', lambda e: e.dma_start(out=z[:, :, :n], in_=Zd[:, :, c0:c0 + n]), reads=[kz], writes=[kzt], dma=True)
            load_x_tile(k, xt, c0, n)
            for dc in range(8):
                b, ps = psum(k)
                for zc in range(nz):
                    P.op('pe', lambda e: e.matmul(ps[:, :n], lhsT=lhs_fn(zc, dc), rhs=z[:, zc, :n], start=(zc == 0), stop=(zc == nz - 1)),
                         reads=[kzt, 'wo'], writes=[('ps', b)])
                gate = k.mods[:, i, 2 * 8 + dc, s:s + 1]
                P.op('dve', lambda e: e.scalar_tensor_tensor(out=xt[:, dc, :n], in0=ps[:, :n], scalar=gate, in1=xt[:, dc, :n],
                                                             op0=ALU.mult, op1=ALU.add),
                     reads=[('ps', b), 'xt', 'mods'], writes=['xt'])
            store_x_tile(k, xt, c0, n)
    P.barrier()


def stage_lru(k, i):
    nc, P = k.nc, k.P
    j = i // 3
    last = (i == 3)
    XR, YG, ZG = k.XR, k.YG, k.ZG
    with ExitStack() as st:
        win = sb(st, nc, "lru_win", [128, 8, 2560], BF16)
        for h2 in range(2):
            P.op('pool', lambda e: e.dma_start(out=win[:, :, h2 * 1280:(h2 + 1) * 1280],
                                               in_=k.lru_w_in[j].rearrange("(kc q) c -> q kc c", q=128)[:, :, h2 * 1280:(h2 + 1) * 1280]),
                 writes=['win'], dma=True)
        drain_precast(k, 3)
        Hs = [sb(st, nc, f"H1{q}", [128, 8, 512], BF16) for q in range(2)]
        xts = [sb(st, nc, f"xt1{q}", [128, 8, 512], F32) for q in range(2)]
        sq = sb(st, nc, "sq1", [128, 8, 512], F32)
        rs = sb(st, nc, "rs1", [128, 512], F32)
        tmps = [sb(st, nc, f"nt1{q}", [128, 512], F32) for q in range(2)]
        xrt = sb(st, nc, "xrt", [128, 10, 512], F32)
        ygt = sb(st, nc, "ygt", [128, 10, 512], BF16)
        g1 = [sb(st, nc, f"g1{q}", [128, 512], F32) for q in range(2)]
        g2 = [sb(st, nc, f"g2{q}", [128, 512], F32) for q in range(2)]
        ng = 0
        def prep(ti):
            c0, n, s = TILES[ti]
            q = ti % 2
            load_x_tile(k, xts[q], c0, n, key=('xt', q))
            norm_tile(k, xts[q], ('xt', q), n, i, 0, s, Hs[q], ('H', q), sq, rs, tmps)
        prep(0)
        for ti, (c0, n, s) in enumerate(TILES):
            H, kH = Hs[ti % 2], ('H', ti % 2)
            if ti + 1 < len(TILES):
                prep(ti + 1)
            for oc in range(20):
                b, ps = psum(k)
                for kc in range(8):
                    P.op('pe', lambda e: e.matmul(ps[:, :n], lhsT=win[:, kc, oc * 128:(oc + 1) * 128], rhs=H[:, kc, :n],
                                                  start=(kc == 0), stop=(kc == 7)),
                         reads=['win', kH], writes=[('ps', b)])
                if oc < 10:
                    P.op('act', lambda e: e.activation(out=ygt[:, oc, :n], in_=ps[:, :n], func=AF.Gelu_apprx_tanh),
                         reads=[('ps', b)], writes=['ygt'])
                else:
                    P.op('act', lambda e: e.activation(out=xrt[:, oc - 10, :n], in_=ps[:, :n], func=AF.Copy),
                         reads=[('ps', b)], writes=['xrt'])
            P.op('sp', lambda e: e.dma_start(out=YG.rearrange("(m q) t -> q m t", q=128)[:, :, c0:c0 + n], in_=ygt[:, :, :n]),
                 reads=['ygt'], writes=['YG'], dma=True)
            P.op('sp', lambda e: e.dma_start(out=XR.rearrange("(m q) t -> q m t", q=128)[:, :, c0:c0 + n], in_=xrt[:, :, :n]),
                 reads=['xrt'], writes=['XR'], dma=True)
    P.barrier()
    with ExitStack() as st:
        wg = sb(st, nc, "lru_wg", [128, 40, 128], BF16)
        for q in range(4):
            P.op('pool', lambda e: e.dma_start(out=wg[:, q * 10:(q + 1) * 10, :], in_=k.lru_wg[j][:, q * 10:(q + 1) * 10, :]),
                 writes=['wg'], dma=True)
        drain_precast(k, 1000)
        if i == 0 and k.mods_split:
            stage_mods(k, layers=(1, 2, 3), final_barrier=False, outer=st, cbw=256)
        B1 = sb(st, nc, "B1", [128, T], F32)
        B2 = sb(st, nc, "B2", [128, T], F32)
        B3 = sb(st, nc, "B3", [128, T], F32)
        B4 = sb(st, nc, "B4", [128, T], F32)
        B5 = sb(st, nc, "B5", [128, T], F32)
        B6 = sb(st, nc, "B6", [128, T], F32)
        B7 = sb(st, nc, "B7", [128, T], F32)
        ub = sb(st, nc, "ub", [128, T], BF16)
        ygc = sb(st, nc, "ygc", [128, T], BF16)
        zc_ = sb(st, nc, "zc", [128, T], BF16)
        sp8 = sb(st, nc, "sp8", [128, 2, 10], F32)
        P.op('act', lambda e: e.activation(out=sp8, in_=k.lru_lam[:, j], func=AF.Exp, scale=-1.0), reads=['lrup'], writes=['sp8'])
        P.op('act', lambda e: e.activation(out=sp8, in_=sp8, func=AF.Ln, bias=k.oneb[:, 0:1], scale=1.0), reads=['sp8'], writes=['sp8'])
        P.op('dve', lambda e: e.tensor_scalar(out=sp8, in0=sp8, scalar1=-8.0, scalar2=None, op0=ALU.mult), reads=['sp8'], writes=['sp8'])
        segs = [(0, NCTX), (NCTX, T)]
        allk = lambda nm: [(nm, ti) for ti in range(len(TILES))]
        PCS = [(0, 1280, (0, 1, 2)), (1280, 2816, (3, 4, 5)), (2816, T, (6, 7, 8))]
        pk = lambda nm, tl: [(nm, ti) for ti in tl]
        for m in range(10):
            P.op('sp', lambda e: e.dma_start(out=B1, in_=XR[m * 128:(m + 1) * 128, :]), reads=['XR'], writes=allk('B1'), dma=True)
            P.op('sp', lambda e: e.dma_start(out=ygc, in_=YG[m * 128:(m + 1) * 128, :]), reads=['YG'], writes=['ygc'], dma=True)
            cw = lambda t: k.lru_cw[:, j, t, m:m + 1]
            for (p0, p1, tl) in PCS:
                P.op('dve', lambda e: e.tensor_scalar(out=B2[:, p0:p1], in0=B1[:, p0:p1], scalar1=cw(2), scalar2=k.lru_cb[:, j, m:m + 1],
                                                      op0=ALU.mult, op1=ALU.add),
                     reads=allk('B1') + ['lrup'], writes=pk('B2', tl))
                for o in (-2, -1, 1):
                    for (s0, s1) in segs:
                        a0 = max(s0 + max(0, -o), p0)
                        a1 = min(s1 - max(0, o), p1)
                        if a1 <= a0:
                            continue
                        P.op('dve', lambda e: e.scalar_tensor_tensor(out=B2[:, a0:a1], in0=B1[:, a0 + o:a1 + o], scalar=cw(o + 2),
                                                                     in1=B2[:, a0:a1], op0=ALU.mult, op1=ALU.add),
                             reads=allk('B1') + pk('B2', tl) + ['lrup'], writes=pk('B2', tl))
                P.op('act', lambda e: e.activation(out=ub[:, p0:p1], in_=B2[:, p0:p1], func=AF.Copy), reads=pk('B2', tl), writes=pk('ub', tl))
            for d in range(2):
                Ba, nA = (B3, 'B3') if d == 0 else (B7, 'B7')
                Bi, nI = (B1, 'B1') if d == 0 else (B6, 'B6')
                for ti, (c0, n, s) in enumerate(TILES):
                    b, ps = psum(k)
                    P.op('pe', lambda e: e.matmul(ps[:, :n], lhsT=wg[:, (d * 2 + 0) * 10 + m, :], rhs=ub[:, c0:c0 + n], start=True, stop=True),
                         reads=['wg', ('ub', ti)], writes=[('ps', b)])
                    P.op('act', lambda e: e.activation(out=Ba[:, c0:c0 + n], in_=ps[:, :n], func=AF.Sigmoid,
                                                       bias=k.lru_bg[:, j, d, 0, m:m + 1], scale=1.0),
                         reads=[('ps', b), 'lrup'], writes=[(nA, ti)])
                    b2, ps2 = psum(k)
                    P.op('pe', lambda e: e.matmul(ps2[:, :n], lhsT=wg[:, (d * 2 + 1) * 10 + m, :], rhs=ub[:, c0:c0 + n], start=True, stop=True),
                         reads=['wg', ('ub', ti)], writes=[('ps', b2)])
                    P.op('act', lambda e: e.activation(out=Bi[:, c0:c0 + n], in_=ps2[:, :n], func=AF.Sigmoid,
                                                       bias=k.lru_bg[:, j, d, 1, m:m + 1], scale=1.0),
                         reads=[('ps', b2), 'lrup'], writes=[(nI, ti)])
                for (p0, p1, tl) in PCS:
                    P.op('act', lambda e: e.activation(out=Ba[:, p0:p1], in_=Ba[:, p0:p1], func=AF.Exp, scale=sp8[:, d, m:m + 1]),
                         reads=pk(nA, tl) + ['sp8'], writes=pk(nA, tl))
                for (p0, p1, tl) in PCS:
                    P.op('act', lambda e: e.activation(out=B4[:, p0:p1], in_=Ba[:, p0:p1], func=AF.Square), reads=pk(nA, tl), writes=pk('B4', tl))
                for (p0, p1, tl) in PCS:
                    P.op('act', lambda e: e.activation(out=B4[:, p0:p1], in_=B4[:, p0:p1], func=AF.Sqrt, scale=-1.0, bias=k.oneb[:, 0:1]),
                         reads=pk('B4', tl), writes=pk('B4', tl))
                    P.op('dve', lambda e: e.tensor_tensor(out=Bi[:, p0:p1], in0=Bi[:, p0:p1], in1=B2[:, p0:p1], op=ALU.mult),
                         reads=pk(nI, tl) + pk('B2', tl), writes=pk(nI, tl))
                    P.op('dve', lambda e: e.tensor_tensor(out=Bi[:, p0:p1], in0=Bi[:, p0:p1], in1=B4[:, p0:p1], op=ALU.mult),
                         reads=pk(nI, tl) + pk('B4', tl), writes=pk(nI, tl))
                if d == 0:
                    P.op('dve', lambda e: e.tensor_tensor_scan(out=B5, data0=B3, data1=B1, initial=0.0, op0=ALU.mult, op1=ALU.add),
                         reads=allk('B3') + allk('B1'), writes=allk('B5'))
                else:
                    rv = lambda buf, a, b_: buf[:, a:b_][:, ::-1]
                    P.op('dve', lambda e: e.tensor_tensor_scan(out=rv(B4, 0, NCTX), data0=rv(B7, 0, NCTX), data1=rv(B6, 0, NCTX),
                                                               initial=0.0, op0=ALU.mult, op1=ALU.add),
                         reads=allk('B7') + allk('B6'), writes=allk('B4'))
                    P.op('dve', lambda e: e.tensor_tensor_scan(out=rv(B4, NCTX, T), data0=rv(B7, NCTX, T), data1=rv(B6, NCTX, T),
                                                               initial=B4[:, 0:1], op0=ALU.mult, op1=ALU.add),
                         reads=allk('B7') + allk('B6') + allk('B4'), writes=allk('B4'))
            for (p0, p1, tl) in PCS:
                P.op('dve', lambda e: e.tensor_tensor(out=B5[:, p0:p1], in0=B5[:, p0:p1], in1=B4[:, p0:p1], op=ALU.add),
                     reads=pk('B5', tl) + pk('B4', tl), writes=pk('B5', tl))
                P.op('dve', lambda e: e.tensor_tensor(out=zc_[:, p0:p1], in0=B5[:, p0:p1], in1=ygc[:, p0:p1], op=ALU.mult),
                     reads=pk('B5', tl) + ['ygc'], writes=pk('zc', tl))
            P.op('sp', lambda e: e.dma_start(out=ZG[m * 128:(m + 1) * 128, :], in_=zc_), reads=allk('zc'), writes=['ZG'], dma=True)
    P.barrier()
    with ExitStack() as st:
        wo = sb(st, nc, "lru_wo", [128, 10, 1024], BF16)
        P.op('pool', lambda e: e.dma_start(out=wo, in_=k.lru_w_out[j].rearrange("(m q) c -> q m c", q=128)), writes=['wo'], dma=True)
        out_proj_residual(k, i, TILES[1:] if last else TILES, ZG.rearrange("(m q) t -> q m t", q=128), 10, 'ZG', wo, 128,
                          lambda zc, dc: wo[:, zc, dc * 128:(dc + 1) * 128])


NCH = T // 128


def stage_ret(k, i):
    nc, P = k.nc, k.P
    QT, KT, KTOK, VTOK, GS, SBd, ZR = k.QT, k.KT, k.KTOK, k.VTOK, k.GS, k.SBd, k.ZR
    tab = k.ret_tab
    with ExitStack() as st:
        win = sb(st, nc, "ret_win", [128, 8, 6144], BF16)
        for q3 in range(3):
            P.op('pool', lambda e: e.dma_start(out=win[:, :, q3 * 2048:(q3 + 1) * 2048],
                                               in_=k.ret_w_in[0].rearrange("(kc q) c -> q kc c", q=128)[:, :, q3 * 2048:(q3 + 1) * 2048]),
                 writes=['win'], dma=True)
        drain_precast(k, 4)
        H = sb(st, nc, "H2", [128, 8, 512], BF16)
        xt = sb(st, nc, "xt2", [128, 8, 512], F32)
        sq = sb(st, nc, "sq2", [128, 8, 512], F32)
        rs = sb(st, nc, "rs2", [128, 512], F32)
        tmps = [sb(st, nc, f"nt2{q}", [128, 512], F32) for q in range(2)]
        qo = sb(st, nc, "qo", [128, 8, 512], BF16)
        cs = sb(st, nc, "cs", [128, 2, 512], F32)
        kt = sb(st, nc, "kt", [128, 1024], F32)
        kto = sb(st, nc, "kto", [128, 1024], BF16)
        vto = sb(st, nc, "vto", [128, 2048], BF16)
        cst = sb(st, nc, "cst", [128, 2, 128], F32)
        gso = sb(st, nc, "gso", [128, 4, 512], BF16)
        for ti, (c0, n, s) in enumerate(TILES):
            load_x_tile(k, xt, c0, n)
            norm_tile(k, xt, 'xt', n, i, 0, s, H, 'H', sq, rs, tmps)
            if s == 0:
                p0 = c0 - NCTX
                P.op('sp', lambda e: e.dma_start(out=cs[:, 0, :n], in_=k.ret_cosT[:, p0:p0 + n]), writes=['cs0'], dma=True)
                P.op('sp', lambda e: e.dma_start(out=cs[:, 1, :n], in_=k.ret_sinT[:, p0:p0 + n]), writes=['cs1'], dma=True)
            for (dst, kd_, off, scale) in ((QT, 'QT', 0, 1.0), (KT, 'KT', 1024, 0.0625)):
                for oc in range(8):
                    b, ps = psum(k)
                    for kc in range(8):
                        P.op('pe', lambda e: e.matmul(ps[:, :n], lhsT=win[:, kc, off + oc * 128:off + (oc + 1) * 128], rhs=H[:, kc, :n],
                                                      start=(kc == 0), stop=(kc == 7)),
                             reads=['win', 'H'], writes=[('ps', b)])
                    P.op('act', lambda e: e.activation(out=xt[:, oc, :n], in_=ps[:, :n], func=AF.Copy, scale=scale),
                         reads=[('ps', b)], writes=['xt'])
                if s == 0:
                    xv = xt.rearrange("p (h two) n -> p h two n", two=2)
                    qv = qo.rearrange("p (h two) n -> p h two n", two=2)
                    x1, x2 = xv[:, :, 0, :n], xv[:, :, 1, :n]
                    o1, o2 = qv[:, :, 0, :n], qv[:, :, 1, :n]
                    cosb = cs[:, 0, :n].unsqueeze(1).to_broadcast([128, 4, n])
                    sinb = cs[:, 1, :n].unsqueeze(1).to_broadcast([128, 4, n])
                    sv = sq.rearrange("p (two h) n -> p two h n", two=2)
                    ta, tb = sv[:, 0, :, :n], sv[:, 1, :, :n]
                    P.op('dve', lambda e: e.tensor_tensor(out=ta, in0=x1, in1=cosb, op=ALU.mult), reads=['xt', 'cs0', 'sq'], writes=['sq'])
                    P.op('dve', lambda e: e.tensor_tensor(out=tb, in0=x2, in1=sinb, op=ALU.mult), reads=['xt', 'cs1', 'sq'], writes=['sq'])
                    P.op('dve', lambda e: e.tensor_tensor(out=o1, in0=ta, in1=tb, op=ALU.subtract), reads=['sq'], writes=['qo'])
                    P.op('dve', lambda e: e.tensor_tensor(out=ta, in0=x2, in1=cosb, op=ALU.mult), reads=['xt', 'cs0', 'qo', 'sq'], writes=['sq'])
                    P.op('dve', lambda e: e.tensor_tensor(out=tb, in0=x1, in1=sinb, op=ALU.mult), reads=['xt', 'cs1', 'qo', 'sq'], writes=['sq'])
                    P.op('dve', lambda e: e.tensor_tensor(out=o2, in0=ta, in1=tb, op=ALU.add), reads=['sq'], writes=['qo'])
                else:
                    P.op('dve', lambda e: e.tensor_copy(out=qo[:, :, :n], in_=xt[:, :, :n]), reads=['xt'], writes=['qo'])
                P.op('sp', lambda e: e.dma_start(out=dst.rearrange("(oc q) t -> q oc t", q=128)[:, :, c0:c0 + n], in_=qo[:, :, :n]),
                     reads=['qo'], writes=[kd_], dma=True)
            for tb_ in range(n // 128):
                r0 = c0 + tb_ * 128
                if s == 0:
                    pp = r0 - NCTX
                    P.op('sp', lambda e: e.dma_start(out=cst[:, 0, :], in_=k.ret_costok[pp:pp + 128, :]), writes=['cst0'], dma=True)
                    P.op('sp', lambda e: e.dma_start(out=cst[:, 1, :], in_=k.ret_sintok[pp:pp + 128, :]), writes=['cst1'], dma=True)
                for half in range(2):
                    b, ps = psum(k)
                    for kc in range(8):
                        P.op('pe', lambda e: e.matmul(ps[:, :], lhsT=H[:, kc, tb_ * 128:(tb_ + 1) * 128],
                                                      rhs=win[:, kc, 1024 + half * 512:1024 + (half + 1) * 512], start=(kc == 0), stop=(kc == 7)),
                             reads=['win', 'H'], writes=[('ps', b)])
                    P.op('act', lambda e: e.activation(out=kt[:, half * 512:(half + 1) * 512], in_=ps[:, :], func=AF.Copy, scale=0.0625),
                         reads=[('ps', b)], writes=['kt'])
                if s == 0:
                    kv = kt.rearrange("p (h two f) -> p h two f", two=2, f=128)
                    ov = kto.rearrange("p (h two f) -> p h two f", two=2, f=128)
                    k1, k2 = kv[:, :, 0, :], kv[:, :, 1, :]
                    o1, o2 = ov[:, :, 0, :], ov[:, :, 1, :]
                    cosb = cst[:, 0, :].unsqueeze(1).to_broadcast([128, 4, 128])
                    sinb = cst[:, 1, :].unsqueeze(1).to_broadcast([128, 4, 128])
                    ta = tmps[0].rearrange("p (h f) -> p h f", f=128)
                    tb = tmps[1].rearrange("p (h f) -> p h f", f=128)
                    ka, kb = ('ntmp', 0), ('ntmp', 1)
                    P.op('dve', lambda e: e.tensor_tensor(out=ta, in0=k1, in1=cosb, op=ALU.mult), reads=['kt', 'cst0'], writes=[ka])
                    P.op('dve', lambda e: e.tensor_tensor(out=tb, in0=k2, in1=sinb, op=ALU.mult), reads=['kt', 'cst1'], writes=[kb])
                    P.op('dve', lambda e: e.tensor_tensor(out=o1, in0=ta, in1=tb, op=ALU.subtract), reads=[ka, kb], writes=['kto'])
                    P.op('dve', lambda e: e.tensor_tensor(out=ta, in0=k2, in1=cosb, op=ALU.mult), reads=['kt', 'cst0', 'kto'], writes=[ka])
                    P.op('dve', lambda e: e.tensor_tensor(out=tb, in0=k1, in1=sinb, op=ALU.mult), reads=['kt', 'cst1', 'kto'], writes=[kb])
                    P.op('dve', lambda e: e.tensor_tensor(out=o2, in0=ta, in1=tb, op=ALU.add), reads=[ka, kb], writes=['kto'])
                else:
                    P.op('dve', lambda e: e.tensor_copy(out=kto, in_=kt), reads=['kt'], writes=['kto'])
                P.op('sp', lambda e: e.dma_start(out=KTOK[r0:r0 + 128, :], in_=kto), reads=['kto'], writes=['KTOK'], dma=True)
                for q4 in range(4):
                    b, ps = psum(k)
                    for kc in range(8):
                        P.op('pe', lambda e: e.matmul(ps[:, :], lhsT=H[:, kc, tb_ * 128:(tb_ + 1) * 128],
                                                      rhs=win[:, kc, 2048 + q4 * 512:2048 + (q4 + 1) * 512], start=(kc == 0), stop=(kc == 7)),
                             reads=['win', 'H'], writes=[('ps', b)])
                    eng = 'act' if q4 % 2 == 0 else 'dve'
                    if eng == 'act':
                        P.op('act', lambda e: e.activation(out=vto[:, q4 * 512:(q4 + 1) * 512], in_=ps[:, :], func=AF.Copy),
                             reads=[('ps', b)], writes=['vto'])
                    else:
                        P.op('dve', lambda e: e.tensor_copy(out=vto[:, q4 * 512:(q4 + 1) * 512], in_=ps[:, :]),
                             reads=[('ps', b)], writes=['vto'])
                P.op('sp', lambda e: e.dma_start(out=VTOK[r0:r0 + 128, :], in_=vto), reads=['vto'], writes=['VTOK'], dma=True)
            for g4 in range(4):
                for gj in range(4):
                    oc = g4 * 4 + gj
                    b, ps = psum(k)
                    for kc in range(8):
                        P.op('pe', lambda e: e.matmul(ps[:, :n], lhsT=win[:, kc, 4096 + oc * 128:4096 + (oc + 1) * 128], rhs=H[:, kc, :n],
                                                      start=(kc == 0), stop=(kc == 7)),
                             reads=['win', 'H'], writes=[('ps', b)])
                    gt = tmps[gj % 2]
                    kg = ('ntmp', gj % 2)
                    P.op('act', lambda e: e.activation(out=gt[:, :n], in_=ps[:, :n], func=AF.Silu), reads=[('ps', b)], writes=[kg])
                    P.op('dve', lambda e: e.tensor_scalar(out=gso[:, gj, :n], in0=gt[:, :n], scalar1=k.ret_gain[:, oc:oc + 1], scalar2=None,
                                                          op0=ALU.mult), reads=[kg, 'retp'], writes=['gso'])
                P.op('sp', lambda e: e.dma_start(out=GS.rearrange("(oc q) t -> q oc t", q=128)[:, g4 * 4:(g4 + 1) * 4, c0:c0 + n],
                                                 in_=gso[:, :, :n]), reads=['gso'], writes=['GS'], dma=True)
    P.barrier()
    with ExitStack() as st:
        drain_precast(k, 1000)
        qT = sb(st, nc, "r_qT", [128, 2, T], BF16)
        kT = sb(st, nc, "r_kT", [128, 2, T], BF16)
        ktk = sb(st, nc, "r_ktk", [128, NCH, 256], BF16)
        vtk = sb(st, nc, "r_vtk", [128, NCH, 512], BF16)
        tb_s = sb(st, nc, "r_tab", [128, 6 * 128 + 3], F32)
        P.op('sp', lambda e: e.dma_start(out=tb_s, in_=tab), writes=['rtab'], dma=True)
        DP, DM, UP, LO, POS1, POSB = (tb_s[:, q * 128:(q + 1) * 128] for q in range(6))
        KPF, KPB, C128 = (tb_s[:, 768 + q:769 + q] for q in range(3))
        M = sb(st, nc, "r_M", [128, 128], F32)
        M2 = sb(st, nc, "r_M2", [128, 128], F32)
        QD = sb(st, nc, "r_QD", [128, 2, 128], F32)
        cv = sb(st, nc, "r_cv", [128, 4], F32)
        S32 = [sb(st, nc, f"r_S32{d}", [128, 2, 512], F32) for d in range(2)]
        Sbf = [sb(st, nc, f"r_Sbf{q}", [128, 2, 512], BF16) for q in range(2)]
        Sfb = sb(st, nc, "r_Sfb", [128, 2, 512], BF16)
        Sin = [sb(st, nc, f"r_Sin{q}", [128, 2, 512], BF16) for q in range(3)]
        gsc = [sb(st, nc, f"r_gsc{q}", [128, 4, 128], BF16) for q in range(4)]
        kd = [sb(st, nc, f"r_kd{q}", [128, 256], BF16) for q in range(2)]
        sm = [sb(st, nc, f"r_sm{q}", [128, 128], BF16) for q in range(2)]
        qs = [sb(st, nc, f"r_qs{q}", [128, 2, 2, 128], BF16) for q in range(2)]
        sqo2 = [sb(st, nc, f"r_sqo{q}", [128, 512], F32) for q in range(2)]
        rn2 = [sb(st, nc, f"r_rn{q}", [128, 128], F32) for q in range(2)]
        zt2 = [sb(st, nc, f"r_zt{q}", [128, 4, 128], F32) for q in range(2)]
        zo = [sb(st, nc, f"r_zo{q}", [128, 4, 128], BF16) for q in range(2)]
        nkd = 0
        for h in range(4):
            P.op('sp', lambda e: e.dma_start(out=ktk, in_=KTOK.rearrange("(c q) f -> q c f", q=128)[:, :, h * 256:(h + 1) * 256]),
                 reads=['KTOK'], writes=['ktk'], dma=True)
            P.op('sp', lambda e: e.dma_start(out=vtk, in_=VTOK.rearrange("(c q) f -> q c f", q=128)[:, :, h * 512:(h + 1) * 512]),
                 reads=['VTOK'], writes=['vtk'], dma=True)
            P.op('sp', lambda e: e.dma_start(out=qT, in_=QT.rearrange("(oc q) t -> q oc t", q=128)[:, 2 * h:2 * h + 2, :]),
                 reads=['QT'], writes=['qT'], dma=True)
            P.op('sp', lambda e: e.dma_start(out=kT, in_=KT.rearrange("(oc q) t -> q oc t", q=128)[:, 2 * h:2 * h + 2, :]),
                 reads=['KT'], writes=['kT'], dma=True)
            lgf = k.ret_ld[:, h:h + 1]
            lgb = k.ret_ld[:, 4 + h:5 + h]
            P.op('act', lambda e: e.activation(out=M, in_=DP, func=AF.Exp, scale=lgf), reads=['rtab', 'retp'], writes=['M'])
            P.op('dve', lambda e: e.tensor_tensor(out=M, in0=M, in1=UP, op=ALU.mult), reads=['M', 'rtab'], writes=['M'])
            P.op('act', lambda e: e.activation(out=M2, in_=DM, func=AF.Exp, scale=lgb), reads=['rtab', 'retp'], writes=['M2'])
            P.op('dve', lambda e: e.tensor_tensor(out=M2, in0=M2, in1=LO, op=ALU.mult), reads=['M2', 'rtab'], writes=['M2'])
            P.op('dve', lambda e: e.tensor_tensor(out=M, in0=M, in1=M2, op=ALU.add), reads=['M', 'M2'], writes=['M'])
            P.op('act', lambda e: e.activation(out=QD[:, 0, :], in_=POS1, func=AF.Exp, scale=lgf), reads=['rtab', 'retp'], writes=['QD'])
            P.op('act', lambda e: e.activation(out=QD[:, 1, :], in_=POSB, func=AF.Exp, scale=lgb), reads=['rtab', 'retp'], writes=['QD'])
            P.op('act', lambda e: e.activation(out=cv[:, 0:1], in_=KPF, func=AF.Exp, scale=lgf), reads=['rtab', 'retp'], writes=['cv'])
            P.op('act', lambda e: e.activation(out=cv[:, 1:2], in_=KPB, func=AF.Exp, scale=lgb), reads=['rtab', 'retp'], writes=['cv'])
            P.op('act', lambda e: e.activation(out=cv[:, 2:3], in_=C128, func=AF.Exp, scale=lgf), reads=['rtab', 'retp'], writes=['cv'])
            P.op('act', lambda e: e.activation(out=cv[:, 3:4], in_=C128, func=AF.Exp, scale=lgb), reads=['rtab', 'retp'], writes=['cv'])

            def state_update(d, cidx, S, kS, Sb_out, kSb, banks=None):
                nonlocal nkd
                kdt = kd[nkd % 2]
                kkd = ('kd', nkd % 2)
                nkd += 1
                P.op('dve', lambda e: e.tensor_scalar(out=kdt, in0=ktk[:, cidx, :], scalar1=cv[:, d:d + 1], scalar2=None, op0=ALU.mult),
                     reads=['ktk', 'cv'], writes=[kkd])
                for dch in range(2):
                    if banks is None:
                        b, ps = psum(k)
                    else:
                        b, ps = banks[dch], k.PS[banks[dch]]
                    P.op('pe', lambda e: e.matmul(ps[:, :], lhsT=kdt[:, dch * 128:(dch + 1) * 128], rhs=vtk[:, cidx, :], start=True, stop=True),
                         reads=[kkd, 'vtk'], writes=[('ps', b)])
                    P.op('dve', lambda e: e.scalar_tensor_tensor(out=S[:, dch, :], in0=S[:, dch, :], scalar=cv[:, 2 + d:3 + d], in1=ps[:, :],
                                                                 op0=ALU.mult, op1=ALU.add),
                         reads=[kS, ('ps', b), 'cv'], writes=[kS])
                P.op('act', lambda e: e.activation(out=Sb_out, in_=S, func=AF.Copy), reads=[kS], writes=[kSb])

            order_b = [1, 0] + list(range(NCH - 1, 1, -1))
            P.op('dve', lambda e: e.memset(S32[1], 0.0), writes=['S32b'])
            P.op('dve', lambda e: e.memset(Sbf[0], 0.0), writes=[('Sbf', 0)])
            for oi, cidx in enumerate(order_b):
                cur = Sbf[oi % 2]
                kcur = ('Sbf', oi % 2)
                P.op('sp', lambda e: e.dma_start(out=SBd[h, cidx].rearrange("(dch q) e -> q dch e", q=128), in_=cur),
                     reads=[kcur], writes=[('SBd', cidx)], dma=True)
                if oi + 1 < len(order_b):
                    state_update(1, cidx, S32[1], 'S32b', Sbf[(oi + 1) % 2], ('Sbf', (oi + 1) % 2))
            P.op('dve', lambda e: e.memset(S32[0], 0.0), writes=['S32f'])
            P.op('dve', lambda e: e.memset(Sfb, 0.0), writes=['Sfb'])

            def prefetch(cidx):
                b3, b4 = cidx % 3, cidx % 4
                P.op('sp', lambda e: e.dma_start(out=Sin[b3], in_=SBd[h, cidx].rearrange("(dch q) e -> q dch e", q=128)),
                     reads=[('SBd', cidx)], writes=[('Sin', b3)], dma=True)
                P.op('sp', lambda e: e.dma_start(out=gsc[b4], in_=GS.rearrange("(oc q) t -> q oc t", q=128)[:, 4 * h:4 * h + 4, cidx * 128:(cidx + 1) * 128]),
                     reads=['GS'], writes=[('gsc', b4)], dma=True)
            def fA(cidx):
                bq = cidx % 2
                cols = slice(cidx * 128, (cidx + 1) * 128)
                b = bq
                ps_s = k.PS[b]
                for dch in range(2):
                    P.op('pe', lambda e: e.matmul(ps_s[:, 0:128], lhsT=kT[:, dch, cols], rhs=qT[:, dch, cols], start=(dch == 0), stop=(dch == 1)),
                         reads=['kT', 'qT'], writes=[('ps', b)])
                P.op('dve', lambda e: e.tensor_tensor(out=sm[bq], in0=ps_s[:, 0:128], in1=M, op=ALU.mult), reads=[('ps', b), 'M'], writes=[('sm', bq)])
                for d in range(2):
                    P.op('dve', lambda e: e.tensor_tensor(out=qs[bq][:, d], in0=qT[:, :, cols], in1=QD[:, d, :].unsqueeze(1).to_broadcast([128, 2, 128]),
                                                          op=ALU.mult), reads=['qT', 'QD'], writes=[('qs', bq, d)])

            def fB(cidx):
                bq = cidx % 2
                bo = 2 + bq
                ps_o = k.PS[bo]
                smt, qst = sm[bq], qs[bq]
                for ech in range(4):
                    es = slice(ech * 128, (ech + 1) * 128)
                    P.op('pe', lambda e: e.matmul(ps_o[:, es], lhsT=vtk[:, cidx, es], rhs=smt, start=True, stop=False),
                         reads=['vtk', ('sm', bq)], writes=[('ps', bo)])
                    for dch in range(2):
                        P.op('pe', lambda e: e.matmul(ps_o[:, es], lhsT=Sfb[:, dch, es], rhs=qst[:, 0, dch, :], start=False, stop=False),
                             reads=['Sfb', ('qs', bq, 0)], writes=[('ps', bo)])
                    for dch in range(2):
                        P.op('pe', lambda e: e.matmul(ps_o[:, es], lhsT=Sin[cidx % 3][:, dch, es], rhs=qst[:, 1, dch, :], start=False, stop=(dch == 1)),
                             reads=[('Sin', cidx % 3), ('qs', bq, 1)], writes=[('ps', bo)])
                P.op('act', lambda e: e.activation(out=sqo2[bq], in_=ps_o, func=AF.Square), reads=[('ps', bo)], writes=[('sqo', bq)])

            def fC(cidx):
                bq = cidx % 2
                bo = 2 + bq
                ps_o = k.PS[bo]
                cols = slice(cidx * 128, (cidx + 1) * 128)
                bn = 4
                ps_n = k.PS[bn]
                sqo, rn, zt = sqo2[bq], rn2[bq], zt2[bq]
                for ech in range(4):
                    P.op('pe', lambda e: e.matmul(ps_n[:, 0:128], lhsT=k.ones_f, rhs=sqo[:, ech * 128:(ech + 1) * 128], start=(ech == 0), stop=(ech == 3)),
                         reads=[('sqo', bq), 'ones'], writes=[('ps', bn)])
                P.op('act', lambda e: e.activation(out=rn, in_=ps_n[:, 0:128], func=AF.Ln, scale=1.0 / 512, bias=k.epsb[:, 0:1]),
                     reads=[('ps', bn)], writes=[('rn', bq)])
                P.op('act', lambda e: e.activation(out=rn, in_=rn, func=AF.Exp, scale=-0.5), reads=[('rn', bq)], writes=[('rn', bq)])
                P.op('dve', lambda e: e.tensor_tensor(out=zt, in0=ps_o.rearrange("p (c i) -> p c i", i=128),
                                                      in1=rn.unsqueeze(1).to_broadcast([128, 4, 128]), op=ALU.mult),
                     reads=[('ps', bo), ('rn', bq)], writes=[('zt', bq)])
                P.op('dve', lambda e: e.tensor_tensor(out=zo[bq], in0=zt, in1=gsc[cidx % 4], op=ALU.mult), reads=[('zt', bq), ('gsc', cidx % 4)], writes=[('zo', bq)])
                P.op('sp', lambda e: e.dma_start(out=ZR.rearrange("(oc q) t -> q oc t", q=128)[:, 4 * h:4 * h + 4, cols], in_=zo[bq]),
                     reads=[('zo', bq)], writes=['ZR'], dma=True)

            prefetch(0)
            prefetch(1)
            fA(0)
            for cidx in range(NCH):
                if cidx + 2 < NCH:
                    prefetch(cidx + 2)
                if cidx + 1 < NCH:
                    fA(cidx + 1)
                fB(cidx)
                if cidx + 1 < NCH:
                    state_update(0, cidx, S32[0], 'S32f', Sfb, 'Sfb', banks=(5, 6))
                if cidx >= 1:
                    fC(cidx - 1)
            fC(NCH - 1)
    P.barrier()
    with ExitStack() as st:
        wo = sb(st, nc, "ret_wo", [128, 16, 1024], BF16)
        P.op('pool', lambda e: e.dma_start(out=wo, in_=k.ret_w_out[0].rearrange("(m q) c -> q m c", q=128)), writes=['wo'], dma=True)
        out_proj_residual(k, i, TILES, ZR.rearrange("(m q) t -> q m t", q=128), 16, 'ZR', wo, 128,
                          lambda zc, dc: wo[:, zc, dc * 128:(dc + 1) * 128])


def stage_swa(k, i):
    nc, P = k.nc, k.P
    QS, KS, VS, OS = k.QS, k.KS, k.VS, k.OS
    with ExitStack() as st:
        win = sb(st, nc, "swa_win", [128, 8, 2816], BF16)
        for (a, b_) in ((0, 1408), (1408, 2816)):
            P.op('pool', lambda e: e.dma_start(out=win[:, :, a:b_], in_=k.swa_w_ext.rearrange("(kc q) c -> q kc c", q=128)[:, :, a:b_]),
                 writes=['win'], dma=True)
        drain_precast(k, 1000)
        Hs = [sb(st, nc, f"H3{q}", [128, 8, 512], BF16) for q in range(2)]
        xts = [sb(st, nc, f"xt3{q}", [128, 8, 512], F32) for q in range(2)]
        sq = sb(st, nc, "sq3", [128, 8, 512], F32)
        rs = sb(st, nc, "rs3", [128, 512], F32)
        tmps = [sb(st, nc, f"nt3{q}", [128, 512], F32) for q in range(2)]
        qo = sb(st, nc, "s_qo", [128, 10, 512], BF16)
        cs = sb(st, nc, "s_cs", [128, 2, 512], F32)
        vto = [sb(st, nc, f"s_vto{q}", [128, 256], BF16) for q in range(2)]
        nt = 0
        def prep(ti):
            c0, n, s = TILES[ti]
            q = ti % 2
            load_x_tile(k, xts[q], c0, n, key=('xt', q))
            norm_tile(k, xts[q], ('xt', q), n, i, 0, s, Hs[q], ('H', q), sq, rs, tmps)
        prep(0)
        for ti, (c0, n, s) in enumerate(TILES):
            H, kH = Hs[ti % 2], ('H', ti % 2)
            if ti + 1 < len(TILES):
                prep(ti + 1)
            if s == 0:
                p0 = c0 - NCTX
                P.op('sp', lambda e: e.dma_start(out=cs[:, 0, :n], in_=k.swa_cos[:, p0:p0 + n]), writes=['cs0'], dma=True)
                P.op('sp', lambda e: e.dma_start(out=cs[:, 1, :n], in_=k.swa_sin[:, p0:p0 + n]), writes=['cs1'], dma=True)
            for hp in range(10):
                off = hp * 128
                off_sw = 1536 + hp * 128
                b, ps = psum(k)
                for kc in range(8):
                    P.op('pe', lambda e: e.matmul(ps[:, :n], lhsT=win[:, kc, off:off + 128], rhs=H[:, kc, :n], start=(kc == 0), stop=(kc == 7)),
                         reads=['win', kH], writes=[('ps', b)])
                if s == 0:
                    b2, ps2 = psum(k)
                    for kc in range(8):
                        P.op('pe', lambda e: e.matmul(ps2[:, :n], lhsT=win[:, kc, off_sw:off_sw + 128], rhs=H[:, kc, :n], start=(kc == 0), stop=(kc == 7)),
                             reads=['win', kH], writes=[('ps', b2)])
                    ta, tb = tmps[0], tmps[1]
                    P.op('dve', lambda e: e.tensor_tensor(out=ta[:, :n], in0=ps[:, :n], in1=cs[:, 0, :n], op=ALU.mult),
                         reads=[('ps', b), 'cs0'], writes=[('ntmp', 0)])
                    P.op('dve', lambda e: e.tensor_tensor(out=tb[:, :n], in0=ps2[:, :n], in1=cs[:, 1, :n], op=ALU.mult),
                         reads=[('ps', b2), 'cs1'], writes=[('ntmp', 1)])
                    P.op('dve', lambda e: e.tensor_tensor(out=qo[:, hp, :n], in0=ta[:, :n], in1=tb[:, :n], op=ALU.add),
                         reads=[('ntmp', 0), ('ntmp', 1)], writes=['qo'])
                else:
                    P.op('act', lambda e: e.activation(out=qo[:, hp, :n], in_=ps[:, :n], func=AF.Copy), reads=[('ps', b)], writes=['qo'])
            for par in range(2):
                psl = slice(par * 64, (par + 1) * 64)
                P.op('sp', lambda e: e.dma_start(out=QS.rearrange("d (j two) t -> d two j t", two=2)[:, par, :, c0:c0 + n], in_=qo[psl, 0:8, :n]),
                     reads=['qo'], writes=['QS'], dma=True)
                P.op('sp', lambda e: e.dma_start(out=KS.rearrange("d (j two) t -> d two j t", two=2)[:, par, :, c0:c0 + n], in_=qo[psl, 8:10, :n]),
                     reads=['qo'], writes=['KS'], dma=True)
            for tb_ in range(n // 128):
                r0 = c0 + tb_ * 128
                b, ps = psum(k)
                for kc in range(8):
                    P.op('pe', lambda e: e.matmul(ps[:, 0:256], lhsT=H[:, kc, tb_ * 128:(tb_ + 1) * 128], rhs=win[:, kc, 1280:1536],
                                                  start=(kc == 0), stop=(kc == 7)),
                         reads=['win', kH], writes=[('ps', b)])
                vt = vto[nt % 2]
                kv_ = ('vto', nt % 2)
                nt += 1
                P.op('act', lambda e: e.activation(out=vt, in_=ps[:, 0:256], func=AF.Copy), reads=[('ps', b)], writes=[kv_])
                P.op('sp', lambda e: e.dma_start(out=VS[r0:r0 + 128, :], in_=vt), reads=[kv_], writes=['VS'], dma=True)
    P.barrier()
    with ExitStack() as st:
        Kt = sb(st, nc, "s_K", [64, 4, T], BF16)
        Vt = sb(st, nc, "s_V", [128, NCH, 256], BF16)
        P.op('sp', lambda e: e.dma_start(out=Kt, in_=KS), reads=['KS'], writes=['Kt'], dma=True)
        P.op('sp', lambda e: e.dma_start(out=Vt, in_=VS.rearrange("(c q) f -> q c f", q=128)), reads=['VS'], writes=['Vt'], dma=True)
        msk = sb(st, nc, "s_msk", [128, 2, 128], F32)
        P.op('sp', lambda e: e.dma_start(out=msk[:, 0, :], in_=k.ret_tab[:, 384:512]), writes=['msk'], dma=True)
        P.op('sp', lambda e: e.dma_start(out=msk[:, 1, :], in_=k.ret_tab[:, 256:384]), writes=['msk'], dma=True)
        mskb = sb(st, nc, "s_mskb", [128, 2, 128], BF16)
        P.op('dve', lambda e: e.tensor_copy(out=mskb, in_=msk), reads=['msk'], writes=['mskb'])
        esink = sb(st, nc, "s_esink", [64, 16], F32)
        P.op('act', lambda e: e.activation(out=esink, in_=k.swa_sink, func=AF.Exp), reads=['swap'], writes=['esink'])
        ones_b = sb(st, nc, "s_ones", [128, 64], BF16)
        P.op('dve', lambda e: e.memset(ones_b, 1.0), writes=['ones_b'])
        qb_ = [sb(st, nc, f"s_qb{q}", [64, 16, 128], BF16) for q in range(2)]
        ob_ = [sb(st, nc, f"s_ob{q}", [64, 16, 128], BF16) for q in range(2)]
        Et = [sb(st, nc, f"s_E{q}", [128, 512], BF16) for q in range(10)]
        rd = sb(st, nc, "s_rd", [64, 4, 128], F32)
        ne = 0

        def loadq(c):
            P.op('sp', lambda e: e.dma_start(out=qb_[c % 2], in_=QS[:, :, c * 128:(c + 1) * 128]), reads=['QS'], writes=[('qb', c % 2)], dma=True)
        loadq(0)
        for c in range(NCH):
            if c + 1 < NCH:
                loadq(c + 1)
            qblk = qb_[c % 2]
            oblk = ob_[c % 2]
            if c < 2:
                kbs = [(0, None), (1, None)]
            else:
                kbs = []
                if c - 1 >= 2:
                    kbs.append((c - 1, 0))
                kbs.append((c, None))
                if c + 1 < NCH:
                    kbs.append((c + 1, 1))
                kbs += [(0, None), (1, None)]
            for hk in range(4):
                Q = qblk[:, hk * 4:(hk + 1) * 4, :]
                es = []
                for (kb, mk) in kbs:
                    b, ps = psum(k)
                    P.op('pe', lambda e: e.matmul(ps[:, :], lhsT=Kt[:, hk, kb * 128:(kb + 1) * 128], rhs=Q, start=True, stop=True),
                         reads=['Kt', ('qb', c % 2)], writes=[('ps', b)])
                    E = Et[ne % 10]
                    kE = ('E', ne % 10)
                    ne += 1
                    P.op('act', lambda e: e.activation(out=E, in_=ps[:, :], func=AF.Exp, scale=0.125), reads=[('ps', b)], writes=[kE])
                    if mk is not None:
                        Ev = E.rearrange("p (g i) -> p g i", i=128)
                        P.op('pool', lambda e: e.tensor_tensor(out=Ev, in0=Ev, in1=mskb[:, mk, :].unsqueeze(1).to_broadcast([128, 4, 128]), op=ALU.mult),
                             reads=[kE, 'mskb'], writes=[kE])
                    es.append((kb, E, kE))
                bo, ps_o = psum(k)
                for q, (kb, E, kE) in enumerate(es):
                    P.op('pe', lambda e: e.matmul(ps_o[0:64, :], lhsT=Vt[:, kb, hk * 64:(hk + 1) * 64], rhs=E, start=(q == 0), stop=(q == len(es) - 1)),
                         reads=['Vt', kE], writes=[('ps', bo)])
                bd, ps_d = psum(k)
                for q, (kb, E, kE) in enumerate(es):
                    P.op('pe', lambda e: e.matmul(ps_d[0:64, :], lhsT=ones_b, rhs=E, start=(q == 0), stop=(q == len(es) - 1)),
                         reads=['ones_b', kE], writes=[('ps', bd)])
                P.op('dve', lambda e: e.tensor_tensor(out=rd, in0=ps_d[0:64, :].rearrange("p (g i) -> p g i", i=128),
                                                      in1=esink[:, hk * 4:(hk + 1) * 4].unsqueeze(2).to_broadcast([64, 4, 128]), op=ALU.add),
                     reads=[('ps', bd), 'esink'], writes=['rd'])
                P.op('act', lambda e: e.activation(out=rd, in_=rd, func=AF.Ln), reads=['rd'], writes=['rd'])
                P.op('act', lambda e: e.activation(out=rd, in_=rd, func=AF.Exp, scale=-1.0), reads=['rd'], writes=['rd'])
                P.op('dve', lambda e: e.tensor_tensor(out=oblk[:, hk * 4:(hk + 1) * 4, :], in0=ps_o[0:64, :].rearrange("p (g i) -> p g i", i=128),
                                                      in1=rd, op=ALU.mult),
                     reads=[('ps', bo), 'rd'], writes=[('ob', c % 2)])
            P.op('sp', lambda e: e.dma_start(out=OS[:, :, c * 128:(c + 1) * 128], in_=oblk), reads=[('ob', c % 2)], writes=['OS'], dma=True)
    P.barrier()
    with ExitStack() as st:
        wo = sb(st, nc, "swa_wo", [64, 16, 1024], BF16)
        P.op('pool', lambda e: e.dma_start(out=wo, in_=k.swa_w_out[0].rearrange("(h d) c -> d h c", d=64)), writes=['wo'], dma=True)
        out_proj_residual(k, i, TILES, OS, 16, 'OS', wo, 64, lambda zc, dc: wo[:, zc, dc * 128:(dc + 1) * 128])


def stage_final(k):
    nc, P = k.nc, k.P
    with ExitStack() as st:
        xts = [sb(st, nc, f"xtf{q}", [128, 8, 512], F32) for q in range(2)]
        sq = sb(st, nc, "sqf", [128, 8, 512], F32)
        rss = [sb(st, nc, f"rsf{q}", [128, 512], F32) for q in range(2)]
        outv = k.out.rearrange("(kc q) t -> q kc t", q=128)
        for ti, (c0, n, s) in enumerate(TILES[1:]):
            xt = xts[ti % 2]
            rs = rss[ti % 2]
            kx = ('xtf', ti % 2)
            kr = ('rsf', ti % 2)
            P.op('sp', lambda e: e.dma_start(out=xt[:, :, :n], in_=xrow(k)[:, :, c0:c0 + n]), reads=['X', ('Xt', c0)], writes=[kx], dma=True)
            sqb = sq.bitcast(BF16)[:, :, 0:512]
            P.op('act', lambda e: e.activation(out=sqb[:, :, :n], in_=xt[:, :, :n], func=AF.Square), reads=[kx], writes=['sq'])
            b, ps = psum(k)
            for kc in range(8):
                P.op('pe', lambda e: e.matmul(ps[:, :n], lhsT=k.ones_b[:, :], rhs=sqb[:, kc, :n], start=(kc == 0), stop=(kc == 7)),
                     reads=['sq', 'ones_b'], writes=[('ps', b)])
            P.op('act', lambda e: e.activation(out=rs[:, :n], in_=ps[:, :n], func=AF.Ln, scale=1.0 / D, bias=k.epsb[:, 0:1]),
                 reads=[('ps', b)], writes=[kr])
            P.op('act', lambda e: e.activation(out=rs[:, :n], in_=rs[:, :n], func=AF.Exp, scale=-0.5), reads=[kr], writes=[kr])
            for kc in range(8):
                P.op('dve', lambda e: e.scalar_tensor_tensor(out=xt[:, kc, :n], in0=xt[:, kc, :n], scalar=k.fg_s[:, kc:kc + 1], in1=rs[:, :n],
                                                             op0=ALU.mult, op1=ALU.mult),
                     reads=[kx, kr, 'fg'], writes=[kx])
            P.op('sp', lambda e: e.dma_start(out=outv[:, :, c0 - NCTX:c0 - NCTX + n], in_=xt[:, :, :n]), reads=[kx], writes=['out'], dma=True)
    P.barrier()


ALL_PARTS = ['init', 'mods', 'mix0', 'ffn0', 'mix1', 'ffn1', 'mix2', 'ffn2', 'mix3', 'ffn3', 'final']


def kernel(**inputs):
    inp = {kk: np.asarray(v) for kk, v in inputs.items()}
    n = inp['x'].shape[0]
    maps = make_in_maps(inp, list(range(n)))
    nc, _ = build_program(ALL_PARTS, debug=False)
    res = run_bass_kernel_spmd(nc, maps, core_ids=list(range(n)))
    out = np.stack([np.ascontiguousarray(res.results[b]['outT'].T) for b in range(n)], axis=0)
    return out.astype(np.float32)


I32 = mybir.dt.int32
NBLK_MAX = 24
MOE_BLK = 512


def precast_moe(k, pe):
    P = k.P
    f, e = pe // 8, pe % 8
    gu, dn = k.moe_w_gu[f, e], k.moe_w_down[f, e]
    for g7 in range(7):
        r0 = (pe * 7 + g7) * 128
        for half in range(2):
            c0 = half * DFF + g7 * 512
            dst = k.WGUx[r0:r0 + 128, :].rearrange("p (kc h c) -> p kc h c", kc=8, h=2)[:, :, half, :]
            src = gu.rearrange("(kc p) n -> p kc n", p=128)[:, :, c0:c0 + 512]
            P.op('pool', lambda e_: e_.dma_start(out=dst, in_=src), writes=[('wgux', pe)], dma=True)
    for dh in range(2):
        r0 = (pe * 2 + dh) * 128
        dst = k.WDx[r0:r0 + 128, :].rearrange("p (fc c) -> p fc c", c=512)
        src = dn.rearrange("(fc p) d -> p fc d", p=128)[:, :, dh * 512:(dh + 1) * 512]
        P.op('pool', lambda e_: e_.dma_start(out=dst, in_=src), writes=[('wdx', pe)], dma=True)


def drain_precast(k, n):
    while n > 0 and k.pc_queue:
        k.pc_queue.pop(0)()
        n -= 1


def stage_moe_sparse(k, i):
    nc, P = k.nc, k.P
    f = i // 2
    last = (i == 3)
    tiles = TILES[1:] if last else TILES
    cols0 = tiles[0][0]
    ntok = sum(t[1] for t in tiles)
    NCK = ntok // 128
    NB = (2 * ntok + 8 * (MOE_BLK - 1)) // MOE_BLK
    drain_precast(k, 1000)
    HC, YC = k.HC, k.YC
    ct = k.moe_tab
    with ExitStack() as st0:
        EQ1 = sb(st0, nc, "m_eq1", [128, NCK, 8], F32)
        EQ2 = sb(st0, nc, "m_eq2", [128, NCK, 8], F32)
        W12 = sb(st0, nc, "m_w12", [128, NCK, 2], F32)
        DI = sb(st0, nc, "m_di", [128, NCK, 2], I32)
        IGU = sb(st0, nc, "m_igu", [128, NB, 7], I32)
        IWD = sb(st0, nc, "m_iwd", [128, NB, 2], I32)
        tabs = sb(st0, nc, "m_tab", [128, 161], F32)
        P.op('sp', lambda e: e.dma_start(out=tabs, in_=ct), writes=['mtab'], dma=True)
        TRI, IOB, CGU, CWD = tabs[:, 0:128], tabs[:, 128:128 + NB], tabs[:, 152:159], tabs[:, 159:161]
        with ExitStack() as st:
            HTOK = sb(st, nc, "m_htok", [128, NCK, 1024], BF16)
            H = sb(st, nc, "m_H", [128, 8, 512], BF16)
            xt = sb(st, nc, "m_xt", [128, 8, 512], F32)
            sq = sb(st, nc, "m_sq", [128, 8, 512], F32)
            hf = sb(st, nc, "m_hf", [128, 8, 512], F32)
            rs = sb(st, nc, "m_rs", [128, 512], F32)
            tmps = [sb(st, nc, f"m_nt{q}", [128, 512], F32) for q in range(2)]
            rsm = sb(st, nc, "m_rsm", [128, 64], F32)
            rsm3 = sb(st, nc, "m_rsm3", [128, 12], F32)
            identb = sb(st, nc, "m_identb", [128, 128], BF16)
            P.op('dve', lambda e: e.tensor_copy(out=identb, in_=k.ident_f), reads=['ident'], writes=['identb'])
            ck = 0
            for ti, (c0, n, s) in enumerate(tiles):
                load_x_tile(k, xt, c0, n)
                norm_tile(k, xt, 'xt', n, i, 1, s, H, 'H', sq, rs, tmps, hf=hf, khf='hf')
                nb = n // 128
                b, ps = psum(k)
                for bi in range(nb):
                    bsl = slice(bi * 128, (bi + 1) * 128)
                    for kc in range(8):
                        P.op('pe', lambda e: e.matmul(ps[:, bi * 8:(bi + 1) * 8], lhsT=hf[:, kc, bsl], rhs=k.rt[:, f, kc, :], start=(kc == 0), stop=(kc == 7)),
                             reads=['hf', 'rt'], writes=[('ps', b)])
                lg = rsm[:, 0:nb * 8]
                lg2 = rsm[:, 32:32 + nb * 8]
                lgv = lg.rearrange("p (c e) -> p c e", e=8)
                lg2v = lg2.rearrange("p (c e) -> p c e", e=8)
                m1, m2, dd = rsm3[:, 0:nb], rsm3[:, 4:4 + nb], rsm3[:, 8:8 + nb]
                eq1, eq2 = EQ1[:, ck:ck + nb, :], EQ2[:, ck:ck + nb, :]
                R = ['rsm']
                KE = [('eq', ck + q) for q in range(nb)]
                P.op('dve', lambda e: e.tensor_copy(out=lg, in_=ps[:, 0:nb * 8]), reads=[('ps', b)], writes=R)
                P.op('dve', lambda e: e.tensor_reduce(out=m1, in_=lgv, axis=AX.X, op=ALU.max), reads=R, writes=R)
                P.op('dve', lambda e: e.tensor_tensor(out=eq1, in0=lgv, in1=m1.unsqueeze(2).to_broadcast([128, nb, 8]), op=ALU.is_equal), reads=R, writes=KE)
                P.op('dve', lambda e: e.scalar_tensor_tensor(out=lg2, in0=eq1.rearrange("p c e -> p (c e)"), scalar=-1e30, in1=lg, op0=ALU.mult, op1=ALU.add),
                     reads=R + KE, writes=R)
                P.op('dve', lambda e: e.tensor_reduce(out=m2, in_=lg2v, axis=AX.X, op=ALU.max), reads=R, writes=R)
                P.op('dve', lambda e: e.tensor_tensor(out=eq2, in0=lg2v, in1=m2.unsqueeze(2).to_broadcast([128, nb, 8]), op=ALU.is_equal), reads=R, writes=KE)
                P.op('dve', lambda e: e.tensor_tensor(out=dd, in0=m2, in1=m1, op=ALU.subtract), reads=R, writes=R)
                P.op('act', lambda e: e.activation(out=W12[:, ck:ck + nb, 0], in_=dd, func=AF.Sigmoid, scale=-1.0), reads=R, writes=KE)
                P.op('act', lambda e: e.activation(out=W12[:, ck:ck + nb, 1], in_=dd, func=AF.Sigmoid, scale=1.0), reads=R, writes=KE)
                for bi in range(nb):
                    bsl = slice(bi * 128, (bi + 1) * 128)
                    bt, pst = psum(k)
                    pstb = pst.bitcast(BF16)
                    for kc in range(8):
                        P.op('pe', lambda e: e.transpose(out=pstb[:, kc * 128:(kc + 1) * 128], in_=H[:, kc, bsl], identity=identb),
                             reads=['H', 'identb'], writes=[('ps', bt)])
                    eng = 'act' if ck % 2 == 0 else 'dve'
                    if eng == 'act':
                        P.op('act', lambda e: e.activation(out=HTOK[:, ck, :], in_=pstb, func=AF.Copy), reads=[('ps', bt)], writes=[('htok', ck)])
                    else:
                        P.op('dve', lambda e: e.tensor_copy(out=HTOK[:, ck, :], in_=pstb), reads=[('ps', bt)], writes=[('htok', ck)])
                    ck += 1
            allE = [('eq', c) for c in range(NCK)]
            SEL = sb(st, nc, "m_sel", [128, NCK, 8], F32)
            PRE = sb(st, nc, "m_pre", [128, NCK + 1, 8], F32)
            RANK = sb(st, nc, "m_rank", [128, NCK, 8], F32)
            sm = sb(st, nc, "m_sm", [128, 64], F32)
            TOT, NBk, CB, OFF, ONE8 = sm[:, 0:8], sm[:, 8:16], sm[:, 16:24], sm[:, 24:32], sm[:, 32:40]
            EB = sb(st, nc, "m_eb", [128, 3, NB], F32)
            DF = sb(st, nc, "m_df", [128, NCK, 2], F32)
            IGf = sb(st, nc, "m_igf", [128, NB, 7], F32)
            IWf = sb(st, nc, "m_iwf", [128, NB, 2], F32)
            P.op('dve', lambda e: e.tensor_tensor(out=SEL, in0=EQ1, in1=EQ2, op=ALU.add), reads=allE, writes=['sel'])
            P.op('dve', lambda e: e.memset(PRE[:, 0, :], 0.0), writes=['pre'])
            P.op('dve', lambda e: e.memset(ONE8, 1.0), writes=['sm'])
            for c in range(NCK):
                P.op('dve', lambda e: e.tensor_tensor(out=PRE[:, c + 1, :], in0=PRE[:, c, :], in1=SEL[:, c, :], op=ALU.add),
                     reads=['pre', 'sel'], writes=['pre'])
            br, psr = psum(k)
            for c in range(NCK):
                P.op('pe', lambda e: e.matmul(psr[:, c * 8:(c + 1) * 8], lhsT=TRI, rhs=SEL[:, c, :], start=True, stop=False),
                     reads=['mtab', 'sel'], writes=[('ps', br)])
                P.op('pe', lambda e: e.matmul(psr[:, c * 8:(c + 1) * 8], lhsT=k.ones_f, rhs=PRE[:, c, :], start=False, stop=True),
                     reads=['ones', 'pre'], writes=[('ps', br)])
            P.op('dve', lambda e: e.tensor_copy(out=RANK, in_=psr[:, 0:NCK * 8].rearrange("p (c e) -> p c e", e=8)), reads=[('ps', br)], writes=['rank'])
            b2, ps2 = psum(k)
            P.op('pe', lambda e: e.matmul(ps2[:, 0:8], lhsT=k.ones_f, rhs=PRE[:, NCK, :], start=True, stop=True), reads=['ones', 'pre'], writes=[('ps', b2)])
            S_ = ['sm']
            P.op('dve', lambda e: e.tensor_copy(out=TOT, in_=ps2[:, 0:8]), reads=[('ps', b2)], writes=S_)
            P.op('dve', lambda e: e.tensor_scalar(out=NBk, in0=TOT, scalar1=0.0, scalar2=None, op0=ALU.is_gt), reads=S_, writes=S_)
            for m in range(1, 9):
                P.op('dve', lambda e: e.scalar_tensor_tensor(out=NBk, in0=TOT, scalar=float(MOE_BLK * m), in1=NBk, op0=ALU.is_gt, op1=ALU.add),
                     reads=S_, writes=S_)
            P.op('dve', lambda e: e.tensor_tensor_scan(out=CB, data0=ONE8, data1=NBk, initial=0.0, op0=ALU.mult, op1=ALU.add), reads=S_, writes=S_)
            P.op('dve', lambda e: e.tensor_tensor(out=OFF, in0=CB, in1=NBk, op=ALU.subtract), reads=S_, writes=S_)
            P.op('dve', lambda e: e.tensor_scalar(out=OFF, in0=OFF, scalar1=float(MOE_BLK), scalar2=None, op0=ALU.mult), reads=S_, writes=S_)
            P.op('dve', lambda e: e.tensor_tensor(out=RANK, in0=RANK, in1=OFF.unsqueeze(1).to_broadcast([128, NCK, 8]), op=ALU.add),
                 reads=['rank'] + S_, writes=['rank'])
            P.op('dve', lambda e: e.tensor_tensor(out=SEL, in0=RANK, in1=EQ1, op=ALU.mult), reads=['rank'] + allE, writes=['sel'])
            P.op('dve', lambda e: e.tensor_reduce(out=DF[:, :, 0], in_=SEL, axis=AX.X, op=ALU.add), reads=['sel'], writes=['df'])
            P.op('dve', lambda e: e.tensor_tensor(out=SEL, in0=RANK, in1=EQ2, op=ALU.mult), reads=['rank', 'df'] + allE, writes=['sel'])
            P.op('dve', lambda e: e.tensor_reduce(out=DF[:, :, 1], in_=SEL, axis=AX.X, op=ALU.add), reads=['sel'], writes=['df'])
            P.op('dve', lambda e: e.tensor_copy(out=DI, in_=DF), reads=['df'], writes=['di'])
            P.op('dve', lambda e: e.memset(EB[:, 0, :], 0.0), writes=['eb'])
            for e8 in range(8):
                P.op('dve', lambda e: e.scalar_tensor_tensor(out=EB[:, 0, :], in0=IOB, scalar=CB[:, e8:e8 + 1], in1=EB[:, 0, :], op0=ALU.is_ge, op1=ALU.add),
                     reads=['mtab', 'eb'] + S_, writes=['eb'])
            P.op('dve', lambda e: e.tensor_scalar(out=EB[:, 0, :], in0=EB[:, 0, :], scalar1=7.0, scalar2=None, op0=ALU.min), reads=['eb'], writes=['eb'])
            P.op('dve', lambda e: e.tensor_scalar(out=EB[:, 1, :], in0=EB[:, 0, :], scalar1=896.0, scalar2=float(f * 8 * 896), op0=ALU.mult, op1=ALU.add),
                 reads=['eb'], writes=['eb'])
            P.op('dve', lambda e: e.tensor_scalar(out=EB[:, 2, :], in0=EB[:, 0, :], scalar1=256.0, scalar2=float(f * 8 * 256), op0=ALU.mult, op1=ALU.add),
                 reads=['eb'], writes=['eb'])
            for b_ in range(NB):
                P.op('dve', lambda e: e.tensor_scalar(out=IGf[:, b_, :], in0=CGU, scalar1=EB[:, 1, b_:b_ + 1], scalar2=None, op0=ALU.add),
                     reads=['eb', 'mtab'], writes=['igf'])
                P.op('dve', lambda e: e.tensor_scalar(out=IWf[:, b_, :], in0=CWD, scalar1=EB[:, 2, b_:b_ + 1], scalar2=None, op0=ALU.add),
                     reads=['eb', 'mtab'], writes=['iwf'])
            P.op('dve', lambda e: e.tensor_copy(out=IGU, in_=IGf), reads=['igf'], writes=['igu'])
            P.op('dve', lambda e: e.tensor_copy(out=IWD, in_=IWf), reads=['iwf'], writes=['iwd'])
            for c in range(NCK):
                for j2 in range(2):
                    P.op('pool', lambda e: e.indirect_dma_start(out=HC, out_offset=bass.IndirectOffsetOnAxis(ap=DI[:, c, j2:j2 + 1], axis=0),
                                                                in_=HTOK[:, c, :], in_offset=None),
                         reads=['di', ('htok', c)], writes=['HC'], dma=True)
        P.barrier()
        with ExitStack() as st:
            wgu = [sb(st, nc, f"m_wgu{q}", [128, 8192], BF16) for q in range(2)]
            wdb = [sb(st, nc, f"m_wd{q}", [128, 28 * 512], BF16) for q in range(2)]
            hs = [sb(st, nc, f"m_hs{q}", [128, 4, 1024], BF16) for q in range(2)]
            HcT = sb(st, nc, "m_HcT", [128, 8, 512], BF16)
            act = sb(st, nc, "m_act", [128, 28, 512], BF16)
            yc = [sb(st, nc, f"m_yc{q}", [128, 1024], F32) for q in range(2)]
            sg_ = [sb(st, nc, f"m_sg{q}", [128, 512], BF16) for q in range(2)]
            identb = sb(st, nc, "m_identb2", [128, 128], BF16)
            P.op('dve', lambda e: e.tensor_copy(out=identb, in_=k.ident_f), reads=['ident'], writes=['identb'])
            n_gu = [0]

            def gather_gu(b_, g7):
                q = n_gu[0] % 2
                n_gu[0] += 1
                P.op('pool', lambda e: e.indirect_dma_start(out=wgu[q], out_offset=None, in_=k.WGUx,
                                                            in_offset=bass.IndirectOffsetOnAxis(ap=IGU[:, b_, g7:g7 + 1], axis=0)),
                     reads=['igu', 'wgux_all'], writes=[('wgu', q)], dma=True)
                return q

            def gather_wd(b_, dh):
                P.op('pool', lambda e: e.indirect_dma_start(out=wdb[dh], out_offset=None, in_=k.WDx,
                                                            in_offset=bass.IndirectOffsetOnAxis(ap=IWD[:, b_, dh:dh + 1], axis=0)),
                     reads=['iwd', 'wdx_all'], writes=[('wd', dh)], dma=True)

            def load_hs(b_):
                P.op('sp', lambda e: e.dma_start(out=hs[b_ % 2], in_=HC[b_ * 512:(b_ + 1) * 512, :].rearrange("(sg p) d -> p sg d", p=128)),
                     reads=['HC'], writes=[('hs', b_ % 2)], dma=True)
            load_hs(0)
            pre_gu = [gather_gu(0, 0), gather_gu(0, 1)]
            gather_wd(0, 0)
            gather_wd(0, 1)
            nsg = 0
            nyc = 0
            for b_ in range(NB):
                if b_ + 1 < NB:
                    load_hs(b_ + 1)
                hsb = hs[b_ % 2]
                for kc in range(8):
                    bt, pst = psum(k)
                    pstb = pst.bitcast(BF16)
                    for sgi in range(4):
                        P.op('pe', lambda e: e.transpose(out=pstb[:, sgi * 128:(sgi + 1) * 128], in_=hsb[:, sgi, kc * 128:(kc + 1) * 128], identity=identb),
                             reads=[('hs', b_ % 2), 'identb'], writes=[('ps', bt)])
                    if kc % 2 == 0:
                        P.op('act', lambda e: e.activation(out=HcT[:, kc, :], in_=pstb[:, 0:512], func=AF.Copy), reads=[('ps', bt)], writes=['HcT'])
                    else:
                        P.op('dve', lambda e: e.tensor_copy(out=HcT[:, kc, :], in_=pstb[:, 0:512]), reads=[('ps', bt)], writes=['HcT'])
                for g7 in range(7):
                    if g7 < 2:
                        q = pre_gu[g7]
                    else:
                        q = gather_gu(b_, g7)
                    wv = wgu[q].rearrange("p (kc h c) -> p kc h c", kc=8, h=2)
                    for j in range(4):
                        fch = g7 * 4 + j
                        bg, psg = psum(k)
                        for kc in range(8):
                            P.op('pe', lambda e: e.matmul(psg, lhsT=wv[:, kc, 0, j * 128:(j + 1) * 128], rhs=HcT[:, kc, :], start=(kc == 0), stop=(kc == 7)),
                                 reads=[('wgu', q), 'HcT'], writes=[('ps', bg)])
                        bu, psu = psum(k)
                        for kc in range(8):
                            P.op('pe', lambda e: e.matmul(psu, lhsT=wv[:, kc, 1, j * 128:(j + 1) * 128], rhs=HcT[:, kc, :], start=(kc == 0), stop=(kc == 7)),
                                 reads=[('wgu', q), 'HcT'], writes=[('ps', bu)])
                        sgt = sg_[nsg % 2]
                        ksg = ('sg', nsg % 2)
                        nsg += 1
                        P.op('act', lambda e: e.activation(out=sgt, in_=psg, func=AF.Silu), reads=[('ps', bg)], writes=[ksg])
                        P.op('dve', lambda e: e.tensor_tensor(out=act[:, fch, :], in0=sgt, in1=psu, op=ALU.mult), reads=[ksg, ('ps', bu)], writes=[('act', fch)])
                if b_ + 1 < NB:
                    pre_gu = [gather_gu(b_ + 1, 0), gather_gu(b_ + 1, 1)]
                for sgi in range(4):
                    y = yc[nyc % 2]
                    ky = ('yc', nyc % 2)
                    nyc += 1
                    for dh in range(2):
                        wv = wdb[dh].rearrange("p (fc c) -> p fc c", c=512)
                        bd, psd = psum(k)
                        for fc in range(28):
                            P.op('pe', lambda e: e.matmul(psd, lhsT=act[:, fc, sgi * 128:(sgi + 1) * 128], rhs=wv[:, fc, :], start=(fc == 0), stop=(fc == 27)),
                                 reads=[('act', fc), ('wd', dh)], writes=[('ps', bd)])
                        if dh == 0:
                            P.op('act', lambda e: e.activation(out=y[:, 0:512], in_=psd, func=AF.Copy), reads=[('ps', bd)], writes=[ky])
                        else:
                            P.op('dve', lambda e: e.tensor_copy(out=y[:, 512:1024], in_=psd), reads=[('ps', bd)], writes=[ky])
                    r0 = b_ * 512 + sgi * 128
                    P.op('sp', lambda e: e.dma_start(out=YC[r0:r0 + 128, :], in_=y), reads=[ky], writes=['YC'], dma=True)
                if b_ + 1 < NB:
                    gather_wd(b_ + 1, 0)
                    gather_wd(b_ + 1, 1)
        P.barrier()
        with ExitStack() as st:
            xt = sb(st, nc, "m_xtE", [128, 8, 512], F32)
            sqE = sb(st, nc, "m_sqE", [128, 8, 512], BF16)
            rsE = sb(st, nc, "m_rsE", [128, 512], F32)
            y1 = [sb(st, nc, f"m_y1{q}", [128, 1024], F32) for q in range(2)]
            y2 = [sb(st, nc, f"m_y2{q}", [128, 1024], F32) for q in range(2)]
            ck = 0
            for ti, (c0, n, s) in enumerate(tiles):
                load_x_tile(k, xt, c0, n)
                banks = [psum(k) for _ in range(8)]
                for bi in range(n // 128):
                    q = ck % 2
                    P.op('pool', lambda e: e.indirect_dma_start(out=y1[q], out_offset=None, in_=YC,
                                                                in_offset=bass.IndirectOffsetOnAxis(ap=DI[:, ck, 0:1], axis=0)),
                         reads=['YC', 'di'], writes=[('y1', q)], dma=True)
                    P.op('pool', lambda e: e.indirect_dma_start(out=y2[q], out_offset=None, in_=YC,
                                                                in_offset=bass.IndirectOffsetOnAxis(ap=DI[:, ck, 1:2], axis=0)),
                         reads=['YC', 'di'], writes=[('y2', q)], dma=True)
                    P.op('dve', lambda e: e.tensor_scalar(out=y1[q], in0=y1[q], scalar1=W12[:, ck, 0:1], scalar2=None, op0=ALU.mult),
                         reads=[('y1', q), ('eq', ck)], writes=[('y1', q)])
                    P.op('dve', lambda e: e.scalar_tensor_tensor(out=y1[q], in0=y2[q], scalar=W12[:, ck, 1:2], in1=y1[q], op0=ALU.mult, op1=ALU.add),
                         reads=[('y1', q), ('y2', q), ('eq', ck)], writes=[('y1', q)])
                    for kc in range(8):
                        bb, pp = banks[kc]
                        P.op('pe', lambda e: e.transpose(out=pp[:, bi * 128:(bi + 1) * 128], in_=y1[q][:, kc * 128:(kc + 1) * 128], identity=k.ident_f),
                             reads=[('y1', q), 'ident'], writes=[('ps', bb)])
                    ck += 1
                for kc in range(8):
                    bb, pp = banks[kc]
                    gate = k.mods[:, i, 5 * 8 + kc, s:s + 1]
                    P.op('dve', lambda e: e.scalar_tensor_tensor(out=xt[:, kc, :n], in0=pp[:, :n], scalar=gate, in1=xt[:, kc, :n], op0=ALU.mult, op1=ALU.add),
                         reads=[('ps', bb), 'xt', 'mods'], writes=['xt'])
                if last and k.fuse_final:
                    P.op('act', lambda e: e.activation(out=sqE[:, :, :n], in_=xt[:, :, :n], func=AF.Square), reads=['xt'], writes=['sqE'])
                    bf_, psf = psum(k)
                    for kc in range(8):
                        P.op('pe', lambda e: e.matmul(psf[:, :n], lhsT=k.ones_b[:, :], rhs=sqE[:, kc, :n], start=(kc == 0), stop=(kc == 7)),
                             reads=['sqE', 'ones_b'], writes=[('ps', bf_)])
                    P.op('act', lambda e: e.activation(out=rsE[:, :n], in_=psf[:, :n], func=AF.Ln, scale=1.0 / D, bias=k.epsb[:, 0:1]),
                         reads=[('ps', bf_)], writes=['rsE'])
                    P.op('act', lambda e: e.activation(out=rsE[:, :n], in_=rsE[:, :n], func=AF.Exp, scale=-0.5), reads=['rsE'], writes=['rsE'])
                    for kc in range(8):
                        P.op('dve', lambda e: e.scalar_tensor_tensor(out=xt[:, kc, :n], in0=xt[:, kc, :n], scalar=k.fg_s[:, kc:kc + 1], in1=rsE[:, :n],
                                                                     op0=ALU.mult, op1=ALU.mult),
                             reads=['xt', 'rsE', 'fg'], writes=['xt'])
                    P.op('sp', lambda e: e.dma_start(out=k.out.rearrange("(kc q) t -> q kc t", q=128)[:, :, c0 - NCTX:c0 - NCTX + n], in_=xt[:, :, :n]),
                         reads=['xt'], writes=['out'], dma=True)
                else:
                    store_x_tile(k, xt, c0, n)
    P.barrier()
```

```python
import numpy as np
from contextlib import ExitStack
import concourse.bass as bass
import concourse.mybir as mybir
from concourse.bass_utils import run_bass_kernel_spmd

F32 = mybir.dt.float32
BF16 = mybir.dt.bfloat16
AF = mybir.ActivationFunctionType
ALU = mybir.AluOpType
AX = mybir.AxisListType

NPOOL = 16
SPARSE_MOE = True
SAME_ENGINE_SYNC = True

T = 4352
NCTX = 256
SEQ = 4096
D = 1024
DFF = 3584
EPS = 1e-6
TILES = [(0, 256, 1)] + [(256 + 512 * i, 512, 0) for i in range(8)]


class Prog:
    def __init__(self, nc, stack):
        self.nc = nc
        self.stack = stack
        self.eng = {'pe': nc.tensor, 'act': nc.scalar, 'dve': nc.vector, 'pool': nc.gpsimd, 'sp': nc.sync}
        self.sems = {}
        self.cnt = {e: 0 for e in self.eng}
        self.seen = {e: {} for e in self.eng}
        self.keys = {}
        self.dma_n = {'sp': 0, 'pool': 0}
        self.n_inst = 0

    def sem(self, sk):
        if sk not in self.sems:
            name = 's_' + (sk if isinstance(sk, str) else f'{sk[0]}q{sk[1]}')
            self.sems[sk] = self.stack.enter_context(self.nc.semaphore(name))
        return self.sems[sk]

    def op(self, eng, fn, reads=(), writes=(), dma=False):
        deps = {}
        own = None if dma else eng

        def add(sk, v):
            if deps.get(sk, 0) < v:
                deps[sk] = v
        for k in reads:
            st = self.keys.get(k)
            if st is not None and st[0] is not None:
                add(*st[0])
        for k in writes:
            st = self.keys.get(k)
            if st is not None:
                if st[0] is not None and st[0][0] != own:
                    add(*st[0])
                for sk, v in st[1].items():
                    if sk != own:
                        add(sk, v)
        if dma:
            n = self.dma_n[eng]
            slot = n % NPOOL
            semkey = (eng, slot)
            val = 16 * (n // NPOOL + 1)
            if n >= NPOOL:
                add(semkey, 16 * (n // NPOOL))
            self.dma_n[eng] += 1
            inc = 16
        else:
            self.cnt[eng] += 1
            semkey = eng
            val = self.cnt[eng]
            inc = 1
        h = self.eng[eng]
        for sk, v in deps.items():
            if sk == eng and (eng == 'pe' or not SAME_ENGINE_SYNC):
                continue
            if self.seen[eng].get(sk, 0) >= v:
                continue
            self.seen[eng][sk] = v
            h.wait_ge(self.sem(sk), v)
        inst = fn(h)
        inst.then_inc(self.sem(semkey), inc)
        self.n_inst += 1
        for k in reads:
            st = self.keys.setdefault(k, [None, {}])
            if st[1].get(semkey, 0) < val:
                st[1][semkey] = val
        for k in writes:
            self.keys[k] = [(semkey, val), {}]

    def _all_now(self):
        cur = {}
        for e in ('pe', 'act', 'dve', 'pool'):
            if self.cnt[e] > 0:
                cur[e] = self.cnt[e]
        for q, n in self.dma_n.items():
            for slot in range(min(n, NPOOL)):
                cur[(q, slot)] = 16 * (((n - 1 - slot) // NPOOL) + 1)
        return cur

    def barrier(self, engines=('pe', 'act', 'dve', 'pool', 'sp')):
        cur = self._all_now()
        for e in engines:
            h = self.eng[e]
            for sk, v in cur.items():
                if sk == e:
                    continue
                if self.seen[e].get(sk, 0) >= v:
                    continue
                self.seen[e][sk] = v
                h.wait_ge(self.sem(sk), v)
        if len(engines) == 5:
            self.keys.clear()

    def finish(self):
        self.barrier(engines=('sp',))


class K:
    pass


_SB_N = [0]


def sb(st, nc, name, shape, dt):
    _SB_N[0] += 1
    h = st.enter_context(nc.sbuf_tensor(f"{name}_u{_SB_N[0]}", shape, dt))
    return h[tuple(slice(None) for _ in shape)]


def col(v, n):
    return np.ascontiguousarray(np.asarray(v).reshape(n, 128).T)


def build_program(parts, debug=False):
    nc = bass.Bass("TRN2", target_bir_lowering=False)
    k = K()
    k.nc = nc
    dr = {}

    def din(name, shape, dt=F32):
        dr[name] = nc.dram_tensor(name, list(shape), dt, kind="ExternalInput").ap()
        return dr[name]

    k.xT = din("xT", [D, T])
    k.cc = din("cc", [128, 8, 2])
    k.ada_w = din("ada_w", [4, D, 6 * D])
    k.adab = din("adab", [128, 4, 48])
    k.adab_row = din("adab_row", [4, 2, 6144])
    k.ng = din("ng", [128, 4, 2, 8])
    k.fg = din("fg", [128, 8])
    k.ones_in = din("ones_f", [128, 128])
    k.ident_in = din("ident_f", [128, 128])
    k.esel_in = din("esel", [8, 8, 128])
    k.ffn_w_gu = din("ffn_w_gu", [2, D, 2 * DFF])
    k.ffn_w_down = din("ffn_w_down", [2, DFF, D])
    k.router = din("router", [128, 2, 8, 8])
    k.moe_w_gu = din("moe_w_gu", [2, 8, D, 2 * DFF])
    k.moe_w_down = din("moe_w_down", [2, 8, DFF, D])
    k.lru_w_in = din("lru_w_in", [2, D, 2560])
    k.lru_wg = din("lru_wg", [2, 128, 40, 128])
    k.lru_w_out = din("lru_w_out", [2, 1280, D])
    k.lru_cw_in = din("lru_cw", [128, 2, 4, 10])
    k.lru_cb_in = din("lru_cb", [128, 2, 10])
    k.lru_bg_in = din("lru_bg", [128, 2, 2, 2, 10])
    k.lru_lam_in = din("lru_lam", [128, 2, 2, 10])
    k.ret_w_in = din("ret_w_in", [1, D, 6144])
    k.ret_w_out = din("ret_w_out", [1, 2048, D])
    k.ret_ld_in = din("ret_ld", [128, 8])
    k.ret_gain_in = din("ret_gain", [128, 16])
    k.ret_cosT = din("ret_cosT", [128, SEQ])
    k.ret_sinT = din("ret_sinT", [128, SEQ])
    k.ret_costok = din("ret_costok", [SEQ, 128])
    k.ret_sintok = din("ret_sintok", [SEQ, 128])
    k.ret_tab = din("ret_tab", [128, 771])
    k.swa_w_ext = din("swa_w_ext", [D, 2816])
    k.swa_w_out = din("swa_w_out", [1, D, D])
    k.swa_sink_in = din("swa_sink", [64, 16])
    k.swa_cos = din("swa_cos", [128, SEQ])
    k.swa_sin = din("swa_sin", [128, SEQ])
    k.moe_tab = din("moe_tab", [128, 161])
    k.dr = dr

    if debug:
        k.out = nc.dram_tensor("xdump", [D, T], F32, kind="ExternalOutput").ap()
    else:
        k.out = nc.dram_tensor("outT", [D, SEQ], F32, kind="ExternalOutput").ap()
    k.X = nc.dram_tensor("Xres", [D, T], F32, kind="Internal").ap()
    k.WGU = nc.dram_tensor("WGUs", [18, D, 2 * DFF], BF16, kind="Internal").ap() if not SPARSE_MOE else nc.dram_tensor("WGUs", [10, D, 2 * DFF], BF16, kind="Internal").ap()
    k.WD = nc.dram_tensor("WDs", [18, DFF, D], BF16, kind="Internal").ap() if not SPARSE_MOE else nc.dram_tensor("WDs", [10, DFF, D], BF16, kind="Internal").ap()
    k.QT = nc.dram_tensor("QTs", [1024, T], BF16, kind="Internal").ap()
    k.KT = nc.dram_tensor("KTs", [1024, T], BF16, kind="Internal").ap()
    k.KTOK = nc.dram_tensor("KTOKs", [T, 1024], BF16, kind="Internal").ap()
    k.VTOK = nc.dram_tensor("VTOKs", [T, 2048], BF16, kind="Internal").ap()
    k.GS = nc.dram_tensor("GSs", [2048, T], BF16, kind="Internal").ap()
    k.SBd = nc.dram_tensor("SBds", [4, NCH, 256, 512], BF16, kind="Internal").ap()
    k.ZR = nc.dram_tensor("ZRs", [2048, T], BF16, kind="Internal").ap()
    k.QS = nc.dram_tensor("QSs", [64, 16, T], BF16, kind="Internal").ap()
    k.KS = nc.dram_tensor("KSs", [64, 4, T], BF16, kind="Internal").ap()
    k.VS = nc.dram_tensor("VSs", [T, 256], BF16, kind="Internal").ap()
    k.OS = nc.dram_tensor("OSs", [64, 16, T], BF16, kind="Internal").ap()
    k.HC = nc.dram_tensor("HCs", [NBLK_MAX * 512, 1024], BF16, kind="Internal").ap()
    k.YC = nc.dram_tensor("YCs", [NBLK_MAX * 512, 1024], F32, kind="Internal").ap()
    k.WGUx = nc.dram_tensor("WGUx", [16 * 7 * 128, 8192], BF16, kind="Internal").ap()
    k.WDx = nc.dram_tensor("WDx", [16 * 2 * 128, 28 * 512], BF16, kind="Internal").ap()
    k.XR = nc.dram_tensor("XRs", [1280, T], F32, kind="Internal").ap()
    k.YG = nc.dram_tensor("YGs", [1280, T], BF16, kind="Internal").ap()
    k.ZG = nc.dram_tensor("ZGs", [1280, T], BF16, kind="Internal").ap()

    with ExitStack() as st:
        P = Prog(nc, st)
        k.P = P
        k.PS = [st.enter_context(nc.psum_tensor(f"ps{i}", [128, 512], F32))[:, :] for i in range(8)]
        k.ps_n = 0
        k.cond = sb(st, nc, "cond", [128, 8, 2], F32)
        k.mods = sb(st, nc, "mods", [128, 4, 48, 2], F32)
        k.acoef = sb(st, nc, "acoef", [128, 4, 2, 8, 2], F32)
        k.adab_s = sb(st, nc, "adab_s", [128, 4, 48], F32)
        k.ng_s = sb(st, nc, "ng_s", [128, 4, 2, 8], F32)
        k.fg_s = sb(st, nc, "fg_s", [128, 8], F32)
        k.ones_f = sb(st, nc, "ones_fs", [128, 128], F32)
        k.ident_f = sb(st, nc, "ident_fs", [128, 128], F32)
        k.esel = sb(st, nc, "esel_s", [8, 8, 128], F32)
        k.rt = sb(st, nc, "rt_s", [128, 2, 8, 8], F32)
        k.epsb = sb(st, nc, "epsb", [128, 1], F32)
        P.op('dve', lambda e: e.memset(k.epsb, EPS), writes=['epsb'])
        k.ones_b = sb(st, nc, "ones_b", [128, 128], BF16)
        P.op('dve', lambda e: e.memset(k.ones_b, 1.0), writes=['ones_b'])
        k.oneb = sb(st, nc, "oneb", [128, 1], F32)
        P.op('dve', lambda e: e.memset(k.oneb, 1.0), writes=['oneb'])
        k.lru_cw = sb(st, nc, "lru_cw_s", [128, 2, 4, 10], F32)
        k.lru_cb = sb(st, nc, "lru_cb_s", [128, 2, 10], F32)
        k.lru_bg = sb(st, nc, "lru_bg_s", [128, 2, 2, 2, 10], F32)
        k.lru_lam = sb(st, nc, "lru_lam_s", [128, 2, 2, 10], F32)
        for dst, src in ((k.lru_cw, k.lru_cw_in), (k.lru_cb, k.lru_cb_in), (k.lru_bg, k.lru_bg_in), (k.lru_lam, k.lru_lam_in)):
            P.op('sp', lambda e, dst=dst, src=src: e.dma_start(out=dst, in_=src), writes=['lrup'], dma=True)
        k.ret_ld = sb(st, nc, "ret_ld_s", [128, 8], F32)
        k.ret_gain = sb(st, nc, "ret_gain_s", [128, 16], F32)
        for dst, src in ((k.ret_ld, k.ret_ld_in), (k.ret_gain, k.ret_gain_in)):
            P.op('sp', lambda e, dst=dst, src=src: e.dma_start(out=dst, in_=src), writes=['retp'], dma=True)
        k.swa_sink = sb(st, nc, "swa_sink_s", [64, 16], F32)
        P.op('sp', lambda e: e.dma_start(out=k.swa_sink, in_=k.swa_sink_in), writes=['swap'], dma=True)
        for dst, src, key in ((k.adab_s, k.adab, 'adab'), (k.ng_s, k.ng, 'ng'), (k.fg_s, k.fg, 'fg'),
                              (k.ones_f, k.ones_in, 'ones'), (k.ident_f, k.ident_in, 'ident'),
                              (k.esel, k.esel_in, 'esel'), (k.rt, k.router, 'rt')):
            P.op('sp', lambda e, dst=dst, src=src: e.dma_start(out=dst, in_=src), writes=[key], dma=True)

        if 'init' in parts:
            P.op('sp', lambda e: e.dma_start(out=k.X, in_=k.xT), writes=['X'], dma=True)
        k.precast_done = set()
        k.cond_done = False
        k.mods_split = ('mods' in parts and 'mix0' in parts)
        k.fuse_final = ('final' in parts and 'ffn3' in parts and SPARSE_MOE and not debug)
        k.pc_queue = []
        if 'mods' in parts:
            stage_mods(k, layers=(0,) if k.mods_split else (0, 1, 2, 3))
        for i in range(4):
            if f'ffn{i}' in parts:
                if SPARSE_MOE and i % 2 == 1:
                    for e8 in range(8):
                        k.pc_queue.append(lambda pe=(i // 2) * 8 + e8: precast_moe(k, pe))
                else:
                    for e8 in (range(8) if i % 2 == 1 else range(1)):
                        k.pc_queue.append(lambda p=ffn_pass_index(i, e8): precast(k, p))
            if f'mix{i}' in parts:
                if i % 3 == 0:
                    stage_lru(k, i)
                elif i % 3 == 1:
                    stage_ret(k, i)
                else:
                    stage_swa(k, i)
            if f'ffn{i}' in parts:
                if SPARSE_MOE and i % 2 == 1:
                    stage_moe_sparse(k, i)
                else:
                    drain_precast(k, 1000)
                    stage_ffn(k, i)
        if 'final' in parts and not k.fuse_final:
            stage_final(k)
        P.barrier()
        if debug:
            P.op('sp', lambda e: e.dma_start(out=k.out, in_=k.X), dma=True)
        P.finish()
        k.n_inst = P.n_inst
    return nc, k


def psum(k):
    b = k.ps_n % 8
    k.ps_n += 1
    return b, k.PS[b]


def stage_mods(k, layers=(0, 1, 2, 3), final_barrier=True, outer=None, cbw=512):
    nc, P = k.nc, k.P
    with ExitStack() as own:
        st = outer if outer is not None else own
        wa = [sb(st, nc, f"wa{j}", [128, 8, cbw], F32) for j in range(2)]
        mrow = [sb(st, nc, f"mrow{j}", [2, cbw], F32) for j in range(2)]
        nj = cbw // 128
        if not k.cond_done:
            ccs = sb(st, nc, "ccs", [128, 8, 2], F32)
            P.op('sp', lambda e: e.dma_start(out=ccs, in_=k.cc), writes=['ccs'], dma=True)
            P.op('act', lambda e: e.activation(out=k.cond, in_=ccs, func=AF.Silu), reads=['ccs'], writes=['cond'])
            k.cond_done = True
        n = 0
        for i in layers:
            wsrc = k.ada_w[i].rearrange("(kc p) j -> p kc j", p=128)
            for cb in range(6144 // cbw):
                buf = wa[n % 2]
                key = ('wa', n % 2)
                mr = mrow[n % 2]
                kmr = ('mrow', n % 2)
                P.op('sp', lambda e: e.dma_start(out=buf, in_=wsrc[:, :, cb * cbw:(cb + 1) * cbw]), writes=[key], dma=True)
                b, ps = psum(k)
                for kc in range(8):
                    P.op('pe', lambda e: e.matmul(ps[0:2, 0:cbw], lhsT=k.cond[:, kc, :], rhs=buf[:, kc, :], start=(kc == 0), stop=(kc == 7)),
                         reads=[key, 'cond'], writes=[('ps', b)])
                P.op('dve', lambda e: e.tensor_copy(out=mr, in_=ps[0:2, 0:cbw]), reads=[('ps', b)], writes=[kmr])
                b2, ps2 = psum(k)
                for jj in range(nj):
                    P.op('pe', lambda e: e.transpose(out=ps2[:, jj * 2:jj * 2 + 2], in_=mr[0:2, jj * 128:(jj + 1) * 128], identity=k.ident_f[0:2, 0:2]),
                         reads=[kmr, 'ident'], writes=[('ps', b2)])
                P.op('dve', lambda e: e.tensor_tensor(out=k.mods[:, i, cb * nj:(cb + 1) * nj, :], in0=ps2[:, 0:2 * nj].rearrange("p (j s) -> p j s", s=2),
                                                      in1=k.adab_s[:, i, cb * nj:(cb + 1) * nj].unsqueeze(2).to_broadcast([128, nj, 2]), op=ALU.add),
                     reads=[('ps', b2), 'adab'], writes=['mods'])
                n += 1
            for j in range(2):
                for s in range(2):
                    m = (1 + 3 * j) * 8
                    P.op('dve', lambda e: e.tensor_scalar(out=k.acoef[:, i, j, :, s], in0=k.mods[:, i, m:m + 8, s],
                                                          scalar1=1.0, scalar2=None, op0=ALU.add),
                         reads=['mods'], writes=['acoef'])
                    P.op('dve', lambda e: e.tensor_tensor(out=k.acoef[:, i, j, :, s], in0=k.acoef[:, i, j, :, s],
                                                          in1=k.ng_s[:, i, j, :], op=ALU.mult),
                         reads=['acoef', 'ng'], writes=['acoef'])
    if final_barrier:
        P.barrier()


def norm_tile(k, xt, kx, n, i, j, s, hb, kh, sq, rs, tmps, hf=None, khf=None):
    P = k.P
    sqb = sq.bitcast(BF16)[:, :, 0:512]
    P.op('act', lambda e: e.activation(out=sqb[:, :, :n], in_=xt[:, :, :n], func=AF.Square), reads=[kx], writes=['sq'])
    b, ps = psum(k)
    for kc in range(8):
        P.op('pe', lambda e: e.matmul(ps[:, :n], lhsT=k.ones_b[:, :], rhs=sqb[:, kc, :n], start=(kc == 0), stop=(kc == 7)),
             reads=['sq', 'ones_b'], writes=[('ps', b)])
    P.op('act', lambda e: e.activation(out=rs[:, :n], in_=ps[:, :n], func=AF.Ln, scale=1.0 / D, bias=k.epsb[:, 0:1]),
         reads=[('ps', b)], writes=['rs'])
    P.op('act', lambda e: e.activation(out=rs[:, :n], in_=rs[:, :n], func=AF.Exp, scale=-0.5), reads=['rs'], writes=['rs'])
    for kc in range(8):
        tmp = tmps[kc % 2]
        kt = ('ntmp', kc % 2)
        P.op('dve', lambda e: e.scalar_tensor_tensor(out=tmp[:, :n], in0=xt[:, kc, :n], scalar=k.acoef[:, i, j, kc, s:s + 1],
                                                     in1=rs[:, :n], op0=ALU.mult, op1=ALU.mult),
             reads=[kx, 'rs', 'acoef'], writes=[kt])
        sh = k.mods[:, i, (3 * j) * 8 + kc, s:s + 1]
        if hf is None:
            P.op('act', lambda e: e.activation(out=hb[:, kc, :n], in_=tmp[:, :n], func=AF.Identity, bias=sh, scale=1.0),
                 reads=[kt, 'mods'], writes=[kh])
        else:
            P.op('act', lambda e: e.activation(out=hf[:, kc, :n], in_=tmp[:, :n], func=AF.Identity, bias=sh, scale=1.0),
                 reads=[kt, 'mods'], writes=[khf])
    if hf is not None:
        P.op('dve', lambda e: e.tensor_copy(out=hb[:, :, :n], in_=hf[:, :, :n]), reads=[khf], writes=[kh])


def ffn_pass_index(i, e):
    return {0: 0, 1: 1 + e, 2: 9, 3: 10 + e}[i]


def precast(k, p):
    if p in k.precast_done:
        return
    k.precast_done.add(p)
    P = k.P
    if p == 0:
        gu, dn = k.ffn_w_gu[0], k.ffn_w_down[0]
    elif p == 9:
        gu, dn = k.ffn_w_gu[1], k.ffn_w_down[1]
    elif p < 9:
        gu, dn = k.moe_w_gu[0, p - 1], k.moe_w_down[0, p - 1]
    else:
        gu, dn = k.moe_w_gu[1, p - 10], k.moe_w_down[1, p - 10]
    P.op('pool', lambda e: e.dma_start(out=k.WGU[p].rearrange("r (a b) -> (r a) b", b=1024),
                                       in_=gu.rearrange("r (a b) -> (r a) b", b=1024)),
         writes=[('wgus', p)], dma=True)
    P.op('pool', lambda e: e.dma_start(out=k.WD[p], in_=dn), writes=[('wds', p)], dma=True)


def stage_ffn(k, i):
    nc, P = k.nc, k.P
    moe = (i % 2 == 1)
    last = (i == 3)
    f = i // 2
    experts = list(range(8)) if moe else [0]
    tiles = TILES[1:] if last else TILES
    precast(k, ffn_pass_index(i, 0))
    with ExitStack() as st:
        wgu = [sb(st, nc, f"wgu{j}", [128, 8, 2, 512], BF16) for j in range(2)]
        wd = [sb(st, nc, f"wd{j}", [128, 28, 256], BF16) for j in range(2)]
        nbuf = 1 if moe else 2
        Hs = [sb(st, nc, f"Hb{q}", [128, 8, 512], BF16) for q in range(nbuf)]
        xts = [sb(st, nc, f"xt{q}", [128, 8, 512], F32) for q in range(nbuf)]
        act = sb(st, nc, "actb", [128, 28, 512], BF16)
        sq = sb(st, nc, "sq", [128, 8, 512], F32)
        rs = sb(st, nc, "rs", [128, 512], F32)
        tmps = [sb(st, nc, f"ntmp{j}", [128, 512], F32) for j in range(2)]
        sg = [sb(st, nc, f"sg{j}", [128, 512], BF16) for j in range(2)]
        if moe:
            hf = sb(st, nc, "hf", [128, 8, 512], F32)
            yacc = sb(st, nc, "yacc", [128, 8, 512], F32)
            cmul = [sb(st, nc, f"cmul{j}", [128, 512], F32) for j in range(2)]
            combT = sb(st, nc, "combT", [8, 512], F32)
            rsm = sb(st, nc, "rsm", [128, 64], F32)

        seq_gu = [(ti, e, g) for ti in range(len(tiles)) for e in experts for g in range(7)]
        seq_wd = [(ti, e, q) for ti in range(len(tiles)) for e in experts for q in range(4)]
        st_gu = {'n': 0}
        st_wd = {'n': 0}

        def ensure_gu(upto):
            while st_gu['n'] <= upto and st_gu['n'] < len(seq_gu):
                a = st_gu['n']
                ti, e, g = seq_gu[a]
                p = ffn_pass_index(i, e)
                if ti == 0:
                    precast(k, p)
                buf = wgu[a % 2]
                for hh in range(2):
                    src = k.WGU[p].rearrange("(kc q) c -> q kc c", q=128)[:, :, hh * DFF + g * 512:hh * DFF + (g + 1) * 512]
                    P.op('sp', lambda e_: e_.dma_start(out=buf[:, :, hh, :], in_=src), reads=[('wgus', p)],
                         writes=[('wgu', a % 2, hh)], dma=True)
                st_gu['n'] += 1

        def ensure_wd(upto):
            while st_wd['n'] <= upto and st_wd['n'] < len(seq_wd):
                a = st_wd['n']
                ti, e, q = seq_wd[a]
                p = ffn_pass_index(i, e)
                src = k.WD[p].rearrange("(fc q) d -> q fc d", q=128)[:, :, q * 256:(q + 1) * 256]
                buf = wd[a % 2]
                P.op('sp', lambda e_: e_.dma_start(out=buf, in_=src), reads=[('wds', p)], writes=[('wd', a % 2)], dma=True)
                st_wd['n'] += 1

        a_gu = 0
        a_wd = 0
        nsg = 0
        def prep(ti):
            c0, n, s = tiles[ti]
            xt, H = xts[ti % nbuf], Hs[ti % nbuf]
            kxt, kH = ('xt', ti % nbuf), ('H', ti % nbuf)
            P.op('sp', lambda e: e.dma_start(out=xt[:, :, :n], in_=k.X.rearrange("(kc q) t -> q kc t", q=128)[:, :, c0:c0 + n]),
                 reads=['X', ('Xt', c0)], writes=[kxt], dma=True)
            if moe:
                norm_tile(k, xt, kxt, n, i, 1, s, H, kH, sq, rs, tmps, hf=hf, khf='hf')
                route_tile(k, f, n, hf, combT, rsm)
            else:
                norm_tile(k, xt, kxt, n, i, 1, s, H, kH, sq, rs, tmps)

        ensure_gu(0)
        if not moe:
            prep(0)
        for ti, (c0, n, s) in enumerate(tiles):
            xt, H = xts[ti % nbuf], Hs[ti % nbuf]
            kxt, kH = ('xt', ti % nbuf), ('H', ti % nbuf)
            if moe:
                prep(ti)
            elif ti + 1 < len(tiles):
                prep(ti + 1)
            ensure_gu(a_gu)
            for e in experts:
                if moe:
                    cm = cmul[e % 2]
                    kcm = ('cmul', e % 2)
                    b, ps = psum(k)
                    P.op('pe', lambda e_: e_.matmul(ps[:, :n], lhsT=k.esel[0:8, e, :], rhs=combT[0:8, :n], start=True, stop=True),
                         reads=['esel', 'combT'], writes=[('ps', b)])
                    P.op('act', lambda e_: e_.activation(out=cm[:, :n], in_=ps[:, :n], func=AF.Copy),
                         reads=[('ps', b)], writes=[kcm])
                ensure_wd(a_wd)
                for g in range(7):
                    ensure_gu(a_gu + 1)
                    buf = wgu[a_gu % 2]
                    kb0 = ('wgu', a_gu % 2, 0)
                    kb1 = ('wgu', a_gu % 2, 1)
                    for j in range(4):
                        fch = g * 4 + j
                        bg, psg = psum(k)
                        for kc in range(8):
                            P.op('pe', lambda e_: e_.matmul(psg[:, :n], lhsT=buf[:, kc, 0, j * 128:(j + 1) * 128], rhs=H[:, kc, :n],
                                                            start=(kc == 0), stop=(kc == 7)),
                                 reads=[kb0, kH], writes=[('ps', bg)])
                        bu, psu = psum(k)
                        for kc in range(8):
                            P.op('pe', lambda e_: e_.matmul(psu[:, :n], lhsT=buf[:, kc, 1, j * 128:(j + 1) * 128], rhs=H[:, kc, :n],
                                                            start=(kc == 0), stop=(kc == 7)),
                                 reads=[kb1, kH], writes=[('ps', bu)])
                        sgt = sg[nsg % 2]
                        ksg = ('sg', nsg % 2)
                        nsg += 1
                        P.op('act', lambda e_: e_.activation(out=sgt[:, :n], in_=psg[:, :n], func=AF.Silu),
                             reads=[('ps', bg)], writes=[ksg])
                        P.op('dve', lambda e_: e_.tensor_tensor(out=act[:, fch, :n], in0=sgt[:, :n], in1=psu[:, :n], op=ALU.mult),
                             reads=[ksg, ('ps', bu)], writes=[('act', fch)])
                    a_gu += 1
                ensure_gu(a_gu)
                for q in range(4):
                    ensure_wd(a_wd + 1)
                    buf = wd[a_wd % 2]
                    kb = ('wd', a_wd % 2)
                    for dj in range(2):
                        dc = q * 2 + dj
                        b, ps = psum(k)
                        for fc in range(28):
                            P.op('pe', lambda e_: e_.matmul(ps[:, :n], lhsT=buf[:, fc, dj * 128:(dj + 1) * 128], rhs=act[:, fc, :n],
                                                            start=(fc == 0), stop=(fc == 27)),
                                 reads=[kb, ('act', fc)], writes=[('ps', b)])
                        gate = k.mods[:, i, 5 * 8 + dc, s:s + 1]
                        if not moe:
                            P.op('dve', lambda e_: e_.scalar_tensor_tensor(out=xt[:, dc, :n], in0=ps[:, :n], scalar=gate,
                                                                           in1=xt[:, dc, :n], op0=ALU.mult, op1=ALU.add),
                                 reads=[('ps', b), kxt, 'mods'], writes=[kxt])
                        else:
                            if e == 0:
                                P.op('dve', lambda e_: e_.tensor_tensor(out=yacc[:, dc, :n], in0=ps[:, :n], in1=cm[:, :n], op=ALU.mult),
                                     reads=[('ps', b), kcm], writes=[('yacc', dc)])
                            else:
                                tmp = tmps[dc % 2]
                                kt = ('ntmp', dc % 2)
                                P.op('dve', lambda e_: e_.tensor_tensor(out=tmp[:, :n], in0=ps[:, :n], in1=cm[:, :n], op=ALU.mult),
                                     reads=[('ps', b), kcm], writes=[kt])
                                P.op('dve', lambda e_: e_.tensor_tensor(out=yacc[:, dc, :n], in0=yacc[:, dc, :n], in1=tmp[:, :n], op=ALU.add),
                                     reads=[kt, ('yacc', dc)], writes=[('yacc', dc)])
                            if e == experts[-1]:
                                P.op('dve', lambda e_: e_.scalar_tensor_tensor(out=xt[:, dc, :n], in0=yacc[:, dc, :n], scalar=gate,
                                                                               in1=xt[:, dc, :n], op0=ALU.mult, op1=ALU.add),
                                     reads=[('yacc', dc), kxt, 'mods'], writes=[kxt])
                    a_wd += 1
            P.op('sp', lambda e: e.dma_start(out=k.X.rearrange("(kc q) t -> q kc t", q=128)[:, :, c0:c0 + n], in_=xt[:, :, :n]),
                 reads=[kxt], writes=[('Xt', c0)], dma=True)
    P.barrier()


def route_tile(k, f, n, hf, combT, rsm):
    P = k.P
    for bi in range(n // 128):
        b, ps = psum(k)
        for kc in range(8):
            P.op('pe', lambda e: e.matmul(ps[:, 0:8], lhsT=hf[:, kc, bi * 128:(bi + 1) * 128], rhs=k.rt[:, f, kc, :],
                                          start=(kc == 0), stop=(kc == 7)),
                 reads=['hf', 'rt'], writes=[('ps', b)])
        lg, eq1, lg2, eq2, comb = (rsm[:, 8 * j:8 * j + 8] for j in range(5))
        m1, m2, dd, w1, w2 = (rsm[:, 40 + j:41 + j] for j in range(5))
        R = ['rsm']
        P.op('dve', lambda e: e.tensor_copy(out=lg, in_=ps[:, 0:8]), reads=[('ps', b)], writes=R)
        P.op('dve', lambda e: e.tensor_reduce(out=m1, in_=lg, axis=AX.X, op=ALU.max), reads=R, writes=R)
        P.op('dve', lambda e: e.tensor_scalar(out=eq1, in0=lg, scalar1=m1, scalar2=None, op0=ALU.is_equal), reads=R, writes=R)
        P.op('dve', lambda e: e.scalar_tensor_tensor(out=lg2, in0=eq1, scalar=-1e30, in1=lg, op0=ALU.mult, op1=ALU.add), reads=R, writes=R)
        P.op('dve', lambda e: e.tensor_reduce(out=m2, in_=lg2, axis=AX.X, op=ALU.max), reads=R, writes=R)
        P.op('dve', lambda e: e.tensor_scalar(out=eq2, in0=lg2, scalar1=m2, scalar2=None, op0=ALU.is_equal), reads=R, writes=R)
        P.op('dve', lambda e: e.tensor_tensor(out=dd, in0=m2, in1=m1, op=ALU.subtract), reads=R, writes=R)
        P.op('act', lambda e: e.activation(out=w1, in_=dd, func=AF.Sigmoid, scale=-1.0), reads=R, writes=R)
        P.op('act', lambda e: e.activation(out=w2, in_=dd, func=AF.Sigmoid, scale=1.0), reads=R, writes=R)
        P.op('dve', lambda e: e.tensor_scalar(out=comb, in0=eq1, scalar1=w1, scalar2=None, op0=ALU.mult), reads=R, writes=R)
        P.op('dve', lambda e: e.scalar_tensor_tensor(out=comb, in0=eq2, scalar=w2, in1=comb, op0=ALU.mult, op1=ALU.add), reads=R, writes=R)
        b2, ps2 = psum(k)
        P.op('pe', lambda e: e.transpose(out=ps2[0:8, 0:128], in_=comb, identity=k.ident_f[:, :]),
             reads=R + ['ident'], writes=[('ps', b2)])
        P.op('act', lambda e: e.activation(out=combT[0:8, bi * 128:(bi + 1) * 128], in_=ps2[0:8, 0:128], func=AF.Copy),
             reads=[('ps', b2)], writes=['combT'])


def _consts():
    ones = np.ones((128, 128), np.float32)
    ident = np.eye(128, dtype=np.float32)
    esel = np.zeros((8, 8, 128), np.float32)
    for e in range(8):
        esel[e, e, :] = 1.0
    return ones, ident, esel


def _ret_consts():
    f32 = np.float32
    theta = (f32(10000.0) ** (-np.arange(128, dtype=f32) / f32(128))).astype(f32)
    ang = (np.arange(SEQ, dtype=f32)[:, None] * theta[None, :]).astype(f32)
    cos, sin = np.cos(ang).astype(f32), np.sin(ang).astype(f32)
    p = np.arange(128, dtype=f32)[:, None]
    fr = np.arange(128, dtype=f32)[None, :]
    tab = np.zeros((128, 771), f32)
    tab[:, 0:128] = np.maximum(fr - p, 0)
    tab[:, 128:256] = np.maximum(p - fr, 0)
    tab[:, 256:384] = (fr >= p)
    tab[:, 384:512] = (p >= fr)
    tab[:, 512:640] = fr + 1
    tab[:, 640:768] = 128 - fr
    tab[:, 768] = 127 - p[:, 0]
    tab[:, 769] = p[:, 0]
    tab[:, 770] = 128
    return (np.ascontiguousarray(cos.T), np.ascontiguousarray(sin.T), cos, sin, tab)


def _swa_consts(w_in):
    f32 = np.float32
    qk = w_in[:, :1280].reshape(D, 20, 2, 32)
    sw = qk[:, :, ::-1, :].reshape(D, 1280)
    w_ext = np.ascontiguousarray(np.concatenate([w_in, sw], axis=1))
    rows = SEQ // 64
    row = np.repeat(np.arange(rows, dtype=f32), 64)
    colp = np.tile(np.arange(64, dtype=f32), rows)
    freq = (f32(10000.0) ** (-np.arange(16, dtype=f32) / f32(16))).astype(f32)
    ang = np.concatenate([row[:, None] * freq, colp[:, None] * freq], axis=-1).astype(f32)
    cos, sin = np.cos(ang).astype(f32), np.sin(ang).astype(f32)
    cos_full = np.concatenate([cos, cos], axis=1).T
    sin_signed = np.concatenate([-sin, sin], axis=1).T
    return w_ext, np.ascontiguousarray(np.tile(cos_full, (2, 1))), np.ascontiguousarray(np.tile(sin_signed, (2, 1)))


def make_in_maps(inp, cores):
    ones, ident, esel = _consts()
    adab = np.ascontiguousarray(inp['ada_b'].reshape(4, 48, 128).transpose(2, 0, 1))
    ng = np.ascontiguousarray(inp['norm_g'].reshape(4, 2, 8, 128).transpose(3, 0, 1, 2))
    fg = col(inp['final_g'], 8)
    router = np.ascontiguousarray(inp['moe_router'].reshape(2, 8, 128, 8).transpose(2, 0, 1, 3))
    lru_wg = np.ascontiguousarray(inp['lru_w_gate'].transpose(0, 4, 1, 2, 3, 5).reshape(2, 128, 40, 128))
    lru_cw = np.ascontiguousarray(inp['lru_conv_w'].reshape(2, 4, 10, 128).transpose(3, 0, 1, 2))
    lru_cb = np.ascontiguousarray(inp['lru_conv_b'].reshape(2, 10, 128).transpose(2, 0, 1))
    lru_bg = np.ascontiguousarray(inp['lru_b_gate'].reshape(2, 2, 2, 10, 128).transpose(4, 0, 1, 2, 3))
    lru_lam = np.ascontiguousarray(inp['lru_lambda'].reshape(2, 2, 10, 128).transpose(3, 0, 1, 2))
    ret_ld = np.ascontiguousarray(np.broadcast_to(inp['ret_log_decay'].reshape(1, 8), (128, 8))).astype(np.float32)
    ret_gain = col(inp['ret_gn_gain'][0], 16)
    ret_cosT, ret_sinT, ret_costok, ret_sintok, ret_tab = _ret_consts()
    swa_w_ext, swa_cos, swa_sin = _swa_consts(inp['swa_w_in'][0])
    swa_sink = np.ascontiguousarray(np.broadcast_to(inp['swa_sink'].reshape(1, 16), (64, 16))).astype(np.float32)
    adab_row = np.ascontiguousarray(np.broadcast_to(inp['ada_b'][:, None, :], (4, 2, 6144))).astype(np.float32)
    moe_tab = np.zeros((128, 161), np.float32)
    pp_ = np.arange(128, dtype=np.float32)
    moe_tab[:, 0:128] = (pp_[:, None] < pp_[None, :])
    moe_tab[:, 128:152] = np.arange(24, dtype=np.float32)[None, :]
    moe_tab[:, 152:159] = np.arange(7, dtype=np.float32)[None, :] * 128 + pp_[:, None]
    moe_tab[:, 159:161] = np.arange(2, dtype=np.float32)[None, :] * 128 + pp_[:, None]
    maps = []
    for b in cores:
        xT = np.ascontiguousarray(np.concatenate([inp['ctx'][b], inp['x'][b]], axis=0).T)
        cc = np.ascontiguousarray(np.stack([inp['c'][b], inp['c_ctx']], -1).reshape(8, 128, 2).transpose(1, 0, 2))
        maps.append(dict(adab_row=adab_row, moe_tab=moe_tab, swa_w_ext=swa_w_ext, swa_w_out=inp['swa_w_out'], swa_sink=swa_sink, swa_cos=swa_cos, swa_sin=swa_sin,
                         ret_w_in=inp['ret_w_in'], ret_w_out=inp['ret_w_out'], ret_ld=ret_ld, ret_gain=ret_gain, ret_cosT=ret_cosT,
                         ret_sinT=ret_sinT, ret_costok=ret_costok, ret_sintok=ret_sintok, ret_tab=ret_tab,
                         lru_w_in=inp['lru_w_in'], lru_wg=lru_wg, lru_w_out=inp['lru_w_out'], lru_cw=lru_cw, lru_cb=lru_cb,
                         lru_bg=lru_bg, lru_lam=lru_lam, xT=xT, cc=cc, ada_w=inp['ada_w'], adab=adab, ng=ng, fg=fg, ones_f=ones, ident_f=ident,
                         esel=esel, ffn_w_gu=inp['ffn_w_gu'], ffn_w_down=inp['ffn_w_down'], router=router,
                         moe_w_gu=inp['moe_w_gu'], moe_w_down=inp['moe_w_down']))
    return maps


def xrow(k):
    return k.X.rearrange("(kc q) t -> q kc t", q=128)


def load_x_tile(k, xt, c0, n, key='xt'):
    k.P.op('sp', lambda e: e.dma_start(out=xt[:, :, :n], in_=xrow(k)[:, :, c0:c0 + n]),
           reads=['X', ('Xt', c0)], writes=[key], dma=True)


def store_x_tile(k, xt, c0, n):
    k.P.op('sp', lambda e: e.dma_start(out=xrow(k)[:, :, c0:c0 + n], in_=xt[:, :, :n]),
           reads=['xt'], writes=[('Xt', c0)], dma=True)


def out_proj_residual(k, i, tiles, Zd, nz, kz, wo, zt_shape_k, lhs_fn):
    nc, P = k.nc, k.P
    with ExitStack() as st:
        xt = sb(st, nc, "xt_o", [128, 8, 512], F32)
        zt = [sb(st, nc, f"zt_o{j}", [zt_shape_k, nz, 512], BF16) for j in range(2)]
        for ti, (c0, n, s) in enumerate(tiles):
            z = zt[ti % 2]
            kzt = ('zt', ti % 2)
            P.op('sp', lambda e: e.dma_start(out=z[:, :, :n], in_=Zd[:, :, c0:c0 + n]), reads=[kz], writes=[kzt], dma=True)
            load_x_tile(k, xt, c0, n)
            for dc in range(8):
                b, ps = psum(k)
                for zc in range(nz):
                    P.op('pe', lambda e: e.matmul(ps[:, :n], lhsT=lhs_fn(zc, dc), rhs=z[:, zc, :n], start=(zc == 0), stop=(zc == nz - 1)),
                         reads=[kzt, 'wo'], writes=[('ps', b)])
                gate = k.mods[:, i, 2 * 8 + dc, s:s + 1]
                P.op('dve', lambda e: e.scalar_tensor_tensor(out=xt[:, dc, :n], in0=ps[:, :n], scalar=gate, in1=xt[:, dc, :n],
                                                             op0=ALU.mult, op1=ALU.add),
                     reads=[('ps', b), 'xt', 'mods'], writes=['xt'])
            store_x_tile(k, xt, c0, n)
    P.barrier()


def stage_lru(k, i):
    nc, P = k.nc, k.P
    j = i // 3
    last = (i == 3)
    XR, YG, ZG = k.XR, k.YG, k.ZG
    with ExitStack() as st:
        win = sb(st, nc, "lru_win", [128, 8, 2560], BF16)
        for h2 in range(2):
            P.op('pool', lambda e: e.dma_start(out=win[:, :, h2 * 1280:(h2 + 1) * 1280],
                                               in_=k.lru_w_in[j].rearrange("(kc q) c -> q kc c", q=128)[:, :, h2 * 1280:(h2 + 1) * 1280]),
                 writes=['win'], dma=True)
        drain_precast(k, 3)
        Hs = [sb(st, nc, f"H1{q}", [128, 8, 512], BF16) for q in range(2)]
        xts = [sb(st, nc, f"xt1{q}", [128, 8, 512], F32) for q in range(2)]
        sq = sb(st, nc, "sq1", [128, 8, 512], F32)
        rs = sb(st, nc, "rs1", [128, 512], F32)
        tmps = [sb(st, nc, f"nt1{q}", [128, 512], F32) for q in range(2)]
        xrt = sb(st, nc, "xrt", [128, 10, 512], F32)
        ygt = sb(st, nc, "ygt", [128, 10, 512], BF16)
        g1 = [sb(st, nc, f"g1{q}", [128, 512], F32) for q in range(2)]
        g2 = [sb(st, nc, f"g2{q}", [128, 512], F32) for q in range(2)]
        ng = 0
        def prep(ti):
            c0, n, s = TILES[ti]
            q = ti % 2
            load_x_tile(k, xts[q], c0, n, key=('xt', q))
            norm_tile(k, xts[q], ('xt', q), n, i, 0, s, Hs[q], ('H', q), sq, rs, tmps)
        prep(0)
        for ti, (c0, n, s) in enumerate(TILES):
            H, kH = Hs[ti % 2], ('H', ti % 2)
            if ti + 1 < len(TILES):
                prep(ti + 1)
            for oc in range(20):
                b, ps = psum(k)
                for kc in range(8):
                    P.op('pe', lambda e: e.matmul(ps[:, :n], lhsT=win[:, kc, oc * 128:(oc + 1) * 128], rhs=H[:, kc, :n],
                                                  start=(kc == 0), stop=(kc == 7)),
                         reads=['win', kH], writes=[('ps', b)])
                if oc < 10:
                    P.op('act', lambda e: e.activation(out=ygt[:, oc, :n], in_=ps[:, :n], func=AF.Gelu_apprx_tanh),
                         reads=[('ps', b)], writes=['ygt'])
                else:
                    P.op('act', lambda e: e.activation(out=xrt[:, oc - 10, :n], in_=ps[:, :n], func=AF.Copy),
                         reads=[('ps', b)], writes=['xrt'])
            P.op('sp', lambda e: e.dma_start(out=YG.rearrange("(m q) t -> q m t", q=128)[:, :, c0:c0 + n], in_=ygt[:, :, :n]),
                 reads=['ygt'], writes=['YG'], dma=True)
            P.op('sp', lambda e: e.dma_start(out=XR.rearrange("(m q) t -> q m t", q=128)[:, :, c0:c0 + n], in_=xrt[:, :, :n]),
                 reads=['xrt'], writes=['XR'], dma=True)
    P.barrier()
    with ExitStack() as st:
        wg = sb(st, nc, "lru_wg", [128, 40, 128], BF16)
        for q in range(4):
            P.op('pool', lambda e: e.dma_start(out=wg[:, q * 10:(q + 1) * 10, :], in_=k.lru_wg[j][:, q * 10:(q + 1) * 10, :]),
                 writes=['wg'], dma=True)
        drain_precast(k, 1000)
        if i == 0 and k.mods_split:
            stage_mods(k, layers=(1, 2, 3), final_barrier=False, outer=st, cbw=256)
        B1 = sb(st, nc, "B1", [128, T], F32)
        B2 = sb(st, nc, "B2", [128, T], F32)
        B3 = sb(st, nc, "B3", [128, T], F32)
        B4 = sb(st, nc, "B4", [128, T], F32)
        B5 = sb(st, nc, "B5", [128, T], F32)
        B6 = sb(st, nc, "B6", [128, T], F32)
        B7 = sb(st, nc, "B7", [128, T], F32)
        ub = sb(st, nc, "ub", [128, T], BF16)
        ygc = sb(st, nc, "ygc", [128, T], BF16)
        zc_ = sb(st, nc, "zc", [128, T], BF16)
        sp8 = sb(st, nc, "sp8", [128, 2, 10], F32)
        P.op('act', lambda e: e.activation(out=sp8, in_=k.lru_lam[:, j], func=AF.Exp, scale=-1.0), reads=['lrup'], writes=['sp8'])
        P.op('act', lambda e: e.activation(out=sp8, in_=sp8, func=AF.Ln, bias=k.oneb[:, 0:1], scale=1.0), reads=['sp8'], writes=['sp8'])
        P.op('dve', lambda e: e.tensor_scalar(out=sp8, in0=sp8, scalar1=-8.0, scalar2=None, op0=ALU.mult), reads=['sp8'], writes=['sp8'])
        segs = [(0, NCTX), (NCTX, T)]
        allk = lambda nm: [(nm, ti) for ti in range(len(TILES))]
        PCS = [(0, 1280, (0, 1, 2)), (1280, 2816, (3, 4, 5)), (2816, T, (6, 7, 8))]
        pk = lambda nm, tl: [(nm, ti) for ti in tl]
        for m in range(10):
            P.op('sp', lambda e: e.dma_start(out=B1, in_=XR[m * 128:(m + 1) * 128, :]), reads=['XR'], writes=allk('B1'), dma=True)
            P.op('sp', lambda e: e.dma_start(out=ygc, in_=YG[m * 128:(m + 1) * 128, :]), reads=['YG'], writes=['ygc'], dma=True)
            cw = lambda t: k.lru_cw[:, j, t, m:m + 1]
            for (p0, p1, tl) in PCS:
                P.op('dve', lambda e: e.tensor_scalar(out=B2[:, p0:p1], in0=B1[:, p0:p1], scalar1=cw(2), scalar2=k.lru_cb[:, j, m:m + 1],
                                                      op0=ALU.mult, op1=ALU.add),
                     reads=allk('B1') + ['lrup'], writes=pk('B2', tl))
                for o in (-2, -1, 1):
                    for (s0, s1) in segs:
                        a0 = max(s0 + max(0, -o), p0)
                        a1 = min(s1 - max(0, o), p1)
                        if a1 <= a0:
                            continue
                        P.op('dve', lambda e: e.scalar_tensor_tensor(out=B2[:, a0:a1], in0=B1[:, a0 + o:a1 + o], scalar=cw(o + 2),
                                                                     in1=B2[:, a0:a1], op0=ALU.mult, op1=ALU.add),
                             reads=allk('B1') + pk('B2', tl) + ['lrup'], writes=pk('B2', tl))
                P.op('act', lambda e: e.activation(out=ub[:, p0:p1], in_=B2[:, p0:p1], func=AF.Copy), reads=pk('B2', tl), writes=pk('ub', tl))
            for d in range(2):
                Ba, nA = (B3, 'B3') if d == 0 else (B7, 'B7')
                Bi, nI = (B1, 'B1') if d == 0 else (B6, 'B6')
                for ti, (c0, n, s) in enumerate(TILES):
                    b, ps = psum(k)
                    P.op('pe', lambda e: e.matmul(ps[:, :n], lhsT=wg[:, (d * 2 + 0) * 10 + m, :], rhs=ub[:, c0:c0 + n], start=True, stop=True),
                         reads=['wg', ('ub', ti)], writes=[('ps', b)])
                    P.op('act', lambda e: e.activation(out=Ba[:, c0:c0 + n], in_=ps[:, :n], func=AF.Sigmoid,
                                                       bias=k.lru_bg[:, j, d, 0, m:m + 1], scale=1.0),
                         reads=[('ps', b), 'lrup'], writes=[(nA, ti)])
                    b2, ps2 = psum(k)
                    P.op('pe', lambda e: e.matmul(ps2[:, :n], lhsT=wg[:, (d * 2 + 1) * 10 + m, :], rhs=ub[:, c0:c0 + n], start=True, stop=True),
                         reads=['wg', ('ub', ti)], writes=[('ps', b2)])
                    P.op('act', lambda e: e.activation(out=Bi[:, c0:c0 + n], in_=ps2[:, :n], func=AF.Sigmoid,
                                                       bias=k.lru_bg[:, j, d, 1, m:m + 1], scale=1.0),
                         reads=[('ps', b2), 'lrup'], writes=[(nI, ti)])
                for (p0, p1, tl) in PCS:
                    P.op('act', lambda e: e.activation(out=Ba[:, p0:p1], in_=Ba[:, p0:p1], func=AF.Exp, scale=sp8[:, d, m:m + 1]),
                         reads=pk(nA, tl) + ['sp8'], writes=pk(nA, tl))
                for (p0, p1, tl) in PCS:
                    P.op('act', lambda e: e.activation(out=B4[:, p0:p1], in_=Ba[:, p0:p1], func=AF.Square), reads=pk(nA, tl), writes=pk('B4', tl))
                for (p0, p1, tl) in PCS:
                    P.op('act', lambda e: e.activation(out=B4[:, p0:p1], in_=B4[:, p0:p1], func=AF.Sqrt, scale=-1.0, bias=k.oneb[:, 0:1]),
                         reads=pk('B4', tl), writes=pk('B4', tl))
                    P.op('dve', lambda e: e.tensor_tensor(out=Bi[:, p0:p1], in0=Bi[:, p0:p1], in1=B2[:, p0:p1], op=ALU.mult),
                         reads=pk(nI, tl) + pk('B2', tl), writes=pk(nI, tl))
                    P.op('dve', lambda e: e.tensor_tensor(out=Bi[:, p0:p1], in0=Bi[:, p0:p1], in1=B4[:, p0:p1], op=ALU.mult),
                         reads=pk(nI, tl) + pk('B4', tl), writes=pk(nI, tl))
                if d == 0:
                    P.op('dve', lambda e: e.tensor_tensor_scan(out=B5, data0=B3, data1=B1, initial=0.0, op0=ALU.mult, op1=ALU.add),
                         reads=allk('B3') + allk('B1'), writes=allk('B5'))
                else:
                    rv = lambda buf, a, b_: buf[:, a:b_][:, ::-1]
                    P.op('dve', lambda e: e.tensor_tensor_scan(out=rv(B4, 0, NCTX), data0=rv(B7, 0, NCTX), data1=rv(B6, 0, NCTX),
                                                               initial=0.0, op0=ALU.mult, op1=ALU.add),
                         reads=allk('B7') + allk('B6'), writes=allk('B4'))
                    P.op('dve', lambda e: e.tensor_tensor_scan(out=rv(B4, NCTX, T), data0=rv(B7, NCTX, T), data1=rv(B6, NCTX, T),
                                                               initial=B4[:, 0:1], op0=ALU.mult, op1=ALU.add),
                         reads=allk('B7') + allk('B6') + allk('B4'), writes=allk('B4'))
            for (p0, p1, tl) in PCS:
                P.op('dve', lambda e: e.tensor_tensor(out=B5[:, p0:p1], in0=B5[:, p0:p1], in1=B4[:, p0:p1], op=ALU.add),
                     reads=pk('B5', tl) + pk('B4', tl), writes=pk('B5', tl))
                P.op('dve', lambda e: e.tensor_tensor(out=zc_[:, p0:p1], in0=B5[:, p0:p1], in1=ygc[:, p0:p1], op=ALU.mult),
                     reads=pk('B5', tl) + ['ygc'], writes=pk('zc', tl))
            P.op('sp', lambda e: e.dma_start(out=ZG[m * 128:(m + 1) * 128, :], in_=zc_), reads=allk('zc'), writes=['ZG'], dma=True)
    P.barrier()
    with ExitStack() as st:
        wo = sb(st, nc, "lru_wo", [128, 10, 1024], BF16)
        P.op('pool', lambda e: e.dma_start(out=wo, in_=k.lru_w_out[j].rearrange("(m q) c -> q m c", q=128)), writes=['wo'], dma=True)
        out_proj_residual(k, i, TILES[1:] if last else TILES, ZG.rearrange("(m q) t -> q m t", q=128), 10, 'ZG', wo, 128,
                          lambda zc, dc: wo[:, zc, dc * 128:(dc + 1) * 128])


NCH = T // 128


def stage_ret(k, i):
    nc, P = k.nc, k.P
    QT, KT, KTOK, VTOK, GS, SBd, ZR = k.QT, k.KT, k.KTOK, k.VTOK, k.GS, k.SBd, k.ZR
    tab = k.ret_tab
    with ExitStack() as st:
        win = sb(st, nc, "ret_win", [128, 8, 6144], BF16)
        for q3 in range(3):
            P.op('pool', lambda e: e.dma_start(out=win[:, :, q3 * 2048:(q3 + 1) * 2048],
                                               in_=k.ret_w_in[0].rearrange("(kc q) c -> q kc c", q=128)[:, :, q3 * 2048:(q3 + 1) * 2048]),
                 writes=['win'], dma=True)
        drain_precast(k, 4)
        H = sb(st, nc, "H2", [128, 8, 512], BF16)
        xt = sb(st, nc, "xt2", [128, 8, 512], F32)
        sq = sb(st, nc, "sq2", [128, 8, 512], F32)
        rs = sb(st, nc, "rs2", [128, 512], F32)
        tmps = [sb(st, nc, f"nt2{q}", [128, 512], F32) for q in range(2)]
        qo = sb(st, nc, "qo", [128, 8, 512], BF16)
        cs = sb(st, nc, "cs", [128, 2, 512], F32)
        kt = sb(st, nc, "kt", [128, 1024], F32)
        kto = sb(st, nc, "kto", [128, 1024], BF16)
        vto = sb(st, nc, "vto", [128, 2048], BF16)
        cst = sb(st, nc, "cst", [128, 2, 128], F32)
        gso = sb(st, nc, "gso", [128, 4, 512], BF16)
        for ti, (c0, n, s) in enumerate(TILES):
            load_x_tile(k, xt, c0, n)
            norm_tile(k, xt, 'xt', n, i, 0, s, H, 'H', sq, rs, tmps)
            if s == 0:
                p0 = c0 - NCTX
                P.op('sp', lambda e: e.dma_start(out=cs[:, 0, :n], in_=k.ret_cosT[:, p0:p0 + n]), writes=['cs0'], dma=True)
                P.op('sp', lambda e: e.dma_start(out=cs[:, 1, :n], in_=k.ret_sinT[:, p0:p0 + n]), writes=['cs1'], dma=True)
            for (dst, kd_, off, scale) in ((QT, 'QT', 0, 1.0), (KT, 'KT', 1024, 0.0625)):
                for oc in range(8):
                    b, ps = psum(k)
                    for kc in range(8):
                        P.op('pe', lambda e: e.matmul(ps[:, :n], lhsT=win[:, kc, off + oc * 128:off + (oc + 1) * 128], rhs=H[:, kc, :n],
                                                      start=(kc == 0), stop=(kc == 7)),
                             reads=['win', 'H'], writes=[('ps', b)])
                    P.op('act', lambda e: e.activation(out=xt[:, oc, :n], in_=ps[:, :n], func=AF.Copy, scale=scale),
                         reads=[('ps', b)], writes=['xt'])
                if s == 0:
                    xv = xt.rearrange("p (h two) n -> p h two n", two=2)
                    qv = qo.rearrange("p (h two) n -> p h two n", two=2)
                    x1, x2 = xv[:, :, 0, :n], xv[:, :, 1, :n]
                    o1, o2 = qv[:, :, 0, :n], qv[:, :, 1, :n]
                    cosb = cs[:, 0, :n].unsqueeze(1).to_broadcast([128, 4, n])
                    sinb = cs[:, 1, :n].unsqueeze(1).to_broadcast([128, 4, n])
                    sv = sq.rearrange("p (two h) n -> p two h n", two=2)
                    ta, tb = sv[:, 0, :, :n], sv[:, 1, :, :n]
                    P.op('dve', lambda e: e.tensor_tensor(out=ta, in0=x1, in1=cosb, op=ALU.mult), reads=['xt', 'cs0', 'sq'], writes=['sq'])
                    P.op('dve', lambda e: e.tensor_tensor(out=tb, in0=x2, in1=sinb, op=ALU.mult), reads=['xt', 'cs1', 'sq'], writes=['sq'])
                    P.op('dve', lambda e: e.tensor_tensor(out=o1, in0=ta, in1=tb, op=ALU.subtract), reads=['sq'], writes=['qo'])
                    P.op('dve', lambda e: e.tensor_tensor(out=ta, in0=x2, in1=cosb, op=ALU.mult), reads=['xt', 'cs0', 'qo', 'sq'], writes=['sq'])
                    P.op('dve', lambda e: e.tensor_tensor(out=tb, in0=x1, in1=sinb, op=ALU.mult), reads=['xt', 'cs1', 'qo', 'sq'], writes=['sq'])
                    P.op('dve', lambda e: e.tensor_tensor(out=o2, in0=ta, in1=tb, op=ALU.add), reads=['sq'], writes=['qo'])
                else:
                    P.op('dve', lambda e: e.tensor_copy(out=qo[:, :, :n], in_=xt[:, :, :n]), reads=['xt'], writes=['qo'])
                P.op('sp', lambda e: e.dma_start(out=dst.rearrange("(oc q) t -> q oc t", q=128)[:, :, c0:c0 + n], in_=qo[:, :, :n]),
                     reads=['qo'], writes=[kd_], dma=True)
            for tb_ in range(n // 128):
                r0 = c0 + tb_ * 128
                if s == 0:
                    pp = r0 - NCTX
                    P.op('sp', lambda e: e.dma_start(out=cst[:, 0, :], in_=k.ret_costok[pp:pp + 128, :]), writes=['cst0'], dma=True)
                    P.op('sp', lambda e: e.dma_start(out=cst[:, 1, :], in_=k.ret_sintok[pp:pp + 128, :]), writes=['cst1'], dma=True)
                for half in range(2):
                    b, ps = psum(k)
                    for kc in range(8):
                        P.op('pe', lambda e: e.matmul(ps[:, :], lhsT=H[:, kc, tb_ * 128:(tb_ + 1) * 128],
                                                      rhs=win[:, kc, 1024 + half * 512:1024 + (half + 1) * 512], start=(kc == 0), stop=(kc == 7)),
                             reads=['win', 'H'], writes=[('ps', b)])
                    P.op('act', lambda e: e.activation(out=kt[:, half * 512:(half + 1) * 512], in_=ps[:, :], func=AF.Copy, scale=0.0625),
                         reads=[('ps', b)], writes=['kt'])
                if s == 0:
                    kv = kt.rearrange("p (h two f) -> p h two f", two=2, f=128)
                    ov = kto.rearrange("p (h two f) -> p h two f", two=2, f=128)
                    k1, k2 = kv[:, :, 0, :], kv[:, :, 1, :]
                    o1, o2 = ov[:, :, 0, :], ov[:, :, 1, :]
                    cosb = cst[:, 0, :].unsqueeze(1).to_broadcast([128, 4, 128])
                    sinb = cst[:, 1, :].unsqueeze(1).to_broadcast([128, 4, 128])
                    ta = tmps[0].rearrange("p (h f) -> p h f", f=128)
                    tb = tmps[1].rearrange("p (h f) -> p h f", f=128)
                    ka, kb = ('ntmp', 0), ('ntmp', 1)
                    P.op('dve', lambda e: e.tensor_tensor(out=ta, in0=k1, in1=cosb, op=ALU.mult), reads=['kt', 'cst0'], writes=[ka])
                    P.op('dve', lambda e: e.tensor_tensor(out=tb, in0=k2, in1=sinb, op=ALU.mult), reads=['kt', 'cst1'], writes=[kb])
                    P.op('dve', lambda e: e.tensor_tensor(out=o1, in0=ta, in1=tb, op=ALU.subtract), reads=[ka, kb], writes=['kto'])
                    P.op('dve', lambda e: e.tensor_tensor(out=ta, in0=k2, in1=cosb, op=ALU.mult), reads=['kt', 'cst0', 'kto'], writes=[ka])
                    P.op('dve', lambda e: e.tensor_tensor(out=tb, in0=k1, in1=sinb, op=ALU.mult), reads=['kt', 'cst1', 'kto'], writes=[kb])
                    P.op('dve', lambda e: e.tensor_tensor(out=o2, in0=ta, in1=tb, op=ALU.add), reads=[ka, kb], writes=['kto'])
                else:
                    P.op('dve', lambda e: e.tensor_copy(out=kto, in_=kt), reads=['kt'], writes=['kto'])
                P.op('sp', lambda e: e.dma_start(out=KTOK[r0:r0 + 128, :], in_=kto), reads=['kto'], writes=['KTOK'], dma=True)
                for q4 in range(4):
                    b, ps = psum(k)
                    for kc in range(8):
                        P.op('pe', lambda e: e.matmul(ps[:, :], lhsT=H[:, kc, tb_ * 128:(tb_ + 1) * 128],
                                                      rhs=win[:, kc, 2048 + q4 * 512:2048 + (q4 + 1) * 512], start=(kc == 0), stop=(kc == 7)),
                             reads=['win', 'H'], writes=[('ps', b)])
                    eng = 'act' if q4 % 2 == 0 else 'dve'
                    if eng == 'act':
                        P.op('act', lambda e: e.activation(out=vto[:, q4 * 512:(q4 + 1) * 512], in_=ps[:, :], func=AF.Copy),
                             reads=[('ps', b)], writes=['vto'])
                    else:
                        P.op('dve', lambda e: e.tensor_copy(out=vto[:, q4 * 512:(q4 + 1) * 512], in_=ps[:, :]),
                             reads=[('ps', b)], writes=['vto'])
                P.op('sp', lambda e: e.dma_start(out=VTOK[r0:r0 + 128, :], in_=vto), reads=['vto'], writes=['VTOK'], dma=True)
            for g4 in range(4):
                for gj in range(4):
                    oc = g4 * 4 + gj
                    b, ps = psum(k)
                    for kc in range(8):
                        P.op('pe', lambda e: e.matmul(ps[:, :n], lhsT=win[:, kc, 4096 + oc * 128:4096 + (oc + 1) * 128], rhs=H[:, kc, :n],
                                                      start=(kc == 0), stop=(kc == 7)),
                             reads=['win', 'H'], writes=[('ps', b)])
                    gt = tmps[gj % 2]
                    kg = ('ntmp', gj % 2)
                    P.op('act', lambda e: e.activation(out=gt[:, :n], in_=ps[:, :n], func=AF.Silu), reads=[('ps', b)], writes=[kg])
                    P.op('dve', lambda e: e.tensor_scalar(out=gso[:, gj, :n], in0=gt[:, :n], scalar1=k.ret_gain[:, oc:oc + 1], scalar2=None,
                                                          op0=ALU.mult), reads=[kg, 'retp'], writes=['gso'])
                P.op('sp', lambda e: e.dma_start(out=GS.rearrange("(oc q) t -> q oc t", q=128)[:, g4 * 4:(g4 + 1) * 4, c0:c0 + n],
                                                 in_=gso[:, :, :n]), reads=['gso'], writes=['GS'], dma=True)
    P.barrier()
    with ExitStack() as st:
        drain_precast(k, 1000)
        qT = sb(st, nc, "r_qT", [128, 2, T], BF16)
        kT = sb(st, nc, "r_kT", [128, 2, T], BF16)
        ktk = sb(st, nc, "r_ktk", [128, NCH, 256], BF16)
        vtk = sb(st, nc, "r_vtk", [128, NCH, 512], BF16)
        tb_s = sb(st, nc, "r_tab", [128, 6 * 128 + 3], F32)
        P.op('sp', lambda e: e.dma_start(out=tb_s, in_=tab), writes=['rtab'], dma=True)
        DP, DM, UP, LO, POS1, POSB = (tb_s[:, q * 128:(q + 1) * 128] for q in range(6))
        KPF, KPB, C128 = (tb_s[:, 768 + q:769 + q] for q in range(3))
        M = sb(st, nc, "r_M", [128, 128], F32)
        M2 = sb(st, nc, "r_M2", [128, 128], F32)
        QD = sb(st, nc, "r_QD", [128, 2, 128], F32)
        cv = sb(st, nc, "r_cv", [128, 4], F32)
        S32 = [sb(st, nc, f"r_S32{d}", [128, 2, 512], F32) for d in range(2)]
        Sbf = [sb(st, nc, f"r_Sbf{q}", [128, 2, 512], BF16) for q in range(2)]
        Sfb = sb(st, nc, "r_Sfb", [128, 2, 512], BF16)
        Sin = [sb(st, nc, f"r_Sin{q}", [128, 2, 512], BF16) for q in range(3)]
        gsc = [sb(st, nc, f"r_gsc{q}", [128, 4, 128], BF16) for q in range(4)]
        kd = [sb(st, nc, f"r_kd{q}", [128, 256], BF16) for q in range(2)]
        sm = [sb(st, nc, f"r_sm{q}", [128, 128], BF16) for q in range(2)]
        qs = [sb(st, nc, f"r_qs{q}", [128, 2, 2, 128], BF16) for q in range(2)]
        sqo2 = [sb(st, nc, f"r_sqo{q}", [128, 512], F32) for q in range(2)]
        rn2 = [sb(st, nc, f"r_rn{q}", [128, 128], F32) for q in range(2)]
        zt2 = [sb(st, nc, f"r_zt{q}", [128, 4, 128], F32) for q in range(2)]
        zo = [sb(st, nc, f"r_zo{q}", [128, 4, 128], BF16) for q in range(2)]
        nkd = 0
        for h in range(4):
            P.op('sp', lambda e: e.dma_start(out=ktk, in_=KTOK.rearrange("(c q) f -> q c f", q=128)[:, :, h * 256:(h + 1) * 256]),
                 reads=['KTOK'], writes=['ktk'], dma=True)
            P.op('sp', lambda e: e.dma_start(out=vtk, in_=VTOK.rearrange("(c q) f -> q c f", q=128)[:, :, h * 512:(h + 1) * 512]),
                 reads=['VTOK'], writes=['vtk'], dma=True)
            P.op('sp', lambda e: e.dma_start(out=qT, in_=QT.rearrange("(oc q) t -> q oc t", q=128)[:, 2 * h:2 * h + 2, :]),
                 reads=['QT'], writes=['qT'], dma=True)
            P.op('sp', lambda e: e.dma_start(out=kT, in_=KT.rearrange("(oc q) t -> q oc t", q=128)[:, 2 * h:2 * h + 2, :]),
                 reads=['KT'], writes=['kT'], dma=True)
            lgf = k.ret_ld[:, h:h + 1]
            lgb = k.ret_ld[:, 4 + h:5 + h]
            P.op('act', lambda e: e.activation(out=M, in_=DP, func=AF.Exp, scale=lgf), reads=['rtab', 'retp'], writes=['M'])
            P.op('dve', lambda e: e.tensor_tensor(out=M, in0=M, in1=UP, op=ALU.mult), reads=['M', 'rtab'], writes=['M'])
            P.op('act', lambda e: e.activation(out=M2, in_=DM, func=AF.Exp, scale=lgb), reads=['rtab', 'retp'], writes=['M2'])
            P.op('dve', lambda e: e.tensor_tensor(out=M2, in0=M2, in1=LO, op=ALU.mult), reads=['M2', 'rtab'], writes=['M2'])
            P.op('dve', lambda e: e.tensor_tensor(out=M, in0=M, in1=M2, op=ALU.add), reads=['M', 'M2'], writes=['M'])
            P.op('act', lambda e: e.activation(out=QD[:, 0, :], in_=POS1, func=AF.Exp, scale=lgf), reads=['rtab', 'retp'], writes=['QD'])
            P.op('act', lambda e: e.activation(out=QD[:, 1, :], in_=POSB, func=AF.Exp, scale=lgb), reads=['rtab', 'retp'], writes=['QD'])
            P.op('act', lambda e: e.activation(out=cv[:, 0:1], in_=KPF, func=AF.Exp, scale=lgf), reads=['rtab', 'retp'], writes=['cv'])
            P.op('act', lambda e: e.activation(out=cv[:, 1:2], in_=KPB, func=AF.Exp, scale=lgb), reads=['rtab', 'retp'], writes=['cv'])
            P.op('act', lambda e: e.activation(out=cv[:, 2:3], in_=C128, func=AF.Exp, scale=lgf), reads=['rtab', 'retp'], writes=['cv'])
            P.op('act', lambda e: e.activation(out=cv[:, 3:4], in_=C128, func=AF.Exp, scale=lgb), reads=['rtab', 'retp'], writes=['cv'])

            def state_update(d, cidx, S, kS, Sb_out, kSb, banks=None):
                nonlocal nkd
                kdt = kd[nkd % 2]
                kkd = ('kd', nkd % 2)
                nkd += 1
                P.op('dve', lambda e: e.tensor_scalar(out=kdt, in0=ktk[:, cidx, :], scalar1=cv[:, d:d + 1], scalar2=None, op0=ALU.mult),
                     reads=['ktk', 'cv'], writes=[kkd])
                for dch in range(2):
                    if banks is None:
                        b, ps = psum(k)
                    else:
                        b, ps = banks[dch], k.PS[banks[dch]]
                    P.op('pe', lambda e: e.matmul(ps[:, :], lhsT=kdt[:, dch * 128:(dch + 1) * 128], rhs=vtk[:, cidx, :], start=True, stop=True),
                         reads=[kkd, 'vtk'], writes=[('ps', b)])
                    P.op('dve', lambda e: e.scalar_tensor_tensor(out=S[:, dch, :], in0=S[:, dch, :], scalar=cv[:, 2 + d:3 + d], in1=ps[:, :],
                                                                 op0=ALU.mult, op1=ALU.add),
                         reads=[kS, ('ps', b), 'cv'], writes=[kS])
                P.op('act', lambda e: e.activation(out=Sb_out, in_=S, func=AF.Copy), reads=[kS], writes=[kSb])

            order_b = [1, 0] + list(range(NCH - 1, 1, -1))
            P.op('dve', lambda e: e.memset(S32[1], 0.0), writes=['S32b'])
            P.op('dve', lambda e: e.memset(Sbf[0], 0.0), writes=[('Sbf', 0)])
            for oi, cidx in enumerate(order_b):
                cur = Sbf[oi % 2]
                kcur = ('Sbf', oi % 2)
                P.op('sp', lambda e: e.dma_start(out=SBd[h, cidx].rearrange("(dch q) e -> q dch e", q=128), in_=cur),
                     reads=[kcur], writes=[('SBd', cidx)], dma=True)
                if oi + 1 < len(order_b):
                    state_update(1, cidx, S32[1], 'S32b', Sbf[(oi + 1) % 2], ('Sbf', (oi + 1) % 2))
            P.op('dve', lambda e: e.memset(S32[0], 0.0), writes=['S32f'])
            P.op('dve', lambda e: e.memset(Sfb, 0.0), writes=['Sfb'])

            def prefetch(cidx):
                b3, b4 = cidx % 3, cidx % 4
                P.op('sp', lambda e: e.dma_start(out=Sin[b3], in_=SBd[h, cidx].rearrange("(dch q) e -> q dch e", q=128)),
                     reads=[('SBd', cidx)], writes=[('Sin', b3)], dma=True)
                P.op('sp', lambda e: e.dma_start(out=gsc[b4], in_=GS.rearrange("(oc q) t -> q oc t", q=128)[:, 4 * h:4 * h + 4, cidx * 128:(cidx + 1) * 128]),
                     reads=['GS'], writes=[('gsc', b4)], dma=True)
            def fA(cidx):
                bq = cidx % 2
                cols = slice(cidx * 128, (cidx + 1) * 128)
                b = bq
                ps_s = k.PS[b]
                for dch in range(2):
                    P.op('pe', lambda e: e.matmul(ps_s[:, 0:128], lhsT=kT[:, dch, cols], rhs=qT[:, dch, cols], start=(dch == 0), stop=(dch == 1)),
                         reads=['kT', 'qT'], writes=[('ps', b)])
                P.op('dve', lambda e: e.tensor_tensor(out=sm[bq], in0=ps_s[:, 0:128], in1=M, op=ALU.mult), reads=[('ps', b), 'M'], writes=[('sm', bq)])
                for d in range(2):
                    P.op('dve', lambda e: e.tensor_tensor(out=qs[bq][:, d], in0=qT[:, :, cols], in1=QD[:, d, :].unsqueeze(1).to_broadcast([128, 2, 128]),
                                                          op=ALU.mult), reads=['qT', 'QD'], writes=[('qs', bq, d)])

            def fB(cidx):
                bq = cidx % 2
                bo = 2 + bq
                ps_o = k.PS[bo]
                smt, qst = sm[bq], qs[bq]
                for ech in range(4):
                    es = slice(ech * 128, (ech + 1) * 128)
                    P.op('pe', lambda e: e.matmul(ps_o[:, es], lhsT=vtk[:, cidx, es], rhs=smt, start=True, stop=False),
                         reads=['vtk', ('sm', bq)], writes=[('ps', bo)])
                    for dch in range(2):
                        P.op('pe', lambda e: e.matmul(ps_o[:, es], lhsT=Sfb[:, dch, es], rhs=qst[:, 0, dch, :], start=False, stop=False),
                             reads=['Sfb', ('qs', bq, 0)], writes=[('ps', bo)])
                    for dch in range(2):
                        P.op('pe', lambda e: e.matmul(ps_o[:, es], lhsT=Sin[cidx % 3][:, dch, es], rhs=qst[:, 1, dch, :], start=False, stop=(dch == 1)),
                             reads=[('Sin', cidx % 3), ('qs', bq, 1)], writes=[('ps', bo)])
                P.op('act', lambda e: e.activation(out=sqo2[bq], in_=ps_o, func=AF.Square), reads=[('ps', bo)], writes=[('sqo', bq)])

            def fC(cidx):
                bq = cidx % 2
                bo = 2 + bq
                ps_o = k.PS[bo]
                cols = slice(cidx * 128, (cidx + 1) * 128)
                bn = 4
                ps_n = k.PS[bn]
                sqo, rn, zt = sqo2[bq], rn2[bq], zt2[bq]
                for ech in range(4):
                    P.op('pe', lambda e: e.matmul(ps_n[:, 0:128], lhsT=k.ones_f, rhs=sqo[:, ech * 128:(ech + 1) * 128], start=(ech == 0), stop=(ech == 3)),
                         reads=[('sqo', bq), 'ones'], writes=[('ps', bn)])
                P.op('act', lambda e: e.activation(out=rn, in_=ps_n[:, 0:128], func=AF.Ln, scale=1.0 / 512, bias=k.epsb[:, 0:1]),
                     reads=[('ps', bn)], writes=[('rn', bq)])
                P.op('act', lambda e: e.activation(out=rn, in_=rn, func=AF.Exp, scale=-0.5), reads=[('rn', bq)], writes=[('rn', bq)])
                P.op('dve', lambda e: e.tensor_tensor(out=zt, in0=ps_o.rearrange("p (c i) -> p c i", i=128),
                                                      in1=rn.unsqueeze(1).to_broadcast([128, 4, 128]), op=ALU.mult),
                     reads=[('ps', bo), ('rn', bq)], writes=[('zt', bq)])
                P.op('dve', lambda e: e.tensor_tensor(out=zo[bq], in0=zt, in1=gsc[cidx % 4], op=ALU.mult), reads=[('zt', bq), ('gsc', cidx % 4)], writes=[('zo', bq)])
                P.op('sp', lambda e: e.dma_start(out=ZR.rearrange("(oc q) t -> q oc t", q=128)[:, 4 * h:4 * h + 4, cols], in_=zo[bq]),
                     reads=[('zo', bq)], writes=['ZR'], dma=True)

            prefetch(0)
            prefetch(1)
            fA(0)
            for cidx in range(NCH):
                if cidx + 2 < NCH:
                    prefetch(cidx + 2)
                if cidx + 1 < NCH:
                    fA(cidx + 1)
                fB(cidx)
                if cidx + 1 < NCH:
                    state_update(0, cidx, S32[0], 'S32f', Sfb, 'Sfb', banks=(5, 6))
                if cidx >= 1:
                    fC(cidx - 1)
            fC(NCH - 1)
    P.barrier()
    with ExitStack() as st:
        wo = sb(st, nc, "ret_wo", [128, 16, 1024], BF16)
        P.op('pool', lambda e: e.dma_start(out=wo, in_=k.ret_w_out[0].rearrange("(m q) c -> q m c", q=128)), writes=['wo'], dma=True)
        out_proj_residual(k, i, TILES, ZR.rearrange("(m q) t -> q m t", q=128), 16, 'ZR', wo, 128,
                          lambda zc, dc: wo[:, zc, dc * 128:(dc + 1) * 128])


def stage_swa(k, i):
    nc, P = k.nc, k.P
    QS, KS, VS, OS = k.QS, k.KS, k.VS, k.OS
    with ExitStack() as st:
        win = sb(st, nc, "swa_win", [128, 8, 2816], BF16)
        for (a, b_) in ((0, 1408), (1408, 2816)):
            P.op('pool', lambda e: e.dma_start(out=win[:, :, a:b_], in_=k.swa_w_ext.rearrange("(kc q) c -> q kc c", q=128)[:, :, a:b_]),
                 writes=['win'], dma=True)
        drain_precast(k, 1000)
        Hs = [sb(st, nc, f"H3{q}", [128, 8, 512], BF16) for q in range(2)]
        xts = [sb(st, nc, f"xt3{q}", [128, 8, 512], F32) for q in range(2)]
        sq = sb(st, nc, "sq3", [128, 8, 512], F32)
        rs = sb(st, nc, "rs3", [128, 512], F32)
        tmps = [sb(st, nc, f"nt3{q}", [128, 512], F32) for q in range(2)]
        qo = sb(st, nc, "s_qo", [128, 10, 512], BF16)
        cs = sb(st, nc, "s_cs", [128, 2, 512], F32)
        vto = [sb(st, nc, f"s_vto{q}", [128, 256], BF16) for q in range(2)]
        nt = 0
        def prep(ti):
            c0, n, s = TILES[ti]
            q = ti % 2
            load_x_tile(k, xts[q], c0, n, key=('xt', q))
            norm_tile(k, xts[q], ('xt', q), n, i, 0, s, Hs[q], ('H', q), sq, rs, tmps)
        prep(0)
        for ti, (c0, n, s) in enumerate(TILES):
            H, kH = Hs[ti % 2], ('H', ti % 2)
            if ti + 1 < len(TILES):
                prep(ti + 1)
            if s == 0:
                p0 = c0 - NCTX
                P.op('sp', lambda e: e.dma_start(out=cs[:, 0, :n], in_=k.swa_cos[:, p0:p0 + n]), writes=['cs0'], dma=True)
                P.op('sp', lambda e: e.dma_start(out=cs[:, 1, :n], in_=k.swa_sin[:, p0:p0 + n]), writes=['cs1'], dma=True)
            for hp in range(10):
                off = hp * 128
                off_sw = 1536 + hp * 128
                b, ps = psum(k)
                for kc in range(8):
                    P.op('pe', lambda e: e.matmul(ps[:, :n], lhsT=win[:, kc, off:off + 128], rhs=H[:, kc, :n], start=(kc == 0), stop=(kc == 7)),
                         reads=['win', kH], writes=[('ps', b)])
                if s == 0:
                    b2, ps2 = psum(k)
                    for kc in range(8):
                        P.op('pe', lambda e: e.matmul(ps2[:, :n], lhsT=win[:, kc, off_sw:off_sw + 128], rhs=H[:, kc, :n], start=(kc == 0), stop=(kc == 7)),
                             reads=['win', kH], writes=[('ps', b2)])
                    ta, tb = tmps[0], tmps[1]
                    P.op('dve', lambda e: e.tensor_tensor(out=ta[:, :n], in0=ps[:, :n], in1=cs[:, 0, :n], op=ALU.mult),
                         reads=[('ps', b), 'cs0'], writes=[('ntmp', 0)])
                    P.op('dve', lambda e: e.tensor_tensor(out=tb[:, :n], in0=ps2[:, :n], in1=cs[:, 1, :n], op=ALU.mult),
                         reads=[('ps', b2), 'cs1'], writes=[('ntmp', 1)])
                    P.op('dve', lambda e: e.tensor_tensor(out=qo[:, hp, :n], in0=ta[:, :n], in1=tb[:, :n], op=ALU.add),
                         reads=[('ntmp', 0), ('ntmp', 1)], writes=['qo'])
                else:
                    P.op('act', lambda e: e.activation(out=qo[:, hp, :n], in_=ps[:, :n], func=AF.Copy), reads=[('ps', b)], writes=['qo'])
            for par in range(2):
                psl = slice(par * 64, (par + 1) * 64)
                P.op('sp', lambda e: e.dma_start(out=QS.rearrange("d (j two) t -> d two j t", two=2)[:, par, :, c0:c0 + n], in_=qo[psl, 0:8, :n]),
                     reads=['qo'], writes=['QS'], dma=True)
                P.op('sp', lambda e: e.dma_start(out=KS.rearrange("d (j two) t -> d two j t", two=2)[:, par, :, c0:c0 + n], in_=qo[psl, 8:10, :n]),
                     reads=['qo'], writes=['KS'], dma=True)
            for tb_ in range(n // 128):
                r0 = c0 + tb_ * 128
                b, ps = psum(k)
                for kc in range(8):
                    P.op('pe', lambda e: e.matmul(ps[:, 0:256], lhsT=H[:, kc, tb_ * 128:(tb_ + 1) * 128], rhs=win[:, kc, 1280:1536],
                                                  start=(kc == 0), stop=(kc == 7)),
                         reads=['win', kH], writes=[('ps', b)])
                vt = vto[nt % 2]
                kv_ = ('vto', nt % 2)
                nt += 1
                P.op('act', lambda e: e.activation(out=vt, in_=ps[:, 0:256], func=AF.Copy), reads=[('ps', b)], writes=[kv_])
                P.op('sp', lambda e: e.dma_start(out=VS[r0:r0 + 128, :], in_=vt), reads=[kv_], writes=['VS'], dma=True)
    P.barrier()
    with ExitStack() as st:
        Kt = sb(st, nc, "s_K", [64, 4, T], BF16)
        Vt = sb(st, nc, "s_V", [128, NCH, 256], BF16)
        P.op('sp', lambda e: e.dma_start(out=Kt, in_=KS), reads=['KS'], writes=['Kt'], dma=True)
        P.op('sp', lambda e: e.dma_start(out=Vt, in_=VS.rearrange("(c q) f -> q c f", q=128)), reads=['VS'], writes=['Vt'], dma=True)
        msk = sb(st, nc, "s_msk", [128, 2, 128], F32)
        P.op('sp', lambda e: e.dma_start(out=msk[:, 0, :], in_=k.ret_tab[:, 384:512]), writes=['msk'], dma=True)
        P.op('sp', lambda e: e.dma_start(out=msk[:, 1, :], in_=k.ret_tab[:, 256:384]), writes=['msk'], dma=True)
        mskb = sb(st, nc, "s_mskb", [128, 2, 128], BF16)
        P.op('dve', lambda e: e.tensor_copy(out=mskb, in_=msk), reads=['msk'], writes=['mskb'])
        esink = sb(st, nc, "s_esink", [64, 16], F32)
        P.op('act', lambda e: e.activation(out=esink, in_=k.swa_sink, func=AF.Exp), reads=['swap'], writes=['esink'])
        ones_b = sb(st, nc, "s_ones", [128, 64], BF16)
        P.op('dve', lambda e: e.memset(ones_b, 1.0), writes=['ones_b'])
        qb_ = [sb(st, nc, f"s_qb{q}", [64, 16, 128], BF16) for q in range(2)]
        ob_ = [sb(st, nc, f"s_ob{q}", [64, 16, 128], BF16) for q in range(2)]
        Et = [sb(st, nc, f"s_E{q}", [128, 512], BF16) for q in range(10)]
        rd = sb(st, nc, "s_rd", [64, 4, 128], F32)
        ne = 0

        def loadq(c):
            P.op('sp', lambda e: e.dma_start(out=qb_[c % 2], in_=QS[:, :, c * 128:(c + 1) * 128]), reads=['QS'], writes=[('qb', c % 2)], dma=True)
        loadq(0)
        for c in range(NCH):
            if c + 1 < NCH:
                loadq(c + 1)
            qblk = qb_[c % 2]
            oblk = ob_[c % 2]
            if c < 2:
                kbs = [(0, None), (1, None)]
            else:
                kbs = []
                if c - 1 >= 2:
                    kbs.append((c - 1, 0))
                kbs.append((c, None))
                if c + 1 < NCH:
                    kbs.append((c + 1, 1))
                kbs += [(0, None), (1, None)]
            for hk in range(4):
                Q = qblk[:, hk * 4:(hk + 1) * 4, :]
                es = []
                for (kb, mk) in kbs:
                    b, ps = psum(k)
                    P.op('pe', lambda e: e.matmul(ps[:, :], lhsT=Kt[:, hk, kb * 128:(kb + 1) * 128], rhs=Q, start=True, stop=True),
                         reads=['Kt', ('qb', c % 2)], writes=[('ps', b)])
                    E = Et[ne % 10]
                    kE = ('E', ne % 10)
                    ne += 1
                    P.op('act', lambda e: e.activation(out=E, in_=ps[:, :], func=AF.Exp, scale=0.125), reads=[('ps', b)], writes=[kE])
                    if mk is not None:
                        Ev = E.rearrange("p (g i) -> p g i", i=128)
                        P.op('pool', lambda e: e.tensor_tensor(out=Ev, in0=Ev, in1=mskb[:, mk, :].unsqueeze(1).to_broadcast([128, 4, 128]), op=ALU.mult),
                             reads=[kE, 'mskb'], writes=[kE])
                    es.append((kb, E, kE))
                bo, ps_o = psum(k)
                for q, (kb, E, kE) in enumerate(es):
                    P.op('pe', lambda e: e.matmul(ps_o[0:64, :], lhsT=Vt[:, kb, hk * 64:(hk + 1) * 64], rhs=E, start=(q == 0), stop=(q == len(es) - 1)),
                         reads=['Vt', kE], writes=[('ps', bo)])
                bd, ps_d = psum(k)
                for q, (kb, E, kE) in enumerate(es):
                    P.op('pe', lambda e: e.matmul(ps_d[0:64, :], lhsT=ones_b, rhs=E, start=(q == 0), stop=(q == len(es) - 1)),
                         reads=['ones_b', kE], writes=[('ps', bd)])
                P.op('dve', lambda e: e.tensor_tensor(out=rd, in0=ps_d[0:64, :].rearrange("p (g i) -> p g i", i=128),
                                                      in1=esink[:, hk * 4:(hk + 1) * 4].unsqueeze(2).to_broadcast([64, 4, 128]), op=ALU.add),
                     reads=[('ps', bd), 'esink'], writes=['rd'])
                P.op('act', lambda e: e.activation(out=rd, in_=rd, func=AF.Ln), reads=['rd'], writes=['rd'])
                P.op('act', lambda e: e.activation(out=rd, in_=rd, func=AF.Exp, scale=-1.0), reads=['rd'], writes=['rd'])
                P.op('dve', lambda e: e.tensor_tensor(out=oblk[:, hk * 4:(hk + 1) * 4, :], in0=ps_o[0:64, :].rearrange("p (g i) -> p g i", i=128),
                                                      in1=rd, op=ALU.mult),
                     reads=[('ps', bo), 'rd'], writes=[('ob', c % 2)])
            P.op('sp', lambda e: e.dma_start(out=OS[:, :, c * 128:(c + 1) * 128], in_=oblk), reads=[('ob', c % 2)], writes=['OS'], dma=True)
    P.barrier()
    with ExitStack() as st:
        wo = sb(st, nc, "swa_wo", [64, 16, 1024], BF16)
        P.op('pool', lambda e: e.dma_start(out=wo, in_=k.swa_w_out[0].rearrange("(h d) c -> d h c", d=64)), writes=['wo'], dma=True)
        out_proj_residual(k, i, TILES, OS, 16, 'OS', wo, 64, lambda zc, dc: wo[:, zc, dc * 128:(dc + 1) * 128])


def stage_final(k):
    nc, P = k.nc, k.P
    with ExitStack() as st:
        xts = [sb(st, nc, f"xtf{q}", [128, 8, 512], F32) for q in range(2)]
        sq = sb(st, nc, "sqf", [128, 8, 512], F32)
        rss = [sb(st, nc, f"rsf{q}", [128, 512], F32) for q in range(2)]
        outv = k.out.rearrange("(kc q) t -> q kc t", q=128)
        for ti, (c0, n, s) in enumerate(TILES[1:]):
            xt = xts[ti % 2]
            rs = rss[ti % 2]
            kx = ('xtf', ti % 2)
            kr = ('rsf', ti % 2)
            P.op('sp', lambda e: e.dma_start(out=xt[:, :, :n], in_=xrow(k)[:, :, c0:c0 + n]), reads=['X', ('Xt', c0)], writes=[kx], dma=True)
            sqb = sq.bitcast(BF16)[:, :, 0:512]
            P.op('act', lambda e: e.activation(out=sqb[:, :, :n], in_=xt[:, :, :n], func=AF.Square), reads=[kx], writes=['sq'])
            b, ps = psum(k)
            for kc in range(8):
                P.op('pe', lambda e: e.matmul(ps[:, :n], lhsT=k.ones_b[:, :], rhs=sqb[:, kc, :n], start=(kc == 0), stop=(kc == 7)),
                     reads=['sq', 'ones_b'], writes=[('ps', b)])
            P.op('act', lambda e: e.activation(out=rs[:, :n], in_=ps[:, :n], func=AF.Ln, scale=1.0 / D, bias=k.epsb[:, 0:1]),
                 reads=[('ps', b)], writes=[kr])
            P.op('act', lambda e: e.activation(out=rs[:, :n], in_=rs[:, :n], func=AF.Exp, scale=-0.5), reads=[kr], writes=[kr])
            for kc in range(8):
                P.op('dve', lambda e: e.scalar_tensor_tensor(out=xt[:, kc, :n], in0=xt[:, kc, :n], scalar=k.fg_s[:, kc:kc + 1], in1=rs[:, :n],
                                                             op0=ALU.mult, op1=ALU.mult),
                     reads=[kx, kr, 'fg'], writes=[kx])
            P.op('sp', lambda e: e.dma_start(out=outv[:, :, c0 - NCTX:c0 - NCTX + n], in_=xt[:, :, :n]), reads=[kx], writes=['out'], dma=True)
    P.barrier()


ALL_PARTS = ['init', 'mods', 'mix0', 'ffn0', 'mix1', 'ffn1', 'mix2', 'ffn2', 'mix3', 'ffn3', 'final']


def kernel(**inputs):
    inp = {kk: np.asarray(v) for kk, v in inputs.items()}
    n = inp['x'].shape[0]
    maps = make_in_maps(inp, list(range(n)))
    nc, _ = build_program(ALL_PARTS, debug=False)
    res = run_bass_kernel_spmd(nc, maps, core_ids=list(range(n)))
    out = np.stack([np.ascontiguousarray(res.results[b]['outT'].T) for b in range(n)], axis=0)
    return out.astype(np.float32)


I32 = mybir.dt.int32
NBLK_MAX = 24
MOE_BLK = 512


def precast_moe(k, pe):
    P = k.P
    f, e = pe // 8, pe % 8
    gu, dn = k.moe_w_gu[f, e], k.moe_w_down[f, e]
    for g7 in range(7):
        r0 = (pe * 7 + g7) * 128
        for half in range(2):
            c0 = half * DFF + g7 * 512
            dst = k.WGUx[r0:r0 + 128, :].rearrange("p (kc h c) -> p kc h c", kc=8, h=2)[:, :, half, :]
            src = gu.rearrange("(kc p) n -> p kc n", p=128)[:, :, c0:c0 + 512]
            P.op('pool', lambda e_: e_.dma_start(out=dst, in_=src), writes=[('wgux', pe)], dma=True)
    for dh in range(2):
        r0 = (pe * 2 + dh) * 128
        dst = k.WDx[r0:r0 + 128, :].rearrange("p (fc c) -> p fc c", c=512)
        src = dn.rearrange("(fc p) d -> p fc d", p=128)[:, :, dh * 512:(dh + 1) * 512]
        P.op('pool', lambda e_: e_.dma_start(out=dst, in_=src), writes=[('wdx', pe)], dma=True)


def drain_precast(k, n):
    while n > 0 and k.pc_queue:
        k.pc_queue.pop(0)()
        n -= 1


def stage_moe_sparse(k, i):
    nc, P = k.nc, k.P
    f = i // 2
    last = (i == 3)
    tiles = TILES[1:] if last else TILES
    cols0 = tiles[0][0]
    ntok = sum(t[1] for t in tiles)
    NCK = ntok // 128
    NB = (2 * ntok + 8 * (MOE_BLK - 1)) // MOE_BLK
    drain_precast(k, 1000)
    HC, YC = k.HC, k.YC
    ct = k.moe_tab
    with ExitStack() as st0:
        EQ1 = sb(st0, nc, "m_eq1", [128, NCK, 8], F32)
        EQ2 = sb(st0, nc, "m_eq2", [128, NCK, 8], F32)
        W12 = sb(st0, nc, "m_w12", [128, NCK, 2], F32)
        DI = sb(st0, nc, "m_di", [128, NCK, 2], I32)
        IGU = sb(st0, nc, "m_igu", [128, NB, 7], I32)
        IWD = sb(st0, nc, "m_iwd", [128, NB, 2], I32)
        tabs = sb(st0, nc, "m_tab", [128, 161], F32)
        P.op('sp', lambda e: e.dma_start(out=tabs, in_=ct), writes=['mtab'], dma=True)
        TRI, IOB, CGU, CWD = tabs[:, 0:128], tabs[:, 128:128 + NB], tabs[:, 152:159], tabs[:, 159:161]
        with ExitStack() as st:
            HTOK = sb(st, nc, "m_htok", [128, NCK, 1024], BF16)
            H = sb(st, nc, "m_H", [128, 8, 512], BF16)
            xt = sb(st, nc, "m_xt", [128, 8, 512], F32)
            sq = sb(st, nc, "m_sq", [128, 8, 512], F32)
            hf = sb(st, nc, "m_hf", [128, 8, 512], F32)
            rs = sb(st, nc, "m_rs", [128, 512], F32)
            tmps = [sb(st, nc, f"m_nt{q}", [128, 512], F32) for q in range(2)]
            rsm = sb(st, nc, "m_rsm", [128, 64], F32)
            rsm3 = sb(st, nc, "m_rsm3", [128, 12], F32)
            identb = sb(st, nc, "m_identb", [128, 128], BF16)
            P.op('dve', lambda e: e.tensor_copy(out=identb, in_=k.ident_f), reads=['ident'], writes=['identb'])
            ck = 0
            for ti, (c0, n, s) in enumerate(tiles):
                load_x_tile(k, xt, c0, n)
                norm_tile(k, xt, 'xt', n, i, 1, s, H, 'H', sq, rs, tmps, hf=hf, khf='hf')
                nb = n // 128
                b, ps = psum(k)
                for bi in range(nb):
                    bsl = slice(bi * 128, (bi + 1) * 128)
                    for kc in range(8):
                        P.op('pe', lambda e: e.matmul(ps[:, bi * 8:(bi + 1) * 8], lhsT=hf[:, kc, bsl], rhs=k.rt[:, f, kc, :], start=(kc == 0), stop=(kc == 7)),
                             reads=['hf', 'rt'], writes=[('ps', b)])
                lg = rsm[:, 0:nb * 8]
                lg2 = rsm[:, 32:32 + nb * 8]
                lgv = lg.rearrange("p (c e) -> p c e", e=8)
                lg2v = lg2.rearrange("p (c e) -> p c e", e=8)
                m1, m2, dd = rsm3[:, 0:nb], rsm3[:, 4:4 + nb], rsm3[:, 8:8 + nb]
                eq1, eq2 = EQ1[:, ck:ck + nb, :], EQ2[:, ck:ck + nb, :]
                R = ['rsm']
                KE = [('eq', ck + q) for q in range(nb)]
                P.op('dve', lambda e: e.tensor_copy(out=lg, in_=ps[:, 0:nb * 8]), reads=[('ps', b)], writes=R)
                P.op('dve', lambda e: e.tensor_reduce(out=m1, in_=lgv, axis=AX.X, op=ALU.max), reads=R, writes=R)
                P.op('dve', lambda e: e.tensor_tensor(out=eq1, in0=lgv, in1=m1.unsqueeze(2).to_broadcast([128, nb, 8]), op=ALU.is_equal), reads=R, writes=KE)
                P.op('dve', lambda e: e.scalar_tensor_tensor(out=lg2, in0=eq1.rearrange("p c e -> p (c e)"), scalar=-1e30, in1=lg, op0=ALU.mult, op1=ALU.add),
                     reads=R + KE, writes=R)
                P.op('dve', lambda e: e.tensor_reduce(out=m2, in_=lg2v, axis=AX.X, op=ALU.max), reads=R, writes=R)
                P.op('dve', lambda e: e.tensor_tensor(out=eq2, in0=lg2v, in1=m2.unsqueeze(2).to_broadcast([128, nb, 8]), op=ALU.is_equal), reads=R, writes=KE)
                P.op('dve', lambda e: e.tensor_tensor(out=dd, in0=m2, in1=m1, op=ALU.subtract), reads=R, writes=R)
                P.op('act', lambda e: e.activation(out=W12[:, ck:ck + nb, 0], in_=dd, func=AF.Sigmoid, scale=-1.0), reads=R, writes=KE)
                P.op('act', lambda e: e.activation(out=W12[:, ck:ck + nb, 1], in_=dd, func=AF.Sigmoid, scale=1.0), reads=R, writes=KE)
                for bi in range(nb):
                    bsl = slice(bi * 128, (bi + 1) * 128)
                    bt, pst = psum(k)
                    pstb = pst.bitcast(BF16)
                    for kc in range(8):
                        P.op('pe', lambda e: e.transpose(out=pstb[:, kc * 128:(kc + 1) * 128], in_=H[:, kc, bsl], identity=identb),
                             reads=['H', 'identb'], writes=[('ps', bt)])
                    eng = 'act' if ck % 2 == 0 else 'dve'
                    if eng == 'act':
                        P.op('act', lambda e: e.activation(out=HTOK[:, ck, :], in_=pstb, func=AF.Copy), reads=[('ps', bt)], writes=[('htok', ck)])
                    else:
                        P.op('dve', lambda e: e.tensor_copy(out=HTOK[:, ck, :], in_=pstb), reads=[('ps', bt)], writes=[('htok', ck)])
                    ck += 1
            allE = [('eq', c) for c in range(NCK)]
            SEL = sb(st, nc, "m_sel", [128, NCK, 8], F32)
            PRE = sb(st, nc, "m_pre", [128, NCK + 1, 8], F32)
            RANK = sb(st, nc, "m_rank", [128, NCK, 8], F32)
            sm = sb(st, nc, "m_sm", [128, 64], F32)
            TOT, NBk, CB, OFF, ONE8 = sm[:, 0:8], sm[:, 8:16], sm[:, 16:24], sm[:, 24:32], sm[:, 32:40]
            EB = sb(st, nc, "m_eb", [128, 3, NB], F32)
            DF = sb(st, nc, "m_df", [128, NCK, 2], F32)
            IGf = sb(st, nc, "m_igf", [128, NB, 7], F32)
            IWf = sb(st, nc, "m_iwf", [128, NB, 2], F32)
            P.op('dve', lambda e: e.tensor_tensor(out=SEL, in0=EQ1, in1=EQ2, op=ALU.add), reads=allE, writes=['sel'])
            P.op('dve', lambda e: e.memset(PRE[:, 0, :], 0.0), writes=['pre'])
            P.op('dve', lambda e: e.memset(ONE8, 1.0), writes=['sm'])
            for c in range(NCK):
                P.op('dve', lambda e: e.tensor_tensor(out=PRE[:, c + 1, :], in0=PRE[:, c, :], in1=SEL[:, c, :], op=ALU.add),
                     reads=['pre', 'sel'], writes=['pre'])
            br, psr = psum(k)
            for c in range(NCK):
                P.op('pe', lambda e: e.matmul(psr[:, c * 8:(c + 1) * 8], lhsT=TRI, rhs=SEL[:, c, :], start=True, stop=False),
                     reads=['mtab', 'sel'], writes=[('ps', br)])
                P.op('pe', lambda e: e.matmul(psr[:, c * 8:(c + 1) * 8], lhsT=k.ones_f, rhs=PRE[:, c, :], start=False, stop=True),
                     reads=['ones', 'pre'], writes=[('ps', br)])
            P.op('dve', lambda e: e.tensor_copy(out=RANK, in_=psr[:, 0:NCK * 8].rearrange("p (c e) -> p c e", e=8)), reads=[('ps', br)], writes=['rank'])
            b2, ps2 = psum(k)
            P.op('pe', lambda e: e.matmul(ps2[:, 0:8], lhsT=k.ones_f, rhs=PRE[:, NCK, :], start=True, stop=True), reads=['ones', 'pre'], writes=[('ps', b2)])
            S_ = ['sm']
            P.op('dve', lambda e: e.tensor_copy(out=TOT, in_=ps2[:, 0:8]), reads=[('ps', b2)], writes=S_)
            P.op('dve', lambda e: e.tensor_scalar(out=NBk, in0=TOT, scalar1=0.0, scalar2=None, op0=ALU.is_gt), reads=S_, writes=S_)
            for m in range(1, 9):
                P.op('dve', lambda e: e.scalar_tensor_tensor(out=NBk, in0=TOT, scalar=float(MOE_BLK * m), in1=NBk, op0=ALU.is_gt, op1=ALU.add),
                     reads=S_, writes=S_)
            P.op('dve', lambda e: e.tensor_tensor_scan(out=CB, data0=ONE8, data1=NBk, initial=0.0, op0=ALU.mult, op1=ALU.add), reads=S_, writes=S_)
            P.op('dve', lambda e: e.tensor_tensor(out=OFF, in0=CB, in1=NBk, op=ALU.subtract), reads=S_, writes=S_)
            P.op('dve', lambda e: e.tensor_scalar(out=OFF, in0=OFF, scalar1=float(MOE_BLK), scalar2=None, op0=ALU.mult), reads=S_, writes=S_)
            P.op('dve', lambda e: e.tensor_tensor(out=RANK, in0=RANK, in1=OFF.unsqueeze(1).to_broadcast([128, NCK, 8]), op=ALU.add),
                 reads=['rank'] + S_, writes=['rank'])
            P.op('dve', lambda e: e.tensor_tensor(out=SEL, in0=RANK, in1=EQ1, op=ALU.mult), reads=['rank'] + allE, writes=['sel'])
            P.op('dve', lambda e: e.tensor_reduce(out=DF[:, :, 0], in_=SEL, axis=AX.X, op=ALU.add), reads=['sel'], writes=['df'])
            P.op('dve', lambda e: e.tensor_tensor(out=SEL, in0=RANK, in1=EQ2, op=ALU.mult), reads=['rank', 'df'] + allE, writes=['sel'])
            P.op('dve', lambda e: e.tensor_reduce(out=DF[:, :, 1], in_=SEL, axis=AX.X, op=ALU.add), reads=['sel'], writes=['df'])
            P.op('dve', lambda e: e.tensor_copy(out=DI, in_=DF), reads=['df'], writes=['di'])
            P.op('dve', lambda e: e.memset(EB[:, 0, :], 0.0), writes=['eb'])
            for e8 in range(8):
                P.op('dve', lambda e: e.scalar_tensor_tensor(out=EB[:, 0, :], in0=IOB, scalar=CB[:, e8:e8 + 1], in1=EB[:, 0, :], op0=ALU.is_ge, op1=ALU.add),
                     reads=['mtab', 'eb'] + S_, writes=['eb'])
            P.op('dve', lambda e: e.tensor_scalar(out=EB[:, 0, :], in0=EB[:, 0, :], scalar1=7.0, scalar2=None, op0=ALU.min), reads=['eb'], writes=['eb'])
            P.op('dve', lambda e: e.tensor_scalar(out=EB[:, 1, :], in0=EB[:, 0, :], scalar1=896.0, scalar2=float(f * 8 * 896), op0=ALU.mult, op1=ALU.add),
                 reads=['eb'], writes=['eb'])
            P.op('dve', lambda e: e.tensor_scalar(out=EB[:, 2, :], in0=EB[:, 0, :], scalar1=256.0, scalar2=float(f * 8 * 256), op0=ALU.mult, op1=ALU.add),
                 reads=['eb'], writes=['eb'])
            for b_ in range(NB):
                P.op('dve', lambda e: e.tensor_scalar(out=IGf[:, b_, :], in0=CGU, scalar1=EB[:, 1, b_:b_ + 1], scalar2=None, op0=ALU.add),
                     reads=['eb', 'mtab'], writes=['igf'])
                P.op('dve', lambda e: e.tensor_scalar(out=IWf[:, b_, :], in0=CWD, scalar1=EB[:, 2, b_:b_ + 1], scalar2=None, op0=ALU.add),
                     reads=['eb', 'mtab'], writes=['iwf'])
            P.op('dve', lambda e: e.tensor_copy(out=IGU, in_=IGf), reads=['igf'], writes=['igu'])
            P.op('dve', lambda e: e.tensor_copy(out=IWD, in_=IWf), reads=['iwf'], writes=['iwd'])
            for c in range(NCK):
                for j2 in range(2):
                    P.op('pool', lambda e: e.indirect_dma_start(out=HC, out_offset=bass.IndirectOffsetOnAxis(ap=DI[:, c, j2:j2 + 1], axis=0),
                                                                in_=HTOK[:, c, :], in_offset=None),
                         reads=['di', ('htok', c)], writes=['HC'], dma=True)
        P.barrier()
        with ExitStack() as st:
            wgu = [sb(st, nc, f"m_wgu{q}", [128, 8192], BF16) for q in range(2)]
            wdb = [sb(st, nc, f"m_wd{q}", [128, 28 * 512], BF16) for q in range(2)]
            hs = [sb(st, nc, f"m_hs{q}", [128, 4, 1024], BF16) for q in range(2)]
            HcT = sb(st, nc, "m_HcT", [128, 8, 512], BF16)
            act = sb(st, nc, "m_act", [128, 28, 512], BF16)
            yc = [sb(st, nc, f"m_yc{q}", [128, 1024], F32) for q in range(2)]
            sg_ = [sb(st, nc, f"m_sg{q}", [128, 512], BF16) for q in range(2)]
            identb = sb(st, nc, "m_identb2", [128, 128], BF16)
            P.op('dve', lambda e: e.tensor_copy(out=identb, in_=k.ident_f), reads=['ident'], writes=['identb'])
            n_gu = [0]

            def gather_gu(b_, g7):
                q = n_gu[0] % 2
                n_gu[0] += 1
                P.op('pool', lambda e: e.indirect_dma_start(out=wgu[q], out_offset=None, in_=k.WGUx,
                                                            in_offset=bass.IndirectOffsetOnAxis(ap=IGU[:, b_, g7:g7 + 1], axis=0)),
                     reads=['igu', 'wgux_all'], writes=[('wgu', q)], dma=True)
                return q

            def gather_wd(b_, dh):
                P.op('pool', lambda e: e.indirect_dma_start(out=wdb[dh], out_offset=None, in_=k.WDx,
                                                            in_offset=bass.IndirectOffsetOnAxis(ap=IWD[:, b_, dh:dh + 1], axis=0)),
                     reads=['iwd', 'wdx_all'], writes=[('wd', dh)], dma=True)

            def load_hs(b_):
                P.op('sp', lambda e: e.dma_start(out=hs[b_ % 2], in_=HC[b_ * 512:(b_ + 1) * 512, :].rearrange("(sg p) d -> p sg d", p=128)),
                     reads=['HC'], writes=[('hs', b_ % 2)], dma=True)
            load_hs(0)
            pre_gu = [gather_gu(0, 0), gather_gu(0, 1)]
            gather_wd(0, 0)
            gather_wd(0, 1)
            nsg = 0
            nyc = 0
            for b_ in range(NB):
                if b_ + 1 < NB:
                    load_hs(b_ + 1)
                hsb = hs[b_ % 2]
                for kc in range(8):
                    bt, pst = psum(k)
                    pstb = pst.bitcast(BF16)
                    for sgi in range(4):
                        P.op('pe', lambda e: e.transpose(out=pstb[:, sgi * 128:(sgi + 1) * 128], in_=hsb[:, sgi, kc * 128:(kc + 1) * 128], identity=identb),
                             reads=[('hs', b_ % 2), 'identb'], writes=[('ps', bt)])
                    if kc % 2 == 0:
                        P.op('act', lambda e: e.activation(out=HcT[:, kc, :], in_=pstb[:, 0:512], func=AF.Copy), reads=[('ps', bt)], writes=['HcT'])
                    else:
                        P.op('dve', lambda e: e.tensor_copy(out=HcT[:, kc, :], in_=pstb[:, 0:512]), reads=[('ps', bt)], writes=['HcT'])
                for g7 in range(7):
                    if g7 < 2:
                        q = pre_gu[g7]
                    else:
                        q = gather_gu(b_, g7)
                    wv = wgu[q].rearrange("p (kc h c) -> p kc h c", kc=8, h=2)
                    for j in range(4):
                        fch = g7 * 4 + j
                        bg, psg = psum(k)
                        for kc in range(8):
                            P.op('pe', lambda e: e.matmul(psg, lhsT=wv[:, kc, 0, j * 128:(j + 1) * 128], rhs=HcT[:, kc, :], start=(kc == 0), stop=(kc == 7)),
                                 reads=[('wgu', q), 'HcT'], writes=[('ps', bg)])
                        bu, psu = psum(k)
                        for kc in range(8):
                            P.op('pe', lambda e: e.matmul(psu, lhsT=wv[:, kc, 1, j * 128:(j + 1) * 128], rhs=HcT[:, kc, :], start=(kc == 0), stop=(kc == 7)),
                                 reads=[('wgu', q), 'HcT'], writes=[('ps', bu)])
                        sgt = sg_[nsg % 2]
                        ksg = ('sg', nsg % 2)
                        nsg += 1
                        P.op('act', lambda e: e.activation(out=sgt, in_=psg, func=AF.Silu), reads=[('ps', bg)], writes=[ksg])
                        P.op('dve', lambda e: e.tensor_tensor(out=act[:, fch, :], in0=sgt, in1=psu, op=ALU.mult), reads=[ksg, ('ps', bu)], writes=[('act', fch)])
                if b_ + 1 < NB:
                    pre_gu = [gather_gu(b_ + 1, 0), gather_gu(b_ + 1, 1)]
                for sgi in range(4):
                    y = yc[nyc % 2]
                    ky = ('yc', nyc % 2)
                    nyc += 1
                    for dh in range(2):
                        wv = wdb[dh].rearrange("p (fc c) -> p fc c", c=512)
                        bd, psd = psum(k)
                        for fc in range(28):
                            P.op('pe', lambda e: e.matmul(psd, lhsT=act[:, fc, sgi * 128:(sgi + 1) * 128], rhs=wv[:, fc, :], start=(fc == 0), stop=(fc == 27)),
                                 reads=[('act', fc), ('wd', dh)], writes=[('ps', bd)])
                        if dh == 0:
                            P.op('act', lambda e: e.activation(out=y[:, 0:512], in_=psd, func=AF.Copy), reads=[('ps', bd)], writes=[ky])
                        else:
                            P.op('dve', lambda e: e.tensor_copy(out=y[:, 512:1024], in_=psd), reads=[('ps', bd)], writes=[ky])
                    r0 = b_ * 512 + sgi * 128
                    P.op('sp', lambda e: e.dma_start(out=YC[r0:r0 + 128, :], in_=y), reads=[ky], writes=['YC'], dma=True)
                if b_ + 1 < NB:
                    gather_wd(b_ + 1, 0)
                    gather_wd(b_ + 1, 1)
        P.barrier()
        with ExitStack() as st:
            xt = sb(st, nc, "m_xtE", [128, 8, 512], F32)
            sqE = sb(st, nc, "m_sqE", [128, 8, 512], BF16)
            rsE = sb(st, nc, "m_rsE", [128, 512], F32)
            y1 = [sb(st, nc, f"m_y1{q}", [128, 1024], F32) for q in range(4)]
            y2 = [sb(st, nc, f"m_y2{q}", [128, 1024], F32) for q in range(4)]
            ck = 0
            for ti, (c0, n, s) in enumerate(tiles):
                load_x_tile(k, xt, c0, n)
                banks = [psum(k) for _ in range(8)]
                for bi in range(n // 128):
                    q = ck % 4
                    P.op('pool', lambda e: e.indirect_dma_start(out=y1[q], out_offset=None, in_=YC,
                                                                in_offset=bass.IndirectOffsetOnAxis(ap=DI[:, ck, 0:1], axis=0)),
                         reads=['YC', 'di'], writes=[('y1', q)], dma=True)
                    P.op('pool', lambda e: e.indirect_dma_start(out=y2[q], out_offset=None, in_=YC,
                                                                in_offset=bass.IndirectOffsetOnAxis(ap=DI[:, ck, 1:2], axis=0)),
                         reads=['YC', 'di'], writes=[('y2', q)], dma=True)
                    P.op('dve', lambda e: e.tensor_scalar(out=y1[q], in0=y1[q], scalar1=W12[:, ck, 0:1], scalar2=None, op0=ALU.mult),
                         reads=[('y1', q), ('eq', ck)], writes=[('y1', q)])
                    P.op('dve', lambda e: e.scalar_tensor_tensor(out=y1[q], in0=y2[q], scalar=W12[:, ck, 1:2], in1=y1[q], op0=ALU.mult, op1=ALU.add),
                         reads=[('y1', q), ('y2', q), ('eq', ck)], writes=[('y1', q)])
                    for kc in range(8):
                        bb, pp = banks[kc]
                        P.op('pe', lambda e: e.transpose(out=pp[:, bi * 128:(bi + 1) * 128], in_=y1[q][:, kc * 128:(kc + 1) * 128], identity=k.ident_f),
                             reads=[('y1', q), 'ident'], writes=[('ps', bb)])
                    ck += 1
                for kc in range(8):
                    bb, pp = banks[kc]
                    gate = k.mods[:, i, 5 * 8 + kc, s:s + 1]
                    P.op('dve', lambda e: e.scalar_tensor_tensor(out=xt[:, kc, :n], in0=pp[:, :n], scalar=gate, in1=xt[:, kc, :n], op0=ALU.mult, op1=ALU.add),
                         reads=[('ps', bb), 'xt', 'mods'], writes=['xt'])
                if last and k.fuse_final:
                    P.op('act', lambda e: e.activation(out=sqE[:, :, :n], in_=xt[:, :, :n], func=AF.Square), reads=['xt'], writes=['sqE'])
                    bf_, psf = psum(k)
                    for kc in range(8):
                        P.op('pe', lambda e: e.matmul(psf[:, :n], lhsT=k.ones_b[:, :], rhs=sqE[:, kc, :n], start=(kc == 0), stop=(kc == 7)),
                             reads=['sqE', 'ones_b'], writes=[('ps', bf_)])
                    P.op('act', lambda e: e.activation(out=rsE[:, :n], in_=psf[:, :n], func=AF.Ln, scale=1.0 / D, bias=k.epsb[:, 0:1]),
                         reads=[('ps', bf_)], writes=['rsE'])
                    P.op('act', lambda e: e.activation(out=rsE[:, :n], in_=rsE[:, :n], func=AF.Exp, scale=-0.5), reads=['rsE'], writes=['rsE'])
                    for kc in range(8):
                        P.op('dve', lambda e: e.scalar_tensor_tensor(out=xt[:, kc, :n], in0=xt[:, kc, :n], scalar=k.fg_s[:, kc:kc + 1], in1=rsE[:, :n],
                                                                     op0=ALU.mult, op1=ALU.mult),
                             reads=['xt', 'rsE', 'fg'], writes=['xt'])
                    P.op('sp', lambda e: e.dma_start(out=k.out.rearrange("(kc q) t -> q kc t", q=128)[:, :, c0 - NCTX:c0 - NCTX + n], in_=xt[:, :, :n]),
                         reads=['xt'], writes=['out'], dma=True)
                else:
                    store_x_tile(k, xt, c0, n)
    P.barrier()
```

```python
import numpy as np
from contextlib import ExitStack
import concourse.bass as bass
import concourse.mybir as mybir
from concourse.bass_utils import run_bass_kernel_spmd

F32 = mybir.dt.float32
BF16 = mybir.dt.bfloat16
AF = mybir.ActivationFunctionType
ALU = mybir.AluOpType
AX = mybir.AxisListType

NPOOL = 8
SPARSE_MOE = True
SAME_ENGINE_SYNC = True

T = 4352
NCTX = 256
SEQ = 4096
D = 1024
DFF = 3584
EPS = 1e-6
TILES = [(0, 256, 1)] + [(256 + 512 * i, 512, 0) for i in range(8)]


class Prog:
    def __init__(self, nc, stack):
        self.nc = nc
        self.stack = stack
        self.eng = {'pe': nc.tensor, 'act': nc.scalar, 'dve': nc.vector, 'pool': nc.gpsimd, 'sp': nc.sync}
        self.sems = {}
        self.cnt = {e: 0 for e in self.eng}
        self.seen = {e: {} for e in self.eng}
        self.keys = {}
        self.dma_n = {'sp': 0, 'pool': 0}
        self.n_inst = 0

    def sem(self, sk):
        if sk not in self.sems:
            name = 's_' + (sk if isinstance(sk, str) else f'{sk[0]}q{sk[1]}')
            self.sems[sk] = self.stack.enter_context(self.nc.semaphore(name))
        return self.sems[sk]

    def op(self, eng, fn, reads=(), writes=(), dma=False):
        deps = {}
        own = None if dma else eng

        def add(sk, v):
            if deps.get(sk, 0) < v:
                deps[sk] = v
        for k in reads:
            st = self.keys.get(k)
            if st is not None and st[0] is not None:
                add(*st[0])
        for k in writes:
            st = self.keys.get(k)
            if st is not None:
                if st[0] is not None and st[0][0] != own:
                    add(*st[0])
                for sk, v in st[1].items():
                    if sk != own:
                        add(sk, v)
        if dma:
            n = self.dma_n[eng]
            slot = n % NPOOL
            semkey = (eng, slot)
            val = 16 * (n // NPOOL + 1)
            if n >= NPOOL:
                add(semkey, 16 * (n // NPOOL))
            self.dma_n[eng] += 1
            inc = 16
        else:
            self.cnt[eng] += 1
            semkey = eng
            val = self.cnt[eng]
            inc = 1
        h = self.eng[eng]
        for sk, v in deps.items():
            if sk == eng and (eng == 'pe' or not SAME_ENGINE_SYNC):
                continue
            if self.seen[eng].get(sk, 0) >= v:
                continue
            self.seen[eng][sk] = v
            h.wait_ge(self.sem(sk), v)
        inst = fn(h)
        inst.then_inc(self.sem(semkey), inc)
        self.n_inst += 1
        for k in reads:
            st = self.keys.setdefault(k, [None, {}])
            if st[1].get(semkey, 0) < val:
                st[1][semkey] = val
        for k in writes:
            self.keys[k] = [(semkey, val), {}]

    def _all_now(self):
        cur = {}
        for e in ('pe', 'act', 'dve', 'pool'):
            if self.cnt[e] > 0:
                cur[e] = self.cnt[e]
        for q, n in self.dma_n.items():
            for slot in range(min(n, NPOOL)):
                cur[(q, slot)] = 16 * (((n - 1 - slot) // NPOOL) + 1)
        return cur

    def barrier(self, engines=('pe', 'act', 'dve', 'pool', 'sp')):
        cur = self._all_now()
        for e in engines:
            h = self.eng[e]
            for sk, v in cur.items():
                if sk == e:
                    continue
                if self.seen[e].get(sk, 0) >= v:
                    continue
                self.seen[e][sk] = v
                h.wait_ge(self.sem(sk), v)
        if len(engines) == 5:
            self.keys.clear()

    def finish(self):
        self.barrier(engines=('sp',))


class K:
    pass


_SB_N = [0]


def sb(st, nc, name, shape, dt):
    _SB_N[0] += 1
    h = st.enter_context(nc.sbuf_tensor(f"{name}_u{_SB_N[0]}", shape, dt))
    return h[tuple(slice(None) for _ in shape)]


def col(v, n):
    return np.ascontiguousarray(np.asarray(v).reshape(n, 128).T)


def build_program(parts, debug=False):
    nc = bass.Bass("TRN2", target_bir_lowering=False)
    k = K()
    k.nc = nc
    dr = {}

    def din(name, shape, dt=F32):
        dr[name] = nc.dram_tensor(name, list(shape), dt, kind="ExternalInput").ap()
        return dr[name]

    k.xT = din("xT", [D, T])
    k.cc = din("cc", [128, 8, 2])
    k.ada_w = din("ada_w", [4, D, 6 * D])
    k.adab = din("adab", [128, 4, 48])
    k.adab_row = din("adab_row", [4, 2, 6144])
    k.ng = din("ng", [128, 4, 2, 8])
    k.fg = din("fg", [128, 8])
    k.ones_in = din("ones_f", [128, 128])
    k.ident_in = din("ident_f", [128, 128])
    k.esel_in = din("esel", [8, 8, 128])
    k.ffn_w_gu = din("ffn_w_gu", [2, D, 2 * DFF])
    k.ffn_w_down = din("ffn_w_down", [2, DFF, D])
    k.router = din("router", [128, 2, 8, 8])
    k.moe_w_gu = din("moe_w_gu", [2, 8, D, 2 * DFF])
    k.moe_w_down = din("moe_w_down", [2, 8, DFF, D])
    k.lru_w_in = din("lru_w_in", [2, D, 2560])
    k.lru_wg = din("lru_wg", [2, 128, 40, 128])
    k.lru_w_out = din("lru_w_out", [2, 1280, D])
    k.lru_cw_in = din("lru_cw", [128, 2, 4, 10])
    k.lru_cb_in = din("lru_cb", [128, 2, 10])
    k.lru_bg_in = din("lru_bg", [128, 2, 2, 2, 10])
    k.lru_lam_in = din("lru_lam", [128, 2, 2, 10])
    k.ret_w_in = din("ret_w_in", [1, D, 6144])
    k.ret_w_out = din("ret_w_out", [1, 2048, D])
    k.ret_ld_in = din("ret_ld", [128, 8])
    k.ret_gain_in = din("ret_gain", [128, 16])
    k.ret_cosT = din("ret_cosT", [128, SEQ])
    k.ret_sinT = din("ret_sinT", [128, SEQ])
    k.ret_costok = din("ret_costok", [SEQ, 128])
    k.ret_sintok = din("ret_sintok", [SEQ, 128])
    k.ret_tab = din("ret_tab", [128, 771])
    k.swa_w_ext = din("swa_w_ext", [D, 2816])
    k.swa_w_out = din("swa_w_out", [1, D, D])
    k.swa_sink_in = din("swa_sink", [64, 16])
    k.swa_cos = din("swa_cos", [128, SEQ])
    k.swa_sin = din("swa_sin", [128, SEQ])
    k.moe_tab = din("moe_tab", [128, 161])
    k.dr = dr

    if debug:
        k.out = nc.dram_tensor("xdump", [D, T], F32, kind="ExternalOutput").ap()
    else:
        k.out = nc.dram_tensor("outT", [D, SEQ], F32, kind="ExternalOutput").ap()
    k.X = nc.dram_tensor("Xres", [D, T], F32, kind="Internal").ap()
    k.WGU = nc.dram_tensor("WGUs", [18, D, 2 * DFF], BF16, kind="Internal").ap() if not SPARSE_MOE else nc.dram_tensor("WGUs", [10, D, 2 * DFF], BF16, kind="Internal").ap()
    k.WD = nc.dram_tensor("WDs", [18, DFF, D], BF16, kind="Internal").ap() if not SPARSE_MOE else nc.dram_tensor("WDs", [10, DFF, D], BF16, kind="Internal").ap()
    k.QT = nc.dram_tensor("QTs", [1024, T], BF16, kind="Internal").ap()
    k.KT = nc.dram_tensor("KTs", [1024, T], BF16, kind="Internal").ap()
    k.KTOK = nc.dram_tensor("KTOKs", [T, 1024], BF16, kind="Internal").ap()
    k.VTOK = nc.dram_tensor("VTOKs", [T, 2048], BF16, kind="Internal").ap()
    k.GS = nc.dram_tensor("GSs", [2048, T], BF16, kind="Internal").ap()
    k.SBd = nc.dram_tensor("SBds", [4, NCH, 256, 512], BF16, kind="Internal").ap()
    k.ZR = nc.dram_tensor("ZRs", [2048, T], BF16, kind="Internal").ap()
    k.QS = nc.dram_tensor("QSs", [64, 16, T], BF16, kind="Internal").ap()
    k.KS = nc.dram_tensor("KSs", [64, 4, T], BF16, kind="Internal").ap()
    k.VS = nc.dram_tensor("VSs", [T, 256], BF16, kind="Internal").ap()
    k.OS = nc.dram_tensor("OSs", [64, 16, T], BF16, kind="Internal").ap()
    k.HC = nc.dram_tensor("HCs", [NBLK_MAX * 512, 1024], BF16, kind="Internal").ap()
    k.YC = nc.dram_tensor("YCs", [NBLK_MAX * 512, 1024], F32, kind="Internal").ap()
    k.WGUx = nc.dram_tensor("WGUx", [16 * 7 * 128, 8192], BF16, kind="Internal").ap()
    k.WDx = nc.dram_tensor("WDx", [16 * 2 * 128, 28 * 512], BF16, kind="Internal").ap()
    k.XR = nc.dram_tensor("XRs", [1280, T], F32, kind="Internal").ap()
    k.YG = nc.dram_tensor("YGs", [1280, T], BF16, kind="Internal").ap()
    k.ZG = nc.dram_tensor("ZGs", [1280, T], BF16, kind="Internal").ap()

    with ExitStack() as st:
        P = Prog(nc, st)
        k.P = P
        k.PS = [st.enter_context(nc.psum_tensor(f"ps{i}", [128, 512], F32))[:, :] for i in range(8)]
        k.ps_n = 0
        k.cond = sb(st, nc, "cond", [128, 8, 2], F32)
        k.mods = sb(st, nc, "mods", [128, 4, 48, 2], F32)
        k.acoef = sb(st, nc, "acoef", [128, 4, 2, 8, 2], F32)
        k.adab_s = sb(st, nc, "adab_s", [128, 4, 48], F32)
        k.ng_s = sb(st, nc, "ng_s", [128, 4, 2, 8], F32)
        k.fg_s = sb(st, nc, "fg_s", [128, 8], F32)
        k.ones_f = sb(st, nc, "ones_fs", [128, 128], F32)
        k.ident_f = sb(st, nc, "ident_fs", [128, 128], F32)
        k.esel = sb(st, nc, "esel_s", [8, 8, 128], F32)
        k.rt = sb(st, nc, "rt_s", [128, 2, 8, 8], F32)
        k.epsb = sb(st, nc, "epsb", [128, 1], F32)
        P.op('dve', lambda e: e.memset(k.epsb, EPS), writes=['epsb'])
        k.ones_b = sb(st, nc, "ones_b", [128, 128], BF16)
        P.op('dve', lambda e: e.memset(k.ones_b, 1.0), writes=['ones_b'])
        k.oneb = sb(st, nc, "oneb", [128, 1], F32)
        P.op('dve', lambda e: e.memset(k.oneb, 1.0), writes=['oneb'])
        k.lru_cw = sb(st, nc, "lru_cw_s", [128, 2, 4, 10], F32)
        k.lru_cb = sb(st, nc, "lru_cb_s", [128, 2, 10], F32)
        k.lru_bg = sb(st, nc, "lru_bg_s", [128, 2, 2, 2, 10], F32)
        k.lru_lam = sb(st, nc, "lru_lam_s", [128, 2, 2, 10], F32)
        for dst, src in ((k.lru_cw, k.lru_cw_in), (k.lru_cb, k.lru_cb_in), (k.lru_bg, k.lru_bg_in), (k.lru_lam, k.lru_lam_in)):
            P.op('sp', lambda e, dst=dst, src=src: e.dma_start(out=dst, in_=src), writes=['lrup'], dma=True)
        k.ret_ld = sb(st, nc, "ret_ld_s", [128, 8], F32)
        k.ret_gain = sb(st, nc, "ret_gain_s", [128, 16], F32)
        for dst, src in ((k.ret_ld, k.ret_ld_in), (k.ret_gain, k.ret_gain_in)):
            P.op('sp', lambda e, dst=dst, src=src: e.dma_start(out=dst, in_=src), writes=['retp'], dma=True)
        k.swa_sink = sb(st, nc, "swa_sink_s", [64, 16], F32)
        P.op('sp', lambda e: e.dma_start(out=k.swa_sink, in_=k.swa_sink_in), writes=['swap'], dma=True)
        for dst, src, key in ((k.adab_s, k.adab, 'adab'), (k.ng_s, k.ng, 'ng'), (k.fg_s, k.fg, 'fg'),
                              (k.ones_f, k.ones_in, 'ones'), (k.ident_f, k.ident_in, 'ident'),
                              (k.esel, k.esel_in, 'esel'), (k.rt, k.router, 'rt')):
            P.op('sp', lambda e, dst=dst, src=src: e.dma_start(out=dst, in_=src), writes=[key], dma=True)

        if 'init' in parts:
            P.op('sp', lambda e: e.dma_start(out=k.X, in_=k.xT), writes=['X'], dma=True)
        k.precast_done = set()
        k.cond_done = False
        k.mods_split = ('mods' in parts and 'mix0' in parts)
        k.fuse_final = ('final' in parts and 'ffn3' in parts and SPARSE_MOE and not debug)
        k.pc_queue = []
        if 'mods' in parts:
            stage_mods(k, layers=(0,) if k.mods_split else (0, 1, 2, 3))
        for i in range(4):
            if f'ffn{i}' in parts:
                if SPARSE_MOE and i % 2 == 1:
                    for e8 in range(8):
                        k.pc_queue.append(lambda pe=(i // 2) * 8 + e8: precast_moe(k, pe))
                else:
                    for e8 in (range(8) if i % 2 == 1 else range(1)):
                        k.pc_queue.append(lambda p=ffn_pass_index(i, e8): precast(k, p))
            if f'mix{i}' in parts:
                if i % 3 == 0:
                    stage_lru(k, i)
                elif i % 3 == 1:
                    stage_ret(k, i)
                else:
                    stage_swa(k, i)
            if f'ffn{i}' in parts:
                if SPARSE_MOE and i % 2 == 1:
                    stage_moe_sparse(k, i)
                else:
                    drain_precast(k, 1000)
                    stage_ffn(k, i)
        if 'final' in parts and not k.fuse_final:
            stage_final(k)
        P.barrier()
        if debug:
            P.op('sp', lambda e: e.dma_start(out=k.out, in_=k.X), dma=True)
        P.finish()
        k.n_inst = P.n_inst
    return nc, k


def psum(k):
    b = k.ps_n % 8
    k.ps_n += 1
    return b, k.PS[b]


def stage_mods(k, layers=(0, 1, 2, 3), final_barrier=True, outer=None, cbw=512):
    nc, P = k.nc, k.P
    with ExitStack() as own:
        st = outer if outer is not None else own
        wa = [sb(st, nc, f"wa{j}", [128, 8, cbw], F32) for j in range(2)]
        mrow = [sb(st, nc, f"mrow{j}", [2, cbw], F32) for j in range(2)]
        nj = cbw // 128
        if not k.cond_done:
            ccs = sb(st, nc, "ccs", [128, 8, 2], F32)
            P.op('sp', lambda e: e.dma_start(out=ccs, in_=k.cc), writes=['ccs'], dma=True)
            P.op('act', lambda e: e.activation(out=k.cond, in_=ccs, func=AF.Silu), reads=['ccs'], writes=['cond'])
            k.cond_done = True
        n = 0
        for i in layers:
            wsrc = k.ada_w[i].rearrange("(kc p) j -> p kc j", p=128)
            for cb in range(6144 // cbw):
                buf = wa[n % 2]
                key = ('wa', n % 2)
                mr = mrow[n % 2]
                kmr = ('mrow', n % 2)
                P.op('sp', lambda e: e.dma_start(out=buf, in_=wsrc[:, :, cb * cbw:(cb + 1) * cbw]), writes=[key], dma=True)
                b, ps = psum(k)
                for kc in range(8):
                    P.op('pe', lambda e: e.matmul(ps[0:2, 0:cbw], lhsT=k.cond[:, kc, :], rhs=buf[:, kc, :], start=(kc == 0), stop=(kc == 7)),
                         reads=[key, 'cond'], writes=[('ps', b)])
                P.op('dve', lambda e: e.tensor_copy(out=mr, in_=ps[0:2, 0:cbw]), reads=[('ps', b)], writes=[kmr])
                b2, ps2 = psum(k)
                for jj in range(nj):
                    P.op('pe', lambda e: e.transpose(out=ps2[:, jj * 2:jj * 2 + 2], in_=mr[0:2, jj * 128:(jj + 1) * 128], identity=k.ident_f[0:2, 0:2]),
                         reads=[kmr, 'ident'], writes=[('ps', b2)])
                P.op('dve', lambda e: e.tensor_tensor(out=k.mods[:, i, cb * nj:(cb + 1) * nj, :], in0=ps2[:, 0:2 * nj].rearrange("p (j s) -> p j s", s=2),
                                                      in1=k.adab_s[:, i, cb * nj:(cb + 1) * nj].unsqueeze(2).to_broadcast([128, nj, 2]), op=ALU.add),
                     reads=[('ps', b2), 'adab'], writes=['mods'])
                n += 1
            for j in range(2):
                for s in range(2):
                    m = (1 + 3 * j) * 8
                    P.op('dve', lambda e: e.tensor_scalar(out=k.acoef[:, i, j, :, s], in0=k.mods[:, i, m:m + 8, s],
                                                          scalar1=1.0, scalar2=None, op0=ALU.add),
                         reads=['mods'], writes=['acoef'])
                    P.op('dve', lambda e: e.tensor_tensor(out=k.acoef[:, i, j, :, s], in0=k.acoef[:, i, j, :, s],
                                                          in1=k.ng_s[:, i, j, :], op=ALU.mult),
                         reads=['acoef', 'ng'], writes=['acoef'])
    if final_barrier:
        P.barrier()


def norm_tile(k, xt, kx, n, i, j, s, hb, kh, sq, rs, tmps, hf=None, khf=None):
    P = k.P
    sqb = sq.bitcast(BF16)[:, :, 0:512]
    P.op('act', lambda e: e.activation(out=sqb[:, :, :n], in_=xt[:, :, :n], func=AF.Square), reads=[kx], writes=['sq'])
    b, ps = psum(k)
    for kc in range(8):
        P.op('pe', lambda e: e.matmul(ps[:, :n], lhsT=k.ones_b[:, :], rhs=sqb[:, kc, :n], start=(kc == 0), stop=(kc == 7)),
             reads=['sq', 'ones_b'], writes=[('ps', b)])
    P.op('act', lambda e: e.activation(out=rs[:, :n], in_=ps[:, :n], func=AF.Ln, scale=1.0 / D, bias=k.epsb[:, 0:1]),
         reads=[('ps', b)], writes=['rs'])
    P.op('act', lambda e: e.activation(out=rs[:, :n], in_=rs[:, :n], func=AF.Exp, scale=-0.5), reads=['rs'], writes=['rs'])
    for kc in range(8):
        tmp = tmps[kc % 2]
        kt = ('ntmp', kc % 2)
        P.op('dve', lambda e: e.scalar_tensor_tensor(out=tmp[:, :n], in0=xt[:, kc, :n], scalar=k.acoef[:, i, j, kc, s:s + 1],
                                                     in1=rs[:, :n], op0=ALU.mult, op1=ALU.mult),
             reads=[kx, 'rs', 'acoef'], writes=[kt])
        sh = k.mods[:, i, (3 * j) * 8 + kc, s:s + 1]
        if hf is None:
            P.op('act', lambda e: e.activation(out=hb[:, kc, :n], in_=tmp[:, :n], func=AF.Identity, bias=sh, scale=1.0),
                 reads=[kt, 'mods'], writes=[kh])
        else:
            P.op('act', lambda e: e.activation(out=hf[:, kc, :n], in_=tmp[:, :n], func=AF.Identity, bias=sh, scale=1.0),
                 reads=[kt, 'mods'], writes=[khf])
    if hf is not None:
        P.op('dve', lambda e: e.tensor_copy(out=hb[:, :, :n], in_=hf[:, :, :n]), reads=[khf], writes=[kh])


def ffn_pass_index(i, e):
    return {0: 0, 1: 1 + e, 2: 9, 3: 10 + e}[i]


def precast(k, p):
    if p in k.precast_done:
        return
    k.precast_done.add(p)
    P = k.P
    if p == 0:
        gu, dn = k.ffn_w_gu[0], k.ffn_w_down[0]
    elif p == 9:
        gu, dn = k.ffn_w_gu[1], k.ffn_w_down[1]
    elif p < 9:
        gu, dn = k.moe_w_gu[0, p - 1], k.moe_w_down[0, p - 1]
    else:
        gu, dn = k.moe_w_gu[1, p - 10], k.moe_w_down[1, p - 10]
    P.op('pool', lambda e: e.dma_start(out=k.WGU[p].rearrange("r (a b) -> (r a) b", b=1024),
                                       in_=gu.rearrange("r (a b) -> (r a) b", b=1024)),
         writes=[('wgus', p)], dma=True)
    P.op('pool', lambda e: e.dma_start(out=k.WD[p], in_=dn), writes=[('wds', p)], dma=True)


def stage_ffn(k, i):
    nc, P = k.nc, k.P
    moe = (i % 2 == 1)
    last = (i == 3)
    f = i // 2
    experts = list(range(8)) if moe else [0]
    tiles = TILES[1:] if last else TILES
    precast(k, ffn_pass_index(i, 0))
    with ExitStack() as st:
        wgu = [sb(st, nc, f"wgu{j}", [128, 8, 2, 512], BF16) for j in range(2)]
        wd = [sb(st, nc, f"wd{j}", [128, 28, 256], BF16) for j in range(2)]
        nbuf = 1 if moe else 2
        Hs = [sb(st, nc, f"Hb{q}", [128, 8, 512], BF16) for q in range(nbuf)]
        xts = [sb(st, nc, f"xt{q}", [128, 8, 512], F32) for q in range(nbuf)]
        act = sb(st, nc, "actb", [128, 28, 512], BF16)
        sq = sb(st, nc, "sq", [128, 8, 512], F32)
        rs = sb(st, nc, "rs", [128, 512], F32)
        tmps = [sb(st, nc, f"ntmp{j}", [128, 512], F32) for j in range(2)]
        sg = [sb(st, nc, f"sg{j}", [128, 512], BF16) for j in range(2)]
        if moe:
            hf = sb(st, nc, "hf", [128, 8, 512], F32)
            yacc = sb(st, nc, "yacc", [128, 8, 512], F32)
            cmul = [sb(st, nc, f"cmul{j}", [128, 512], F32) for j in range(2)]
            combT = sb(st, nc, "combT", [8, 512], F32)
            rsm = sb(st, nc, "rsm", [128, 64], F32)

        seq_gu = [(ti, e, g) for ti in range(len(tiles)) for e in experts for g in range(7)]
        seq_wd = [(ti, e, q) for ti in range(len(tiles)) for e in experts for q in range(4)]
        st_gu = {'n': 0}
        st_wd = {'n': 0}

        def ensure_gu(upto):
            while st_gu['n'] <= upto and st_gu['n'] < len(seq_gu):
                a = st_gu['n']
                ti, e, g = seq_gu[a]
                p = ffn_pass_index(i, e)
                if ti == 0:
                    precast(k, p)
                buf = wgu[a % 2]
                for hh in range(2):
                    src = k.WGU[p].rearrange("(kc q) c -> q kc c", q=128)[:, :, hh * DFF + g * 512:hh * DFF + (g + 1) * 512]
                    P.op('sp', lambda e_: e_.dma_start(out=buf[:, :, hh, :], in_=src), reads=[('wgus', p)],
                         writes=[('wgu', a % 2, hh)], dma=True)
                st_gu['n'] += 1

        def ensure_wd(upto):
            while st_wd['n'] <= upto and st_wd['n'] < len(seq_wd):
                a = st_wd['n']
                ti, e, q = seq_wd[a]
                p = ffn_pass_index(i, e)
                src = k.WD[p].rearrange("(fc q) d -> q fc d", q=128)[:, :, q * 256:(q + 1) * 256]
                buf = wd[a % 2]
                P.op('sp', lambda e_: e_.dma_start(out=buf, in_=src), reads=[('wds', p)], writes=[('wd', a % 2)], dma=True)
                st_wd['n'] += 1

        a_gu = 0
        a_wd = 0
        nsg = 0
        def prep(ti):
            c0, n, s = tiles[ti]
            xt, H = xts[ti % nbuf], Hs[ti % nbuf]
            kxt, kH = ('xt', ti % nbuf), ('H', ti % nbuf)
            P.op('sp', lambda e: e.dma_start(out=xt[:, :, :n], in_=k.X.rearrange("(kc q) t -> q kc t", q=128)[:, :, c0:c0 + n]),
                 reads=['X', ('Xt', c0)], writes=[kxt], dma=True)
            if moe:
                norm_tile(k, xt, kxt, n, i, 1, s, H, kH, sq, rs, tmps, hf=hf, khf='hf')
                route_tile(k, f, n, hf, combT, rsm)
            else:
                norm_tile(k, xt, kxt, n, i, 1, s, H, kH, sq, rs, tmps)

        ensure_gu(0)
        if not moe:
            prep(0)
        for ti, (c0, n, s) in enumerate(tiles):
            xt, H = xts[ti % nbuf], Hs[ti % nbuf]
            kxt, kH = ('xt', ti % nbuf), ('H', ti % nbuf)
            if moe:
                prep(ti)
            elif ti + 1 < len(tiles):
                prep(ti + 1)
            ensure_gu(a_gu)
            for e in experts:
                if moe:
                    cm = cmul[e % 2]
                    kcm = ('cmul', e % 2)
                    b, ps = psum(k)
                    P.op('pe', lambda e_: e_.matmul(ps[:, :n], lhsT=k.esel[0:8, e, :], rhs=combT[0:8, :n], start=True, stop=True),
                         reads=['esel', 'combT'], writes=[('ps', b)])
                    P.op('act', lambda e_: e_.activation(out=cm[:, :n], in_=ps[:, :n], func=AF.Copy),
                         reads=[('ps', b)], writes=[kcm])
                ensure_wd(a_wd)
                for g in range(7):
                    ensure_gu(a_gu + 1)
                    buf = wgu[a_gu % 2]
                    kb0 = ('wgu', a_gu % 2, 0)
                    kb1 = ('wgu', a_gu % 2, 1)
                    for j in range(4):
                        fch = g * 4 + j
                        bg, psg = psum(k)
                        for kc in range(8):
                            P.op('pe', lambda e_: e_.matmul(psg[:, :n], lhsT=buf[:, kc, 0, j * 128:(j + 1) * 128], rhs=H[:, kc, :n],
                                                            start=(kc == 0), stop=(kc == 7)),
                                 reads=[kb0, kH], writes=[('ps', bg)])
                        bu, psu = psum(k)
                        for kc in range(8):
                            P.op('pe', lambda e_: e_.matmul(psu[:, :n], lhsT=buf[:, kc, 1, j * 128:(j + 1) * 128], rhs=H[:, kc, :n],
                                                            start=(kc == 0), stop=(kc == 7)),
                                 reads=[kb1, kH], writes=[('ps', bu)])
                        sgt = sg[nsg % 2]
                        ksg = ('sg', nsg % 2)
                        nsg += 1
                        P.op('act', lambda e_: e_.activation(out=sgt[:, :n], in_=psg[:, :n], func=AF.Silu),
                             reads=[('ps', bg)], writes=[ksg])
                        P.op('dve', lambda e_: e_.tensor_tensor(out=act[:, fch, :n], in0=sgt[:, :n], in1=psu[:, :n], op=ALU.mult),
                             reads=[ksg, ('ps', bu)], writes=[('act', fch)])
                    a_gu += 1
                ensure_gu(a_gu)
                for q in range(4):
                    ensure_wd(a_wd + 1)
                    buf = wd[a_wd % 2]
                    kb = ('wd', a_wd % 2)
                    for dj in range(2):
                        dc = q * 2 + dj
                        b, ps = psum(k)
                        for fc in range(28):
                            P.op('pe', lambda e_: e_.matmul(ps[:, :n], lhsT=buf[:, fc, dj * 128:(dj + 1) * 128], rhs=act[:, fc, :n],
                                                            start=(fc == 0), stop=(fc == 27)),
                                 reads=[kb, ('act', fc)], writes=[('ps', b)])
                        gate = k.mods[:, i, 5 * 8 + dc, s:s + 1]
                        if not moe:
                            P.op('dve', lambda e_: e_.scalar_tensor_tensor(out=xt[:, dc, :n], in0=ps[:, :n], scalar=gate,
                                                                           in1=xt[:, dc, :n], op0=ALU.mult, op1=ALU.add),
                                 reads=[('ps', b), kxt, 'mods'], writes=[kxt])
                        else:
                            if e == 0:
                                P.op('dve', lambda e_: e_.tensor_tensor(out=yacc[:, dc, :n], in0=ps[:, :n], in1=cm[:, :n], op=ALU.mult),
                                     reads=[('ps', b), kcm], writes=[('yacc', dc)])
                            else:
                                tmp = tmps[dc % 2]
                                kt = ('ntmp', dc % 2)
                                P.op('dve', lambda e_: e_.tensor_tensor(out=tmp[:, :n], in0=ps[:, :n], in1=cm[:, :n], op=ALU.mult),
                                     reads=[('ps', b), kcm], writes=[kt])
                                P.op('dve', lambda e_: e_.tensor_tensor(out=yacc[:, dc, :n], in0=yacc[:, dc, :n], in1=tmp[:, :n], op=ALU.add),
                                     reads=[kt, ('yacc', dc)], writes=[('yacc', dc)])
                            if e == experts[-1]:
                                P.op('dve', lambda e_: e_.scalar_tensor_tensor(out=xt[:, dc, :n], in0=yacc[:, dc, :n], scalar=gate,
                                                                               in1=xt[:, dc, :n], op0=ALU.mult, op1=ALU.add),
                                     reads=[('yacc', dc), kxt, 'mods'], writes=[kxt])
                    a_wd += 1
            P.op('sp', lambda e: e.dma_start(out=k.X.rearrange("(kc q) t -> q kc t", q=128)[:, :, c0:c0 + n], in_=xt[:, :, :n]),
                 reads=[kxt], writes=[('Xt', c0)], dma=True)
    P.barrier()


def route_tile(k, f, n, hf, combT, rsm):
    P = k.P
    for bi in range(n // 128):
        b, ps = psum(k)
        for kc in range(8):
            P.op('pe', lambda e: e.matmul(ps[:, 0:8], lhsT=hf[:, kc, bi * 128:(bi + 1) * 128], rhs=k.rt[:, f, kc, :],
                                          start=(kc == 0), stop=(kc == 7)),
                 reads=['hf', 'rt'], writes=[('ps', b)])
        lg, eq1, lg2, eq2, comb = (rsm[:, 8 * j:8 * j + 8] for j in range(5))
        m1, m2, dd, w1, w2 = (rsm[:, 40 + j:41 + j] for j in range(5))
        R = ['rsm']
        P.op('dve', lambda e: e.tensor_copy(out=lg, in_=ps[:, 0:8]), reads=[('ps', b)], writes=R)
        P.op('dve', lambda e: e.tensor_reduce(out=m1, in_=lg, axis=AX.X, op=ALU.max), reads=R, writes=R)
        P.op('dve', lambda e: e.tensor_scalar(out=eq1, in0=lg, scalar1=m1, scalar2=None, op0=ALU.is_equal), reads=R, writes=R)
        P.op('dve', lambda e: e.scalar_tensor_tensor(out=lg2, in0=eq1, scalar=-1e30, in1=lg, op0=ALU.mult, op1=ALU.add), reads=R, writes=R)
        P.op('dve', lambda e: e.tensor_reduce(out=m2, in_=lg2, axis=AX.X, op=ALU.max), reads=R, writes=R)
        P.op('dve', lambda e: e.tensor_scalar(out=eq2, in0=lg2, scalar1=m2, scalar2=None, op0=ALU.is_equal), reads=R, writes=R)
        P.op('dve', lambda e: e.tensor_tensor(out=dd, in0=m2, in1=m1, op=ALU.subtract), reads=R, writes=R)
        P.op('act', lambda e: e.activation(out=w1, in_=dd, func=AF.Sigmoid, scale=-1.0), reads=R, writes=R)
        P.op('act', lambda e: e.activation(out=w2, in_=dd, func=AF.Sigmoid, scale=1.0), reads=R, writes=R)
        P.op('dve', lambda e: e.tensor_scalar(out=comb, in0=eq1, scalar1=w1, scalar2=None, op0=ALU.mult), reads=R, writes=R)
        P.op('dve', lambda e: e.scalar_tensor_tensor(out=comb, in0=eq2, scalar=w2, in1=comb, op0=ALU.mult, op1=ALU.add), reads=R, writes=R)
        b2, ps2 = psum(k)
        P.op('pe', lambda e: e.transpose(out=ps2[0:8, 0:128], in_=comb, identity=k.ident_f[:, :]),
             reads=R + ['ident'], writes=[('ps', b2)])
        P.op('act', lambda e: e.activation(out=combT[0:8, bi * 128:(bi + 1) * 128], in_=ps2[0:8, 0:128], func=AF.Copy),
             reads=[('ps', b2)], writes=['combT'])


def _consts():
    ones = np.ones((128, 128), np.float32)
    ident = np.eye(128, dtype=np.float32)
    esel = np.zeros((8, 8, 128), np.float32)
    for e in range(8):
        esel[e, e, :] = 1.0
    return ones, ident, esel


def _ret_consts():
    f32 = np.float32
    theta = (f32(10000.0) ** (-np.arange(128, dtype=f32) / f32(128))).astype(f32)
    ang = (np.arange(SEQ, dtype=f32)[:, None] * theta[None, :]).astype(f32)
    cos, sin = np.cos(ang).astype(f32), np.sin(ang).astype(f32)
    p = np.arange(128, dtype=f32)[:, None]
    fr = np.arange(128, dtype=f32)[None, :]
    tab = np.zeros((128, 771), f32)
    tab[:, 0:128] = np.maximum(fr - p, 0)
    tab[:, 128:256] = np.maximum(p - fr, 0)
    tab[:, 256:384] = (fr >= p)
    tab[:, 384:512] = (p >= fr)
    tab[:, 512:640] = fr + 1
    tab[:, 640:768] = 128 - fr
    tab[:, 768] = 127 - p[:, 0]
    tab[:, 769] = p[:, 0]
    tab[:, 770] = 128
    return (np.ascontiguousarray(cos.T), np.ascontiguousarray(sin.T), cos, sin, tab)


def _swa_consts(w_in):
    f32 = np.float32
    qk = w_in[:, :1280].reshape(D, 20, 2, 32)
    sw = qk[:, :, ::-1, :].reshape(D, 1280)
    w_ext = np.ascontiguousarray(np.concatenate([w_in, sw], axis=1))
    rows = SEQ // 64
    row = np.repeat(np.arange(rows, dtype=f32), 64)
    colp = np.tile(np.arange(64, dtype=f32), rows)
    freq = (f32(10000.0) ** (-np.arange(16, dtype=f32) / f32(16))).astype(f32)
    ang = np.concatenate([row[:, None] * freq, colp[:, None] * freq], axis=-1).astype(f32)
    cos, sin = np.cos(ang).astype(f32), np.sin(ang).astype(f32)
    cos_full = np.concatenate([cos, cos], axis=1).T
    sin_signed = np.concatenate([-sin, sin], axis=1).T
    return w_ext, np.ascontiguousarray(np.tile(cos_full, (2, 1))), np.ascontiguousarray(np.tile(sin_signed, (2, 1)))


def make_in_maps(inp, cores):
    ones, ident, esel = _consts()
    adab = np.ascontiguousarray(inp['ada_b'].reshape(4, 48, 128).transpose(2, 0, 1))
    ng = np.ascontiguousarray(inp['norm_g'].reshape(4, 2, 8, 128).transpose(3, 0, 1, 2))
    fg = col(inp['final_g'], 8)
    router = np.ascontiguousarray(inp['moe_router'].reshape(2, 8, 128, 8).transpose(2, 0, 1, 3))
    lru_wg = np.ascontiguousarray(inp['lru_w_gate'].transpose(0, 4, 1, 2, 3, 5).reshape(2, 128, 40, 128))
    lru_cw = np.ascontiguousarray(inp['lru_conv_w'].reshape(2, 4, 10, 128).transpose(3, 0, 1, 2))
    lru_cb = np.ascontiguousarray(inp['lru_conv_b'].reshape(2, 10, 128).transpose(2, 0, 1))
    lru_bg = np.ascontiguousarray(inp['lru_b_gate'].reshape(2, 2, 2, 10, 128).transpose(4, 0, 1, 2, 3))
    lru_lam = np.ascontiguousarray(inp['lru_lambda'].reshape(2, 2, 10, 128).transpose(3, 0, 1, 2))
    ret_ld = np.ascontiguousarray(np.broadcast_to(inp['ret_log_decay'].reshape(1, 8), (128, 8))).astype(np.float32)
    ret_gain = col(inp['ret_gn_gain'][0], 16)
    ret_cosT, ret_sinT, ret_costok, ret_sintok, ret_tab = _ret_consts()
    swa_w_ext, swa_cos, swa_sin = _swa_consts(inp['swa_w_in'][0])
    swa_sink = np.ascontiguousarray(np.broadcast_to(inp['swa_sink'].reshape(1, 16), (64, 16))).astype(np.float32)
    adab_row = np.ascontiguousarray(np.broadcast_to(inp['ada_b'][:, None, :], (4, 2, 6144))).astype(np.float32)
    moe_tab = np.zeros((128, 161), np.float32)
    pp_ = np.arange(128, dtype=np.float32)
    moe_tab[:, 0:128] = (pp_[:, None] < pp_[None, :])
    moe_tab[:, 128:152] = np.arange(24, dtype=np.float32)[None, :]
    moe_tab[:, 152:159] = np.arange(7, dtype=np.float32)[None, :] * 128 + pp_[:, None]
    moe_tab[:, 159:161] = np.arange(2, dtype=np.float32)[None, :] * 128 + pp_[:, None]
    maps = []
    for b in cores:
        xT = np.ascontiguousarray(np.concatenate([inp['ctx'][b], inp['x'][b]], axis=0).T)
        cc = np.ascontiguousarray(np.stack([inp['c'][b], inp['c_ctx']], -1).reshape(8, 128, 2).transpose(1, 0, 2))
        maps.append(dict(adab_row=adab_row, moe_tab=moe_tab, swa_w_ext=swa_w_ext, swa_w_out=inp['swa_w_out'], swa_sink=swa_sink, swa_cos=swa_cos, swa_sin=swa_sin,
                         ret_w_in=inp['ret_w_in'], ret_w_out=inp['ret_w_out'], ret_ld=ret_ld, ret_gain=ret_gain, ret_cosT=ret_cosT,
                         ret_sinT=ret_sinT, ret_costok=ret_costok, ret_sintok=ret_sintok, ret_tab=ret_tab,
                         lru_w_in=inp['lru_w_in'], lru_wg=lru_wg, lru_w_out=inp['lru_w_out'], lru_cw=lru_cw, lru_cb=lru_cb,
                         lru_bg=lru_bg, lru_lam=lru_lam, xT=xT, cc=cc, ada_w=inp['ada_w'], adab=adab, ng=ng, fg=fg, ones_f=ones, ident_f=ident,
                         esel=esel, ffn_w_gu=inp['ffn_w_gu'], ffn_w_down=inp['ffn_w_down'], router=router,
                         moe_w_gu=inp['moe_w_gu'], moe_w_down=inp['moe_w_down']))
    return maps


def xrow(k):
    return k.X.rearrange("(kc q) t -> q kc t", q=128)


def load_x_tile(k, xt, c0, n, key='xt'):
    k.P.op('sp', lambda e: e.dma_start(out=xt[:, :, :n], in_=xrow(k)[:, :, c0:c0 + n]),
           reads=['X', ('Xt', c0)], writes=[key], dma=True)


def store_x_tile(k, xt, c0, n, key='xt'):
    k.P.op('sp', lambda e: e.dma_start(out=xrow(k)[:, :, c0:c0 + n], in_=xt[:, :, :n]),
           reads=[key], writes=[('Xt', c0)], dma=True)


def out_proj_residual(k, i, tiles, Zd, nz, kz, wo, zt_shape_k, lhs_fn):
    nc, P = k.nc, k.P
    with ExitStack() as st:
        xts = [sb(st, nc, f"xt_o{j}", [128, 8, 512], F32) for j in range(2)]
        zt = [sb(st, nc, f"zt_o{j}", [zt_shape_k, nz, 512], BF16) for j in range(2)]
        for ti, (c0, n, s) in enumerate(tiles):
            z = zt[ti % 2]
            kzt = ('zt', ti % 2)
            xt = xts[ti % 2]
            kxt = ('xt', ti % 2)
            P.op('sp', lambda e: e.dma_start(out=z[:, :, :n], in_=Zd[:, :, c0:c0 + n]), reads=[kz], writes=[kzt], dma=True)
            load_x_tile(k, xt, c0, n, key=kxt)
            for dc in range(8):
                b, ps = psum(k)
                for zc in range(nz):
                    P.op('pe', lambda e: e.matmul(ps[:, :n], lhsT=lhs_fn(zc, dc), rhs=z[:, zc, :n], start=(zc == 0), stop=(zc == nz - 1)),
                         reads=[kzt, 'wo'], writes=[('ps', b)])
                gate = k.mods[:, i, 2 * 8 + dc, s:s + 1]
                P.op('dve', lambda e: e.scalar_tensor_tensor(out=xt[:, dc, :n], in0=ps[:, :n], scalar=gate, in1=xt[:, dc, :n],
                                                             op0=ALU.mult, op1=ALU.add),
                     reads=[('ps', b), kxt, 'mods'], writes=[kxt])
            store_x_tile(k, xt, c0, n, key=kxt)
    P.barrier()


def stage_lru(k, i):
    nc, P = k.nc, k.P
    j = i // 3
    last = (i == 3)
    XR, YG, ZG = k.XR, k.YG, k.ZG
    with ExitStack() as st:
        win = sb(st, nc, "lru_win", [128, 8, 2560], BF16)
        for h2 in range(2):
            P.op('pool', lambda e: e.dma_start(out=win[:, :, h2 * 1280:(h2 + 1) * 1280],
                                               in_=k.lru_w_in[j].rearrange("(kc q) c -> q kc c", q=128)[:, :, h2 * 1280:(h2 + 1) * 1280]),
                 writes=['win'], dma=True)
        drain_precast(k, 3)
        Hs = [sb(st, nc, f"H1{q}", [128, 8, 512], BF16) for q in range(2)]
        xts = [sb(st, nc, f"xt1{q}", [128, 8, 512], F32) for q in range(2)]
        sq = sb(st, nc, "sq1", [128, 8, 512], F32)
        rs = sb(st, nc, "rs1", [128, 512], F32)
        tmps = [sb(st, nc, f"nt1{q}", [128, 512], F32) for q in range(2)]
        xrt = sb(st, nc, "xrt", [128, 10, 512], F32)
        ygt = sb(st, nc, "ygt", [128, 10, 512], BF16)
        g1 = [sb(st, nc, f"g1{q}", [128, 512], F32) for q in range(2)]
        g2 = [sb(st, nc, f"g2{q}", [128, 512], F32) for q in range(2)]
        ng = 0
        def prep(ti):
            c0, n, s = TILES[ti]
            q = ti % 2
            load_x_tile(k, xts[q], c0, n, key=('xt', q))
            norm_tile(k, xts[q], ('xt', q), n, i, 0, s, Hs[q], ('H', q), sq, rs, tmps)
        prep(0)
        for ti, (c0, n, s) in enumerate(TILES):
            H, kH = Hs[ti % 2], ('H', ti % 2)
            if ti + 1 < len(TILES):
                prep(ti + 1)
            for oc in range(20):
                b, ps = psum(k)
                for kc in range(8):
                    P.op('pe', lambda e: e.matmul(ps[:, :n], lhsT=win[:, kc, oc * 128:(oc + 1) * 128], rhs=H[:, kc, :n],
                                                  start=(kc == 0), stop=(kc == 7)),
                         reads=['win', kH], writes=[('ps', b)])
                if oc < 10:
                    P.op('act', lambda e: e.activation(out=ygt[:, oc, :n], in_=ps[:, :n], func=AF.Gelu_apprx_tanh),
                         reads=[('ps', b)], writes=['ygt'])
                else:
                    P.op('act', lambda e: e.activation(out=xrt[:, oc - 10, :n], in_=ps[:, :n], func=AF.Copy),
                         reads=[('ps', b)], writes=['xrt'])
            P.op('sp', lambda e: e.dma_start(out=YG.rearrange("(m q) t -> q m t", q=128)[:, :, c0:c0 + n], in_=ygt[:, :, :n]),
                 reads=['ygt'], writes=['YG'], dma=True)
            P.op('sp', lambda e: e.dma_start(out=XR.rearrange("(m q) t -> q m t", q=128)[:, :, c0:c0 + n], in_=xrt[:, :, :n]),
                 reads=['xrt'], writes=['XR'], dma=True)
    P.barrier()
    with ExitStack() as st:
        wg = sb(st, nc, "lru_wg", [128, 40, 128], BF16)
        for q in range(4):
            P.op('pool', lambda e: e.dma_start(out=wg[:, q * 10:(q + 1) * 10, :], in_=k.lru_wg[j][:, q * 10:(q + 1) * 10, :]),
                 writes=['wg'], dma=True)
        drain_precast(k, 1000)
        if i == 0 and k.mods_split:
            stage_mods(k, layers=(1, 2, 3), final_barrier=False, outer=st, cbw=256)
        B1 = sb(st, nc, "B1", [128, T], F32)
        B2 = sb(st, nc, "B2", [128, T], F32)
        B3 = sb(st, nc, "B3", [128, T], F32)
        B4 = sb(st, nc, "B4", [128, T], F32)
        B5 = sb(st, nc, "B5", [128, T], F32)
        B6 = sb(st, nc, "B6", [128, T], F32)
        B7 = sb(st, nc, "B7", [128, T], F32)
        ub = sb(st, nc, "ub", [128, T], BF16)
        ygc = sb(st, nc, "ygc", [128, T], BF16)
        zc_ = sb(st, nc, "zc", [128, T], BF16)
        sp8 = sb(st, nc, "sp8", [128, 2, 10], F32)
        P.op('act', lambda e: e.activation(out=sp8, in_=k.lru_lam[:, j], func=AF.Exp, scale=-1.0), reads=['lrup'], writes=['sp8'])
        P.op('act', lambda e: e.activation(out=sp8, in_=sp8, func=AF.Ln, bias=k.oneb[:, 0:1], scale=1.0), reads=['sp8'], writes=['sp8'])
        P.op('dve', lambda e: e.tensor_scalar(out=sp8, in0=sp8, scalar1=-8.0, scalar2=None, op0=ALU.mult), reads=['sp8'], writes=['sp8'])
        segs = [(0, NCTX), (NCTX, T)]
        allk = lambda nm: [(nm, ti) for ti in range(len(TILES))]
        PCS = [(0, 1280, (0, 1, 2)), (1280, 2816, (3, 4, 5)), (2816, T, (6, 7, 8))]
        pk = lambda nm, tl: [(nm, ti) for ti in tl]
        for m in range(10):
            P.op('sp', lambda e: e.dma_start(out=B1, in_=XR[m * 128:(m + 1) * 128, :]), reads=['XR'], writes=allk('B1'), dma=True)
            P.op('sp', lambda e: e.dma_start(out=ygc, in_=YG[m * 128:(m + 1) * 128, :]), reads=['YG'], writes=['ygc'], dma=True)
            cw = lambda t: k.lru_cw[:, j, t, m:m + 1]
            for (p0, p1, tl) in PCS:
                P.op('dve', lambda e: e.tensor_scalar(out=B2[:, p0:p1], in0=B1[:, p0:p1], scalar1=cw(2), scalar2=k.lru_cb[:, j, m:m + 1],
                                                      op0=ALU.mult, op1=ALU.add),
                     reads=allk('B1') + ['lrup'], writes=pk('B2', tl))
                for o in (-2, -1, 1):
                    for (s0, s1) in segs:
                        a0 = max(s0 + max(0, -o), p0)
                        a1 = min(s1 - max(0, o), p1)
                        if a1 <= a0:
                            continue
                        P.op('dve', lambda e: e.scalar_tensor_tensor(out=B2[:, a0:a1], in0=B1[:, a0 + o:a1 + o], scalar=cw(o + 2),
                                                                     in1=B2[:, a0:a1], op0=ALU.mult, op1=ALU.add),
                             reads=allk('B1') + pk('B2', tl) + ['lrup'], writes=pk('B2', tl))
                P.op('act', lambda e: e.activation(out=ub[:, p0:p1], in_=B2[:, p0:p1], func=AF.Copy), reads=pk('B2', tl), writes=pk('ub', tl))
            for d in range(2):
                Ba, nA = (B3, 'B3') if d == 0 else (B7, 'B7')
                Bi, nI = (B1, 'B1') if d == 0 else (B6, 'B6')
                for ti, (c0, n, s) in enumerate(TILES):
                    b, ps = psum(k)
                    P.op('pe', lambda e: e.matmul(ps[:, :n], lhsT=wg[:, (d * 2 + 0) * 10 + m, :], rhs=ub[:, c0:c0 + n], start=True, stop=True),
                         reads=['wg', ('ub', ti)], writes=[('ps', b)])
                    P.op('act', lambda e: e.activation(out=Ba[:, c0:c0 + n], in_=ps[:, :n], func=AF.Sigmoid,
                                                       bias=k.lru_bg[:, j, d, 0, m:m + 1], scale=1.0),
                         reads=[('ps', b), 'lrup'], writes=[(nA, ti)])
                    b2, ps2 = psum(k)
                    P.op('pe', lambda e: e.matmul(ps2[:, :n], lhsT=wg[:, (d * 2 + 1) * 10 + m, :], rhs=ub[:, c0:c0 + n], start=True, stop=True),
                         reads=['wg', ('ub', ti)], writes=[('ps', b2)])
                    P.op('act', lambda e: e.activation(out=Bi[:, c0:c0 + n], in_=ps2[:, :n], func=AF.Sigmoid,
                                                       bias=k.lru_bg[:, j, d, 1, m:m + 1], scale=1.0),
                         reads=[('ps', b2), 'lrup'], writes=[(nI, ti)])
                for (p0, p1, tl) in PCS:
                    P.op('act', lambda e: e.activation(out=Ba[:, p0:p1], in_=Ba[:, p0:p1], func=AF.Exp, scale=sp8[:, d, m:m + 1]),
                         reads=pk(nA, tl) + ['sp8'], writes=pk(nA, tl))
                for (p0, p1, tl) in PCS:
                    P.op('act', lambda e: e.activation(out=B4[:, p0:p1], in_=Ba[:, p0:p1], func=AF.Square), reads=pk(nA, tl), writes=pk('B4', tl))
                for (p0, p1, tl) in PCS:
                    P.op('act', lambda e: e.activation(out=B4[:, p0:p1], in_=B4[:, p0:p1], func=AF.Sqrt, scale=-1.0, bias=k.oneb[:, 0:1]),
                         reads=pk('B4', tl), writes=pk('B4', tl))
                    P.op('dve', lambda e: e.tensor_tensor(out=Bi[:, p0:p1], in0=Bi[:, p0:p1], in1=B2[:, p0:p1], op=ALU.mult),
                         reads=pk(nI, tl) + pk('B2', tl), writes=pk(nI, tl))
                    P.op('dve', lambda e: e.tensor_tensor(out=Bi[:, p0:p1], in0=Bi[:, p0:p1], in1=B4[:, p0:p1], op=ALU.mult),
                         reads=pk(nI, tl) + pk('B4', tl), writes=pk(nI, tl))
                if d == 0:
                    P.op('dve', lambda e: e.tensor_tensor_scan(out=B5, data0=B3, data1=B1, initial=0.0, op0=ALU.mult, op1=ALU.add),
                         reads=allk('B3') + allk('B1'), writes=allk('B5'))
                else:
                    rv = lambda buf, a, b_: buf[:, a:b_][:, ::-1]
                    P.op('dve', lambda e: e.tensor_tensor_scan(out=rv(B4, 0, NCTX), data0=rv(B7, 0, NCTX), data1=rv(B6, 0, NCTX),
                                                               initial=0.0, op0=ALU.mult, op1=ALU.add),
                         reads=allk('B7') + allk('B6'), writes=allk('B4'))
                    P.op('dve', lambda e: e.tensor_tensor_scan(out=rv(B4, NCTX, T), data0=rv(B7, NCTX, T), data1=rv(B6, NCTX, T),
                                                               initial=B4[:, 0:1], op0=ALU.mult, op1=ALU.add),
                         reads=allk('B7') + allk('B6') + allk('B4'), writes=allk('B4'))
            for (p0, p1, tl) in PCS:
                P.op('dve', lambda e: e.tensor_tensor(out=B5[:, p0:p1], in0=B5[:, p0:p1], in1=B4[:, p0:p1], op=ALU.add),
                     reads=pk('B5', tl) + pk('B4', tl), writes=pk('B5', tl))
                P.op('dve', lambda e: e.tensor_tensor(out=zc_[:, p0:p1], in0=B5[:, p0:p1], in1=ygc[:, p0:p1], op=ALU.mult),
                     reads=pk('B5', tl) + ['ygc'], writes=pk('zc', tl))
            P.op('sp', lambda e: e.dma_start(out=ZG[m * 128:(m + 1) * 128, :], in_=zc_), reads=allk('zc'), writes=['ZG'], dma=True)
    P.barrier()
    with ExitStack() as st:
        wo = sb(st, nc, "lru_wo", [128, 10, 1024], BF16)
        P.op('pool', lambda e: e.dma_start(out=wo, in_=k.lru_w_out[j].rearrange("(m q) c -> q m c", q=128)), writes=['wo'], dma=True)
        out_proj_residual(k, i, TILES[1:] if last else TILES, ZG.rearrange("(m q) t -> q m t", q=128), 10, 'ZG', wo, 128,
                          lambda zc, dc: wo[:, zc, dc * 128:(dc + 1) * 128])


NCH = T // 128


def stage_ret(k, i):
    nc, P = k.nc, k.P
    QT, KT, KTOK, VTOK, GS, SBd, ZR = k.QT, k.KT, k.KTOK, k.VTOK, k.GS, k.SBd, k.ZR
    tab = k.ret_tab
    with ExitStack() as st:
        win = sb(st, nc, "ret_win", [128, 8, 6144], BF16)
        for q3 in range(3):
            P.op('pool', lambda e: e.dma_start(out=win[:, :, q3 * 2048:(q3 + 1) * 2048],
                                               in_=k.ret_w_in[0].rearrange("(kc q) c -> q kc c", q=128)[:, :, q3 * 2048:(q3 + 1) * 2048]),
                 writes=['win'], dma=True)
        drain_precast(k, 4)
        H = sb(st, nc, "H2", [128, 8, 512], BF16)
        xt = sb(st, nc, "xt2", [128, 8, 512], F32)
        sq = sb(st, nc, "sq2", [128, 8, 512], F32)
        rs = sb(st, nc, "rs2", [128, 512], F32)
        tmps = [sb(st, nc, f"nt2{q}", [128, 512], F32) for q in range(2)]
        qo = sb(st, nc, "qo", [128, 8, 512], BF16)
        cs = sb(st, nc, "cs", [128, 2, 512], F32)
        kt = sb(st, nc, "kt", [128, 1024], F32)
        kto = sb(st, nc, "kto", [128, 1024], BF16)
        vto = sb(st, nc, "vto", [128, 2048], BF16)
        cst = sb(st, nc, "cst", [128, 2, 128], F32)
        gso = sb(st, nc, "gso", [128, 4, 512], BF16)
        for ti, (c0, n, s) in enumerate(TILES):
            load_x_tile(k, xt, c0, n)
            norm_tile(k, xt, 'xt', n, i, 0, s, H, 'H', sq, rs, tmps)
            if s == 0:
                p0 = c0 - NCTX
                P.op('sp', lambda e: e.dma_start(out=cs[:, 0, :n], in_=k.ret_cosT[:, p0:p0 + n]), writes=['cs0'], dma=True)
                P.op('sp', lambda e: e.dma_start(out=cs[:, 1, :n], in_=k.ret_sinT[:, p0:p0 + n]), writes=['cs1'], dma=True)
            for (dst, kd_, off, scale) in ((QT, 'QT', 0, 1.0), (KT, 'KT', 1024, 0.0625)):
                for oc in range(8):
                    b, ps = psum(k)
                    for kc in range(8):
                        P.op('pe', lambda e: e.matmul(ps[:, :n], lhsT=win[:, kc, off + oc * 128:off + (oc + 1) * 128], rhs=H[:, kc, :n],
                                                      start=(kc == 0), stop=(kc == 7)),
                             reads=['win', 'H'], writes=[('ps', b)])
                    P.op('act', lambda e: e.activation(out=xt[:, oc, :n], in_=ps[:, :n], func=AF.Copy, scale=scale),
                         reads=[('ps', b)], writes=['xt'])
                if s == 0:
                    xv = xt.rearrange("p (h two) n -> p h two n", two=2)
                    qv = qo.rearrange("p (h two) n -> p h two n", two=2)
                    x1, x2 = xv[:, :, 0, :n], xv[:, :, 1, :n]
                    o1, o2 = qv[:, :, 0, :n], qv[:, :, 1, :n]
                    cosb = cs[:, 0, :n].unsqueeze(1).to_broadcast([128, 4, n])
                    sinb = cs[:, 1, :n].unsqueeze(1).to_broadcast([128, 4, n])
                    sv = sq.rearrange("p (two h) n -> p two h n", two=2)
                    ta, tb = sv[:, 0, :, :n], sv[:, 1, :, :n]
                    P.op('dve', lambda e: e.tensor_tensor(out=ta, in0=x1, in1=cosb, op=ALU.mult), reads=['xt', 'cs0', 'sq'], writes=['sq'])
                    P.op('dve', lambda e: e.tensor_tensor(out=tb, in0=x2, in1=sinb, op=ALU.mult), reads=['xt', 'cs1', 'sq'], writes=['sq'])
                    P.op('dve', lambda e: e.tensor_tensor(out=o1, in0=ta, in1=tb, op=ALU.subtract), reads=['sq'], writes=['qo'])
                    P.op('dve', lambda e: e.tensor_tensor(out=ta, in0=x2, in1=cosb, op=ALU.mult), reads=['xt', 'cs0', 'qo', 'sq'], writes=['sq'])
                    P.op('dve', lambda e: e.tensor_tensor(out=tb, in0=x1, in1=sinb, op=ALU.mult), reads=['xt', 'cs1', 'qo', 'sq'], writes=['sq'])
                    P.op('dve', lambda e: e.tensor_tensor(out=o2, in0=ta, in1=tb, op=ALU.add), reads=['sq'], writes=['qo'])
                else:
                    P.op('dve', lambda e: e.tensor_copy(out=qo[:, :, :n], in_=xt[:, :, :n]), reads=['xt'], writes=['qo'])
                P.op('sp', lambda e: e.dma_start(out=dst.rearrange("(oc q) t -> q oc t", q=128)[:, :, c0:c0 + n], in_=qo[:, :, :n]),
                     reads=['qo'], writes=[kd_], dma=True)
            for tb_ in range(n // 128):
                r0 = c0 + tb_ * 128
                if s == 0:
                    pp = r0 - NCTX
                    P.op('sp', lambda e: e.dma_start(out=cst[:, 0, :], in_=k.ret_costok[pp:pp + 128, :]), writes=['cst0'], dma=True)
                    P.op('sp', lambda e: e.dma_start(out=cst[:, 1, :], in_=k.ret_sintok[pp:pp + 128, :]), writes=['cst1'], dma=True)
                for half in range(2):
                    b, ps = psum(k)
                    for kc in range(8):
                        P.op('pe', lambda e: e.matmul(ps[:, :], lhsT=H[:, kc, tb_ * 128:(tb_ + 1) * 128],
                                                      rhs=win[:, kc, 1024 + half * 512:1024 + (half + 1) * 512], start=(kc == 0), stop=(kc == 7)),
                             reads=['win', 'H'], writes=[('ps', b)])
                    P.op('act', lambda e: e.activation(out=kt[:, half * 512:(half + 1) * 512], in_=ps[:, :], func=AF.Copy, scale=0.0625),
                         reads=[('ps', b)], writes=['kt'])
                if s == 0:
                    kv = kt.rearrange("p (h two f) -> p h two f", two=2, f=128)
                    ov = kto.rearrange("p (h two f) -> p h two f", two=2, f=128)
                    k1, k2 = kv[:, :, 0, :], kv[:, :, 1, :]
                    o1, o2 = ov[:, :, 0, :], ov[:, :, 1, :]
                    cosb = cst[:, 0, :].unsqueeze(1).to_broadcast([128, 4, 128])
                    sinb = cst[:, 1, :].unsqueeze(1).to_broadcast([128, 4, 128])
                    ta = tmps[0].rearrange("p (h f) -> p h f", f=128)
                    tb = tmps[1].rearrange("p (h f) -> p h f", f=128)
                    ka, kb = ('ntmp', 0), ('ntmp', 1)
                    P.op('dve', lambda e: e.tensor_tensor(out=ta, in0=k1, in1=cosb, op=ALU.mult), reads=['kt', 'cst0'], writes=[ka])
                    P.op('dve', lambda e: e.tensor_tensor(out=tb, in0=k2, in1=sinb, op=ALU.mult), reads=['kt', 'cst1'], writes=[kb])
                    P.op('dve', lambda e: e.tensor_tensor(out=o1, in0=ta, in1=tb, op=ALU.subtract), reads=[ka, kb], writes=['kto'])
                    P.op('dve', lambda e: e.tensor_tensor(out=ta, in0=k2, in1=cosb, op=ALU.mult), reads=['kt', 'cst0', 'kto'], writes=[ka])
                    P.op('dve', lambda e: e.tensor_tensor(out=tb, in0=k1, in1=sinb, op=ALU.mult), reads=['kt', 'cst1', 'kto'], writes=[kb])
                    P.op('dve', lambda e: e.tensor_tensor(out=o2, in0=ta, in1=tb, op=ALU.add), reads=[ka, kb], writes=['kto'])
                else:
                    P.op('dve', lambda e: e.tensor_copy(out=kto, in_=kt), reads=['kt'], writes=['kto'])
                P.op('sp', lambda e: e.dma_start(out=KTOK[r0:r0 + 128, :], in_=kto), reads=['kto'], writes=['KTOK'], dma=True)
                for q4 in range(4):
                    b, ps = psum(k)
                    for kc in range(8):
                        P.op('pe', lambda e: e.matmul(ps[:, :], lhsT=H[:, kc, tb_ * 128:(tb_ + 1) * 128],
                                                      rhs=win[:, kc, 2048 + q4 * 512:2048 + (q4 + 1) * 512], start=(kc == 0), stop=(kc == 7)),
                             reads=['win', 'H'], writes=[('ps', b)])
                    eng = 'act' if q4 % 2 == 0 else 'dve'
                    if eng == 'act':
                        P.op('act', lambda e: e.activation(out=vto[:, q4 * 512:(q4 + 1) * 512], in_=ps[:, :], func=AF.Copy),
                             reads=[('ps', b)], writes=['vto'])
                    else:
                        P.op('dve', lambda e: e.tensor_copy(out=vto[:, q4 * 512:(q4 + 1) * 512], in_=ps[:, :]),
                             reads=[('ps', b)], writes=['vto'])
                P.op('sp', lambda e: e.dma_start(out=VTOK[r0:r0 + 128, :], in_=vto), reads=['vto'], writes=['VTOK'], dma=True)
            for g4 in range(4):
                for gj in range(4):
                    oc = g4 * 4 + gj
                    b, ps = psum(k)
                    for kc in range(8):
                        P.op('pe', lambda e: e.matmul(ps[:, :n], lhsT=win[:, kc, 4096 + oc * 128:4096 + (oc + 1) * 128], rhs=H[:, kc, :n],
                                                      start=(kc == 0), stop=(kc == 7)),
                             reads=['win', 'H'], writes=[('ps', b)])
                    gt = tmps[gj % 2]
                    kg = ('ntmp', gj % 2)
                    P.op('act', lambda e: e.activation(out=gt[:, :n], in_=ps[:, :n], func=AF.Silu), reads=[('ps', b)], writes=[kg])
                    P.op('dve', lambda e: e.tensor_scalar(out=gso[:, gj, :n], in0=gt[:, :n], scalar1=k.ret_gain[:, oc:oc + 1], scalar2=None,
                                                          op0=ALU.mult), reads=[kg, 'retp'], writes=['gso'])
                P.op('sp', lambda e: e.dma_start(out=GS.rearrange("(oc q) t -> q oc t", q=128)[:, g4 * 4:(g4 + 1) * 4, c0:c0 + n],
                                                 in_=gso[:, :, :n]), reads=['gso'], writes=['GS'], dma=True)
    P.barrier()
    with ExitStack() as st:
        drain_precast(k, 1000)
        qT = sb(st, nc, "r_qT", [128, 2, T], BF16)
        kT = sb(st, nc, "r_kT", [128, 2, T], BF16)
        ktk = sb(st, nc, "r_ktk", [128, NCH, 256], BF16)
        vtk = sb(st, nc, "r_vtk", [128, NCH, 512], BF16)
        tb_s = sb(st, nc, "r_tab", [128, 6 * 128 + 3], F32)
        P.op('sp', lambda e: e.dma_start(out=tb_s, in_=tab), writes=['rtab'], dma=True)
        DP, DM, UP, LO, POS1, POSB = (tb_s[:, q * 128:(q + 1) * 128] for q in range(6))
        KPF, KPB, C128 = (tb_s[:, 768 + q:769 + q] for q in range(3))
        M = sb(st, nc, "r_M", [128, 128], F32)
        M2 = sb(st, nc, "r_M2", [128, 128], F32)
        QD = sb(st, nc, "r_QD", [128, 2, 128], F32)
        cv = sb(st, nc, "r_cv", [128, 4], F32)
        S32 = [sb(st, nc, f"r_S32{d}", [128, 2, 512], F32) for d in range(2)]
        Sbf = [sb(st, nc, f"r_Sbf{q}", [128, 2, 512], BF16) for q in range(2)]
        Sfb = sb(st, nc, "r_Sfb", [128, 2, 512], BF16)
        Sin = [sb(st, nc, f"r_Sin{q}", [128, 2, 512], BF16) for q in range(3)]
        gsc = [sb(st, nc, f"r_gsc{q}", [128, 4, 128], BF16) for q in range(4)]
        kd = [sb(st, nc, f"r_kd{q}", [128, 256], BF16) for q in range(2)]
        sm = [sb(st, nc, f"r_sm{q}", [128, 128], BF16) for q in range(2)]
        qs = [sb(st, nc, f"r_qs{q}", [128, 2, 2, 128], BF16) for q in range(2)]
        sqo2 = [sb(st, nc, f"r_sqo{q}", [128, 512], F32) for q in range(2)]
        rn2 = [sb(st, nc, f"r_rn{q}", [128, 128], F32) for q in range(2)]
        zt2 = [sb(st, nc, f"r_zt{q}", [128, 4, 128], F32) for q in range(2)]
        zo = [sb(st, nc, f"r_zo{q}", [128, 4, 128], BF16) for q in range(2)]
        nkd = 0
        for h in range(4):
            P.op('sp', lambda e: e.dma_start(out=ktk, in_=KTOK.rearrange("(c q) f -> q c f", q=128)[:, :, h * 256:(h + 1) * 256]),
                 reads=['KTOK'], writes=['ktk'], dma=True)
            P.op('sp', lambda e: e.dma_start(out=vtk, in_=VTOK.rearrange("(c q) f -> q c f", q=128)[:, :, h * 512:(h + 1) * 512]),
                 reads=['VTOK'], writes=['vtk'], dma=True)
            P.op('sp', lambda e: e.dma_start(out=qT, in_=QT.rearrange("(oc q) t -> q oc t", q=128)[:, 2 * h:2 * h + 2, :]),
                 reads=['QT'], writes=['qT'], dma=True)
            P.op('sp', lambda e: e.dma_start(out=kT, in_=KT.rearrange("(oc q) t -> q oc t", q=128)[:, 2 * h:2 * h + 2, :]),
                 reads=['KT'], writes=['kT'], dma=True)
            lgf = k.ret_ld[:, h:h + 1]
            lgb = k.ret_ld[:, 4 + h:5 + h]
            P.op('act', lambda e: e.activation(out=M, in_=DP, func=AF.Exp, scale=lgf), reads=['rtab', 'retp'], writes=['M'])
            P.op('dve', lambda e: e.tensor_tensor(out=M, in0=M, in1=UP, op=ALU.mult), reads=['M', 'rtab'], writes=['M'])
            P.op('act', lambda e: e.activation(out=M2, in_=DM, func=AF.Exp, scale=lgb), reads=['rtab', 'retp'], writes=['M2'])
            P.op('dve', lambda e: e.tensor_tensor(out=M2, in0=M2, in1=LO, op=ALU.mult), reads=['M2', 'rtab'], writes=['M2'])
            P.op('dve', lambda e: e.tensor_tensor(out=M, in0=M, in1=M2, op=ALU.add), reads=['M', 'M2'], writes=['M'])
            P.op('act', lambda e: e.activation(out=QD[:, 0, :], in_=POS1, func=AF.Exp, scale=lgf), reads=['rtab', 'retp'], writes=['QD'])
            P.op('act', lambda e: e.activation(out=QD[:, 1, :], in_=POSB, func=AF.Exp, scale=lgb), reads=['rtab', 'retp'], writes=['QD'])
            P.op('act', lambda e: e.activation(out=cv[:, 0:1], in_=KPF, func=AF.Exp, scale=lgf), reads=['rtab', 'retp'], writes=['cv'])
            P.op('act', lambda e: e.activation(out=cv[:, 1:2], in_=KPB, func=AF.Exp, scale=lgb), reads=['rtab', 'retp'], writes=['cv'])
            P.op('act', lambda e: e.activation(out=cv[:, 2:3], in_=C128, func=AF.Exp, scale=lgf), reads=['rtab', 'retp'], writes=['cv'])
            P.op('act', lambda e: e.activation(out=cv[:, 3:4], in_=C128, func=AF.Exp, scale=lgb), reads=['rtab', 'retp'], writes=['cv'])

            def state_update(d, cidx, S, kS, Sb_out, kSb, banks=None):
                nonlocal nkd
                kdt = kd[nkd % 2]
                kkd = ('kd', nkd % 2)
                nkd += 1
                P.op('dve', lambda e: e.tensor_scalar(out=kdt, in0=ktk[:, cidx, :], scalar1=cv[:, d:d + 1], scalar2=None, op0=ALU.mult),
                     reads=['ktk', 'cv'], writes=[kkd])
                for dch in range(2):
                    if banks is None:
                        b, ps = psum(k)
                    else:
                        b, ps = banks[dch], k.PS[banks[dch]]
                    P.op('pe', lambda e: e.matmul(ps[:, :], lhsT=kdt[:, dch * 128:(dch + 1) * 128], rhs=vtk[:, cidx, :], start=True, stop=True),
                         reads=[kkd, 'vtk'], writes=[('ps', b)])
                    P.op('dve', lambda e: e.scalar_tensor_tensor(out=S[:, dch, :], in0=S[:, dch, :], scalar=cv[:, 2 + d:3 + d], in1=ps[:, :],
                                                                 op0=ALU.mult, op1=ALU.add),
                         reads=[kS, ('ps', b), 'cv'], writes=[kS])
                P.op('act', lambda e: e.activation(out=Sb_out, in_=S, func=AF.Copy), reads=[kS], writes=[kSb])

            order_b = [1, 0] + list(range(NCH - 1, 1, -1))
            P.op('dve', lambda e: e.memset(S32[1], 0.0), writes=['S32b'])
            P.op('dve', lambda e: e.memset(Sbf[0], 0.0), writes=[('Sbf', 0)])
            for oi, cidx in enumerate(order_b):
                cur = Sbf[oi % 2]
                kcur = ('Sbf', oi % 2)
                P.op('sp', lambda e: e.dma_start(out=SBd[h, cidx].rearrange("(dch q) e -> q dch e", q=128), in_=cur),
                     reads=[kcur], writes=[('SBd', cidx)], dma=True)
                if oi + 1 < len(order_b):
                    state_update(1, cidx, S32[1], 'S32b', Sbf[(oi + 1) % 2], ('Sbf', (oi + 1) % 2))
            P.op('dve', lambda e: e.memset(S32[0], 0.0), writes=['S32f'])
            P.op('dve', lambda e: e.memset(Sfb, 0.0), writes=['Sfb'])

            def prefetch(cidx):
                b3, b4 = cidx % 3, cidx % 4
                P.op('sp', lambda e: e.dma_start(out=Sin[b3], in_=SBd[h, cidx].rearrange("(dch q) e -> q dch e", q=128)),
                     reads=[('SBd', cidx)], writes=[('Sin', b3)], dma=True)
                P.op('sp', lambda e: e.dma_start(out=gsc[b4], in_=GS.rearrange("(oc q) t -> q oc t", q=128)[:, 4 * h:4 * h + 4, cidx * 128:(cidx + 1) * 128]),
                     reads=['GS'], writes=[('gsc', b4)], dma=True)
            def fA(cidx):
                bq = cidx % 2
                cols = slice(cidx * 128, (cidx + 1) * 128)
                b = bq
                ps_s = k.PS[b]
                for dch in range(2):
                    P.op('pe', lambda e: e.matmul(ps_s[:, 0:128], lhsT=kT[:, dch, cols], rhs=qT[:, dch, cols], start=(dch == 0), stop=(dch == 1)),
                         reads=['kT', 'qT'], writes=[('ps', b)])
                P.op('dve', lambda e: e.tensor_tensor(out=sm[bq], in0=ps_s[:, 0:128], in1=M, op=ALU.mult), reads=[('ps', b), 'M'], writes=[('sm', bq)])
                for d in range(2):
                    P.op('dve', lambda e: e.tensor_tensor(out=qs[bq][:, d], in0=qT[:, :, cols], in1=QD[:, d, :].unsqueeze(1).to_broadcast([128, 2, 128]),
                                                          op=ALU.mult), reads=['qT', 'QD'], writes=[('qs', bq, d)])

            def fB(cidx):
                bq = cidx % 2
                bo = 2 + bq
                ps_o = k.PS[bo]
                smt, qst = sm[bq], qs[bq]
                for ech in range(4):
                    es = slice(ech * 128, (ech + 1) * 128)
                    P.op('pe', lambda e: e.matmul(ps_o[:, es], lhsT=vtk[:, cidx, es], rhs=smt, start=True, stop=False),
                         reads=['vtk', ('sm', bq)], writes=[('ps', bo)])
                    for dch in range(2):
                        P.op('pe', lambda e: e.matmul(ps_o[:, es], lhsT=Sfb[:, dch, es], rhs=qst[:, 0, dch, :], start=False, stop=False),
                             reads=['Sfb', ('qs', bq, 0)], writes=[('ps', bo)])
                    for dch in range(2):
                        P.op('pe', lambda e: e.matmul(ps_o[:, es], lhsT=Sin[cidx % 3][:, dch, es], rhs=qst[:, 1, dch, :], start=False, stop=(dch == 1)),
                             reads=[('Sin', cidx % 3), ('qs', bq, 1)], writes=[('ps', bo)])
                P.op('act', lambda e: e.activation(out=sqo2[bq], in_=ps_o, func=AF.Square), reads=[('ps', bo)], writes=[('sqo', bq)])

            def fC(cidx):
                bq = cidx % 2
                bo = 2 + bq
                ps_o = k.PS[bo]
                cols = slice(cidx * 128, (cidx + 1) * 128)
                bn = 4
                ps_n = k.PS[bn]
                sqo, rn, zt = sqo2[bq], rn2[bq], zt2[bq]
                for ech in range(4):
                    P.op('pe', lambda e: e.matmul(ps_n[:, 0:128], lhsT=k.ones_f, rhs=sqo[:, ech * 128:(ech + 1) * 128], start=(ech == 0), stop=(ech == 3)),
                         reads=[('sqo', bq), 'ones'], writes=[('ps', bn)])
                P.op('act', lambda e: e.activation(out=rn, in_=ps_n[:, 0:128], func=AF.Ln, scale=1.0 / 512, bias=k.epsb[:, 0:1]),
                     reads=[('ps', bn)], writes=[('rn', bq)])
                P.op('act', lambda e: e.activation(out=rn, in_=rn, func=AF.Exp, scale=-0.5), reads=[('rn', bq)], writes=[('rn', bq)])
                P.op('dve', lambda e: e.tensor_tensor(out=zt, in0=ps_o.rearrange("p (c i) -> p c i", i=128),
                                                      in1=rn.unsqueeze(1).to_broadcast([128, 4, 128]), op=ALU.mult),
                     reads=[('ps', bo), ('rn', bq)], writes=[('zt', bq)])
                P.op('dve', lambda e: e.tensor_tensor(out=zo[bq], in0=zt, in1=gsc[cidx % 4], op=ALU.mult), reads=[('zt', bq), ('gsc', cidx % 4)], writes=[('zo', bq)])
                P.op('sp', lambda e: e.dma_start(out=ZR.rearrange("(oc q) t -> q oc t", q=128)[:, 4 * h:4 * h + 4, cols], in_=zo[bq]),
                     reads=[('zo', bq)], writes=['ZR'], dma=True)

            prefetch(0)
            prefetch(1)
            fA(0)
            for cidx in range(NCH):
                if cidx + 2 < NCH:
                    prefetch(cidx + 2)
                if cidx + 1 < NCH:
                    fA(cidx + 1)
                fB(cidx)
                if cidx + 1 < NCH:
                    state_update(0, cidx, S32[0], 'S32f', Sfb, 'Sfb', banks=(5, 6))
                if cidx >= 1:
                    fC(cidx - 1)
            fC(NCH - 1)
    P.barrier()
    with ExitStack() as st:
        wo = sb(st, nc, "ret_wo", [128, 16, 1024], BF16)
        P.op('pool', lambda e: e.dma_start(out=wo, in_=k.ret_w_out[0].rearrange("(m q) c -> q m c", q=128)), writes=['wo'], dma=True)
        out_proj_residual(k, i, TILES, ZR.rearrange("(m q) t -> q m t", q=128), 16, 'ZR', wo, 128,
                          lambda zc, dc: wo[:, zc, dc * 128:(dc + 1) * 128])


def stage_swa(k, i):
    nc, P = k.nc, k.P
    QS, KS, VS, OS = k.QS, k.KS, k.VS, k.OS
    with ExitStack() as st:
        win = sb(st, nc, "swa_win", [128, 8, 2816], BF16)
        for (a, b_) in ((0, 1408), (1408, 2816)):
            P.op('pool', lambda e: e.dma_start(out=win[:, :, a:b_], in_=k.swa_w_ext.rearrange("(kc q) c -> q kc c", q=128)[:, :, a:b_]),
                 writes=['win'], dma=True)
        drain_precast(k, 1000)
        Hs = [sb(st, nc, f"H3{q}", [128, 8, 512], BF16) for q in range(2)]
        xts = [sb(st, nc, f"xt3{q}", [128, 8, 512], F32) for q in range(2)]
        sq = sb(st, nc, "sq3", [128, 8, 512], F32)
        rs = sb(st, nc, "rs3", [128, 512], F32)
        tmps = [sb(st, nc, f"nt3{q}", [128, 512], F32) for q in range(2)]
        qo = sb(st, nc, "s_qo", [128, 10, 512], BF16)
        cs = sb(st, nc, "s_cs", [128, 2, 512], F32)
        vto = [sb(st, nc, f"s_vto{q}", [128, 256], BF16) for q in range(2)]
        nt = 0
        def prep(ti):
            c0, n, s = TILES[ti]
            q = ti % 2
            load_x_tile(k, xts[q], c0, n, key=('xt', q))
            norm_tile(k, xts[q], ('xt', q), n, i, 0, s, Hs[q], ('H', q), sq, rs, tmps)
        prep(0)
        for ti, (c0, n, s) in enumerate(TILES):
            H, kH = Hs[ti % 2], ('H', ti % 2)
            if ti + 1 < len(TILES):
                prep(ti + 1)
            if s == 0:
                p0 = c0 - NCTX
                P.op('sp', lambda e: e.dma_start(out=cs[:, 0, :n], in_=k.swa_cos[:, p0:p0 + n]), writes=['cs0'], dma=True)
                P.op('sp', lambda e: e.dma_start(out=cs[:, 1, :n], in_=k.swa_sin[:, p0:p0 + n]), writes=['cs1'], dma=True)
            for hp in range(10):
                off = hp * 128
                off_sw = 1536 + hp * 128
                b, ps = psum(k)
                for kc in range(8):
                    P.op('pe', lambda e: e.matmul(ps[:, :n], lhsT=win[:, kc, off:off + 128], rhs=H[:, kc, :n], start=(kc == 0), stop=(kc == 7)),
                         reads=['win', kH], writes=[('ps', b)])
                if s == 0:
                    b2, ps2 = psum(k)
                    for kc in range(8):
                        P.op('pe', lambda e: e.matmul(ps2[:, :n], lhsT=win[:, kc, off_sw:off_sw + 128], rhs=H[:, kc, :n], start=(kc == 0), stop=(kc == 7)),
                             reads=['win', kH], writes=[('ps', b2)])
                    ta, tb = tmps[0], tmps[1]
                    P.op('dve', lambda e: e.tensor_tensor(out=ta[:, :n], in0=ps[:, :n], in1=cs[:, 0, :n], op=ALU.mult),
                         reads=[('ps', b), 'cs0'], writes=[('ntmp', 0)])
                    P.op('dve', lambda e: e.tensor_tensor(out=tb[:, :n], in0=ps2[:, :n], in1=cs[:, 1, :n], op=ALU.mult),
                         reads=[('ps', b2), 'cs1'], writes=[('ntmp', 1)])
                    P.op('dve', lambda e: e.tensor_tensor(out=qo[:, hp, :n], in0=ta[:, :n], in1=tb[:, :n], op=ALU.add),
                         reads=[('ntmp', 0), ('ntmp', 1)], writes=['qo'])
                else:
                    P.op('act', lambda e: e.activation(out=qo[:, hp, :n], in_=ps[:, :n], func=AF.Copy), reads=[('ps', b)], writes=['qo'])
            for par in range(2):
                psl = slice(par * 64, (par + 1) * 64)
                P.op('sp', lambda e: e.dma_start(out=QS.rearrange("d (j two) t -> d two j t", two=2)[:, par, :, c0:c0 + n], in_=qo[psl, 0:8, :n]),
                     reads=['qo'], writes=['QS'], dma=True)
                P.op('sp', lambda e: e.dma_start(out=KS.rearrange("d (j two) t -> d two j t", two=2)[:, par, :, c0:c0 + n], in_=qo[psl, 8:10, :n]),
                     reads=['qo'], writes=['KS'], dma=True)
            for tb_ in range(n // 128):
                r0 = c0 + tb_ * 128
                b, ps = psum(k)
                for kc in range(8):
                    P.op('pe', lambda e: e.matmul(ps[:, 0:256], lhsT=H[:, kc, tb_ * 128:(tb_ + 1) * 128], rhs=win[:, kc, 1280:1536],
                                                  start=(kc == 0), stop=(kc == 7)),
                         reads=['win', kH], writes=[('ps', b)])
                vt = vto[nt % 2]
                kv_ = ('vto', nt % 2)
                nt += 1
                P.op('act', lambda e: e.activation(out=vt, in_=ps[:, 0:256], func=AF.Copy), reads=[('ps', b)], writes=[kv_])
                P.op('sp', lambda e: e.dma_start(out=VS[r0:r0 + 128, :], in_=vt), reads=[kv_], writes=['VS'], dma=True)
    P.barrier()
    with ExitStack() as st:
        Kt = sb(st, nc, "s_K", [64, 4, T], BF16)
        Vt = sb(st, nc, "s_V", [128, NCH, 256], BF16)
        P.op('sp', lambda e: e.dma_start(out=Kt, in_=KS), reads=['KS'], writes=['Kt'], dma=True)
        P.op('sp', lambda e: e.dma_start(out=Vt, in_=VS.rearrange("(c q) f -> q c f", q=128)), reads=['VS'], writes=['Vt'], dma=True)
        msk = sb(st, nc, "s_msk", [128, 2, 128], F32)
        P.op('sp', lambda e: e.dma_start(out=msk[:, 0, :], in_=k.ret_tab[:, 384:512]), writes=['msk'], dma=True)
        P.op('sp', lambda e: e.dma_start(out=msk[:, 1, :], in_=k.ret_tab[:, 256:384]), writes=['msk'], dma=True)
        mskb = sb(st, nc, "s_mskb", [128, 2, 128], BF16)
        P.op('dve', lambda e: e.tensor_copy(out=mskb, in_=msk), reads=['msk'], writes=['mskb'])
        esink = sb(st, nc, "s_esink", [64, 16], F32)
        P.op('act', lambda e: e.activation(out=esink, in_=k.swa_sink, func=AF.Exp), reads=['swap'], writes=['esink'])
        ones_b = sb(st, nc, "s_ones", [128, 64], BF16)
        P.op('dve', lambda e: e.memset(ones_b, 1.0), writes=['ones_b'])
        qb_ = [sb(st, nc, f"s_qb{q}", [64, 16, 128], BF16) for q in range(2)]
        ob_ = [sb(st, nc, f"s_ob{q}", [64, 16, 128], BF16) for q in range(2)]
        Et = [sb(st, nc, f"s_E{q}", [128, 512], BF16) for q in range(10)]
        rd = sb(st, nc, "s_rd", [64, 4, 128], F32)
        ne = 0

        def loadq(c):
            P.op('sp', lambda e: e.dma_start(out=qb_[c % 2], in_=QS[:, :, c * 128:(c + 1) * 128]), reads=['QS'], writes=[('qb', c % 2)], dma=True)
        loadq(0)
        for c in range(NCH):
            if c + 1 < NCH:
                loadq(c + 1)
            qblk = qb_[c % 2]
            oblk = ob_[c % 2]
            if c < 2:
                kbs = [(0, None), (1, None)]
            else:
                kbs = []
                if c - 1 >= 2:
                    kbs.append((c - 1, 0))
                kbs.append((c, None))
                if c + 1 < NCH:
                    kbs.append((c + 1, 1))
                kbs += [(0, None), (1, None)]
            for hk in range(4):
                Q = qblk[:, hk * 4:(hk + 1) * 4, :]
                es = []
                for (kb, mk) in kbs:
                    b, ps = psum(k)
                    P.op('pe', lambda e: e.matmul(ps[:, :], lhsT=Kt[:, hk, kb * 128:(kb + 1) * 128], rhs=Q, start=True, stop=True),
                         reads=['Kt', ('qb', c % 2)], writes=[('ps', b)])
                    E = Et[ne % 10]
                    kE = ('E', ne % 10)
                    ne += 1
                    P.op('act', lambda e: e.activation(out=E, in_=ps[:, :], func=AF.Exp, scale=0.125), reads=[('ps', b)], writes=[kE])
                    if mk is not None:
                        Ev = E.rearrange("p (g i) -> p g i", i=128)
                        P.op('pool', lambda e: e.tensor_tensor(out=Ev, in0=Ev, in1=mskb[:, mk, :].unsqueeze(1).to_broadcast([128, 4, 128]), op=ALU.mult),
                             reads=[kE, 'mskb'], writes=[kE])
                    es.append((kb, E, kE))
                bo, ps_o = psum(k)
                for q, (kb, E, kE) in enumerate(es):
                    P.op('pe', lambda e: e.matmul(ps_o[0:64, :], lhsT=Vt[:, kb, hk * 64:(hk + 1) * 64], rhs=E, start=(q == 0), stop=(q == len(es) - 1)),
                         reads=['Vt', kE], writes=[('ps', bo)])
                bd, ps_d = psum(k)
                for q, (kb, E, kE) in enumerate(es):
                    P.op('pe', lambda e: e.matmul(ps_d[0:64, :], lhsT=ones_b, rhs=E, start=(q == 0), stop=(q == len(es) - 1)),
                         reads=['ones_b', kE], writes=[('ps', bd)])
                P.op('dve', lambda e: e.tensor_tensor(out=rd, in0=ps_d[0:64, :].rearrange("p (g i) -> p g i", i=128),
                                                      in1=esink[:, hk * 4:(hk + 1) * 4].unsqueeze(2).to_broadcast([64, 4, 128]), op=ALU.add),
                     reads=[('ps', bd), 'esink'], writes=['rd'])
                P.op('act', lambda e: e.activation(out=rd, in_=rd, func=AF.Ln), reads=['rd'], writes=['rd'])
                P.op('act', lambda e: e.activation(out=rd, in_=rd, func=AF.Exp, scale=-1.0), reads=['rd'], writes=['rd'])
                P.op('dve', lambda e: e.tensor_tensor(out=oblk[:, hk * 4:(hk + 1) * 4, :], in0=ps_o[0:64, :].rearrange("p (g i) -> p g i", i=128),
                                                      in1=rd, op=ALU.mult),
                     reads=[('ps', bo), 'rd'], writes=[('ob', c % 2)])
            P.op('sp', lambda e: e.dma_start(out=OS[:, :, c * 128:(c + 1) * 128], in_=oblk), reads=[('ob', c % 2)], writes=['OS'], dma=True)
    P.barrier()
    with ExitStack() as st:
        wo = sb(st, nc, "swa_wo", [64, 16, 1024], BF16)
        P.op('pool', lambda e: e.dma_start(out=wo, in_=k.swa_w_out[0].rearrange("(h d) c -> d h c", d=64)), writes=['wo'], dma=True)
        out_proj_residual(k, i, TILES, OS, 16, 'OS', wo, 64, lambda zc, dc: wo[:, zc, dc * 128:(dc + 1) * 128])


def stage_final(k):
    nc, P = k.nc, k.P
    with ExitStack() as st:
        xts = [sb(st, nc, f"xtf{q}", [128, 8, 512], F32) for q in range(2)]
        sq = sb(st, nc, "sqf", [128, 8, 512], F32)
        rss = [sb(st, nc, f"rsf{q}", [128, 512], F32) for q in range(2)]
        outv = k.out.rearrange("(kc q) t -> q kc t", q=128)
        for ti, (c0, n, s) in enumerate(TILES[1:]):
            xt = xts[ti % 2]
            rs = rss[ti % 2]
            kx = ('xtf', ti % 2)
            kr = ('rsf', ti % 2)
            P.op('sp', lambda e: e.dma_start(out=xt[:, :, :n], in_=xrow(k)[:, :, c0:c0 + n]), reads=['X', ('Xt', c0)], writes=[kx], dma=True)
            sqb = sq.bitcast(BF16)[:, :, 0:512]
            P.op('act', lambda e: e.activation(out=sqb[:, :, :n], in_=xt[:, :, :n], func=AF.Square), reads=[kx], writes=['sq'])
            b, ps = psum(k)
            for kc in range(8):
                P.op('pe', lambda e: e.matmul(ps[:, :n], lhsT=k.ones_b[:, :], rhs=sqb[:, kc, :n], start=(kc == 0), stop=(kc == 7)),
                     reads=['sq', 'ones_b'], writes=[('ps', b)])
            P.op('act', lambda e: e.activation(out=rs[:, :n], in_=ps[:, :n], func=AF.Ln, scale=1.0 / D, bias=k.epsb[:, 0:1]),
                 reads=[('ps', b)], writes=[kr])
            P.op('act', lambda e: e.activation(out=rs[:, :n], in_=rs[:, :n], func=AF.Exp, scale=-0.5), reads=[kr], writes=[kr])
            for kc in range(8):
                P.op('dve', lambda e: e.scalar_tensor_tensor(out=xt[:, kc, :n], in0=xt[:, kc, :n], scalar=k.fg_s[:, kc:kc + 1], in1=rs[:, :n],
                                                             op0=ALU.mult, op1=ALU.mult),
                     reads=[kx, kr, 'fg'], writes=[kx])
            P.op('sp', lambda e: e.dma_start(out=outv[:, :, c0 - NCTX:c0 - NCTX + n], in_=xt[:, :, :n]), reads=[kx], writes=['out'], dma=True)
    P.barrier()


ALL_PARTS = ['init', 'mods', 'mix0', 'ffn0', 'mix1', 'ffn1', 'mix2', 'ffn2', 'mix3', 'ffn3', 'final']


def kernel(**inputs):
    inp = {kk: np.asarray(v) for kk, v in inputs.items()}
    n = inp['x'].shape[0]
    maps = make_in_maps(inp, list(range(n)))
    nc, _ = build_program(ALL_PARTS, debug=False)
    res = run_bass_kernel_spmd(nc, maps, core_ids=list(range(n)))
    out = np.stack([np.ascontiguousarray(res.results[b]['outT'].T) for b in range(n)], axis=0)
    return out.astype(np.float32)


I32 = mybir.dt.int32
NBLK_MAX = 24
MOE_BLK = 512


def precast_moe(k, pe):
    P = k.P
    f, e = pe // 8, pe % 8
    gu, dn = k.moe_w_gu[f, e], k.moe_w_down[f, e]
    for g7 in range(7):
        r0 = (pe * 7 + g7) * 128
        for half in range(2):
            c0 = half * DFF + g7 * 512
            dst = k.WGUx[r0:r0 + 128, :].rearrange("p (kc h c) -> p kc h c", kc=8, h=2)[:, :, half, :]
            src = gu.rearrange("(kc p) n -> p kc n", p=128)[:, :, c0:c0 + 512]
            P.op('pool', lambda e_: e_.dma_start(out=dst, in_=src), writes=[('wgux', pe)], dma=True)
    for dh in range(2):
        r0 = (pe * 2 + dh) * 128
        dst = k.WDx[r0:r0 + 128, :].rearrange("p (fc c) -> p fc c", c=512)
        src = dn.rearrange("(fc p) d -> p fc d", p=128)[:, :, dh * 512:(dh + 1) * 512]
        P.op('pool', lambda e_: e_.dma_start(out=dst, in_=src), writes=[('wdx', pe)], dma=True)


def drain_precast(k, n):
    while n > 0 and k.pc_queue:
        k.pc_queue.pop(0)()
        n -= 1


def stage_moe_sparse(k, i):
    nc, P = k.nc, k.P
    f = i // 2
    last = (i == 3)
    tiles = TILES[1:] if last else TILES
    cols0 = tiles[0][0]
    ntok = sum(t[1] for t in tiles)
    NCK = ntok // 128
    NB = (2 * ntok + 8 * (MOE_BLK - 1)) // MOE_BLK
    drain_precast(k, 1000)
    HC, YC = k.HC, k.YC
    ct = k.moe_tab
    with ExitStack() as st0:
        EQ1 = sb(st0, nc, "m_eq1", [128, NCK, 8], F32)
        EQ2 = sb(st0, nc, "m_eq2", [128, NCK, 8], F32)
        W12 = sb(st0, nc, "m_w12", [128, NCK, 2], F32)
        DI = sb(st0, nc, "m_di", [128, NCK, 2], I32)
        IGU = sb(st0, nc, "m_igu", [128, NB, 7], I32)
        IWD = sb(st0, nc, "m_iwd", [128, NB, 2], I32)
        tabs = sb(st0, nc, "m_tab", [128, 161], F32)
        P.op('sp', lambda e: e.dma_start(out=tabs, in_=ct), writes=['mtab'], dma=True)
        TRI, IOB, CGU, CWD = tabs[:, 0:128], tabs[:, 128:128 + NB], tabs[:, 152:159], tabs[:, 159:161]
        with ExitStack() as st:
            HTOK = sb(st, nc, "m_htok", [128, NCK, 1024], BF16)
            H = sb(st, nc, "m_H", [128, 8, 512], BF16)
            xt = sb(st, nc, "m_xt", [128, 8, 512], F32)
            sq = sb(st, nc, "m_sq", [128, 8, 512], F32)
            hf = sb(st, nc, "m_hf", [128, 8, 512], F32)
            rs = sb(st, nc, "m_rs", [128, 512], F32)
            tmps = [sb(st, nc, f"m_nt{q}", [128, 512], F32) for q in range(2)]
            rsm = sb(st, nc, "m_rsm", [128, 64], F32)
            rsm3 = sb(st, nc, "m_rsm3", [128, 12], F32)
            identb = sb(st, nc, "m_identb", [128, 128], BF16)
            P.op('dve', lambda e: e.tensor_copy(out=identb, in_=k.ident_f), reads=['ident'], writes=['identb'])
            ck = 0
            for ti, (c0, n, s) in enumerate(tiles):
                load_x_tile(k, xt, c0, n)
                norm_tile(k, xt, 'xt', n, i, 1, s, H, 'H', sq, rs, tmps, hf=hf, khf='hf')
                nb = n // 128
                b, ps = psum(k)
                for bi in range(nb):
                    bsl = slice(bi * 128, (bi + 1) * 128)
                    for kc in range(8):
                        P.op('pe', lambda e: e.matmul(ps[:, bi * 8:(bi + 1) * 8], lhsT=hf[:, kc, bsl], rhs=k.rt[:, f, kc, :], start=(kc == 0), stop=(kc == 7)),
                             reads=['hf', 'rt'], writes=[('ps', b)])
                lg = rsm[:, 0:nb * 8]
                lg2 = rsm[:, 32:32 + nb * 8]
                lgv = lg.rearrange("p (c e) -> p c e", e=8)
                lg2v = lg2.rearrange("p (c e) -> p c e", e=8)
                m1, m2, dd = rsm3[:, 0:nb], rsm3[:, 4:4 + nb], rsm3[:, 8:8 + nb]
                eq1, eq2 = EQ1[:, ck:ck + nb, :], EQ2[:, ck:ck + nb, :]
                R = ['rsm']
                KE = [('eq', ck + q) for q in range(nb)]
                P.op('dve', lambda e: e.tensor_copy(out=lg, in_=ps[:, 0:nb * 8]), reads=[('ps', b)], writes=R)
                P.op('dve', lambda e: e.tensor_reduce(out=m1, in_=lgv, axis=AX.X, op=ALU.max), reads=R, writes=R)
                P.op('dve', lambda e: e.tensor_tensor(out=eq1, in0=lgv, in1=m1.unsqueeze(2).to_broadcast([128, nb, 8]), op=ALU.is_equal), reads=R, writes=KE)
                P.op('dve', lambda e: e.scalar_tensor_tensor(out=lg2, in0=eq1.rearrange("p c e -> p (c e)"), scalar=-1e30, in1=lg, op0=ALU.mult, op1=ALU.add),
                     reads=R + KE, writes=R)
                P.op('dve', lambda e: e.tensor_reduce(out=m2, in_=lg2v, axis=AX.X, op=ALU.max), reads=R, writes=R)
                P.op('dve', lambda e: e.tensor_tensor(out=eq2, in0=lg2v, in1=m2.unsqueeze(2).to_broadcast([128, nb, 8]), op=ALU.is_equal), reads=R, writes=KE)
                P.op('dve', lambda e: e.tensor_tensor(out=dd, in0=m2, in1=m1, op=ALU.subtract), reads=R, writes=R)
                P.op('act', lambda e: e.activation(out=W12[:, ck:ck + nb, 0], in_=dd, func=AF.Sigmoid, scale=-1.0), reads=R, writes=KE)
                P.op('act', lambda e: e.activation(out=W12[:, ck:ck + nb, 1], in_=dd, func=AF.Sigmoid, scale=1.0), reads=R, writes=KE)
                for bi in range(nb):
                    bsl = slice(bi * 128, (bi + 1) * 128)
                    bt, pst = psum(k)
                    pstb = pst.bitcast(BF16)
                    for kc in range(8):
                        P.op('pe', lambda e: e.transpose(out=pstb[:, kc * 128:(kc + 1) * 128], in_=H[:, kc, bsl], identity=identb),
                             reads=['H', 'identb'], writes=[('ps', bt)])
                    eng = 'act' if ck % 2 == 0 else 'dve'
                    if eng == 'act':
                        P.op('act', lambda e: e.activation(out=HTOK[:, ck, :], in_=pstb, func=AF.Copy), reads=[('ps', bt)], writes=[('htok', ck)])
                    else:
                        P.op('dve', lambda e: e.tensor_copy(out=HTOK[:, ck, :], in_=pstb), reads=[('ps', bt)], writes=[('htok', ck)])
                    ck += 1
            allE = [('eq', c) for c in range(NCK)]
            SEL = sb(st, nc, "m_sel", [128, NCK, 8], F32)
            PRE = sb(st, nc, "m_pre", [128, NCK + 1, 8], F32)
            RANK = sb(st, nc, "m_rank", [128, NCK, 8], F32)
            sm = sb(st, nc, "m_sm", [128, 64], F32)
            TOT, NBk, CB, OFF, ONE8 = sm[:, 0:8], sm[:, 8:16], sm[:, 16:24], sm[:, 24:32], sm[:, 32:40]
            EB = sb(st, nc, "m_eb", [128, 3, NB], F32)
            DF = sb(st, nc, "m_df", [128, NCK, 2], F32)
            IGf = sb(st, nc, "m_igf", [128, NB, 7], F32)
            IWf = sb(st, nc, "m_iwf", [128, NB, 2], F32)
            P.op('dve', lambda e: e.tensor_tensor(out=SEL, in0=EQ1, in1=EQ2, op=ALU.add), reads=allE, writes=['sel'])
            P.op('dve', lambda e: e.memset(PRE[:, 0, :], 0.0), writes=['pre'])
            P.op('dve', lambda e: e.memset(ONE8, 1.0), writes=['sm'])
            for c in range(NCK):
                P.op('dve', lambda e: e.tensor_tensor(out=PRE[:, c + 1, :], in0=PRE[:, c, :], in1=SEL[:, c, :], op=ALU.add),
                     reads=['pre', 'sel'], writes=['pre'])
            br, psr = psum(k)
            for c in range(NCK):
                P.op('pe', lambda e: e.matmul(psr[:, c * 8:(c + 1) * 8], lhsT=TRI, rhs=SEL[:, c, :], start=True, stop=False),
                     reads=['mtab', 'sel'], writes=[('ps', br)])
                P.op('pe', lambda e: e.matmul(psr[:, c * 8:(c + 1) * 8], lhsT=k.ones_f, rhs=PRE[:, c, :], start=False, stop=True),
                     reads=['ones', 'pre'], writes=[('ps', br)])
            P.op('dve', lambda e: e.tensor_copy(out=RANK, in_=psr[:, 0:NCK * 8].rearrange("p (c e) -> p c e", e=8)), reads=[('ps', br)], writes=['rank'])
            b2, ps2 = psum(k)
            P.op('pe', lambda e: e.matmul(ps2[:, 0:8], lhsT=k.ones_f, rhs=PRE[:, NCK, :], start=True, stop=True), reads=['ones', 'pre'], writes=[('ps', b2)])
            S_ = ['sm']
            P.op('dve', lambda e: e.tensor_copy(out=TOT, in_=ps2[:, 0:8]), reads=[('ps', b2)], writes=S_)
            P.op('dve', lambda e: e.tensor_scalar(out=NBk, in0=TOT, scalar1=0.0, scalar2=None, op0=ALU.is_gt), reads=S_, writes=S_)
            for m in range(1, 9):
                P.op('dve', lambda e: e.scalar_tensor_tensor(out=NBk, in0=TOT, scalar=float(MOE_BLK * m), in1=NBk, op0=ALU.is_gt, op1=ALU.add),
                     reads=S_, writes=S_)
            P.op('dve', lambda e: e.tensor_tensor_scan(out=CB, data0=ONE8, data1=NBk, initial=0.0, op0=ALU.mult, op1=ALU.add), reads=S_, writes=S_)
            P.op('dve', lambda e: e.tensor_tensor(out=OFF, in0=CB, in1=NBk, op=ALU.subtract), reads=S_, writes=S_)
            P.op('dve', lambda e: e.tensor_scalar(out=OFF, in0=OFF, scalar1=float(MOE_BLK), scalar2=None, op0=ALU.mult), reads=S_, writes=S_)
            P.op('dve', lambda e: e.tensor_tensor(out=RANK, in0=RANK, in1=OFF.unsqueeze(1).to_broadcast([128, NCK, 8]), op=ALU.add),
                 reads=['rank'] + S_, writes=['rank'])
            P.op('dve', lambda e: e.tensor_tensor(out=SEL, in0=RANK, in1=EQ1, op=ALU.mult), reads=['rank'] + allE, writes=['sel'])
            P.op('dve', lambda e: e.tensor_reduce(out=DF[:, :, 0], in_=SEL, axis=AX.X, op=ALU.add), reads=['sel'], writes=['df'])
            P.op('dve', lambda e: e.tensor_tensor(out=SEL, in0=RANK, in1=EQ2, op=ALU.mult), reads=['rank', 'df'] + allE, writes=['sel'])
            P.op('dve', lambda e: e.tensor_reduce(out=DF[:, :, 1], in_=SEL, axis=AX.X, op=ALU.add), reads=['sel'], writes=['df'])
            P.op('dve', lambda e: e.tensor_copy(out=DI, in_=DF), reads=['df'], writes=['di'])
            P.op('dve', lambda e: e.memset(EB[:, 0, :], 0.0), writes=['eb'])
            for e8 in range(8):
                P.op('dve', lambda e: e.scalar_tensor_tensor(out=EB[:, 0, :], in0=IOB, scalar=CB[:, e8:e8 + 1], in1=EB[:, 0, :], op0=ALU.is_ge, op1=ALU.add),
                     reads=['mtab', 'eb'] + S_, writes=['eb'])
            P.op('dve', lambda e: e.tensor_scalar(out=EB[:, 0, :], in0=EB[:, 0, :], scalar1=7.0, scalar2=None, op0=ALU.min), reads=['eb'], writes=['eb'])
            P.op('dve', lambda e: e.tensor_scalar(out=EB[:, 1, :], in0=EB[:, 0, :], scalar1=896.0, scalar2=float(f * 8 * 896), op0=ALU.mult, op1=ALU.add),
                 reads=['eb'], writes=['eb'])
            P.op('dve', lambda e: e.tensor_scalar(out=EB[:, 2, :], in0=EB[:, 0, :], scalar1=256.0, scalar2=float(f * 8 * 256), op0=ALU.mult, op1=ALU.add),
                 reads=['eb'], writes=['eb'])
            for b_ in range(NB):
                P.op('dve', lambda e: e.tensor_scalar(out=IGf[:, b_, :], in0=CGU, scalar1=EB[:, 1, b_:b_ + 1], scalar2=None, op0=ALU.add),
                     reads=['eb', 'mtab'], writes=['igf'])
                P.op('dve', lambda e: e.tensor_scalar(out=IWf[:, b_, :], in0=CWD, scalar1=EB[:, 2, b_:b_ + 1], scalar2=None, op0=ALU.add),
                     reads=['eb', 'mtab'], writes=['iwf'])
            P.op('dve', lambda e: e.tensor_copy(out=IGU, in_=IGf), reads=['igf'], writes=['igu'])
            P.op('dve', lambda e: e.tensor_copy(out=IWD, in_=IWf), reads=['iwf'], writes=['iwd'])
            for c in range(NCK):
                for j2 in range(2):
                    P.op('pool', lambda e: e.indirect_dma_start(out=HC, out_offset=bass.IndirectOffsetOnAxis(ap=DI[:, c, j2:j2 + 1], axis=0),
                                                                in_=HTOK[:, c, :], in_offset=None),
                         reads=['di', ('htok', c)], writes=['HC'], dma=True)
        P.barrier()
        with ExitStack() as st:
            wgu = [sb(st, nc, f"m_wgu{q}", [128, 8192], BF16) for q in range(2)]
            wdb = [sb(st, nc, f"m_wd{q}", [128, 28 * 512], BF16) for q in range(2)]
            hs = [sb(st, nc, f"m_hs{q}", [128, 4, 1024], BF16) for q in range(2)]
            HcT = sb(st, nc, "m_HcT", [128, 8, 512], BF16)
            act = sb(st, nc, "m_act", [128, 28, 512], BF16)
            yc = [sb(st, nc, f"m_yc{q}", [128, 1024], F32) for q in range(2)]
            sg_ = [sb(st, nc, f"m_sg{q}", [128, 512], BF16) for q in range(2)]
            identb = sb(st, nc, "m_identb2", [128, 128], BF16)
            P.op('dve', lambda e: e.tensor_copy(out=identb, in_=k.ident_f), reads=['ident'], writes=['identb'])
            n_gu = [0]

            def gather_gu(b_, g7):
                q = n_gu[0] % 2
                n_gu[0] += 1
                P.op('pool', lambda e: e.indirect_dma_start(out=wgu[q], out_offset=None, in_=k.WGUx,
                                                            in_offset=bass.IndirectOffsetOnAxis(ap=IGU[:, b_, g7:g7 + 1], axis=0)),
                     reads=['igu', 'wgux_all'], writes=[('wgu', q)], dma=True)
                return q

            def gather_wd(b_, dh):
                P.op('pool', lambda e: e.indirect_dma_start(out=wdb[dh], out_offset=None, in_=k.WDx,
                                                            in_offset=bass.IndirectOffsetOnAxis(ap=IWD[:, b_, dh:dh + 1], axis=0)),
                     reads=['iwd', 'wdx_all'], writes=[('wd', dh)], dma=True)

            def load_hs(b_):
                P.op('sp', lambda e: e.dma_start(out=hs[b_ % 2], in_=HC[b_ * 512:(b_ + 1) * 512, :].rearrange("(sg p) d -> p sg d", p=128)),
                     reads=['HC'], writes=[('hs', b_ % 2)], dma=True)
            load_hs(0)
            pre_gu = [gather_gu(0, 0), gather_gu(0, 1)]
            gather_wd(0, 0)
            gather_wd(0, 1)
            nsg = 0
            nyc = 0
            for b_ in range(NB):
                if b_ + 1 < NB:
                    load_hs(b_ + 1)
                hsb = hs[b_ % 2]
                for kc in range(8):
                    bt, pst = psum(k)
                    pstb = pst.bitcast(BF16)
                    for sgi in range(4):
                        P.op('pe', lambda e: e.transpose(out=pstb[:, sgi * 128:(sgi + 1) * 128], in_=hsb[:, sgi, kc * 128:(kc + 1) * 128], identity=identb),
                             reads=[('hs', b_ % 2), 'identb'], writes=[('ps', bt)])
                    if kc % 2 == 0:
                        P.op('act', lambda e: e.activation(out=HcT[:, kc, :], in_=pstb[:, 0:512], func=AF.Copy), reads=[('ps', bt)], writes=['HcT'])
                    else:
                        P.op('dve', lambda e: e.tensor_copy(out=HcT[:, kc, :], in_=pstb[:, 0:512]), reads=[('ps', bt)], writes=['HcT'])
                for g7 in range(7):
                    if g7 < 2:
                        q = pre_gu[g7]
                    else:
                        q = gather_gu(b_, g7)
                    wv = wgu[q].rearrange("p (kc h c) -> p kc h c", kc=8, h=2)
                    for j in range(4):
                        fch = g7 * 4 + j
                        bg, psg = psum(k)
                        for kc in range(8):
                            P.op('pe', lambda e: e.matmul(psg, lhsT=wv[:, kc, 0, j * 128:(j + 1) * 128], rhs=HcT[:, kc, :], start=(kc == 0), stop=(kc == 7)),
                                 reads=[('wgu', q), 'HcT'], writes=[('ps', bg)])
                        bu, psu = psum(k)
                        for kc in range(8):
                            P.op('pe', lambda e: e.matmul(psu, lhsT=wv[:, kc, 1, j * 128:(j + 1) * 128], rhs=HcT[:, kc, :], start=(kc == 0), stop=(kc == 7)),
                                 reads=[('wgu', q), 'HcT'], writes=[('ps', bu)])
                        sgt = sg_[nsg % 2]
                        ksg = ('sg', nsg % 2)
                        nsg += 1
                        P.op('act', lambda e: e.activation(out=sgt, in_=psg, func=AF.Silu), reads=[('ps', bg)], writes=[ksg])
                        P.op('dve', lambda e: e.tensor_tensor(out=act[:, fch, :], in0=sgt, in1=psu, op=ALU.mult), reads=[ksg, ('ps', bu)], writes=[('act', fch)])
                if b_ + 1 < NB:
                    pre_gu = [gather_gu(b_ + 1, 0), gather_gu(b_ + 1, 1)]
                for sgi in range(4):
                    y = yc[nyc % 2]
                    ky = ('yc', nyc % 2)
                    nyc += 1
                    for dh in range(2):
                        wv = wdb[dh].rearrange("p (fc c) -> p fc c", c=512)
                        bd, psd = psum(k)
                        for fc in range(28):
                            P.op('pe', lambda e: e.matmul(psd, lhsT=act[:, fc, sgi * 128:(sgi + 1) * 128], rhs=wv[:, fc, :], start=(fc == 0), stop=(fc == 27)),
                                 reads=[('act', fc), ('wd', dh)], writes=[('ps', bd)])
                        if dh == 0:
                            P.op('act', lambda e: e.activation(out=y[:, 0:512], in_=psd, func=AF.Copy), reads=[('ps', bd)], writes=[ky])
                        else:
                            P.op('dve', lambda e: e.tensor_copy(out=y[:, 512:1024], in_=psd), reads=[('ps', bd)], writes=[ky])
                    r0 = b_ * 512 + sgi * 128
                    P.op('sp', lambda e: e.dma_start(out=YC[r0:r0 + 128, :], in_=y), reads=[ky], writes=['YC'], dma=True)
                if b_ + 1 < NB:
                    gather_wd(b_ + 1, 0)
                    gather_wd(b_ + 1, 1)
        P.barrier()
        with ExitStack() as st:
            xt = sb(st, nc, "m_xtE", [128, 8, 512], F32)
            sqE = sb(st, nc, "m_sqE", [128, 8, 512], BF16)
            rsE = sb(st, nc, "m_rsE", [128, 512], F32)
            y1 = [sb(st, nc, f"m_y1{q}", [128, 1024], F32) for q in range(2)]
            y2 = [sb(st, nc, f"m_y2{q}", [128, 1024], F32) for q in range(2)]
            ck = 0
            for ti, (c0, n, s) in enumerate(tiles):
                load_x_tile(k, xt, c0, n)
                banks = [psum(k) for _ in range(8)]
                for bi in range(n // 128):
                    q = ck % 2
                    P.op('pool', lambda e: e.indirect_dma_start(out=y1[q], out_offset=None, in_=YC,
                                                                in_offset=bass.IndirectOffsetOnAxis(ap=DI[:, ck, 0:1], axis=0)),
                         reads=['YC', 'di'], writes=[('y1', q)], dma=True)
                    P.op('pool', lambda e: e.indirect_dma_start(out=y2[q], out_offset=None, in_=YC,
                                                                in_offset=bass.IndirectOffsetOnAxis(ap=DI[:, ck, 1:2], axis=0)),
                         reads=['YC', 'di'], writes=[('y2', q)], dma=True)
                    P.op('dve', lambda e: e.tensor_scalar(out=y1[q], in0=y1[q], scalar1=W12[:, ck, 0:1], scalar2=None, op0=ALU.mult),
                         reads=[('y1', q), ('eq', ck)], writes=[('y1', q)])
                    P.op('dve', lambda e: e.scalar_tensor_tensor(out=y1[q], in0=y2[q], scalar=W12[:, ck, 1:2], in1=y1[q], op0=ALU.mult, op1=ALU.add),
                         reads=[('y1', q), ('y2', q), ('eq', ck)], writes=[('y1', q)])
                    for kc in range(8):
                        bb, pp = banks[kc]
                        P.op('pe', lambda e: e.transpose(out=pp[:, bi * 128:(bi + 1) * 128], in_=y1[q][:, kc * 128:(kc + 1) * 128], identity=k.ident_f),
                             reads=[('y1', q), 'ident'], writes=[('ps', bb)])
                    ck += 1
                for kc in range(8):
                    bb, pp = banks[kc]
                    gate = k.mods[:, i, 5 * 8 + kc, s:s + 1]
                    P.op('dve', lambda e: e.scalar_tensor_tensor(out=xt[:, kc, :n], in0=pp[:, :n], scalar=gate, in1=xt[:, kc, :n], op0=ALU.mult, op1=ALU.add),
                         reads=[('ps', bb), 'xt', 'mods'], writes=['xt'])
                if last and k.fuse_final:
                    P.op('act', lambda e: e.activation(out=sqE[:, :, :n], in_=xt[:, :, :n], func=AF.Square), reads=['xt'], writes=['sqE'])
                    bf_, psf = psum(k)
                    for kc in range(8):
                        P.op('pe', lambda e: e.matmul(psf[:, :n], lhsT=k.ones_b[:, :], rhs=sqE[:, kc, :n], start=(kc == 0), stop=(kc == 7)),
                             reads=['sqE', 'ones_b'], writes=[('ps', bf_)])
                    P.op('act', lambda e: e.activation(out=rsE[:, :n], in_=psf[:, :n], func=AF.Ln, scale=1.0 / D, bias=k.epsb[:, 0:1]),
                         reads=[('ps', bf_)], writes=['rsE'])
                    P.op('act', lambda e: e.activation(out=rsE[:, :n], in_=rsE[:, :n], func=AF.Exp, scale=-0.5), reads=['rsE'], writes=['rsE'])
                    for kc in range(8):
                        P.op('dve', lambda e: e.scalar_tensor_tensor(out=xt[:, kc, :n], in0=xt[:, kc, :n], scalar=k.fg_s[:, kc:kc + 1], in1=rsE[:, :n],
                                                                     op0=ALU.mult, op1=ALU.mult),
                             reads=['xt', 'rsE', 'fg'], writes=['xt'])
                    P.op('sp', lambda e: e.dma_start(out=k.out.rearrange("(kc q) t -> q kc t", q=128)[:, :, c0 - NCTX:c0 - NCTX + n], in_=xt[:, :, :n]),
                         reads=['xt'], writes=['out'], dma=True)
                else:
                    store_x_tile(k, xt, c0, n)
    P.barrier()
```

```python
import numpy as np
from contextlib import ExitStack
import concourse.bass as bass
import concourse.mybir as mybir
from concourse.bass_utils import run_bass_kernel_spmd

F32 = mybir.dt.float32
BF16 = mybir.dt.bfloat16
AF = mybir.ActivationFunctionType
ALU = mybir.AluOpType
AX = mybir.AxisListType

NPOOL = 8
SPARSE_MOE = True
SAME_ENGINE_SYNC = True

T = 4352
NCTX = 256
SEQ = 4096
D = 1024
DFF = 3584
EPS = 1e-6
TILES = [(0, 256, 1)] + [(256 + 512 * i, 512, 0) for i in range(8)]


class Prog:
    def __init__(self, nc, stack):
        self.nc = nc
        self.stack = stack
        self.eng = {'pe': nc.tensor, 'act': nc.scalar, 'dve': nc.vector, 'pool': nc.gpsimd, 'sp': nc.sync}
        self.sems = {}
        self.cnt = {e: 0 for e in self.eng}
        self.seen = {e: {} for e in self.eng}
        self.keys = {}
        self.dma_n = {'sp': 0, 'pool': 0, 'act': 0}
        self.n_inst = 0

    def sem(self, sk):
        if sk not in self.sems:
            name = 's_' + (sk if isinstance(sk, str) else f'{sk[0]}q{sk[1]}')
            self.sems[sk] = self.stack.enter_context(self.nc.semaphore(name))
        return self.sems[sk]

    def op(self, eng, fn, reads=(), writes=(), dma=False):
        deps = {}
        own = None if dma else eng

        def add(sk, v):
            if deps.get(sk, 0) < v:
                deps[sk] = v
        for k in reads:
            st = self.keys.get(k)
            if st is not None and st[0] is not None:
                add(*st[0])
        for k in writes:
            st = self.keys.get(k)
            if st is not None:
                if st[0] is not None and st[0][0] != own:
                    add(*st[0])
                for sk, v in st[1].items():
                    if sk != own:
                        add(sk, v)
        if dma:
            n = self.dma_n[eng]
            slot = n % NPOOL
            semkey = (eng, slot)
            val = 16 * (n // NPOOL + 1)
            if n >= NPOOL:
                add(semkey, 16 * (n // NPOOL))
            self.dma_n[eng] += 1
            inc = 16
        else:
            self.cnt[eng] += 1
            semkey = eng
            val = self.cnt[eng]
            inc = 1
        h = self.eng[eng]
        for sk, v in deps.items():
            if sk == eng and (eng == 'pe' or not SAME_ENGINE_SYNC):
                continue
            if self.seen[eng].get(sk, 0) >= v:
                continue
            self.seen[eng][sk] = v
            h.wait_ge(self.sem(sk), v)
        inst = fn(h)
        inst.then_inc(self.sem(semkey), inc)
        self.n_inst += 1
        for k in reads:
            st = self.keys.setdefault(k, [None, {}])
            if st[1].get(semkey, 0) < val:
                st[1][semkey] = val
        for k in writes:
            self.keys[k] = [(semkey, val), {}]

    def _all_now(self):
        cur = {}
        for e in ('pe', 'act', 'dve', 'pool'):
            if self.cnt[e] > 0:
                cur[e] = self.cnt[e]
        for q, n in self.dma_n.items():
            for slot in range(min(n, NPOOL)):
                cur[(q, slot)] = 16 * (((n - 1 - slot) // NPOOL) + 1)
        return cur

    def barrier(self, engines=('pe', 'act', 'dve', 'pool', 'sp')):
        cur = self._all_now()
        for e in engines:
            h = self.eng[e]
            for sk, v in cur.items():
                if sk == e:
                    continue
                if self.seen[e].get(sk, 0) >= v:
                    continue
                self.seen[e][sk] = v
                h.wait_ge(self.sem(sk), v)
        if len(engines) == 5:
            self.keys.clear()

    def finish(self):
        self.barrier(engines=('sp',))


class K:
    pass


_SB_N = [0]


def sb(st, nc, name, shape, dt):
    _SB_N[0] += 1
    h = st.enter_context(nc.sbuf_tensor(f"{name}_u{_SB_N[0]}", shape, dt))
    return h[tuple(slice(None) for _ in shape)]


def col(v, n):
    return np.ascontiguousarray(np.asarray(v).reshape(n, 128).T)


def build_program(parts, debug=False):
    nc = bass.Bass("TRN2", target_bir_lowering=False)
    k = K()
    k.nc = nc
    dr = {}

    def din(name, shape, dt=F32):
        dr[name] = nc.dram_tensor(name, list(shape), dt, kind="ExternalInput").ap()
        return dr[name]

    k.xT = din("xT", [D, T])
    k.cc = din("cc", [128, 8, 2])
    k.ada_w = din("ada_w", [4, D, 6 * D])
    k.adab = din("adab", [128, 4, 48])
    k.adab_row = din("adab_row", [4, 2, 6144])
    k.ng = din("ng", [128, 4, 2, 8])
    k.fg = din("fg", [128, 8])
    k.ones_in = din("ones_f", [128, 128])
    k.ident_in = din("ident_f", [128, 128])
    k.esel_in = din("esel", [8, 8, 128])
    k.ffn_w_gu = din("ffn_w_gu", [2, D, 2 * DFF])
    k.ffn_w_down = din("ffn_w_down", [2, DFF, D])
    k.router = din("router", [128, 2, 8, 8])
    k.moe_w_gu = din("moe_w_gu", [2, 8, D, 2 * DFF])
    k.moe_w_down = din("moe_w_down", [2, 8, DFF, D])
    k.lru_w_in = din("lru_w_in", [2, D, 2560])
    k.lru_wg = din("lru_wg", [2, 128, 40, 128])
    k.lru_w_out = din("lru_w_out", [2, 1280, D])
    k.lru_cw_in = din("lru_cw", [128, 2, 4, 10])
    k.lru_cb_in = din("lru_cb", [128, 2, 10])
    k.lru_bg_in = din("lru_bg", [128, 2, 2, 2, 10])
    k.lru_lam_in = din("lru_lam", [128, 2, 2, 10])
    k.ret_w_in = din("ret_w_in", [1, D, 6144])
    k.ret_w_out = din("ret_w_out", [1, 2048, D])
    k.ret_ld_in = din("ret_ld", [128, 8])
    k.ret_gain_in = din("ret_gain", [128, 16])
    k.ret_cosT = din("ret_cosT", [128, SEQ])
    k.ret_sinT = din("ret_sinT", [128, SEQ])
    k.ret_costok = din("ret_costok", [SEQ, 128])
    k.ret_sintok = din("ret_sintok", [SEQ, 128])
    k.ret_tab = din("ret_tab", [128, 771])
    k.swa_w_ext = din("swa_w_ext", [D, 2816])
    k.swa_w_out = din("swa_w_out", [1, D, D])
    k.swa_sink_in = din("swa_sink", [64, 16])
    k.swa_cos = din("swa_cos", [128, SEQ])
    k.swa_sin = din("swa_sin", [128, SEQ])
    k.moe_tab = din("moe_tab", [128, 161])
    k.dr = dr

    if debug:
        k.out = nc.dram_tensor("xdump", [D, T], F32, kind="ExternalOutput").ap()
    else:
        k.out = nc.dram_tensor("outT", [D, SEQ], F32, kind="ExternalOutput").ap()
    k.X = nc.dram_tensor("Xres", [D, T], F32, kind="Internal").ap()
    k.WGU = nc.dram_tensor("WGUs", [18, D, 2 * DFF], BF16, kind="Internal").ap() if not SPARSE_MOE else nc.dram_tensor("WGUs", [10, D, 2 * DFF], BF16, kind="Internal").ap()
    k.WD = nc.dram_tensor("WDs", [18, DFF, D], BF16, kind="Internal").ap() if not SPARSE_MOE else nc.dram_tensor("WDs", [10, DFF, D], BF16, kind="Internal").ap()
    k.QT = nc.dram_tensor("QTs", [1024, T], BF16, kind="Internal").ap()
    k.KT = nc.dram_tensor("KTs", [1024, T], BF16, kind="Internal").ap()
    k.KTOK = nc.dram_tensor("KTOKs", [T, 1024], BF16, kind="Internal").ap()
    k.VTOK = nc.dram_tensor("VTOKs", [T, 2048], BF16, kind="Internal").ap()
    k.GS = nc.dram_tensor("GSs", [2048, T], BF16, kind="Internal").ap()
    k.SBd = nc.dram_tensor("SBds", [4, NCH, 256, 512], BF16, kind="Internal").ap()
    k.ZR = nc.dram_tensor("ZRs", [2048, T], BF16, kind="Internal").ap()
    k.QS = nc.dram_tensor("QSs", [64, 16, T], BF16, kind="Internal").ap()
    k.KS = nc.dram_tensor("KSs", [64, 4, T], BF16, kind="Internal").ap()
    k.VS = nc.dram_tensor("VSs", [T, 256], BF16, kind="Internal").ap()
    k.OS = nc.dram_tensor("OSs", [64, 16, T], BF16, kind="Internal").ap()
    k.HC = nc.dram_tensor("HCs", [NBLK_MAX * 512, 1024], BF16, kind="Internal").ap()
    k.YC = nc.dram_tensor("YCs", [NBLK_MAX * 512, 1024], F32, kind="Internal").ap()
    k.WGUx = nc.dram_tensor("WGUx", [16 * 7 * 128, 8192], BF16, kind="Internal").ap()
    k.WDx = nc.dram_tensor("WDx", [16 * 2 * 128, 28 * 512], BF16, kind="Internal").ap()
    k.XR = nc.dram_tensor("XRs", [1280, T], F32, kind="Internal").ap()
    k.YG = nc.dram_tensor("YGs", [1280, T], BF16, kind="Internal").ap()
    k.ZG = nc.dram_tensor("ZGs", [1280, T], BF16, kind="Internal").ap()

    with ExitStack() as st:
        P = Prog(nc, st)
        k.P = P
        k.PS = [st.enter_context(nc.psum_tensor(f"ps{i}", [128, 512], F32))[:, :] for i in range(8)]
        k.ps_n = 0
        k.cond = sb(st, nc, "cond", [128, 8, 2], F32)
        k.mods = sb(st, nc, "mods", [128, 4, 48, 2], F32)
        k.acoef = sb(st, nc, "acoef", [128, 4, 2, 8, 2], F32)
        k.adab_s = sb(st, nc, "adab_s", [128, 4, 48], F32)
        k.ng_s = sb(st, nc, "ng_s", [128, 4, 2, 8], F32)
        k.fg_s = sb(st, nc, "fg_s", [128, 8], F32)
        k.ones_f = sb(st, nc, "ones_fs", [128, 128], F32)
        k.ident_f = sb(st, nc, "ident_fs", [128, 128], F32)
        k.esel = sb(st, nc, "esel_s", [8, 8, 128], F32)
        k.rt = sb(st, nc, "rt_s", [128, 2, 8, 8], F32)
        k.epsb = sb(st, nc, "epsb", [128, 1], F32)
        P.op('dve', lambda e: e.memset(k.epsb, EPS), writes=['epsb'])
        k.ones_b = sb(st, nc, "ones_b", [128, 128], BF16)
        P.op('dve', lambda e: e.memset(k.ones_b, 1.0), writes=['ones_b'])
        k.oneb = sb(st, nc, "oneb", [128, 1], F32)
        P.op('dve', lambda e: e.memset(k.oneb, 1.0), writes=['oneb'])
        k.lru_cw = sb(st, nc, "lru_cw_s", [128, 2, 4, 10], F32)
        k.lru_cb = sb(st, nc, "lru_cb_s", [128, 2, 10], F32)
        k.lru_bg = sb(st, nc, "lru_bg_s", [128, 2, 2, 2, 10], F32)
        k.lru_lam = sb(st, nc, "lru_lam_s", [128, 2, 2, 10], F32)
        for dst, src in ((k.lru_cw, k.lru_cw_in), (k.lru_cb, k.lru_cb_in), (k.lru_bg, k.lru_bg_in), (k.lru_lam, k.lru_lam_in)):
            P.op('sp', lambda e, dst=dst, src=src: e.dma_start(out=dst, in_=src), writes=['lrup'], dma=True)
        k.ret_ld = sb(st, nc, "ret_ld_s", [128, 8], F32)
        k.ret_gain = sb(st, nc, "ret_gain_s", [128, 16], F32)
        for dst, src in ((k.ret_ld, k.ret_ld_in), (k.ret_gain, k.ret_gain_in)):
            P.op('sp', lambda e, dst=dst, src=src: e.dma_start(out=dst, in_=src), writes=['retp'], dma=True)
        k.swa_sink = sb(st, nc, "swa_sink_s", [64, 16], F32)
        P.op('sp', lambda e: e.dma_start(out=k.swa_sink, in_=k.swa_sink_in), writes=['swap'], dma=True)
        for dst, src, key in ((k.adab_s, k.adab, 'adab'), (k.ng_s, k.ng, 'ng'), (k.fg_s, k.fg, 'fg'),
                              (k.ones_f, k.ones_in, 'ones'), (k.ident_f, k.ident_in, 'ident'),
                              (k.esel, k.esel_in, 'esel'), (k.rt, k.router, 'rt')):
            P.op('sp', lambda e, dst=dst, src=src: e.dma_start(out=dst, in_=src), writes=[key], dma=True)

        if 'init' in parts:
            P.op('sp', lambda e: e.dma_start(out=k.X, in_=k.xT), writes=['X'], dma=True)
        k.precast_done = set()
        k.cond_done = False
        k.mods_split = ('mods' in parts and 'mix0' in parts)
        k.fuse_final = ('final' in parts and 'ffn3' in parts and SPARSE_MOE and not debug)
        k.pc_queue = []
        if 'mods' in parts:
            stage_mods(k, layers=(0,) if k.mods_split else (0, 1, 2, 3))
        for i in range(4):
            if f'ffn{i}' in parts:
                if SPARSE_MOE and i % 2 == 1:
                    for e8 in range(8):
                        k.pc_queue.append(lambda pe=(i // 2) * 8 + e8: precast_moe(k, pe))
                else:
                    for e8 in (range(8) if i % 2 == 1 else range(1)):
                        k.pc_queue.append(lambda p=ffn_pass_index(i, e8): precast(k, p))
            if f'mix{i}' in parts:
                if i % 3 == 0:
                    stage_lru(k, i)
                elif i % 3 == 1:
                    stage_ret(k, i)
                else:
                    stage_swa(k, i)
            if f'ffn{i}' in parts:
                if SPARSE_MOE and i % 2 == 1:
                    stage_moe_sparse(k, i)
                else:
                    drain_precast(k, 1000)
                    stage_ffn(k, i)
        if 'final' in parts and not k.fuse_final:
            stage_final(k)
        P.barrier()
        if debug:
            P.op('sp', lambda e: e.dma_start(out=k.out, in_=k.X), dma=True)
        P.finish()
        k.n_inst = P.n_inst
    return nc, k


def psum(k):
    b = k.ps_n % 8
    k.ps_n += 1
    return b, k.PS[b]


def stage_mods(k, layers=(0, 1, 2, 3), final_barrier=True, outer=None, cbw=512):
    nc, P = k.nc, k.P
    with ExitStack() as own:
        st = outer if outer is not None else own
        wa = [sb(st, nc, f"wa{j}", [128, 8, cbw], F32) for j in range(2)]
        mrow = [sb(st, nc, f"mrow{j}", [2, cbw], F32) for j in range(2)]
        nj = cbw // 128
        if not k.cond_done:
            ccs = sb(st, nc, "ccs", [128, 8, 2], F32)
            P.op('sp', lambda e: e.dma_start(out=ccs, in_=k.cc), writes=['ccs'], dma=True)
            P.op('act', lambda e: e.activation(out=k.cond, in_=ccs, func=AF.Silu), reads=['ccs'], writes=['cond'])
            k.cond_done = True
        n = 0
        for i in layers:
            wsrc = k.ada_w[i].rearrange("(kc p) j -> p kc j", p=128)
            for cb in range(6144 // cbw):
                buf = wa[n % 2]
                key = ('wa', n % 2)
                mr = mrow[n % 2]
                kmr = ('mrow', n % 2)
                P.op('sp', lambda e: e.dma_start(out=buf, in_=wsrc[:, :, cb * cbw:(cb + 1) * cbw]), writes=[key], dma=True)
                b, ps = psum(k)
                for kc in range(8):
                    P.op('pe', lambda e: e.matmul(ps[0:2, 0:cbw], lhsT=k.cond[:, kc, :], rhs=buf[:, kc, :], start=(kc == 0), stop=(kc == 7)),
                         reads=[key, 'cond'], writes=[('ps', b)])
                P.op('dve', lambda e: e.tensor_copy(out=mr, in_=ps[0:2, 0:cbw]), reads=[('ps', b)], writes=[kmr])
                b2, ps2 = psum(k)
                for jj in range(nj):
                    P.op('pe', lambda e: e.transpose(out=ps2[:, jj * 2:jj * 2 + 2], in_=mr[0:2, jj * 128:(jj + 1) * 128], identity=k.ident_f[0:2, 0:2]),
                         reads=[kmr, 'ident'], writes=[('ps', b2)])
                P.op('dve', lambda e: e.tensor_tensor(out=k.mods[:, i, cb * nj:(cb + 1) * nj, :], in0=ps2[:, 0:2 * nj].rearrange("p (j s) -> p j s", s=2),
                                                      in1=k.adab_s[:, i, cb * nj:(cb + 1) * nj].unsqueeze(2).to_broadcast([128, nj, 2]), op=ALU.add),
                     reads=[('ps', b2), 'adab'], writes=['mods'])
                n += 1
            for j in range(2):
                for s in range(2):
                    m = (1 + 3 * j) * 8
                    P.op('dve', lambda e: e.tensor_scalar(out=k.acoef[:, i, j, :, s], in0=k.mods[:, i, m:m + 8, s],
                                                          scalar1=1.0, scalar2=None, op0=ALU.add),
                         reads=['mods'], writes=['acoef'])
                    P.op('dve', lambda e: e.tensor_tensor(out=k.acoef[:, i, j, :, s], in0=k.acoef[:, i, j, :, s],
                                                          in1=k.ng_s[:, i, j, :], op=ALU.mult),
                         reads=['acoef', 'ng'], writes=['acoef'])
    if final_barrier:
        P.barrier()


def norm_tile(k, xt, kx, n, i, j, s, hb, kh, sq, rs, tmps, hf=None, khf=None):
    P = k.P
    sqb = sq.bitcast(BF16)[:, :, 0:512]
    P.op('act', lambda e: e.activation(out=sqb[:, :, :n], in_=xt[:, :, :n], func=AF.Square), reads=[kx], writes=['sq'])
    b, ps = psum(k)
    for kc in range(8):
        P.op('pe', lambda e: e.matmul(ps[:, :n], lhsT=k.ones_b[:, :], rhs=sqb[:, kc, :n], start=(kc == 0), stop=(kc == 7)),
             reads=['sq', 'ones_b'], writes=[('ps', b)])
    P.op('act', lambda e: e.activation(out=rs[:, :n], in_=ps[:, :n], func=AF.Ln, scale=1.0 / D, bias=k.epsb[:, 0:1]),
         reads=[('ps', b)], writes=['rs'])
    P.op('act', lambda e: e.activation(out=rs[:, :n], in_=rs[:, :n], func=AF.Exp, scale=-0.5), reads=['rs'], writes=['rs'])
    for kc in range(8):
        tmp = tmps[kc % 2]
        kt = ('ntmp', kc % 2)
        P.op('dve', lambda e: e.scalar_tensor_tensor(out=tmp[:, :n], in0=xt[:, kc, :n], scalar=k.acoef[:, i, j, kc, s:s + 1],
                                                     in1=rs[:, :n], op0=ALU.mult, op1=ALU.mult),
             reads=[kx, 'rs', 'acoef'], writes=[kt])
        sh = k.mods[:, i, (3 * j) * 8 + kc, s:s + 1]
        if hf is None:
            P.op('act', lambda e: e.activation(out=hb[:, kc, :n], in_=tmp[:, :n], func=AF.Identity, bias=sh, scale=1.0),
                 reads=[kt, 'mods'], writes=[kh])
        else:
            P.op('act', lambda e: e.activation(out=hf[:, kc, :n], in_=tmp[:, :n], func=AF.Identity, bias=sh, scale=1.0),
                 reads=[kt, 'mods'], writes=[khf])
    if hf is not None:
        P.op('dve', lambda e: e.tensor_copy(out=hb[:, :, :n], in_=hf[:, :, :n]), reads=[khf], writes=[kh])


def ffn_pass_index(i, e):
    return {0: 0, 1: 1 + e, 2: 9, 3: 10 + e}[i]


def precast(k, p):
    if p in k.precast_done:
        return
    k.precast_done.add(p)
    P = k.P
    if p == 0:
        gu, dn = k.ffn_w_gu[0], k.ffn_w_down[0]
    elif p == 9:
        gu, dn = k.ffn_w_gu[1], k.ffn_w_down[1]
    elif p < 9:
        gu, dn = k.moe_w_gu[0, p - 1], k.moe_w_down[0, p - 1]
    else:
        gu, dn = k.moe_w_gu[1, p - 10], k.moe_w_down[1, p - 10]
    P.op('pool', lambda e: e.dma_start(out=k.WGU[p].rearrange("r (a b) -> (r a) b", b=1024),
                                       in_=gu.rearrange("r (a b) -> (r a) b", b=1024)),
         writes=[('wgus', p)], dma=True)
    P.op('pool', lambda e: e.dma_start(out=k.WD[p], in_=dn), writes=[('wds', p)], dma=True)


def stage_ffn(k, i):
    nc, P = k.nc, k.P
    moe = (i % 2 == 1)
    last = (i == 3)
    f = i // 2
    experts = list(range(8)) if moe else [0]
    tiles = TILES[1:] if last else TILES
    precast(k, ffn_pass_index(i, 0))
    with ExitStack() as st:
        wgu = [sb(st, nc, f"wgu{j}", [128, 8, 2, 512], BF16) for j in range(2)]
        wd = [sb(st, nc, f"wd{j}", [128, 28, 256], BF16) for j in range(2)]
        nbuf = 1 if moe else 2
        Hs = [sb(st, nc, f"Hb{q}", [128, 8, 512], BF16) for q in range(nbuf)]
        xts = [sb(st, nc, f"xt{q}", [128, 8, 512], F32) for q in range(nbuf)]
        act = sb(st, nc, "actb", [128, 28, 512], BF16)
        sq = sb(st, nc, "sq", [128, 8, 512], F32)
        rs = sb(st, nc, "rs", [128, 512], F32)
        tmps = [sb(st, nc, f"ntmp{j}", [128, 512], F32) for j in range(2)]
        sg = [sb(st, nc, f"sg{j}", [128, 512], BF16) for j in range(2)]
        if moe:
            hf = sb(st, nc, "hf", [128, 8, 512], F32)
            yacc = sb(st, nc, "yacc", [128, 8, 512], F32)
            cmul = [sb(st, nc, f"cmul{j}", [128, 512], F32) for j in range(2)]
            combT = sb(st, nc, "combT", [8, 512], F32)
            rsm = sb(st, nc, "rsm", [128, 64], F32)

        seq_gu = [(ti, e, g) for ti in range(len(tiles)) for e in experts for g in range(7)]
        seq_wd = [(ti, e, q) for ti in range(len(tiles)) for e in experts for q in range(4)]
        st_gu = {'n': 0}
        st_wd = {'n': 0}

        def ensure_gu(upto):
            while st_gu['n'] <= upto and st_gu['n'] < len(seq_gu):
                a = st_gu['n']
                ti, e, g = seq_gu[a]
                p = ffn_pass_index(i, e)
                if ti == 0:
                    precast(k, p)
                buf = wgu[a % 2]
                for hh in range(2):
                    src = k.WGU[p].rearrange("(kc q) c -> q kc c", q=128)[:, :, hh * DFF + g * 512:hh * DFF + (g + 1) * 512]
                    P.op('sp', lambda e_: e_.dma_start(out=buf[:, :, hh, :], in_=src), reads=[('wgus', p)],
                         writes=[('wgu', a % 2, hh)], dma=True)
                st_gu['n'] += 1

        def ensure_wd(upto):
            while st_wd['n'] <= upto and st_wd['n'] < len(seq_wd):
                a = st_wd['n']
                ti, e, q = seq_wd[a]
                p = ffn_pass_index(i, e)
                src = k.WD[p].rearrange("(fc q) d -> q fc d", q=128)[:, :, q * 256:(q + 1) * 256]
                buf = wd[a % 2]
                P.op('sp' if moe else 'act', lambda e_: e_.dma_start(out=buf, in_=src), reads=[('wds', p)], writes=[('wd', a % 2)], dma=True)
                st_wd['n'] += 1

        a_gu = 0
        a_wd = 0
        nsg = 0
        def prep(ti):
            c0, n, s = tiles[ti]
            xt, H = xts[ti % nbuf], Hs[ti % nbuf]
            kxt, kH = ('xt', ti % nbuf), ('H', ti % nbuf)
            P.op('sp', lambda e: e.dma_start(out=xt[:, :, :n], in_=k.X.rearrange("(kc q) t -> q kc t", q=128)[:, :, c0:c0 + n]),
                 reads=['X', ('Xt', c0)], writes=[kxt], dma=True)
            if moe:
                norm_tile(k, xt, kxt, n, i, 1, s, H, kH, sq, rs, tmps, hf=hf, khf='hf')
                route_tile(k, f, n, hf, combT, rsm)
            else:
                norm_tile(k, xt, kxt, n, i, 1, s, H, kH, sq, rs, tmps)

        ensure_gu(0)
        if not moe:
            prep(0)
        for ti, (c0, n, s) in enumerate(tiles):
            xt, H = xts[ti % nbuf], Hs[ti % nbuf]
            kxt, kH = ('xt', ti % nbuf), ('H', ti % nbuf)
            if moe:
                prep(ti)
            elif ti + 1 < len(tiles):
                prep(ti + 1)
            ensure_gu(a_gu)
            for e in experts:
                if moe:
                    cm = cmul[e % 2]
                    kcm = ('cmul', e % 2)
                    b, ps = psum(k)
                    P.op('pe', lambda e_: e_.matmul(ps[:, :n], lhsT=k.esel[0:8, e, :], rhs=combT[0:8, :n], start=True, stop=True),
                         reads=['esel', 'combT'], writes=[('ps', b)])
                    P.op('act', lambda e_: e_.activation(out=cm[:, :n], in_=ps[:, :n], func=AF.Copy),
                         reads=[('ps', b)], writes=[kcm])
                ensure_wd(a_wd)
                for g in range(7):
                    ensure_gu(a_gu + 1)
                    buf = wgu[a_gu % 2]
                    kb0 = ('wgu', a_gu % 2, 0)
                    kb1 = ('wgu', a_gu % 2, 1)
                    for j in range(4):
                        fch = g * 4 + j
                        bg, psg = psum(k)
                        for kc in range(8):
                            P.op('pe', lambda e_: e_.matmul(psg[:, :n], lhsT=buf[:, kc, 0, j * 128:(j + 1) * 128], rhs=H[:, kc, :n],
                                                            start=(kc == 0), stop=(kc == 7)),
                                 reads=[kb0, kH], writes=[('ps', bg)])
                        bu, psu = psum(k)
                        for kc in range(8):
                            P.op('pe', lambda e_: e_.matmul(psu[:, :n], lhsT=buf[:, kc, 1, j * 128:(j + 1) * 128], rhs=H[:, kc, :n],
                                                            start=(kc == 0), stop=(kc == 7)),
                                 reads=[kb1, kH], writes=[('ps', bu)])
                        sgt = sg[nsg % 2]
                        ksg = ('sg', nsg % 2)
                        nsg += 1
                        P.op('act', lambda e_: e_.activation(out=sgt[:, :n], in_=psg[:, :n], func=AF.Silu),
                             reads=[('ps', bg)], writes=[ksg])
                        P.op('dve', lambda e_: e_.tensor_tensor(out=act[:, fch, :n], in0=sgt[:, :n], in1=psu[:, :n], op=ALU.mult),
                             reads=[ksg, ('ps', bu)], writes=[('act', fch)])
                    a_gu += 1
                ensure_gu(a_gu)
                for q in range(4):
                    ensure_wd(a_wd + 1)
                    buf = wd[a_wd % 2]
                    kb = ('wd', a_wd % 2)
                    for dj in range(2):
                        dc = q * 2 + dj
                        b, ps = psum(k)
                        for fc in range(28):
                            P.op('pe', lambda e_: e_.matmul(ps[:, :n], lhsT=buf[:, fc, dj * 128:(dj + 1) * 128], rhs=act[:, fc, :n],
                                                            start=(fc == 0), stop=(fc == 27)),
                                 reads=[kb, ('act', fc)], writes=[('ps', b)])
                        gate = k.mods[:, i, 5 * 8 + dc, s:s + 1]
                        if not moe:
                            P.op('dve', lambda e_: e_.scalar_tensor_tensor(out=xt[:, dc, :n], in0=ps[:, :n], scalar=gate,
                                                                           in1=xt[:, dc, :n], op0=ALU.mult, op1=ALU.add),
                                 reads=[('ps', b), kxt, 'mods'], writes=[kxt])
                        else:
                            if e == 0:
                                P.op('dve', lambda e_: e_.tensor_tensor(out=yacc[:, dc, :n], in0=ps[:, :n], in1=cm[:, :n], op=ALU.mult),
                                     reads=[('ps', b), kcm], writes=[('yacc', dc)])
                            else:
                                tmp = tmps[dc % 2]
                                kt = ('ntmp', dc % 2)
                                P.op('dve', lambda e_: e_.tensor_tensor(out=tmp[:, :n], in0=ps[:, :n], in1=cm[:, :n], op=ALU.mult),
                                     reads=[('ps', b), kcm], writes=[kt])
                                P.op('dve', lambda e_: e_.tensor_tensor(out=yacc[:, dc, :n], in0=yacc[:, dc, :n], in1=tmp[:, :n], op=ALU.add),
                                     reads=[kt, ('yacc', dc)], writes=[('yacc', dc)])
                            if e == experts[-1]:
                                P.op('dve', lambda e_: e_.scalar_tensor_tensor(out=xt[:, dc, :n], in0=yacc[:, dc, :n], scalar=gate,
                                                                               in1=xt[:, dc, :n], op0=ALU.mult, op1=ALU.add),
                                     reads=[('yacc', dc), kxt, 'mods'], writes=[kxt])
                    a_wd += 1
            P.op('sp', lambda e: e.dma_start(out=k.X.rearrange("(kc q) t -> q kc t", q=128)[:, :, c0:c0 + n], in_=xt[:, :, :n]),
                 reads=[kxt], writes=[('Xt', c0)], dma=True)
    P.barrier()


def route_tile(k, f, n, hf, combT, rsm):
    P = k.P
    for bi in range(n // 128):
        b, ps = psum(k)
        for kc in range(8):
            P.op('pe', lambda e: e.matmul(ps[:, 0:8], lhsT=hf[:, kc, bi * 128:(bi + 1) * 128], rhs=k.rt[:, f, kc, :],
                                          start=(kc == 0), stop=(kc == 7)),
                 reads=['hf', 'rt'], writes=[('ps', b)])
        lg, eq1, lg2, eq2, comb = (rsm[:, 8 * j:8 * j + 8] for j in range(5))
        m1, m2, dd, w1, w2 = (rsm[:, 40 + j:41 + j] for j in range(5))
        R = ['rsm']
        P.op('dve', lambda e: e.tensor_copy(out=lg, in_=ps[:, 0:8]), reads=[('ps', b)], writes=R)
        P.op('dve', lambda e: e.tensor_reduce(out=m1, in_=lg, axis=AX.X, op=ALU.max), reads=R, writes=R)
        P.op('dve', lambda e: e.tensor_scalar(out=eq1, in0=lg, scalar1=m1, scalar2=None, op0=ALU.is_equal), reads=R, writes=R)
        P.op('dve', lambda e: e.scalar_tensor_tensor(out=lg2, in0=eq1, scalar=-1e30, in1=lg, op0=ALU.mult, op1=ALU.add), reads=R, writes=R)
        P.op('dve', lambda e: e.tensor_reduce(out=m2, in_=lg2, axis=AX.X, op=ALU.max), reads=R, writes=R)
        P.op('dve', lambda e: e.tensor_scalar(out=eq2, in0=lg2, scalar1=m2, scalar2=None, op0=ALU.is_equal), reads=R, writes=R)
        P.op('dve', lambda e: e.tensor_tensor(out=dd, in0=m2, in1=m1, op=ALU.subtract), reads=R, writes=R)
        P.op('act', lambda e: e.activation(out=w1, in_=dd, func=AF.Sigmoid, scale=-1.0), reads=R, writes=R)
        P.op('act', lambda e: e.activation(out=w2, in_=dd, func=AF.Sigmoid, scale=1.0), reads=R, writes=R)
        P.op('dve', lambda e: e.tensor_scalar(out=comb, in0=eq1, scalar1=w1, scalar2=None, op0=ALU.mult), reads=R, writes=R)
        P.op('dve', lambda e: e.scalar_tensor_tensor(out=comb, in0=eq2, scalar=w2, in1=comb, op0=ALU.mult, op1=ALU.add), reads=R, writes=R)
        b2, ps2 = psum(k)
        P.op('pe', lambda e: e.transpose(out=ps2[0:8, 0:128], in_=comb, identity=k.ident_f[:, :]),
             reads=R + ['ident'], writes=[('ps', b2)])
        P.op('act', lambda e: e.activation(out=combT[0:8, bi * 128:(bi + 1) * 128], in_=ps2[0:8, 0:128], func=AF.Copy),
             reads=[('ps', b2)], writes=['combT'])


def _consts():
    ones = np.ones((128, 128), np.float32)
    ident = np.eye(128, dtype=np.float32)
    esel = np.zeros((8, 8, 128), np.float32)
    for e in range(8):
        esel[e, e, :] = 1.0
    return ones, ident, esel


def _ret_consts():
    f32 = np.float32
    theta = (f32(10000.0) ** (-np.arange(128, dtype=f32) / f32(128))).astype(f32)
    ang = (np.arange(SEQ, dtype=f32)[:, None] * theta[None, :]).astype(f32)
    cos, sin = np.cos(ang).astype(f32), np.sin(ang).astype(f32)
    p = np.arange(128, dtype=f32)[:, None]
    fr = np.arange(128, dtype=f32)[None, :]
    tab = np.zeros((128, 771), f32)
    tab[:, 0:128] = np.maximum(fr - p, 0)
    tab[:, 128:256] = np.maximum(p - fr, 0)
    tab[:, 256:384] = (fr >= p)
    tab[:, 384:512] = (p >= fr)
    tab[:, 512:640] = fr + 1
    tab[:, 640:768] = 128 - fr
    tab[:, 768] = 127 - p[:, 0]
    tab[:, 769] = p[:, 0]
    tab[:, 770] = 128
    return (np.ascontiguousarray(cos.T), np.ascontiguousarray(sin.T), cos, sin, tab)


def _swa_consts(w_in):
    f32 = np.float32
    qk = w_in[:, :1280].reshape(D, 20, 2, 32)
    sw = qk[:, :, ::-1, :].reshape(D, 1280)
    w_ext = np.ascontiguousarray(np.concatenate([w_in, sw], axis=1))
    rows = SEQ // 64
    row = np.repeat(np.arange(rows, dtype=f32), 64)
    colp = np.tile(np.arange(64, dtype=f32), rows)
    freq = (f32(10000.0) ** (-np.arange(16, dtype=f32) / f32(16))).astype(f32)
    ang = np.concatenate([row[:, None] * freq, colp[:, None] * freq], axis=-1).astype(f32)
    cos, sin = np.cos(ang).astype(f32), np.sin(ang).astype(f32)
    cos_full = np.concatenate([cos, cos], axis=1).T
    sin_signed = np.concatenate([-sin, sin], axis=1).T
    return w_ext, np.ascontiguousarray(np.tile(cos_full, (2, 1))), np.ascontiguousarray(np.tile(sin_signed, (2, 1)))


def make_in_maps(inp, cores):
    ones, ident, esel = _consts()
    adab = np.ascontiguousarray(inp['ada_b'].reshape(4, 48, 128).transpose(2, 0, 1))
    ng = np.ascontiguousarray(inp['norm_g'].reshape(4, 2, 8, 128).transpose(3, 0, 1, 2))
    fg = col(inp['final_g'], 8)
    router = np.ascontiguousarray(inp['moe_router'].reshape(2, 8, 128, 8).transpose(2, 0, 1, 3))
    lru_wg = np.ascontiguousarray(inp['lru_w_gate'].transpose(0, 4, 1, 2, 3, 5).reshape(2, 128, 40, 128))
    lru_cw = np.ascontiguousarray(inp['lru_conv_w'].reshape(2, 4, 10, 128).transpose(3, 0, 1, 2))
    lru_cb = np.ascontiguousarray(inp['lru_conv_b'].reshape(2, 10, 128).transpose(2, 0, 1))
    lru_bg = np.ascontiguousarray(inp['lru_b_gate'].reshape(2, 2, 2, 10, 128).transpose(4, 0, 1, 2, 3))
    lru_lam = np.ascontiguousarray(inp['lru_lambda'].reshape(2, 2, 10, 128).transpose(3, 0, 1, 2))
    ret_ld = np.ascontiguousarray(np.broadcast_to(inp['ret_log_decay'].reshape(1, 8), (128, 8))).astype(np.float32)
    ret_gain = col(inp['ret_gn_gain'][0], 16)
    ret_cosT, ret_sinT, ret_costok, ret_sintok, ret_tab = _ret_consts()
    swa_w_ext, swa_cos, swa_sin = _swa_consts(inp['swa_w_in'][0])
    swa_sink = np.ascontiguousarray(np.broadcast_to(inp['swa_sink'].reshape(1, 16), (64, 16))).astype(np.float32)
    adab_row = np.ascontiguousarray(np.broadcast_to(inp['ada_b'][:, None, :], (4, 2, 6144))).astype(np.float32)
    moe_tab = np.zeros((128, 161), np.float32)
    pp_ = np.arange(128, dtype=np.float32)
    moe_tab[:, 0:128] = (pp_[:, None] < pp_[None, :])
    moe_tab[:, 128:152] = np.arange(24, dtype=np.float32)[None, :]
    moe_tab[:, 152:159] = np.arange(7, dtype=np.float32)[None, :] * 128 + pp_[:, None]
    moe_tab[:, 159:161] = np.arange(2, dtype=np.float32)[None, :] * 128 + pp_[:, None]
    maps = []
    for b in cores:
        xT = np.ascontiguousarray(np.concatenate([inp['ctx'][b], inp['x'][b]], axis=0).T)
        cc = np.ascontiguousarray(np.stack([inp['c'][b], inp['c_ctx']], -1).reshape(8, 128, 2).transpose(1, 0, 2))
        maps.append(dict(adab_row=adab_row, moe_tab=moe_tab, swa_w_ext=swa_w_ext, swa_w_out=inp['swa_w_out'], swa_sink=swa_sink, swa_cos=swa_cos, swa_sin=swa_sin,
                         ret_w_in=inp['ret_w_in'], ret_w_out=inp['ret_w_out'], ret_ld=ret_ld, ret_gain=ret_gain, ret_cosT=ret_cosT,
                         ret_sinT=ret_sinT, ret_costok=ret_costok, ret_sintok=ret_sintok, ret_tab=ret_tab,
                         lru_w_in=inp['lru_w_in'], lru_wg=lru_wg, lru_w_out=inp['lru_w_out'], lru_cw=lru_cw, lru_cb=lru_cb,
                         lru_bg=lru_bg, lru_lam=lru_lam, xT=xT, cc=cc, ada_w=inp['ada_w'], adab=adab, ng=ng, fg=fg, ones_f=ones, ident_f=ident,
                         esel=esel, ffn_w_gu=inp['ffn_w_gu'], ffn_w_down=inp['ffn_w_down'], router=router,
                         moe_w_gu=inp['moe_w_gu'], moe_w_down=inp['moe_w_down']))
    return maps


def xrow(k):
    return k.X.rearrange("(kc q) t -> q kc t", q=128)


def load_x_tile(k, xt, c0, n, key='xt'):
    k.P.op('sp', lambda e: e.dma_start(out=xt[:, :, :n], in_=xrow(k)[:, :, c0:c0 + n]),
           reads=['X', ('Xt', c0)], writes=[key], dma=True)


def store_x_tile(k, xt, c0, n):
    k.P.op('sp', lambda e: e.dma_start(out=xrow(k)[:, :, c0:c0 + n], in_=xt[:, :, :n]),
           reads=['xt'], writes=[('Xt', c0)], dma=True)


def out_proj_residual(k, i, tiles, Zd, nz, kz, wo, zt_shape_k, lhs_fn):
    nc, P = k.nc, k.P
    with ExitStack() as st:
        xt = sb(st, nc, "xt_o", [128, 8, 512], F32)
        zt = [sb(st, nc, f"zt_o{j}", [zt_shape_k, nz, 512], BF16) for j in range(2)]
        for ti, (c0, n, s) in enumerate(tiles):
            z = zt[ti % 2]
            kzt = ('zt', ti % 2)
            P.op('sp', lambda e: e.dma_start(out=z[:, :, :n], in_=Zd[:, :, c0:c0 + n]), reads=[kz], writes=[kzt], dma=True)
            load_x_tile(k, xt, c0, n)
            for dc in range(8):
                b, ps = psum(k)
                for zc in range(nz):
                    P.op('pe', lambda e: e.matmul(ps[:, :n], lhsT=lhs_fn(zc, dc), rhs=z[:, zc, :n], start=(zc == 0), stop=(zc == nz - 1)),
                         reads=[kzt, 'wo'], writes=[('ps', b)])
                gate = k.mods[:, i, 2 * 8 + dc, s:s + 1]
                P.op('dve', lambda e: e.scalar_tensor_tensor(out=xt[:, dc, :n], in0=ps[:, :n], scalar=gate, in1=xt[:, dc, :n],
                                                             op0=ALU.mult, op1=ALU.add),
                     reads=[('ps', b), 'xt', 'mods'], writes=['xt'])
            store_x_tile(k, xt, c0, n)
    P.barrier()


def stage_lru(k, i):
    nc, P = k.nc, k.P
    j = i // 3
    last = (i == 3)
    XR, YG, ZG = k.XR, k.YG, k.ZG
    with ExitStack() as st:
        win = sb(st, nc, "lru_win", [128, 8, 2560], BF16)
        for h2 in range(2):
            P.op('pool', lambda e: e.dma_start(out=win[:, :, h2 * 1280:(h2 + 1) * 1280],
                                               in_=k.lru_w_in[j].rearrange("(kc q) c -> q kc c", q=128)[:, :, h2 * 1280:(h2 + 1) * 1280]),
                 writes=['win'], dma=True)
        drain_precast(k, 3)
        Hs = [sb(st, nc, f"H1{q}", [128, 8, 512], BF16) for q in range(2)]
        xts = [sb(st, nc, f"xt1{q}", [128, 8, 512], F32) for q in range(2)]
        sq = sb(st, nc, "sq1", [128, 8, 512], F32)
        rs = sb(st, nc, "rs1", [128, 512], F32)
        tmps = [sb(st, nc, f"nt1{q}", [128, 512], F32) for q in range(2)]
        xrt = sb(st, nc, "xrt", [128, 10, 512], F32)
        ygt = sb(st, nc, "ygt", [128, 10, 512], BF16)
        g1 = [sb(st, nc, f"g1{q}", [128, 512], F32) for q in range(2)]
        g2 = [sb(st, nc, f"g2{q}", [128, 512], F32) for q in range(2)]
        ng = 0
        def prep(ti):
            c0, n, s = TILES[ti]
            q = ti % 2
            load_x_tile(k, xts[q], c0, n, key=('xt', q))
            norm_tile(k, xts[q], ('xt', q), n, i, 0, s, Hs[q], ('H', q), sq, rs, tmps)
        prep(0)
        for ti, (c0, n, s) in enumerate(TILES):
            H, kH = Hs[ti % 2], ('H', ti % 2)
            if ti + 1 < len(TILES):
                prep(ti + 1)
            for oc in range(20):
                b, ps = psum(k)
                for kc in range(8):
                    P.op('pe', lambda e: e.matmul(ps[:, :n], lhsT=win[:, kc, oc * 128:(oc + 1) * 128], rhs=H[:, kc, :n],
                                                  start=(kc == 0), stop=(kc == 7)),
                         reads=['win', kH], writes=[('ps', b)])
                if oc < 10:
                    P.op('act', lambda e: e.activation(out=ygt[:, oc, :n], in_=ps[:, :n], func=AF.Gelu_apprx_tanh),
                         reads=[('ps', b)], writes=['ygt'])
                else:
                    P.op('act', lambda e: e.activation(out=xrt[:, oc - 10, :n], in_=ps[:, :n], func=AF.Copy),
                         reads=[('ps', b)], writes=['xrt'])
            P.op('sp', lambda e: e.dma_start(out=YG.rearrange("(m q) t -> q m t", q=128)[:, :, c0:c0 + n], in_=ygt[:, :, :n]),
                 reads=['ygt'], writes=['YG'], dma=True)
            P.op('sp', lambda e: e.dma_start(out=XR.rearrange("(m q) t -> q m t", q=128)[:, :, c0:c0 + n], in_=xrt[:, :, :n]),
                 reads=['xrt'], writes=['XR'], dma=True)
    P.barrier()
    with ExitStack() as st:
        wg = sb(st, nc, "lru_wg", [128, 40, 128], BF16)
        for q in range(4):
            P.op('pool', lambda e: e.dma_start(out=wg[:, q * 10:(q + 1) * 10, :], in_=k.lru_wg[j][:, q * 10:(q + 1) * 10, :]),
                 writes=['wg'], dma=True)
        drain_precast(k, 1000)
        if i == 0 and k.mods_split:
            stage_mods(k, layers=(1, 2, 3), final_barrier=False, outer=st, cbw=256)
        B1 = sb(st, nc, "B1", [128, T], F32)
        B2 = sb(st, nc, "B2", [128, T], F32)
        B3 = sb(st, nc, "B3", [128, T], F32)
        B4 = sb(st, nc, "B4", [128, T], F32)
        B5 = sb(st, nc, "B5", [128, T], F32)
        B6 = sb(st, nc, "B6", [128, T], F32)
        B7 = sb(st, nc, "B7", [128, T], F32)
        ub = sb(st, nc, "ub", [128, T], BF16)
        ygc = sb(st, nc, "ygc", [128, T], BF16)
        zc_ = sb(st, nc, "zc", [128, T], BF16)
        sp8 = sb(st, nc, "sp8", [128, 2, 10], F32)
        P.op('act', lambda e: e.activation(out=sp8, in_=k.lru_lam[:, j], func=AF.Exp, scale=-1.0), reads=['lrup'], writes=['sp8'])
        P.op('act', lambda e: e.activation(out=sp8, in_=sp8, func=AF.Ln, bias=k.oneb[:, 0:1], scale=1.0), reads=['sp8'], writes=['sp8'])
        P.op('dve', lambda e: e.tensor_scalar(out=sp8, in0=sp8, scalar1=-8.0, scalar2=None, op0=ALU.mult), reads=['sp8'], writes=['sp8'])
        segs = [(0, NCTX), (NCTX, T)]
        allk = lambda nm: [(nm, ti) for ti in range(len(TILES))]
        PCS = [(0, 1280, (0, 1, 2)), (1280, 2816, (3, 4, 5)), (2816, T, (6, 7, 8))]
        pk = lambda nm, tl: [(nm, ti) for ti in tl]
        for m in range(10):
            P.op('sp', lambda e: e.dma_start(out=B1, in_=XR[m * 128:(m + 1) * 128, :]), reads=['XR'], writes=allk('B1'), dma=True)
            P.op('sp', lambda e: e.dma_start(out=ygc, in_=YG[m * 128:(m + 1) * 128, :]), reads=['YG'], writes=['ygc'], dma=True)
            cw = lambda t: k.lru_cw[:, j, t, m:m + 1]
            for (p0, p1, tl) in PCS:
                P.op('dve', lambda e: e.tensor_scalar(out=B2[:, p0:p1], in0=B1[:, p0:p1], scalar1=cw(2), scalar2=k.lru_cb[:, j, m:m + 1],
                                                      op0=ALU.mult, op1=ALU.add),
                     reads=allk('B1') + ['lrup'], writes=pk('B2', tl))
                for o in (-2, -1, 1):
                    for (s0, s1) in segs:
                        a0 = max(s0 + max(0, -o), p0)
                        a1 = min(s1 - max(0, o), p1)
                        if a1 <= a0:
                            continue
                        P.op('dve', lambda e: e.scalar_tensor_tensor(out=B2[:, a0:a1], in0=B1[:, a0 + o:a1 + o], scalar=cw(o + 2),
                                                                     in1=B2[:, a0:a1], op0=ALU.mult, op1=ALU.add),
                             reads=allk('B1') + pk('B2', tl) + ['lrup'], writes=pk('B2', tl))
                P.op('act', lambda e: e.activation(out=ub[:, p0:p1], in_=B2[:, p0:p1], func=AF.Copy), reads=pk('B2', tl), writes=pk('ub', tl))
            for d in range(2):
                Ba, nA = (B3, 'B3') if d == 0 else (B7, 'B7')
                Bi, nI = (B1, 'B1') if d == 0 else (B6, 'B6')
                for ti, (c0, n, s) in enumerate(TILES):
                    b, ps = psum(k)
                    P.op('pe', lambda e: e.matmul(ps[:, :n], lhsT=wg[:, (d * 2 + 0) * 10 + m, :], rhs=ub[:, c0:c0 + n], start=True, stop=True),
                         reads=['wg', ('ub', ti)], writes=[('ps', b)])
                    P.op('act', lambda e: e.activation(out=Ba[:, c0:c0 + n], in_=ps[:, :n], func=AF.Sigmoid,
                                                       bias=k.lru_bg[:, j, d, 0, m:m + 1], scale=1.0),
                         reads=[('ps', b), 'lrup'], writes=[(nA, ti)])
                    b2, ps2 = psum(k)
                    P.op('pe', lambda e: e.matmul(ps2[:, :n], lhsT=wg[:, (d * 2 + 1) * 10 + m, :], rhs=ub[:, c0:c0 + n], start=True, stop=True),
                         reads=['wg', ('ub', ti)], writes=[('ps', b2)])
                    P.op('act', lambda e: e.activation(out=Bi[:, c0:c0 + n], in_=ps2[:, :n], func=AF.Sigmoid,
                                                       bias=k.lru_bg[:, j, d, 1, m:m + 1], scale=1.0),
                         reads=[('ps', b2), 'lrup'], writes=[(nI, ti)])
                for (p0, p1, tl) in PCS:
                    P.op('act', lambda e: e.activation(out=Ba[:, p0:p1], in_=Ba[:, p0:p1], func=AF.Exp, scale=sp8[:, d, m:m + 1]),
                         reads=pk(nA, tl) + ['sp8'], writes=pk(nA, tl))
                for (p0, p1, tl) in PCS:
                    P.op('act', lambda e: e.activation(out=B4[:, p0:p1], in_=Ba[:, p0:p1], func=AF.Square), reads=pk(nA, tl), writes=pk('B4', tl))
                for (p0, p1, tl) in PCS:
                    P.op('act', lambda e: e.activation(out=B4[:, p0:p1], in_=B4[:, p0:p1], func=AF.Sqrt, scale=-1.0, bias=k.oneb[:, 0:1]),
                         reads=pk('B4', tl), writes=pk('B4', tl))
                    P.op('dve', lambda e: e.tensor_tensor(out=Bi[:, p0:p1], in0=Bi[:, p0:p1], in1=B2[:, p0:p1], op=ALU.mult),
                         reads=pk(nI, tl) + pk('B2', tl), writes=pk(nI, tl))
                    P.op('dve', lambda e: e.tensor_tensor(out=Bi[:, p0:p1], in0=Bi[:, p0:p1], in1=B4[:, p0:p1], op=ALU.mult),
                         reads=pk(nI, tl) + pk('B4', tl), writes=pk(nI, tl))
                if d == 0:
                    P.op('dve', lambda e: e.tensor_tensor_scan(out=B5, data0=B3, data1=B1, initial=0.0, op0=ALU.mult, op1=ALU.add),
                         reads=allk('B3') + allk('B1'), writes=allk('B5'))
                else:
                    rv = lambda buf, a, b_: buf[:, a:b_][:, ::-1]
                    P.op('dve', lambda e: e.tensor_tensor_scan(out=rv(B4, 0, NCTX), data0=rv(B7, 0, NCTX), data1=rv(B6, 0, NCTX),
                                                               initial=0.0, op0=ALU.mult, op1=ALU.add),
                         reads=allk('B7') + allk('B6'), writes=allk('B4'))
                    P.op('dve', lambda e: e.tensor_tensor_scan(out=rv(B4, NCTX, T), data0=rv(B7, NCTX, T), data1=rv(B6, NCTX, T),
                                                               initial=B4[:, 0:1], op0=ALU.mult, op1=ALU.add),
                         reads=allk('B7') + allk('B6') + allk('B4'), writes=allk('B4'))
            for (p0, p1, tl) in PCS:
                P.op('dve', lambda e: e.tensor_tensor(out=B5[:, p0:p1], in0=B5[:, p0:p1], in1=B4[:, p0:p1], op=ALU.add),
                     reads=pk('B5', tl) + pk('B4', tl), writes=pk('B5', tl))
                P.op('dve', lambda e: e.tensor_tensor(out=zc_[:, p0:p1], in0=B5[:, p0:p1], in1=ygc[:, p0:p1], op=ALU.mult),
                     reads=pk('B5', tl) + ['ygc'], writes=pk('zc', tl))
            P.op('sp', lambda e: e.dma_start(out=ZG[m * 128:(m + 1) * 128, :], in_=zc_), reads=allk('zc'), writes=['ZG'], dma=True)
    P.barrier()
    with ExitStack() as st:
        wo = sb(st, nc, "lru_wo", [128, 10, 1024], BF16)
        P.op('pool', lambda e: e.dma_start(out=wo, in_=k.lru_w_out[j].rearrange("(m q) c -> q m c", q=128)), writes=['wo'], dma=True)
        out_proj_residual(k, i, TILES[1:] if last else TILES, ZG.rearrange("(m q) t -> q m t", q=128), 10, 'ZG', wo, 128,
                          lambda zc, dc: wo[:, zc, dc * 128:(dc + 1) * 128])


NCH = T // 128


def stage_ret(k, i):
    nc, P = k.nc, k.P
    QT, KT, KTOK, VTOK, GS, SBd, ZR = k.QT, k.KT, k.KTOK, k.VTOK, k.GS, k.SBd, k.ZR
    tab = k.ret_tab
    with ExitStack() as st:
        win = sb(st, nc, "ret_win", [128, 8, 6144], BF16)
        for q3 in range(3):
            P.op('pool', lambda e: e.dma_start(out=win[:, :, q3 * 2048:(q3 + 1) * 2048],
                                               in_=k.ret_w_in[0].rearrange("(kc q) c -> q kc c", q=128)[:, :, q3 * 2048:(q3 + 1) * 2048]),
                 writes=['win'], dma=True)
        drain_precast(k, 4)
        H = sb(st, nc, "H2", [128, 8, 512], BF16)
        xt = sb(st, nc, "xt2", [128, 8, 512], F32)
        sq = sb(st, nc, "sq2", [128, 8, 512], F32)
        rs = sb(st, nc, "rs2", [128, 512], F32)
        tmps = [sb(st, nc, f"nt2{q}", [128, 512], F32) for q in range(2)]
        qo = sb(st, nc, "qo", [128, 8, 512], BF16)
        cs = sb(st, nc, "cs", [128, 2, 512], F32)
        kt = sb(st, nc, "kt", [128, 1024], F32)
        kto = sb(st, nc, "kto", [128, 1024], BF16)
        vto = sb(st, nc, "vto", [128, 2048], BF16)
        cst = sb(st, nc, "cst", [128, 2, 128], F32)
        gso = sb(st, nc, "gso", [128, 4, 512], BF16)
        for ti, (c0, n, s) in enumerate(TILES):
            load_x_tile(k, xt, c0, n)
            norm_tile(k, xt, 'xt', n, i, 0, s, H, 'H', sq, rs, tmps)
            if s == 0:
                p0 = c0 - NCTX
                P.op('sp', lambda e: e.dma_start(out=cs[:, 0, :n], in_=k.ret_cosT[:, p0:p0 + n]), writes=['cs0'], dma=True)
                P.op('sp', lambda e: e.dma_start(out=cs[:, 1, :n], in_=k.ret_sinT[:, p0:p0 + n]), writes=['cs1'], dma=True)
            for (dst, kd_, off, scale) in ((QT, 'QT', 0, 1.0), (KT, 'KT', 1024, 0.0625)):
                for oc in range(8):
                    b, ps = psum(k)
                    for kc in range(8):
                        P.op('pe', lambda e: e.matmul(ps[:, :n], lhsT=win[:, kc, off + oc * 128:off + (oc + 1) * 128], rhs=H[:, kc, :n],
                                                      start=(kc == 0), stop=(kc == 7)),
                             reads=['win', 'H'], writes=[('ps', b)])
                    P.op('act', lambda e: e.activation(out=xt[:, oc, :n], in_=ps[:, :n], func=AF.Copy, scale=scale),
                         reads=[('ps', b)], writes=['xt'])
                if s == 0:
                    xv = xt.rearrange("p (h two) n -> p h two n", two=2)
                    qv = qo.rearrange("p (h two) n -> p h two n", two=2)
                    x1, x2 = xv[:, :, 0, :n], xv[:, :, 1, :n]
                    o1, o2 = qv[:, :, 0, :n], qv[:, :, 1, :n]
                    cosb = cs[:, 0, :n].unsqueeze(1).to_broadcast([128, 4, n])
                    sinb = cs[:, 1, :n].unsqueeze(1).to_broadcast([128, 4, n])
                    sv = sq.rearrange("p (two h) n -> p two h n", two=2)
                    ta, tb = sv[:, 0, :, :n], sv[:, 1, :, :n]
                    P.op('dve', lambda e: e.tensor_tensor(out=ta, in0=x1, in1=cosb, op=ALU.mult), reads=['xt', 'cs0', 'sq'], writes=['sq'])
                    P.op('dve', lambda e: e.tensor_tensor(out=tb, in0=x2, in1=sinb, op=ALU.mult), reads=['xt', 'cs1', 'sq'], writes=['sq'])
                    P.op('dve', lambda e: e.tensor_tensor(out=o1, in0=ta, in1=tb, op=ALU.subtract), reads=['sq'], writes=['qo'])
                    P.op('dve', lambda e: e.tensor_tensor(out=ta, in0=x2, in1=cosb, op=ALU.mult), reads=['xt', 'cs0', 'qo', 'sq'], writes=['sq'])
                    P.op('dve', lambda e: e.tensor_tensor(out=tb, in0=x1, in1=sinb, op=ALU.mult), reads=['xt', 'cs1', 'qo', 'sq'], writes=['sq'])
                    P.op('dve', lambda e: e.tensor_tensor(out=o2, in0=ta, in1=tb, op=ALU.add), reads=['sq'], writes=['qo'])
                else:
                    P.op('dve', lambda e: e.tensor_copy(out=qo[:, :, :n], in_=xt[:, :, :n]), reads=['xt'], writes=['qo'])
                P.op('sp', lambda e: e.dma_start(out=dst.rearrange("(oc q) t -> q oc t", q=128)[:, :, c0:c0 + n], in_=qo[:, :, :n]),
                     reads=['qo'], writes=[kd_], dma=True)
            for tb_ in range(n // 128):
                r0 = c0 + tb_ * 128
                if s == 0:
                    pp = r0 - NCTX
                    P.op('sp', lambda e: e.dma_start(out=cst[:, 0, :], in_=k.ret_costok[pp:pp + 128, :]), writes=['cst0'], dma=True)
                    P.op('sp', lambda e: e.dma_start(out=cst[:, 1, :], in_=k.ret_sintok[pp:pp + 128, :]), writes=['cst1'], dma=True)
                for half in range(2):
                    b, ps = psum(k)
                    for kc in range(8):
                        P.op('pe', lambda e: e.matmul(ps[:, :], lhsT=H[:, kc, tb_ * 128:(tb_ + 1) * 128],
                                                      rhs=win[:, kc, 1024 + half * 512:1024 + (half + 1) * 512], start=(kc == 0), stop=(kc == 7)),
                             reads=['win', 'H'], writes=[('ps', b)])
                    P.op('act', lambda e: e.activation(out=kt[:, half * 512:(half + 1) * 512], in_=ps[:, :], func=AF.Copy, scale=0.0625),
                         reads=[('ps', b)], writes=['kt'])
                if s == 0:
                    kv = kt.rearrange("p (h two f) -> p h two f", two=2, f=128)
                    ov = kto.rearrange("p (h two f) -> p h two f", two=2, f=128)
                    k1, k2 = kv[:, :, 0, :], kv[:, :, 1, :]
                    o1, o2 = ov[:, :, 0, :], ov[:, :, 1, :]
                    cosb = cst[:, 0, :].unsqueeze(1).to_broadcast([128, 4, 128])
                    sinb = cst[:, 1, :].unsqueeze(1).to_broadcast([128, 4, 128])
                    ta = tmps[0].rearrange("p (h f) -> p h f", f=128)
                    tb = tmps[1].rearrange("p (h f) -> p h f", f=128)
                    ka, kb = ('ntmp', 0), ('ntmp', 1)
                    P.op('dve', lambda e: e.tensor_tensor(out=ta, in0=k1, in1=cosb, op=ALU.mult), reads=['kt', 'cst0'], writes=[ka])
                    P.op('dve', lambda e: e.tensor_tensor(out=tb, in0=k2, in1=sinb, op=ALU.mult), reads=['kt', 'cst1'], writes=[kb])
                    P.op('dve', lambda e: e.tensor_tensor(out=o1, in0=ta, in1=tb, op=ALU.subtract), reads=[ka, kb], writes=['kto'])
                    P.op('dve', lambda e: e.tensor_tensor(out=ta, in0=k2, in1=cosb, op=ALU.mult), reads=['kt', 'cst0', 'kto'], writes=[ka])
                    P.op('dve', lambda e: e.tensor_tensor(out=tb, in0=k1, in1=sinb, op=ALU.mult), reads=['kt', 'cst1', 'kto'], writes=[kb])
                    P.op('dve', lambda e: e.tensor_tensor(out=o2, in0=ta, in1=tb, op=ALU.add), reads=[ka, kb], writes=['kto'])
                else:
                    P.op('dve', lambda e: e.tensor_copy(out=kto, in_=kt), reads=['kt'], writes=['kto'])
                P.op('sp', lambda e: e.dma_start(out=KTOK[r0:r0 + 128, :], in_=kto), reads=['kto'], writes=['KTOK'], dma=True)
                for q4 in range(4):
                    b, ps = psum(k)
                    for kc in range(8):
                        P.op('pe', lambda e: e.matmul(ps[:, :], lhsT=H[:, kc, tb_ * 128:(tb_ + 1) * 128],
                                                      rhs=win[:, kc, 2048 + q4 * 512:2048 + (q4 + 1) * 512], start=(kc == 0), stop=(kc == 7)),
                             reads=['win', 'H'], writes=[('ps', b)])
                    eng = 'act' if q4 % 2 == 0 else 'dve'
                    if eng == 'act':
                        P.op('act', lambda e: e.activation(out=vto[:, q4 * 512:(q4 + 1) * 512], in_=ps[:, :], func=AF.Copy),
                             reads=[('ps', b)], writes=['vto'])
                    else:
                        P.op('dve', lambda e: e.tensor_copy(out=vto[:, q4 * 512:(q4 + 1) * 512], in_=ps[:, :]),
                             reads=[('ps', b)], writes=['vto'])
                P.op('sp', lambda e: e.dma_start(out=VTOK[r0:r0 + 128, :], in_=vto), reads=['vto'], writes=['VTOK'], dma=True)
            for g4 in range(4):
                for gj in range(4):
                    oc = g4 * 4 + gj
                    b, ps = psum(k)
                    for kc in range(8):
                        P.op('pe', lambda e: e.matmul(ps[:, :n], lhsT=win[:, kc, 4096 + oc * 128:4096 + (oc + 1) * 128], rhs=H[:, kc, :n],
                                                      start=(kc == 0), stop=(kc == 7)),
                             reads=['win', 'H'], writes=[('ps', b)])
                    gt = tmps[gj % 2]
                    kg = ('ntmp', gj % 2)
                    P.op('act', lambda e: e.activation(out=gt[:, :n], in_=ps[:, :n], func=AF.Silu), reads=[('ps', b)], writes=[kg])
                    P.op('dve', lambda e: e.tensor_scalar(out=gso[:, gj, :n], in0=gt[:, :n], scalar1=k.ret_gain[:, oc:oc + 1], scalar2=None,
                                                          op0=ALU.mult), reads=[kg, 'retp'], writes=['gso'])
                P.op('sp', lambda e: e.dma_start(out=GS.rearrange("(oc q) t -> q oc t", q=128)[:, g4 * 4:(g4 + 1) * 4, c0:c0 + n],
                                                 in_=gso[:, :, :n]), reads=['gso'], writes=['GS'], dma=True)
    P.barrier()
    with ExitStack() as st:
        drain_precast(k, 1000)
        qT = sb(st, nc, "r_qT", [128, 2, T], BF16)
        kT = sb(st, nc, "r_kT", [128, 2, T], BF16)
        ktk = sb(st, nc, "r_ktk", [128, NCH, 256], BF16)
        vtk = sb(st, nc, "r_vtk", [128, NCH, 512], BF16)
        tb_s = sb(st, nc, "r_tab", [128, 6 * 128 + 3], F32)
        P.op('sp', lambda e: e.dma_start(out=tb_s, in_=tab), writes=['rtab'], dma=True)
        DP, DM, UP, LO, POS1, POSB = (tb_s[:, q * 128:(q + 1) * 128] for q in range(6))
        KPF, KPB, C128 = (tb_s[:, 768 + q:769 + q] for q in range(3))
        M = sb(st, nc, "r_M", [128, 128], F32)
        M2 = sb(st, nc, "r_M2", [128, 128], F32)
        QD = sb(st, nc, "r_QD", [128, 2, 128], F32)
        cv = sb(st, nc, "r_cv", [128, 4], F32)
        S32 = [sb(st, nc, f"r_S32{d}", [128, 2, 512], F32) for d in range(2)]
        Sbf = [sb(st, nc, f"r_Sbf{q}", [128, 2, 512], BF16) for q in range(2)]
        Sfb = sb(st, nc, "r_Sfb", [128, 2, 512], BF16)
        Sin = [sb(st, nc, f"r_Sin{q}", [128, 2, 512], BF16) for q in range(3)]
        gsc = [sb(st, nc, f"r_gsc{q}", [128, 4, 128], BF16) for q in range(4)]
        kd = [sb(st, nc, f"r_kd{q}", [128, 256], BF16) for q in range(2)]
        sm = [sb(st, nc, f"r_sm{q}", [128, 128], BF16) for q in range(2)]
        qs = [sb(st, nc, f"r_qs{q}", [128, 2, 2, 128], BF16) for q in range(2)]
        sqo2 = [sb(st, nc, f"r_sqo{q}", [128, 512], F32) for q in range(2)]
        rn2 = [sb(st, nc, f"r_rn{q}", [128, 128], F32) for q in range(2)]
        zt2 = [sb(st, nc, f"r_zt{q}", [128, 4, 128], F32) for q in range(2)]
        zo = [sb(st, nc, f"r_zo{q}", [128, 4, 128], BF16) for q in range(2)]
        nkd = 0
        for h in range(4):
            P.op('sp', lambda e: e.dma_start(out=ktk, in_=KTOK.rearrange("(c q) f -> q c f", q=128)[:, :, h * 256:(h + 1) * 256]),
                 reads=['KTOK'], writes=['ktk'], dma=True)
            P.op('sp', lambda e: e.dma_start(out=vtk, in_=VTOK.rearrange("(c q) f -> q c f", q=128)[:, :, h * 512:(h + 1) * 512]),
                 reads=['VTOK'], writes=['vtk'], dma=True)
            P.op('sp', lambda e: e.dma_start(out=qT, in_=QT.rearrange("(oc q) t -> q oc t", q=128)[:, 2 * h:2 * h + 2, :]),
                 reads=['QT'], writes=['qT'], dma=True)
            P.op('sp', lambda e: e.dma_start(out=kT, in_=KT.rearrange("(oc q) t -> q oc t", q=128)[:, 2 * h:2 * h + 2, :]),
                 reads=['KT'], writes=['kT'], dma=True)
            lgf = k.ret_ld[:, h:h + 1]
            lgb = k.ret_ld[:, 4 + h:5 + h]
            P.op('act', lambda e: e.activation(out=M, in_=DP, func=AF.Exp, scale=lgf), reads=['rtab', 'retp'], writes=['M'])
            P.op('dve', lambda e: e.tensor_tensor(out=M, in0=M, in1=UP, op=ALU.mult), reads=['M', 'rtab'], writes=['M'])
            P.op('act', lambda e: e.activation(out=M2, in_=DM, func=AF.Exp, scale=lgb), reads=['rtab', 'retp'], writes=['M2'])
            P.op('dve', lambda e: e.tensor_tensor(out=M2, in0=M2, in1=LO, op=ALU.mult), reads=['M2', 'rtab'], writes=['M2'])
            P.op('dve', lambda e: e.tensor_tensor(out=M, in0=M, in1=M2, op=ALU.add), reads=['M', 'M2'], writes=['M'])
            P.op('act', lambda e: e.activation(out=QD[:, 0, :], in_=POS1, func=AF.Exp, scale=lgf), reads=['rtab', 'retp'], writes=['QD'])
            P.op('act', lambda e: e.activation(out=QD[:, 1, :], in_=POSB, func=AF.Exp, scale=lgb), reads=['rtab', 'retp'], writes=['QD'])
            P.op('act', lambda e: e.activation(out=cv[:, 0:1], in_=KPF, func=AF.Exp, scale=lgf), reads=['rtab', 'retp'], writes=['cv'])
            P.op('act', lambda e: e.activation(out=cv[:, 1:2], in_=KPB, func=AF.Exp, scale=lgb), reads=['rtab', 'retp'], writes=['cv'])
            P.op('act', lambda e: e.activation(out=cv[:, 2:3], in_=C128, func=AF.Exp, scale=lgf), reads=['rtab', 'retp'], writes=['cv'])
            P.op('act', lambda e: e.activation(out=cv[:, 3:4], in_=C128, func=AF.Exp, scale=lgb), reads=['rtab', 'retp'], writes=['cv'])

            def state_update(d, cidx, S, kS, Sb_out, kSb, banks=None):
                nonlocal nkd
                kdt = kd[nkd % 2]
                kkd = ('kd', nkd % 2)
                nkd += 1
                P.op('dve', lambda e: e.tensor_scalar(out=kdt, in0=ktk[:, cidx, :], scalar1=cv[:, d:d + 1], scalar2=None, op0=ALU.mult),
                     reads=['ktk', 'cv'], writes=[kkd])
                for dch in range(2):
                    if banks is None:
                        b, ps = psum(k)
                    else:
                        b, ps = banks[dch], k.PS[banks[dch]]
                    P.op('pe', lambda e: e.matmul(ps[:, :], lhsT=kdt[:, dch * 128:(dch + 1) * 128], rhs=vtk[:, cidx, :], start=True, stop=True),
                         reads=[kkd, 'vtk'], writes=[('ps', b)])
                    P.op('dve', lambda e: e.scalar_tensor_tensor(out=S[:, dch, :], in0=S[:, dch, :], scalar=cv[:, 2 + d:3 + d], in1=ps[:, :],
                                                                 op0=ALU.mult, op1=ALU.add),
                         reads=[kS, ('ps', b), 'cv'], writes=[kS])
                P.op('act', lambda e: e.activation(out=Sb_out, in_=S, func=AF.Copy), reads=[kS], writes=[kSb])

            order_b = [1, 0] + list(range(NCH - 1, 1, -1))
            P.op('dve', lambda e: e.memset(S32[1], 0.0), writes=['S32b'])
            P.op('dve', lambda e: e.memset(Sbf[0], 0.0), writes=[('Sbf', 0)])
            for oi, cidx in enumerate(order_b):
                cur = Sbf[oi % 2]
                kcur = ('Sbf', oi % 2)
                P.op('sp', lambda e: e.dma_start(out=SBd[h, cidx].rearrange("(dch q) e -> q dch e", q=128), in_=cur),
                     reads=[kcur], writes=[('SBd', cidx)], dma=True)
                if oi + 1 < len(order_b):
                    state_update(1, cidx, S32[1], 'S32b', Sbf[(oi + 1) % 2], ('Sbf', (oi + 1) % 2))
            P.op('dve', lambda e: e.memset(S32[0], 0.0), writes=['S32f'])
            P.op('dve', lambda e: e.memset(Sfb, 0.0), writes=['Sfb'])

            def prefetch(cidx):
                b3, b4 = cidx % 3, cidx % 4
                P.op('sp', lambda e: e.dma_start(out=Sin[b3], in_=SBd[h, cidx].rearrange("(dch q) e -> q dch e", q=128)),
                     reads=[('SBd', cidx)], writes=[('Sin', b3)], dma=True)
                P.op('sp', lambda e: e.dma_start(out=gsc[b4], in_=GS.rearrange("(oc q) t -> q oc t", q=128)[:, 4 * h:4 * h + 4, cidx * 128:(cidx + 1) * 128]),
                     reads=['GS'], writes=[('gsc', b4)], dma=True)
            def fA(cidx):
                bq = cidx % 2
                cols = slice(cidx * 128, (cidx + 1) * 128)
                b = bq
                ps_s = k.PS[b]
                for dch in range(2):
                    P.op('pe', lambda e: e.matmul(ps_s[:, 0:128], lhsT=kT[:, dch, cols], rhs=qT[:, dch, cols], start=(dch == 0), stop=(dch == 1)),
                         reads=['kT', 'qT'], writes=[('ps', b)])
                P.op('dve', lambda e: e.tensor_tensor(out=sm[bq], in0=ps_s[:, 0:128], in1=M, op=ALU.mult), reads=[('ps', b), 'M'], writes=[('sm', bq)])
                for d in range(2):
                    P.op('dve', lambda e: e.tensor_tensor(out=qs[bq][:, d], in0=qT[:, :, cols], in1=QD[:, d, :].unsqueeze(1).to_broadcast([128, 2, 128]),
                                                          op=ALU.mult), reads=['qT', 'QD'], writes=[('qs', bq, d)])

            def fB(cidx):
                bq = cidx % 2
                bo = 2 + bq
                ps_o = k.PS[bo]
                smt, qst = sm[bq], qs[bq]
                for ech in range(4):
                    es = slice(ech * 128, (ech + 1) * 128)
                    P.op('pe', lambda e: e.matmul(ps_o[:, es], lhsT=vtk[:, cidx, es], rhs=smt, start=True, stop=False),
                         reads=['vtk', ('sm', bq)], writes=[('ps', bo)])
                    for dch in range(2):
                        P.op('pe', lambda e: e.matmul(ps_o[:, es], lhsT=Sfb[:, dch, es], rhs=qst[:, 0, dch, :], start=False, stop=False),
                             reads=['Sfb', ('qs', bq, 0)], writes=[('ps', bo)])
                    for dch in range(2):
                        P.op('pe', lambda e: e.matmul(ps_o[:, es], lhsT=Sin[cidx % 3][:, dch, es], rhs=qst[:, 1, dch, :], start=False, stop=(dch == 1)),
                             reads=[('Sin', cidx % 3), ('qs', bq, 1)], writes=[('ps', bo)])
                P.op('act', lambda e: e.activation(out=sqo2[bq], in_=ps_o, func=AF.Square), reads=[('ps', bo)], writes=[('sqo', bq)])

            def fC(cidx):
                bq = cidx % 2
                bo = 2 + bq
                ps_o = k.PS[bo]
                cols = slice(cidx * 128, (cidx + 1) * 128)
                bn = 4
                ps_n = k.PS[bn]
                sqo, rn, zt = sqo2[bq], rn2[bq], zt2[bq]
                for ech in range(4):
                    P.op('pe', lambda e: e.matmul(ps_n[:, 0:128], lhsT=k.ones_f, rhs=sqo[:, ech * 128:(ech + 1) * 128], start=(ech == 0), stop=(ech == 3)),
                         reads=[('sqo', bq), 'ones'], writes=[('ps', bn)])
                P.op('act', lambda e: e.activation(out=rn, in_=ps_n[:, 0:128], func=AF.Ln, scale=1.0 / 512, bias=k.epsb[:, 0:1]),
                     reads=[('ps', bn)], writes=[('rn', bq)])
                P.op('act', lambda e: e.activation(out=rn, in_=rn, func=AF.Exp, scale=-0.5), reads=[('rn', bq)], writes=[('rn', bq)])
                P.op('dve', lambda e: e.tensor_tensor(out=zt, in0=ps_o.rearrange("p (c i) -> p c i", i=128),
                                                      in1=rn.unsqueeze(1).to_broadcast([128, 4, 128]), op=ALU.mult),
                     reads=[('ps', bo), ('rn', bq)], writes=[('zt', bq)])
                P.op('dve', lambda e: e.tensor_tensor(out=zo[bq], in0=zt, in1=gsc[cidx % 4], op=ALU.mult), reads=[('zt', bq), ('gsc', cidx % 4)], writes=[('zo', bq)])
                P.op('sp', lambda e: e.dma_start(out=ZR.rearrange("(oc q) t -> q oc t", q=128)[:, 4 * h:4 * h + 4, cols], in_=zo[bq]),
                     reads=[('zo', bq)], writes=['ZR'], dma=True)

            prefetch(0)
            prefetch(1)
            fA(0)
            for cidx in range(NCH):
                if cidx + 2 < NCH:
                    prefetch(cidx + 2)
                if cidx + 1 < NCH:
                    fA(cidx + 1)
                fB(cidx)
                if cidx + 1 < NCH:
                    state_update(0, cidx, S32[0], 'S32f', Sfb, 'Sfb', banks=(5, 6))
                if cidx >= 1:
                    fC(cidx - 1)
            fC(NCH - 1)
    P.barrier()
    with ExitStack() as st:
        wo = sb(st, nc, "ret_wo", [128, 16, 1024], BF16)
        P.op('pool', lambda e: e.dma_start(out=wo, in_=k.ret_w_out[0].rearrange("(m q) c -> q m c", q=128)), writes=['wo'], dma=True)
        out_proj_residual(k, i, TILES, ZR.rearrange("(m q) t -> q m t", q=128), 16, 'ZR', wo, 128,
                          lambda zc, dc: wo[:, zc, dc * 128:(dc + 1) * 128])


def stage_swa(k, i):
    nc, P = k.nc, k.P
    QS, KS, VS, OS = k.QS, k.KS, k.VS, k.OS
    with ExitStack() as st:
        win = sb(st, nc, "swa_win", [128, 8, 2816], BF16)
        for (a, b_) in ((0, 1408), (1408, 2816)):
            P.op('pool', lambda e: e.dma_start(out=win[:, :, a:b_], in_=k.swa_w_ext.rearrange("(kc q) c -> q kc c", q=128)[:, :, a:b_]),
                 writes=['win'], dma=True)
        drain_precast(k, 1000)
        Hs = [sb(st, nc, f"H3{q}", [128, 8, 512], BF16) for q in range(2)]
        xts = [sb(st, nc, f"xt3{q}", [128, 8, 512], F32) for q in range(2)]
        sq = sb(st, nc, "sq3", [128, 8, 512], F32)
        rs = sb(st, nc, "rs3", [128, 512], F32)
        tmps = [sb(st, nc, f"nt3{q}", [128, 512], F32) for q in range(2)]
        qo = sb(st, nc, "s_qo", [128, 10, 512], BF16)
        cs = sb(st, nc, "s_cs", [128, 2, 512], F32)
        vto = [sb(st, nc, f"s_vto{q}", [128, 256], BF16) for q in range(2)]
        nt = 0
        def prep(ti):
            c0, n, s = TILES[ti]
            q = ti % 2
            load_x_tile(k, xts[q], c0, n, key=('xt', q))
            norm_tile(k, xts[q], ('xt', q), n, i, 0, s, Hs[q], ('H', q), sq, rs, tmps)
        prep(0)
        for ti, (c0, n, s) in enumerate(TILES):
            H, kH = Hs[ti % 2], ('H', ti % 2)
            if ti + 1 < len(TILES):
                prep(ti + 1)
            if s == 0:
                p0 = c0 - NCTX
                P.op('sp', lambda e: e.dma_start(out=cs[:, 0, :n], in_=k.swa_cos[:, p0:p0 + n]), writes=['cs0'], dma=True)
                P.op('sp', lambda e: e.dma_start(out=cs[:, 1, :n], in_=k.swa_sin[:, p0:p0 + n]), writes=['cs1'], dma=True)
            for hp in range(10):
                off = hp * 128
                off_sw = 1536 + hp * 128
                b, ps = psum(k)
                for kc in range(8):
                    P.op('pe', lambda e: e.matmul(ps[:, :n], lhsT=win[:, kc, off:off + 128], rhs=H[:, kc, :n], start=(kc == 0), stop=(kc == 7)),
                         reads=['win', kH], writes=[('ps', b)])
                if s == 0:
                    b2, ps2 = psum(k)
                    for kc in range(8):
                        P.op('pe', lambda e: e.matmul(ps2[:, :n], lhsT=win[:, kc, off_sw:off_sw + 128], rhs=H[:, kc, :n], start=(kc == 0), stop=(kc == 7)),
                             reads=['win', kH], writes=[('ps', b2)])
                    ta, tb = tmps[0], tmps[1]
                    P.op('dve', lambda e: e.tensor_tensor(out=ta[:, :n], in0=ps[:, :n], in1=cs[:, 0, :n], op=ALU.mult),
                         reads=[('ps', b), 'cs0'], writes=[('ntmp', 0)])
                    P.op('dve', lambda e: e.tensor_tensor(out=tb[:, :n], in0=ps2[:, :n], in1=cs[:, 1, :n], op=ALU.mult),
                         reads=[('ps', b2), 'cs1'], writes=[('ntmp', 1)])
                    P.op('dve', lambda e: e.tensor_tensor(out=qo[:, hp, :n], in0=ta[:, :n], in1=tb[:, :n], op=ALU.add),
                         reads=[('ntmp', 0), ('ntmp', 1)], writes=['qo'])
                else:
                    P.op('act', lambda e: e.activation(out=qo[:, hp, :n], in_=ps[:, :n], func=AF.Copy), reads=[('ps', b)], writes=['qo'])
            for par in range(2):
                psl = slice(par * 64, (par + 1) * 64)
                P.op('sp', lambda e: e.dma_start(out=QS.rearrange("d (j two) t -> d two j t", two=2)[:, par, :, c0:c0 + n], in_=qo[psl, 0:8, :n]),
                     reads=['qo'], writes=['QS'], dma=True)
                P.op('sp', lambda e: e.dma_start(out=KS.rearrange("d (j two) t -> d two j t", two=2)[:, par, :, c0:c0 + n], in_=qo[psl, 8:10, :n]),
                     reads=['qo'], writes=['KS'], dma=True)
            for tb_ in range(n // 128):
                r0 = c0 + tb_ * 128
                b, ps = psum(k)
                for kc in range(8):
                    P.op('pe', lambda e: e.matmul(ps[:, 0:256], lhsT=H[:, kc, tb_ * 128:(tb_ + 1) * 128], rhs=win[:, kc, 1280:1536],
                                                  start=(kc == 0), stop=(kc == 7)),
                         reads=['win', kH], writes=[('ps', b)])
                vt = vto[nt % 2]
                kv_ = ('vto', nt % 2)
                nt += 1
                P.op('act', lambda e: e.activation(out=vt, in_=ps[:, 0:256], func=AF.Copy), reads=[('ps', b)], writes=[kv_])
                P.op('sp', lambda e: e.dma_start(out=VS[r0:r0 + 128, :], in_=vt), reads=[kv_], writes=['VS'], dma=True)
    P.barrier()
    with ExitStack() as st:
        Kt = sb(st, nc, "s_K", [64, 4, T], BF16)
        Vt = sb(st, nc, "s_V", [128, NCH, 256], BF16)
        P.op('sp', lambda e: e.dma_start(out=Kt, in_=KS), reads=['KS'], writes=['Kt'], dma=True)
        P.op('sp', lambda e: e.dma_start(out=Vt, in_=VS.rearrange("(c q) f -> q c f", q=128)), reads=['VS'], writes=['Vt'], dma=True)
        msk = sb(st, nc, "s_msk", [128, 2, 128], F32)
        P.op('sp', lambda e: e.dma_start(out=msk[:, 0, :], in_=k.ret_tab[:, 384:512]), writes=['msk'], dma=True)
        P.op('sp', lambda e: e.dma_start(out=msk[:, 1, :], in_=k.ret_tab[:, 256:384]), writes=['msk'], dma=True)
        mskb = sb(st, nc, "s_mskb", [128, 2, 128], BF16)
        P.op('dve', lambda e: e.tensor_copy(out=mskb, in_=msk), reads=['msk'], writes=['mskb'])
        esink = sb(st, nc, "s_esink", [64, 16], F32)
        P.op('act', lambda e: e.activation(out=esink, in_=k.swa_sink, func=AF.Exp), reads=['swap'], writes=['esink'])
        ones_b = sb(st, nc, "s_ones", [128, 64], BF16)
        P.op('dve', lambda e: e.memset(ones_b, 1.0), writes=['ones_b'])
        qb_ = [sb(st, nc, f"s_qb{q}", [64, 16, 128], BF16) for q in range(2)]
        ob_ = [sb(st, nc, f"s_ob{q}", [64, 16, 128], BF16) for q in range(2)]
        Et = [sb(st, nc, f"s_E{q}", [128, 512], BF16) for q in range(10)]
        rd = sb(st, nc, "s_rd", [64, 4, 128], F32)
        ne = 0

        def loadq(c):
            P.op('sp', lambda e: e.dma_start(out=qb_[c % 2], in_=QS[:, :, c * 128:(c + 1) * 128]), reads=['QS'], writes=[('qb', c % 2)], dma=True)
        loadq(0)
        for c in range(NCH):
            if c + 1 < NCH:
                loadq(c + 1)
            qblk = qb_[c % 2]
            oblk = ob_[c % 2]
            if c < 2:
                kbs = [(0, None), (1, None)]
            else:
                kbs = []
                if c - 1 >= 2:
                    kbs.append((c - 1, 0))
                kbs.append((c, None))
                if c + 1 < NCH:
                    kbs.append((c + 1, 1))
                kbs += [(0, None), (1, None)]
            for hk in range(4):
                Q = qblk[:, hk * 4:(hk + 1) * 4, :]
                es = []
                for (kb, mk) in kbs:
                    b, ps = psum(k)
                    P.op('pe', lambda e: e.matmul(ps[:, :], lhsT=Kt[:, hk, kb * 128:(kb + 1) * 128], rhs=Q, start=True, stop=True),
                         reads=['Kt', ('qb', c % 2)], writes=[('ps', b)])
                    E = Et[ne % 10]
                    kE = ('E', ne % 10)
                    ne += 1
                    P.op('act', lambda e: e.activation(out=E, in_=ps[:, :], func=AF.Exp, scale=0.125), reads=[('ps', b)], writes=[kE])
                    if mk is not None:
                        Ev = E.rearrange("p (g i) -> p g i", i=128)
                        P.op('pool', lambda e: e.tensor_tensor(out=Ev, in0=Ev, in1=mskb[:, mk, :].unsqueeze(1).to_broadcast([128, 4, 128]), op=ALU.mult),
                             reads=[kE, 'mskb'], writes=[kE])
                    es.append((kb, E, kE))
                bo, ps_o = psum(k)
                for q, (kb, E, kE) in enumerate(es):
                    P.op('pe', lambda e: e.matmul(ps_o[0:64, :], lhsT=Vt[:, kb, hk * 64:(hk + 1) * 64], rhs=E, start=(q == 0), stop=(q == len(es) - 1)),
                         reads=['Vt', kE], writes=[('ps', bo)])
                bd, ps_d = psum(k)
                for q, (kb, E, kE) in enumerate(es):
                    P.op('pe', lambda e: e.matmul(ps_d[0:64, :], lhsT=ones_b, rhs=E, start=(q == 0), stop=(q == len(es) - 1)),
                         reads=['ones_b', kE], writes=[('ps', bd)])
                P.op('dve', lambda e: e.tensor_tensor(out=rd, in0=ps_d[0:64, :].rearrange("p (g i) -> p g i", i=128),
                                                      in1=esink[:, hk * 4:(hk + 1) * 4].unsqueeze(2).to_broadcast([64, 4, 128]), op=ALU.add),
                     reads=[('ps', bd), 'esink'], writes=['rd'])
                P.op('act', lambda e: e.activation(out=rd, in_=rd, func=AF.Ln), reads=['rd'], writes=['rd'])
                P.op('act', lambda e: e.activation(out=rd, in_=rd, func=AF.Exp, scale=-1.0), reads=['rd'], writes=['rd'])
                P.op('dve', lambda e: e.tensor_tensor(out=oblk[:, hk * 4:(hk + 1) * 4, :], in0=ps_o[0:64, :].rearrange("p (g i) -> p g i", i=128),
                                                      in1=rd, op=ALU.mult),
                     reads=[('ps', bo), 'rd'], writes=[('ob', c % 2)])
            P.op('sp', lambda e: e.dma_start(out=OS[:, :, c * 128:(c + 1) * 128], in_=oblk), reads=[('ob', c % 2)], writes=['OS'], dma=True)
    P.barrier()
    with ExitStack() as st:
        wo = sb(st, nc, "swa_wo", [64, 16, 1024], BF16)
        P.op('pool', lambda e: e.dma_start(out=wo, in_=k.swa_w_out[0].rearrange("(h d) c -> d h c", d=64)), writes=['wo'], dma=True)
        out_proj_residual(k, i, TILES, OS, 16, 'OS', wo, 64, lambda zc, dc: wo[:, zc, dc * 128:(dc + 1) * 128])


def stage_final(k):
    nc, P = k.nc, k.P
    with ExitStack() as st:
        xts = [sb(st, nc, f"xtf{q}", [128, 8, 512], F32) for q in range(2)]
        sq = sb(st, nc, "sqf", [128, 8, 512], F32)
        rss = [sb(st, nc, f"rsf{q}", [128, 512], F32) for q in range(2)]
        outv = k.out.rearrange("(kc q) t -> q kc t", q=128)
        for ti, (c0, n, s) in enumerate(TILES[1:]):
            xt = xts[ti % 2]
            rs = rss[ti % 2]
            kx = ('xtf', ti % 2)
            kr = ('rsf', ti % 2)
            P.op('sp', lambda e: e.dma_start(out=xt[:, :, :n], in_=xrow(k)[:, :, c0:c0 + n]), reads=['X', ('Xt', c0)], writes=[kx], dma=True)
            sqb = sq.bitcast(BF16)[:, :, 0:512]
            P.op('act', lambda e: e.activation(out=sqb[:, :, :n], in_=xt[:, :, :n], func=AF.Square), reads=[kx], writes=['sq'])
            b, ps = psum(k)
            for kc in range(8):
                P.op('pe', lambda e: e.matmul(ps[:, :n], lhsT=k.ones_b[:, :], rhs=sqb[:, kc, :n], start=(kc == 0), stop=(kc == 7)),
                     reads=['sq', 'ones_b'], writes=[('ps', b)])
            P.op('act', lambda e: e.activation(out=rs[:, :n], in_=ps[:, :n], func=AF.Ln, scale=1.0 / D, bias=k.epsb[:, 0:1]),
                 reads=[('ps', b)], writes=[kr])
            P.op('act', lambda e: e.activation(out=rs[:, :n], in_=rs[:, :n], func=AF.Exp, scale=-0.5), reads=[kr], writes=[kr])
            for kc in range(8):
                P.op('dve', lambda e: e.scalar_tensor_tensor(out=xt[:, kc, :n], in0=xt[:, kc, :n], scalar=k.fg_s[:, kc:kc + 1], in1=rs[:, :n],
                                                             op0=ALU.mult, op1=ALU.mult),
                     reads=[kx, kr, 'fg'], writes=[kx])
            P.op('sp', lambda e: e.dma_start(out=outv[:, :, c0 - NCTX:c0 - NCTX + n], in_=xt[:, :, :n]), reads=[kx], writes=['out'], dma=True)
    P.barrier()


ALL_PARTS = ['init', 'mods', 'mix0', 'ffn0', 'mix1', 'ffn1', 'mix2', 'ffn2', 'mix3', 'ffn3', 'final']


def kernel(**inputs):
    inp = {kk: np.asarray(v) for kk, v in inputs.items()}
    n = inp['x'].shape[0]
    maps = make_in_maps(inp, list(range(n)))
    nc, _ = build_program(ALL_PARTS, debug=False)
    res = run_bass_kernel_spmd(nc, maps, core_ids=list(range(n)))
    out = np.stack([np.ascontiguousarray(res.results[b]['outT'].T) for b in range(n)], axis=0)
    return out.astype(np.float32)


I32 = mybir.dt.int32
NBLK_MAX = 24
MOE_BLK = 512


def precast_moe(k, pe):
    P = k.P
    f, e = pe // 8, pe % 8
    gu, dn = k.moe_w_gu[f, e], k.moe_w_down[f, e]
    for g7 in range(7):
        r0 = (pe * 7 + g7) * 128
        for half in range(2):
            c0 = half * DFF + g7 * 512
            dst = k.WGUx[r0:r0 + 128, :].rearrange("p (kc h c) -> p kc h c", kc=8, h=2)[:, :, half, :]
            src = gu.rearrange("(kc p) n -> p kc n", p=128)[:, :, c0:c0 + 512]
            P.op('pool', lambda e_: e_.dma_start(out=dst, in_=src), writes=[('wgux', pe)], dma=True)
    for dh in range(2):
        r0 = (pe * 2 + dh) * 128
        dst = k.WDx[r0:r0 + 128, :].rearrange("p (fc c) -> p fc c", c=512)
        src = dn.rearrange("(fc p) d -> p fc d", p=128)[:, :, dh * 512:(dh + 1) * 512]
        P.op('pool', lambda e_: e_.dma_start(out=dst, in_=src), writes=[('wdx', pe)], dma=True)


def drain_precast(k, n):
    while n > 0 and k.pc_queue:
        k.pc_queue.pop(0)()
        n -= 1


def stage_moe_sparse(k, i):
    nc, P = k.nc, k.P
    f = i // 2
    last = (i == 3)
    tiles = TILES[1:] if last else TILES
    cols0 = tiles[0][0]
    ntok = sum(t[1] for t in tiles)
    NCK = ntok // 128
    NB = (2 * ntok + 8 * (MOE_BLK - 1)) // MOE_BLK
    drain_precast(k, 1000)
    HC, YC = k.HC, k.YC
    ct = k.moe_tab
    with ExitStack() as st0:
        EQ1 = sb(st0, nc, "m_eq1", [128, NCK, 8], F32)
        EQ2 = sb(st0, nc, "m_eq2", [128, NCK, 8], F32)
        W12 = sb(st0, nc, "m_w12", [128, NCK, 2], F32)
        DI = sb(st0, nc, "m_di", [128, NCK, 2], I32)
        IGU = sb(st0, nc, "m_igu", [128, NB, 7], I32)
        IWD = sb(st0, nc, "m_iwd", [128, NB, 2], I32)
        tabs = sb(st0, nc, "m_tab", [128, 161], F32)
        P.op('sp', lambda e: e.dma_start(out=tabs, in_=ct), writes=['mtab'], dma=True)
        TRI, IOB, CGU, CWD = tabs[:, 0:128], tabs[:, 128:128 + NB], tabs[:, 152:159], tabs[:, 159:161]
        with ExitStack() as st:
            HTOK = sb(st, nc, "m_htok", [128, NCK, 1024], BF16)
            H = sb(st, nc, "m_H", [128, 8, 512], BF16)
            xt = sb(st, nc, "m_xt", [128, 8, 512], F32)
            sq = sb(st, nc, "m_sq", [128, 8, 512], F32)
            hf = sb(st, nc, "m_hf", [128, 8, 512], F32)
            rs = sb(st, nc, "m_rs", [128, 512], F32)
            tmps = [sb(st, nc, f"m_nt{q}", [128, 512], F32) for q in range(2)]
            rsm = sb(st, nc, "m_rsm", [128, 64], F32)
            rsm3 = sb(st, nc, "m_rsm3", [128, 12], F32)
            identb = sb(st, nc, "m_identb", [128, 128], BF16)
            P.op('dve', lambda e: e.tensor_copy(out=identb, in_=k.ident_f), reads=['ident'], writes=['identb'])
            ck = 0
            for ti, (c0, n, s) in enumerate(tiles):
                load_x_tile(k, xt, c0, n)
                norm_tile(k, xt, 'xt', n, i, 1, s, H, 'H', sq, rs, tmps, hf=hf, khf='hf')
                nb = n // 128
                b, ps = psum(k)
                for bi in range(nb):
                    bsl = slice(bi * 128, (bi + 1) * 128)
                    for kc in range(8):
                        P.op('pe', lambda e: e.matmul(ps[:, bi * 8:(bi + 1) * 8], lhsT=hf[:, kc, bsl], rhs=k.rt[:, f, kc, :], start=(kc == 0), stop=(kc == 7)),
                             reads=['hf', 'rt'], writes=[('ps', b)])
                lg = rsm[:, 0:nb * 8]
                lg2 = rsm[:, 32:32 + nb * 8]
                lgv = lg.rearrange("p (c e) -> p c e", e=8)
                lg2v = lg2.rearrange("p (c e) -> p c e", e=8)
                m1, m2, dd = rsm3[:, 0:nb], rsm3[:, 4:4 + nb], rsm3[:, 8:8 + nb]
                eq1, eq2 = EQ1[:, ck:ck + nb, :], EQ2[:, ck:ck + nb, :]
                R = ['rsm']
                KE = [('eq', ck + q) for q in range(nb)]
                P.op('dve', lambda e: e.tensor_copy(out=lg, in_=ps[:, 0:nb * 8]), reads=[('ps', b)], writes=R)
                P.op('dve', lambda e: e.tensor_reduce(out=m1, in_=lgv, axis=AX.X, op=ALU.max), reads=R, writes=R)
                P.op('dve', lambda e: e.tensor_tensor(out=eq1, in0=lgv, in1=m1.unsqueeze(2).to_broadcast([128, nb, 8]), op=ALU.is_equal), reads=R, writes=KE)
                P.op('dve', lambda e: e.scalar_tensor_tensor(out=lg2, in0=eq1.rearrange("p c e -> p (c e)"), scalar=-1e30, in1=lg, op0=ALU.mult, op1=ALU.add),
                     reads=R + KE, writes=R)
                P.op('dve', lambda e: e.tensor_reduce(out=m2, in_=lg2v, axis=AX.X, op=ALU.max), reads=R, writes=R)
                P.op('dve', lambda e: e.tensor_tensor(out=eq2, in0=lg2v, in1=m2.unsqueeze(2).to_broadcast([128, nb, 8]), op=ALU.is_equal), reads=R, writes=KE)
                P.op('dve', lambda e: e.tensor_tensor(out=dd, in0=m2, in1=m1, op=ALU.subtract), reads=R, writes=R)
                P.op('act', lambda e: e.activation(out=W12[:, ck:ck + nb, 0], in_=dd, func=AF.Sigmoid, scale=-1.0), reads=R, writes=KE)
                P.op('act', lambda e: e.activation(out=W12[:, ck:ck + nb, 1], in_=dd, func=AF.Sigmoid, scale=1.0), reads=R, writes=KE)
                for bi in range(nb):
                    bsl = slice(bi * 128, (bi + 1) * 128)
                    bt, pst = psum(k)
                    pstb = pst.bitcast(BF16)
                    for kc in range(8):
                        P.op('pe', lambda e: e.transpose(out=pstb[:, kc * 128:(kc + 1) * 128], in_=H[:, kc, bsl], identity=identb),
                             reads=['H', 'identb'], writes=[('ps', bt)])
                    eng = 'act' if ck % 2 == 0 else 'dve'
                    if eng == 'act':
                        P.op('act', lambda e: e.activation(out=HTOK[:, ck, :], in_=pstb, func=AF.Copy), reads=[('ps', bt)], writes=[('htok', ck)])
                    else:
                        P.op('dve', lambda e: e.tensor_copy(out=HTOK[:, ck, :], in_=pstb), reads=[('ps', bt)], writes=[('htok', ck)])
                    ck += 1
            allE = [('eq', c) for c in range(NCK)]
            SEL = sb(st, nc, "m_sel", [128, NCK, 8], F32)
            PRE = sb(st, nc, "m_pre", [128, NCK + 1, 8], F32)
            RANK = sb(st, nc, "m_rank", [128, NCK, 8], F32)
            sm = sb(st, nc, "m_sm", [128, 64], F32)
            TOT, NBk, CB, OFF, ONE8 = sm[:, 0:8], sm[:, 8:16], sm[:, 16:24], sm[:, 24:32], sm[:, 32:40]
            EB = sb(st, nc, "m_eb", [128, 3, NB], F32)
            DF = sb(st, nc, "m_df", [128, NCK, 2], F32)
            IGf = sb(st, nc, "m_igf", [128, NB, 7], F32)
            IWf = sb(st, nc, "m_iwf", [128, NB, 2], F32)
            P.op('dve', lambda e: e.tensor_tensor(out=SEL, in0=EQ1, in1=EQ2, op=ALU.add), reads=allE, writes=['sel'])
            P.op('dve', lambda e: e.memset(PRE[:, 0, :], 0.0), writes=['pre'])
            P.op('dve', lambda e: e.memset(ONE8, 1.0), writes=['sm'])
            for c in range(NCK):
                P.op('dve', lambda e: e.tensor_tensor(out=PRE[:, c + 1, :], in0=PRE[:, c, :], in1=SEL[:, c, :], op=ALU.add),
                     reads=['pre', 'sel'], writes=['pre'])
            br, psr = psum(k)
            for c in range(NCK):
                P.op('pe', lambda e: e.matmul(psr[:, c * 8:(c + 1) * 8], lhsT=TRI, rhs=SEL[:, c, :], start=True, stop=False),
                     reads=['mtab', 'sel'], writes=[('ps', br)])
                P.op('pe', lambda e: e.matmul(psr[:, c * 8:(c + 1) * 8], lhsT=k.ones_f, rhs=PRE[:, c, :], start=False, stop=True),
                     reads=['ones', 'pre'], writes=[('ps', br)])
            P.op('dve', lambda e: e.tensor_copy(out=RANK, in_=psr[:, 0:NCK * 8].rearrange("p (c e) -> p c e", e=8)), reads=[('ps', br)], writes=['rank'])
            b2, ps2 = psum(k)
            P.op('pe', lambda e: e.matmul(ps2[:, 0:8], lhsT=k.ones_f, rhs=PRE[:, NCK, :], start=True, stop=True), reads=['ones', 'pre'], writes=[('ps', b2)])
            S_ = ['sm']
            P.op('dve', lambda e: e.tensor_copy(out=TOT, in_=ps2[:, 0:8]), reads=[('ps', b2)], writes=S_)
            P.op('dve', lambda e: e.tensor_scalar(out=NBk, in0=TOT, scalar1=0.0, scalar2=None, op0=ALU.is_gt), reads=S_, writes=S_)
            for m in range(1, 9):
                P.op('dve', lambda e: e.scalar_tensor_tensor(out=NBk, in0=TOT, scalar=float(MOE_BLK * m), in1=NBk, op0=ALU.is_gt, op1=ALU.add),
                     reads=S_, writes=S_)
            P.op('dve', lambda e: e.tensor_tensor_scan(out=CB, data0=ONE8, data1=NBk, initial=0.0, op0=ALU.mult, op1=ALU.add), reads=S_, writes=S_)
            P.op('dve', lambda e: e.tensor_tensor(out=OFF, in0=CB, in1=NBk, op=ALU.subtract), reads=S_, writes=S_)
            P.op('dve', lambda e: e.tensor_scalar(out=OFF, in0=OFF, scalar1=float(MOE_BLK), scalar2=None, op0=ALU.mult), reads=S_, writes=S_)
            P.op('dve', lambda e: e.tensor_tensor(out=RANK, in0=RANK, in1=OFF.unsqueeze(1).to_broadcast([128, NCK, 8]), op=ALU.add),
                 reads=['rank'] + S_, writes=['rank'])
            P.op('dve', lambda e: e.tensor_tensor(out=SEL, in0=RANK, in1=EQ1, op=ALU.mult), reads=['rank'] + allE, writes=['sel'])
            P.op('dve', lambda e: e.tensor_reduce(out=DF[:, :, 0], in_=SEL, axis=AX.X, op=ALU.add), reads=['sel'], writes=['df'])
            P.op('dve', lambda e: e.tensor_tensor(out=SEL, in0=RANK, in1=EQ2, op=ALU.mult), reads=['rank', 'df'] + allE, writes=['sel'])
            P.op('dve', lambda e: e.tensor_reduce(out=DF[:, :, 1], in_=SEL, axis=AX.X, op=ALU.add), reads=['sel'], writes=['df'])
            P.op('dve', lambda e: e.tensor_copy(out=DI, in_=DF), reads=['df'], writes=['di'])
            P.op('dve', lambda e: e.memset(EB[:, 0, :], 0.0), writes=['eb'])
            for e8 in range(8):
                P.op('dve', lambda e: e.scalar_tensor_tensor(out=EB[:, 0, :], in0=IOB, scalar=CB[:, e8:e8 + 1], in1=EB[:, 0, :], op0=ALU.is_ge, op1=ALU.add),
                     reads=['mtab', 'eb'] + S_, writes=['eb'])
            P.op('dve', lambda e: e.tensor_scalar(out=EB[:, 0, :], in0=EB[:, 0, :], scalar1=7.0, scalar2=None, op0=ALU.min), reads=['eb'], writes=['eb'])
            P.op('dve', lambda e: e.tensor_scalar(out=EB[:, 1, :], in0=EB[:, 0, :], scalar1=896.0, scalar2=float(f * 8 * 896), op0=ALU.mult, op1=ALU.add),
                 reads=['eb'], writes=['eb'])
            P.op('dve', lambda e: e.tensor_scalar(out=EB[:, 2, :], in0=EB[:, 0, :], scalar1=256.0, scalar2=float(f * 8 * 256), op0=ALU.mult, op1=ALU.add),
                 reads=['eb'], writes=['eb'])
            for b_ in range(NB):
                P.op('dve', lambda e: e.tensor_scalar(out=IGf[:, b_, :], in0=CGU, scalar1=EB[:, 1, b_:b_ + 1], scalar2=None, op0=ALU.add),
                     reads=['eb', 'mtab'], writes=['igf'])
                P.op('dve', lambda e: e.tensor_scalar(out=IWf[:, b_, :], in0=CWD, scalar1=EB[:, 2, b_:b_ + 1], scalar2=None, op0=ALU.add),
                     reads=['eb', 'mtab'], writes=['iwf'])
            P.op('dve', lambda e: e.tensor_copy(out=IGU, in_=IGf), reads=['igf'], writes=['igu'])
            P.op('dve', lambda e: e.tensor_copy(out=IWD, in_=IWf), reads=['iwf'], writes=['iwd'])
            for c in range(NCK):
                for j2 in range(2):
                    P.op('pool', lambda e: e.indirect_dma_start(out=HC, out_offset=bass.IndirectOffsetOnAxis(ap=DI[:, c, j2:j2 + 1], axis=0),
                                                                in_=HTOK[:, c, :], in_offset=None),
                         reads=['di', ('htok', c)], writes=['HC'], dma=True)
        P.barrier()
        with ExitStack() as st:
            wgu = [sb(st, nc, f"m_wgu{q}", [128, 8192], BF16) for q in range(2)]
            wdb = [sb(st, nc, f"m_wd{q}", [128, 28 * 512], BF16) for q in range(2)]
            hs = [sb(st, nc, f"m_hs{q}", [128, 4, 1024], BF16) for q in range(2)]
            HcT = sb(st, nc, "m_HcT", [128, 8, 512], BF16)
            act = sb(st, nc, "m_act", [128, 28, 512], BF16)
            yc = [sb(st, nc, f"m_yc{q}", [128, 1024], F32) for q in range(2)]
            sg_ = [sb(st, nc, f"m_sg{q}", [128, 512], BF16) for q in range(2)]
            identb = sb(st, nc, "m_identb2", [128, 128], BF16)
            P.op('dve', lambda e: e.tensor_copy(out=identb, in_=k.ident_f), reads=['ident'], writes=['identb'])
            n_gu = [0]

            def gather_gu(b_, g7):
                q = n_gu[0] % 2
                n_gu[0] += 1
                P.op('pool', lambda e: e.indirect_dma_start(out=wgu[q], out_offset=None, in_=k.WGUx,
                                                            in_offset=bass.IndirectOffsetOnAxis(ap=IGU[:, b_, g7:g7 + 1], axis=0)),
                     reads=['igu', 'wgux_all'], writes=[('wgu', q)], dma=True)
                return q

            def gather_wd(b_, dh):
                P.op('pool', lambda e: e.indirect_dma_start(out=wdb[dh], out_offset=None, in_=k.WDx,
                                                            in_offset=bass.IndirectOffsetOnAxis(ap=IWD[:, b_, dh:dh + 1], axis=0)),
                     reads=['iwd', 'wdx_all'], writes=[('wd', dh)], dma=True)

            def load_hs(b_):
                P.op('sp', lambda e: e.dma_start(out=hs[b_ % 2], in_=HC[b_ * 512:(b_ + 1) * 512, :].rearrange("(sg p) d -> p sg d", p=128)),
                     reads=['HC'], writes=[('hs', b_ % 2)], dma=True)
            load_hs(0)
            pre_gu = [gather_gu(0, 0), gather_gu(0, 1)]
            gather_wd(0, 0)
            gather_wd(0, 1)
            nsg = 0
            nyc = 0
            for b_ in range(NB):
                if b_ + 1 < NB:
                    load_hs(b_ + 1)
                hsb = hs[b_ % 2]
                for kc in range(8):
                    bt, pst = psum(k)
                    pstb = pst.bitcast(BF16)
                    for sgi in range(4):
                        P.op('pe', lambda e: e.transpose(out=pstb[:, sgi * 128:(sgi + 1) * 128], in_=hsb[:, sgi, kc * 128:(kc + 1) * 128], identity=identb),
                             reads=[('hs', b_ % 2), 'identb'], writes=[('ps', bt)])
                    if kc % 2 == 0:
                        P.op('act', lambda e: e.activation(out=HcT[:, kc, :], in_=pstb[:, 0:512], func=AF.Copy), reads=[('ps', bt)], writes=['HcT'])
                    else:
                        P.op('dve', lambda e: e.tensor_copy(out=HcT[:, kc, :], in_=pstb[:, 0:512]), reads=[('ps', bt)], writes=['HcT'])
                for g7 in range(7):
                    if g7 < 2:
                        q = pre_gu[g7]
                    else:
                        q = gather_gu(b_, g7)
                    wv = wgu[q].rearrange("p (kc h c) -> p kc h c", kc=8, h=2)
                    for j in range(4):
                        fch = g7 * 4 + j
                        bg, psg = psum(k)
                        for kc in range(8):
                            P.op('pe', lambda e: e.matmul(psg, lhsT=wv[:, kc, 0, j * 128:(j + 1) * 128], rhs=HcT[:, kc, :], start=(kc == 0), stop=(kc == 7)),
                                 reads=[('wgu', q), 'HcT'], writes=[('ps', bg)])
                        bu, psu = psum(k)
                        for kc in range(8):
                            P.op('pe', lambda e: e.matmul(psu, lhsT=wv[:, kc, 1, j * 128:(j + 1) * 128], rhs=HcT[:, kc, :], start=(kc == 0), stop=(kc == 7)),
                                 reads=[('wgu', q), 'HcT'], writes=[('ps', bu)])
                        sgt = sg_[nsg % 2]
                        ksg = ('sg', nsg % 2)
                        nsg += 1
                        P.op('act', lambda e: e.activation(out=sgt, in_=psg, func=AF.Silu), reads=[('ps', bg)], writes=[ksg])
                        P.op('dve', lambda e: e.tensor_tensor(out=act[:, fch, :], in0=sgt, in1=psu, op=ALU.mult), reads=[ksg, ('ps', bu)], writes=[('act', fch)])
                if b_ + 1 < NB:
                    pre_gu = [gather_gu(b_ + 1, 0), gather_gu(b_ + 1, 1)]
                for sgi in range(4):
                    y = yc[nyc % 2]
                    ky = ('yc', nyc % 2)
                    nyc += 1
                    for dh in range(2):
                        wv = wdb[dh].rearrange("p (fc c) -> p fc c", c=512)
                        bd, psd = psum(k)
                        for fc in range(28):
                            P.op('pe', lambda e: e.matmul(psd, lhsT=act[:, fc, sgi * 128:(sgi + 1) * 128], rhs=wv[:, fc, :], start=(fc == 0), stop=(fc == 27)),
                                 reads=[('act', fc), ('wd', dh)], writes=[('ps', bd)])
                        if dh == 0:
                            P.op('act', lambda e: e.activation(out=y[:, 0:512], in_=psd, func=AF.Copy), reads=[('ps', bd)], writes=[ky])
                        else:
                            P.op('dve', lambda e: e.tensor_copy(out=y[:, 512:1024], in_=psd), reads=[('ps', bd)], writes=[ky])
                    r0 = b_ * 512 + sgi * 128
                    P.op('sp', lambda e: e.dma_start(out=YC[r0:r0 + 128, :], in_=y), reads=[ky], writes=['YC'], dma=True)
                if b_ + 1 < NB:
                    gather_wd(b_ + 1, 0)
                    gather_wd(b_ + 1, 1)
        P.barrier()
        with ExitStack() as st:
            xt = sb(st, nc, "m_xtE", [128, 8, 512], F32)
            sqE = sb(st, nc, "m_sqE", [128, 8, 512], BF16)
            rsE = sb(st, nc, "m_rsE", [128, 512], F32)
            y1 = [sb(st, nc, f"m_y1{q}", [128, 1024], F32) for q in range(2)]
            y2 = [sb(st, nc, f"m_y2{q}", [128, 1024], F32) for q in range(2)]
            ck = 0
            for ti, (c0, n, s) in enumerate(tiles):
                load_x_tile(k, xt, c0, n)
                banks = [psum(k) for _ in range(8)]
                for bi in range(n // 128):
                    q = ck % 2
                    P.op('pool', lambda e: e.indirect_dma_start(out=y1[q], out_offset=None, in_=YC,
                                                                in_offset=bass.IndirectOffsetOnAxis(ap=DI[:, ck, 0:1], axis=0)),
                         reads=['YC', 'di'], writes=[('y1', q)], dma=True)
                    P.op('pool', lambda e: e.indirect_dma_start(out=y2[q], out_offset=None, in_=YC,
                                                                in_offset=bass.IndirectOffsetOnAxis(ap=DI[:, ck, 1:2], axis=0)),
                         reads=['YC', 'di'], writes=[('y2', q)], dma=True)
                    P.op('dve', lambda e: e.tensor_scalar(out=y1[q], in0=y1[q], scalar1=W12[:, ck, 0:1], scalar2=None, op0=ALU.mult),
                         reads=[('y1', q), ('eq', ck)], writes=[('y1', q)])
                    P.op('dve', lambda e: e.scalar_tensor_tensor(out=y1[q], in0=y2[q], scalar=W12[:, ck, 1:2], in1=y1[q], op0=ALU.mult, op1=ALU.add),
                         reads=[('y1', q), ('y2', q), ('eq', ck)], writes=[('y1', q)])
                    for kc in range(8):
                        bb, pp = banks[kc]
                        P.op('pe', lambda e: e.transpose(out=pp[:, bi * 128:(bi + 1) * 128], in_=y1[q][:, kc * 128:(kc + 1) * 128], identity=k.ident_f),
                             reads=[('y1', q), 'ident'], writes=[('ps', bb)])
                    ck += 1
                for kc in range(8):
                    bb, pp = banks[kc]
                    gate = k.mods[:, i, 5 * 8 + kc, s:s + 1]
                    P.op('dve', lambda e: e.scalar_tensor_tensor(out=xt[:, kc, :n], in0=pp[:, :n], scalar=gate, in1=xt[:, kc, :n], op0=ALU.mult, op1=ALU.add),
                         reads=[('ps', bb), 'xt', 'mods'], writes=['xt'])
                if last and k.fuse_final:
                    P.op('act', lambda e: e.activation(out=sqE[:, :, :n], in_=xt[:, :, :n], func=AF.Square), reads=['xt'], writes=['sqE'])
                    bf_, psf = psum(k)
                    for kc in range(8):
                        P.op('pe', lambda e: e.matmul(psf[:, :n], lhsT=k.ones_b[:, :], rhs=sqE[:, kc, :n], start=(kc == 0), stop=(kc == 7)),
                             reads=['sqE', 'ones_b'], writes=[('ps', bf_)])
                    P.op('act', lambda e: e.activation(out=rsE[:, :n], in_=psf[:, :n], func=AF.Ln, scale=1.0 / D, bias=k.epsb[:, 0:1]),
                         reads=[('ps', bf_)], writes=['rsE'])
                    P.op('act', lambda e: e.activation(out=rsE[:, :n], in_=rsE[:, :n], func=AF.Exp, scale=-0.5), reads=['rsE'], writes=['rsE'])
                    for kc in range(8):
                        P.op('dve', lambda e: e.scalar_tensor_tensor(out=xt[:, kc, :n], in0=xt[:, kc, :n], scalar=k.fg_s[:, kc:kc + 1], in1=rsE[:, :n],
                                                                     op0=ALU.mult, op1=ALU.mult),
                             reads=['xt', 'rsE', 'fg'], writes=['xt'])
                    P.op('sp', lambda e: e.dma_start(out=k.out.rearrange("(kc q) t -> q kc t", q=128)[:, :, c0 - NCTX:c0 - NCTX + n], in_=xt[:, :, :n]),
                         reads=['xt'], writes=['out'], dma=True)
                else:
                    store_x_tile(k, xt, c0, n)
    P.barrier()
```
